# Optimizing a Trainium2 kernel written in Bass

```python
import math
import jax
import jax.numpy as jnp
from jax import lax
import numpy as np

D_MODEL = 2048
BATCH = 4
SEQ = 2048
DEPTH = 2

CTX_LEN = 256
GRID_W = 64

HEAD_DIM = D_MODEL // 16
HY_GROUPS = 4
NA_HEADS = 6
GLA_HEADS = 6
HY_CH = HY_GROUPS * HEAD_DIM
NA_DIM = NA_HEADS * HEAD_DIM
GLA_DK = HEAD_DIM // 2
GLA_DV = HEAD_DIM
GLA_K = GLA_HEADS * GLA_DK
GLA_V = GLA_HEADS * GLA_DV
MIX_DIM = HY_CH + NA_DIM + GLA_V

SHORT_CONV = 3
HY_EMB_BANDS = 16
HY_EMB_DIM = 1 + 2 * HY_EMB_BANDS
HY_FILTER_WIDTH = 64
HY_DECAY_TARGET = 1e-2
HY_FAST_DECAY = 0.3
HY_SLOW_DECAY = 1.5
HY_MIN_DECAY = math.log(HY_DECAY_TARGET) / HY_SLOW_DECAY
HY_MAX_DECAY = math.log(HY_DECAY_TARGET) / HY_FAST_DECAY

NA_WIN_ROWS = 8
NA_WIN_COLS = 16
NEG_INF = -1e30

GLA_RANK = 16
GLA_GATE_NORM = 16.0
GLA_CHUNK = 64
ROPE_BASE = 10000.0

N_GROUPS = 4
EXPERTS_PER_GROUP = 8
TOP_K_IN_GROUP = 2
D_EXPERT = D_MODEL // 4

ALPHA = (2 * DEPTH) ** 0.25
BETA = (8 * DEPTH) ** -0.25
LN_EPS = 1e-6

IN_SPLITS = (3 * HY_CH, NA_DIM, NA_DIM, NA_DIM, GLA_K, GLA_K, GLA_V, GLA_V, 2 * GLA_RANK)
IN_COLS = sum(IN_SPLITS)

kernel_name = 'hybrid_hyena_natten_gla_hmoe_prefix_dit'


def layer_norm(x, g=None, b=None):
    xf = x.astype(jnp.float32)
    mu = jnp.mean(xf, axis=-1, keepdims=True)
    xc = xf - mu
    y = xc * lax.rsqrt(jnp.mean(xc * xc, axis=-1, keepdims=True) + LN_EPS)
    if g is not None:
        y = y * g.astype(jnp.float32) + b.astype(jnp.float32)
    return y.astype(x.dtype)


def rms_norm(x, w):
    xf = x.astype(jnp.float32)
    y = xf * lax.rsqrt(jnp.mean(xf * xf, axis=-1, keepdims=True) + LN_EPS) * w.astype(jnp.float32)
    return y.astype(x.dtype)


def split_columns(p):
    idx = [int(i) for i in np.cumsum(IN_SPLITS)[:-1]]
    return jnp.split(p, idx, axis=-1)


def to_heads(x, dh):
    b, l, _ = x.shape
    return x.reshape(b, l, -1, dh).transpose(0, 2, 1, 3)


def short_conv(u, w, b):
    l = u.shape[1]
    pad = SHORT_CONV // 2
    up = jnp.pad(u, ((0, 0), (pad, pad), (0, 0)))
    out = up[:, 0:l] * w[0]
    for j in range(1, SHORT_CONV):
        out = out + up[:, j:j + l] * w[j]
    return out + b


def hyena_filter(l, w1, b1, w2, b2, w3, sin_freq):
    f32 = jnp.float32
    n = jnp.arange(l, dtype=f32)
    t = n / max(l - 1, 1)
    bands = jnp.linspace(1e-4, HY_EMB_BANDS - 1, HY_EMB_BANDS, dtype=f32)
    ang = (2.0 * math.pi / l) * n[:, None] * bands[None, :]
    z = jnp.concatenate([t[:, None], jnp.cos(ang), -jnp.sin(ang)], axis=-1)
    fr = sin_freq.astype(f32)
    hid = jnp.sin(fr * (z @ w1.astype(f32) + b1.astype(f32)))
    hid = jnp.sin(fr * (hid @ w2.astype(f32) + b2.astype(f32)))
    h = (hid @ w3.astype(f32)).reshape(l, 2, HY_CH)
    deltas = jnp.abs(jnp.linspace(HY_MIN_DECAY, HY_MAX_DECAY, HY_CH, dtype=f32))
    h = h * jnp.exp(-t[:, None, None] * deltas)
    h_fwd = h[:, 0].T
    h_bwd = h[:, 1].T
    return jnp.concatenate([h_fwd, jnp.zeros((HY_CH, 1), f32), h_bwd[:, :0:-1]], axis=1)


def bidir_long_conv(u, k_circ, bias):
    l = u.shape[1]
    uf = u.astype(jnp.float32)
    u_f = jnp.fft.rfft(uf, n=2 * l, axis=1)
    k_f = jnp.fft.rfft(k_circ, axis=-1).T
    y = jnp.fft.irfft(u_f * k_f, n=2 * l, axis=1)[:, :l]
    return (y + uf * bias.astype(jnp.float32)).astype(u.dtype)


def hyena_branch(u, short_w, short_b, f_w1, f_b1, f_w2, f_b2, f_w3, sin_freq, bias):
    l = u.shape[1]
    u = short_conv(u, short_w, short_b)
    x0, x1, v = jnp.split(u, 3, axis=-1)
    k = hyena_filter(l, f_w1, f_b1, f_w2, f_b2, f_w3, sin_freq)
    return x0 * bidir_long_conv(x1 * v, k, bias)


def na_latent(q, k, v, k_ctx, v_ctx, rpb):
    b, l, _ = q.shape
    lc = k_ctx.shape[1]
    rows_n = l // GRID_W
    wr = min(NA_WIN_ROWS, rows_n)
    wc = NA_WIN_COLS
    scale = HEAD_DIM ** -0.5
    q = q.reshape(b, rows_n, GRID_W, NA_HEADS, HEAD_DIM) * scale
    k = k.reshape(b, rows_n, GRID_W, NA_HEADS, HEAD_DIM)
    v = v.reshape(b, rows_n, GRID_W, NA_HEADS, HEAD_DIM)
    k_ctx = k_ctx.reshape(b, lc, NA_HEADS, HEAD_DIM)
    v_ctx = v_ctx.reshape(b, lc, NA_HEADS, HEAD_DIM)
    r = jnp.arange(rows_n)
    r0 = jnp.clip(r - wr // 2, 0, rows_n - wr)
    rows = r0[:, None] + jnp.arange(wr)[None, :]
    col = jnp.arange(GRID_W)
    c0 = jnp.clip(col - wc // 2, 0, GRID_W - wc)
    col_ok = (col[None, :] >= c0[:, None]) & (col[None, :] < c0[:, None] + wc)
    k_blk = k[:, rows]
    v_blk = v[:, rows]
    s_loc = jnp.einsum('brqhd,brikhd->bhrqik', q, k_blk).astype(jnp.float32)
    rel_r = rows - r[:, None] + (NA_WIN_ROWS - 1)
    rel_c = jnp.clip(col[None, :] - col[:, None] + (wc - 1), 0, 2 * wc - 2)
    bias = rpb.astype(jnp.float32)[:, rel_r[:, None, :, None], rel_c[None, :, None, :]]
    s_loc = jnp.where(col_ok[None, None, None, :, None, :], s_loc + bias, NEG_INF)
    s_loc = s_loc.reshape(b, NA_HEADS, rows_n, GRID_W, wr * GRID_W)
    s_ctx = jnp.einsum('brqhd,bkhd->bhrqk', q, k_ctx).astype(jnp.float32)
    p = jax.nn.softmax(jnp.concatenate([s_loc, s_ctx], axis=-1), axis=-1).astype(v.dtype)
    p_loc = p[..., :wr * GRID_W].reshape(b, NA_HEADS, rows_n, GRID_W, wr, GRID_W)
    p_ctx = p[..., wr * GRID_W:]
    o = (jnp.einsum('bhrqik,brikhd->brqhd', p_loc, v_blk)
         + jnp.einsum('bhrqk,bkhd->brqhd', p_ctx, v_ctx))
    return o.reshape(b, l, NA_DIM)


def na_context(q, k, v):
    b, l, _ = q.shape
    q = q.reshape(b, l, NA_HEADS, HEAD_DIM) * (HEAD_DIM ** -0.5)
    k = k.reshape(b, l, NA_HEADS, HEAD_DIM)
    v = v.reshape(b, l, NA_HEADS, HEAD_DIM)
    s = jnp.einsum('bqhd,bkhd->bhqk', q, k).astype(jnp.float32)
    p = jax.nn.softmax(s, axis=-1).astype(v.dtype)
    return jnp.einsum('bhqk,bkhd->bqhd', p, v).reshape(b, l, NA_DIM)


def rope_2d(x, rows, cols):
    half = x.shape[-1] // 2
    quarter = half // 2
    inv = ROPE_BASE ** (-jnp.arange(quarter, dtype=jnp.float32) / quarter)

    def rot(xa, pos):
        ang = pos[:, None] * inv[None, :]
        cos = jnp.cos(ang).astype(x.dtype)
        sin = jnp.sin(ang).astype(x.dtype)
        x1, x2 = xa[..., :quarter], xa[..., quarter:]
        return jnp.concatenate([x1 * cos - x2 * sin, x1 * sin + x2 * cos], axis=-1)

    return jnp.concatenate([rot(x[..., :half], rows), rot(x[..., half:], cols)], axis=-1)


def gla_log_decay(a_lr, w2, b):
    outs = []
    for d in range(2):
        z = a_lr[..., d * GLA_RANK:(d + 1) * GLA_RANK] @ w2[d] + b[d]
        outs.append(to_heads(jax.nn.log_sigmoid(z.astype(jnp.float32)) / GLA_GATE_NORM, GLA_DK))
    return outs[0], outs[1]


def gla_scan(q, k, v, log_a, h0):
    b, h, l, _ = k.shape
    dv = v.shape[-1]
    n = l // GLA_CHUNK

    def chunks(a):
        return jnp.moveaxis(a.astype(jnp.float32).reshape(b, h, n, GLA_CHUNK, a.shape[-1]), 2, 0)

    with_out = q is not None
    causal = jnp.tril(jnp.ones((GLA_CHUNK, GLA_CHUNK), dtype=bool))
    xs = (chunks(k), chunks(v), chunks(log_a)) + ((chunks(q),) if with_out else ())

    def step(state, xs_i):
        k_i, v_i, a_i = xs_i[0], xs_i[1], xs_i[2]
        cum = jnp.cumsum(a_i, axis=-2)
        cum_last = cum[..., -1:, :]
        new_state = (state * jnp.exp(cum_last)[..., 0, :, None]
                     + jnp.einsum('bhck,bhcv->bhkv', k_i * jnp.exp(cum_last - cum), v_i))
        if not with_out:
            return new_state, None
        q_i = xs_i[3]
        diff = cum[..., :, None, :] - cum[..., None, :, :]
        decay = jnp.exp(jnp.where(causal[:, :, None], diff, -jnp.inf))
        att = jnp.einsum('bhtk,bhsk,bhtsk->bhts', q_i, k_i, decay)
        o = (jnp.einsum('bhck,bhkv->bhcv', q_i * jnp.exp(cum), state)
             + jnp.einsum('bhts,bhsv->bhtv', att, v_i))
        return new_state, o

    state, o = lax.scan(step, h0, xs)
    if with_out:
        o = jnp.moveaxis(o, 0, 2).reshape(b, h, l, dv).astype(v.dtype)
    return o, state


def gla_output(o, g, norm_w):
    b, _, l, _ = o.shape
    o = rms_norm(o.transpose(0, 2, 1, 3), norm_w).reshape(b, l, GLA_V)
    return o * jax.nn.silu(g)


def token_mixers(h, hc, w_in, hy_short_w, hy_short_b, hy_f_w1, hy_f_b1, hy_f_w2, hy_f_b2, hy_f_w3,
                 hy_sin_freq, hy_bias, hy_norm_w, na_rpb, na_norm_w, gla_a_w2, gla_a_b, gla_norm_w,
                 w_out, ctx_out):
    hy_l, nq_l, nk_l, nv_l, gq_l, gk_l, gv_l, gg_l, ga_l = split_columns(h @ w_in)
    hy_c, nq_c, nk_c, nv_c, gq_c, gk_c, gv_c, gg_c, ga_c = split_columns(hc @ w_in)
    hy_args = (hy_short_w, hy_short_b, hy_f_w1, hy_f_b1, hy_f_w2, hy_f_b2, hy_f_w3, hy_sin_freq, hy_bias)

    y_a = hyena_branch(hy_l, *hy_args)
    y_b = na_latent(nq_l, nk_l, nv_l, nk_c, nv_c, na_rpb)
    b, l, _ = h.shape
    t = jnp.arange(l)
    rows = (t // GRID_W).astype(jnp.float32)
    cols = (t % GRID_W).astype(jnp.float32)
    qs = GLA_DK ** -0.5
    q = rope_2d(to_heads(gq_l * qs, GLA_DK), rows, cols)
    k = rope_2d(to_heads(gk_l, GLA_DK), rows, cols)
    v = to_heads(gv_l, GLA_DV)
    la_f, la_b = gla_log_decay(ga_l, gla_a_w2, gla_a_b)
    k_c = to_heads(gk_c, GLA_DK)
    v_c = to_heads(gv_c, GLA_DV)
    la_fc, la_bc = gla_log_decay(ga_c, gla_a_w2, gla_a_b)
    q_c = to_heads(gq_c * qs, GLA_DK) if ctx_out else None
    q_cb = jnp.flip(q_c, 2) if ctx_out else None
    h0 = jnp.zeros((b, GLA_HEADS, GLA_DK, GLA_DV), jnp.float32)
    o_cf, s_f = gla_scan(q_c, k_c, v_c, la_fc, h0)
    o_cb, s_b = gla_scan(q_cb, jnp.flip(k_c, 2), jnp.flip(v_c, 2), jnp.flip(la_bc, 2), h0)
    o_f, _ = gla_scan(q, k, v, la_f, s_f)
    o_b, _ = gla_scan(jnp.flip(q, 2), jnp.flip(k, 2), jnp.flip(v, 2), jnp.flip(la_b, 2), s_b)
    y_c = gla_output(o_f + jnp.flip(o_b, 2), gg_l, gla_norm_w)

    y = jnp.concatenate([rms_norm(y_a, hy_norm_w), rms_norm(y_b, na_norm_w), y_c], axis=-1) @ w_out
    if not ctx_out:
        return y, None
    yc_a = hyena_branch(hy_c, *hy_args)
    yc_b = na_context(nq_c, nk_c, nv_c)
    yc_c = gla_output(o_cf + jnp.flip(o_cb, 2), gg_c, gla_norm_w)
    yc = jnp.concatenate([rms_norm(yc_a, hy_norm_w), rms_norm(yc_b, na_norm_w), yc_c], axis=-1) @ w_out
    return y, yc


def hier_moe(h, w_rg, b_rg, w_re, b_re, w_up, w_down):
    n = h.shape[0]
    p_group = jax.nn.softmax((h @ w_rg).astype(jnp.float32) + b_rg.astype(jnp.float32), axis=-1)
    top_pg, gidx = lax.top_k(p_group, 1)
    gsel = jax.nn.one_hot(gidx[:, 0], N_GROUPS, dtype=jnp.float32)
    le = ((h @ w_re).astype(jnp.float32) + b_re.astype(jnp.float32)).reshape(n, N_GROUPS, EXPERTS_PER_GROUP)
    le_sel = jnp.einsum('nge,ng->ne', le, gsel)
    top_v, eidx = lax.top_k(le_sel, TOP_K_IN_GROUP)
    w_sel = jax.nn.softmax(top_v, axis=-1) * top_pg
    e_gate = jnp.einsum('nke,nk->ne', jax.nn.one_hot(eidx, EXPERTS_PER_GROUP, dtype=jnp.float32), w_sel)
    gate = (gsel[:, :, None] * e_gate[:, None, :]).astype(h.dtype)
    out = jnp.zeros_like(h)
    for gi in range(N_GROUPS):
        gu = jnp.einsum('nd,edf->nef', h, w_up[gi])
        act = jax.nn.silu(gu[..., :D_EXPERT]) * gu[..., D_EXPERT:]
        out = out + jnp.einsum('nef,efd->nd', act * gate[:, gi, :, None], w_down[gi])
    return out


def trunk_layer(x, xc, c, c_ctx, w_ada, b_ada, w_in, hy_short_w, hy_short_b, hy_f_w1, hy_f_b1, hy_f_w2,
                hy_f_b2, hy_f_w3, hy_sin_freq, hy_bias, hy_norm_w, na_rpb, na_norm_w, gla_a_w2, gla_a_b,
                gla_norm_w, w_out, ln1_g, ln1_b, w_rg, b_rg, w_re, b_re, w_up, w_down, ln2_g, ln2_b, last):
    d = x.shape[-1]
    mod = jax.nn.silu(c) @ w_ada + b_ada
    mod_c = jax.nn.silu(c_ctx) @ w_ada + b_ada
    sh1, sc1, g1, sh2, sc2, g2 = [m[:, None, :] for m in jnp.split(mod, 6, axis=-1)]
    sh1c, sc1c, g1c, sh2c, sc2c, g2c = jnp.split(mod_c, 6)

    h = layer_norm(x) * (1 + sc1) + sh1
    hc = layer_norm(xc) * (1 + sc1c) + sh1c
    y, yc = token_mixers(h, hc, w_in, hy_short_w, hy_short_b, hy_f_w1, hy_f_b1, hy_f_w2, hy_f_b2, hy_f_w3,
                         hy_sin_freq, hy_bias, hy_norm_w, na_rpb, na_norm_w, gla_a_w2, gla_a_b, gla_norm_w,
                         w_out, not last)
    x = layer_norm(ALPHA * x + g1 * y, ln1_g, ln1_b)
    h2 = layer_norm(x) * (1 + sc2) + sh2
    b, l, _ = x.shape
    if last:
        f = hier_moe(h2.reshape(b * l, d), w_rg, b_rg, w_re, b_re, w_up, w_down).reshape(b, l, d)
        return layer_norm(ALPHA * x + g2 * f, ln2_g, ln2_b), xc
    xc = layer_norm(ALPHA * xc + g1c * yc, ln1_g, ln1_b)
    h2c = layer_norm(xc) * (1 + sc2c) + sh2c
    lc = xc.shape[1]
    f_all = hier_moe(jnp.concatenate([h2.reshape(b * l, d), h2c.reshape(b * lc, d)], axis=0),
                     w_rg, b_rg, w_re, b_re, w_up, w_down)
    f = f_all[:b * l].reshape(b, l, d)
    fc = f_all[b * l:].reshape(b, lc, d)
    x = layer_norm(ALPHA * x + g2 * f, ln2_g, ln2_b)
    xc = layer_norm(ALPHA * xc + g2c * fc, ln2_g, ln2_b)
    return x, xc


def setup_inputs(seed: int = 0) -> dict:
    key = jax.random.key(seed)
    ks = iter(jax.random.split(key, 48))
    d = D_MODEL
    ne = N_GROUPS * EXPERTS_PER_GROUP

    def nrm(shape, scale):
        return jax.random.normal(next(ks), shape, jnp.float32) * scale

    return {
        'x': nrm((BATCH, SEQ, d), 1.0),
        'c': nrm((BATCH, d), 1.0),
        'ctx': nrm((BATCH, CTX_LEN, d), 1.0),
        'c_ctx': nrm((d,), 1.0),
        'w_ada': nrm((DEPTH, d, 6 * d), d ** -0.5),
        'b_ada': nrm((DEPTH, 6 * d), 0.01),
        'w_in': nrm((DEPTH, d, IN_COLS), d ** -0.5),
        'hy_short_w': nrm((DEPTH, SHORT_CONV, 3 * HY_CH), SHORT_CONV ** -0.5),
        'hy_short_b': nrm((DEPTH, 3 * HY_CH), 0.01),
        'hy_f_w1': nrm((DEPTH, HY_EMB_DIM, HY_FILTER_WIDTH), HY_EMB_DIM ** -0.5),
        'hy_f_b1': nrm((DEPTH, HY_FILTER_WIDTH), 0.02),
        'hy_f_w2': nrm((DEPTH, HY_FILTER_WIDTH, HY_FILTER_WIDTH), HY_FILTER_WIDTH ** -0.5),
        'hy_f_b2': nrm((DEPTH, HY_FILTER_WIDTH), 0.02),
        'hy_f_w3': nrm((DEPTH, HY_FILTER_WIDTH, 2 * HY_CH), HY_FILTER_WIDTH ** -0.5),
        'hy_sin_freq': 1.0 + nrm((DEPTH, HY_FILTER_WIDTH), 0.01),
        'hy_bias': nrm((DEPTH, HY_CH), 0.1),
        'hy_norm_w': 1.0 + nrm((DEPTH, HY_CH), 0.01),
        'na_rpb': nrm((DEPTH, NA_HEADS, 2 * NA_WIN_ROWS - 1, 2 * NA_WIN_COLS - 1), 0.02),
        'na_norm_w': 1.0 + nrm((DEPTH, NA_DIM), 0.01),
        'gla_a_w2': nrm((DEPTH, 2, GLA_RANK, GLA_K), GLA_RANK ** -0.5),
        'gla_a_b': nrm((DEPTH, 2, GLA_K), 0.01),
        'gla_norm_w': 1.0 + nrm((DEPTH, GLA_DV), 0.01),
        'w_out': nrm((DEPTH, MIX_DIM, d), MIX_DIM ** -0.5 * BETA),
        'ln1_g': 1.0 + nrm((DEPTH, d), 0.01),
        'ln1_b': nrm((DEPTH, d), 0.01),
        'w_rg': nrm((DEPTH, d, N_GROUPS), d ** -0.5),
        'b_rg': nrm((DEPTH, N_GROUPS), 0.01),
        'w_re': nrm((DEPTH, d, ne), d ** -0.5),
        'b_re': nrm((DEPTH, ne), 0.01),
        'w_up': nrm((DEPTH, N_GROUPS, EXPERTS_PER_GROUP, d, 2 * D_EXPERT), d ** -0.5),
        'w_down': nrm((DEPTH, N_GROUPS, EXPERTS_PER_GROUP, D_EXPERT, d), D_EXPERT ** -0.5 * BETA),
        'ln2_g': 1.0 + nrm((DEPTH, d), 0.01),
        'ln2_b': nrm((DEPTH, d), 0.01),
    }


def reference(x, c, ctx, c_ctx, w_ada, b_ada, w_in, hy_short_w, hy_short_b, hy_f_w1, hy_f_b1, hy_f_w2,
              hy_f_b2, hy_f_w3, hy_sin_freq, hy_bias, hy_norm_w, na_rpb, na_norm_w, gla_a_w2, gla_a_b,
              gla_norm_w, w_out, ln1_g, ln1_b, w_rg, b_rg, w_re, b_re, w_up, w_down, ln2_g, ln2_b):
    xc = ctx
    for i in range(DEPTH):
        x, xc = trunk_layer(x, xc, c, c_ctx, w_ada[i], b_ada[i], w_in[i], hy_short_w[i], hy_short_b[i],
                            hy_f_w1[i], hy_f_b1[i], hy_f_w2[i], hy_f_b2[i], hy_f_w3[i], hy_sin_freq[i],
                            hy_bias[i], hy_norm_w[i], na_rpb[i], na_norm_w[i], gla_a_w2[i], gla_a_b[i],
                            gla_norm_w[i], w_out[i], ln1_g[i], ln1_b[i], w_rg[i], b_rg[i], w_re[i], b_re[i],
                            w_up[i], w_down[i], ln2_g[i], ln2_b[i], i == DEPTH - 1)
    return x
```

```python
import math
import numpy as np
import concourse.bass as bass
import concourse.mybir as mybir
from concourse.bass_utils import run_bass_kernel_spmd

F32 = mybir.dt.float32
BF16 = mybir.dt.bfloat16
I32 = mybir.dt.int32
AF = mybir.ActivationFunctionType
ALU = mybir.AluOpType
AX = mybir.AxisListType

D = 2048
L = 2048
LC = 256
T = L + LC
NCH = D // 128
DEPTH = 2
INC = 6176
HY = 512
NAH = 6
GH = 6
GDK = 64
EPS = 1e-6
ALPHA = (2 * DEPTH) ** 0.25
NE = 32
DE = 512

O_HY, O_NQ, O_NK, O_NV, O_GQ, O_GK, O_GV, O_GG, O_GA = 0, 1536, 2304, 3072, 3840, 4224, 4608, 5376, 6144


class K:
    NDMA = 24

    def __init__(self, nc):
        self.nc = nc
        self.eng = {'pe': nc.tensor, 'act': nc.scalar, 'dve': nc.vector, 'pool': nc.gpsimd, 'sp': nc.sync}
        self.sem = {e: nc.semaphore("s_" + e).__enter__() for e in ('pe', 'act', 'dve', 'pool')}
        self.cnt = {e: 0 for e in self.sem}
        self.dsem = [nc.semaphore("d%d" % i).__enter__() for i in range(self.NDMA)]
        self.dcnt = [0] * self.NDMA
        self.dnext = 0
        self.seen = {e: {} for e in self.eng}
        self.lastw = {}
        self.readers = {}
        self.nins = 0

    @staticmethod
    def key(x):
        if isinstance(x, (str, tuple)):
            return x
        return x.tensor.name if hasattr(x, 'tensor') else x.name

    def _wait(self, e, tok):
        if tok is None:
            return
        sem, val, src = tok
        if src == e and e == 'pe':
            return
        if self.seen[e].get(id(sem), 0) >= val:
            return
        self.eng[e].wait_ge(sem, val)
        self.seen[e][id(sem)] = val

    def _deps(self, e, reads, writes):
        for r in reads:
            self._wait(e, self.lastw.get(r))
        for w in writes:
            self._wait(e, self.lastw.get(w))
            for tok in self.readers.get(w, {}).values():
                self._wait(e, tok)

    def _record(self, tok, reads, writes):
        for w in writes:
            self.lastw[w] = tok
            self.readers[w] = {}
        for r in reads:
            if r in writes:
                continue
            self.readers.setdefault(r, {})[id(tok[0])] = tok

    def op(self, e, fn, reads=(), writes=()):
        reads = [self.key(r) for r in reads]
        writes = [self.key(w) for w in writes]
        self._deps(e, reads, writes)
        ins = fn(self.eng[e])
        self.cnt[e] += 1
        ins.then_inc(self.sem[e], 1)
        self._record((self.sem[e], self.cnt[e], e), reads, writes)
        self.nins += 1
        return ins

    def dma(self, e, out, in_, reads=None, writes=None, **kw):
        reads = [self.key(r) for r in (reads if reads is not None else [in_])]
        writes = [self.key(w) for w in (writes if writes is not None else [out])]
        self._deps(e, reads, writes)
        i = self.dnext
        self.dnext = (self.dnext + 1) % self.NDMA
        sem = self.dsem[i]
        self._wait(e, (sem, self.dcnt[i], 'dma'))
        self.eng[e].dma_start(out=out, in_=in_, **kw).then_inc(sem, 16)
        self.dcnt[i] += 16
        self._record((sem, self.dcnt[i], 'dma'), reads, writes)
        self.nins += 1

    def barrier(self):
        toks = [(self.sem[e], self.cnt[e], e) for e in self.sem if self.cnt[e] > 0]
        toks += [(self.dsem[i], self.dcnt[i], 'dma') for i in range(self.NDMA) if self.dcnt[i] > 0]
        for e in self.eng:
            for t in toks:
                if t[2] == e:
                    continue
                self._wait(e, t)
        self.lastw.clear()
        self.readers.clear()

    def finish(self):
        self.barrier()


def sb(nc, name, shape, dt):
    return nc.sbuf_tensor(name, list(shape), dt).__enter__()


def ps(nc, name, shape, dt=F32):
    return nc.psum_tensor(name, list(shape), dt).__enter__()


class Prog:
    def __init__(self, dbg=()):
        self.nc = nc = bass.Bass("TRN2", target_bir_lowering=False)
        self.k = K(nc)
        self.dbg = set(dbg)
        self.dram = {}
        self._ctx = []

    def inp(self, name, shape, dt=F32):
        t = self.nc.dram_tensor(name, list(shape), dt, kind="ExternalInput")
        self.dram[name] = t
        return t.ap()

    def outp(self, name, shape, dt=F32):
        t = self.nc.dram_tensor(name, list(shape), dt, kind="ExternalOutput")
        self.dram[name] = t
        return t.ap()

    def scratch(self, name, shape, dt=F32):
        kind = "ExternalOutput" if name in self.dbg else "Internal"
        t = self.nc.dram_tensor(name, list(shape), dt, kind=kind)
        self.dram[name] = t
        return t.ap()

    def alloc_sb(self, name, shape, dt):
        self._uid = getattr(self, '_uid', 0) + 1
        name = "%s_u%d" % (name, self._uid)
        g = self.nc.sbuf_tensor(name, list(shape), dt)
        t = g.__enter__()
        self._ctx.append(g)
        return t

    def alloc_ps(self, name, shape, dt=F32):
        self._uid = getattr(self, '_uid', 0) + 1
        name = "%s_u%d" % (name, self._uid)
        g = self.nc.psum_tensor(name, list(shape), dt)
        t = g.__enter__()
        self._ctx.append(g)
        return t

    def dump(self, name, tile, dt=F32):
        if name in self.dbg:
            o = self.outp("dbg_" + name, list(tile.shape), dt)
            self.k.dma('sp', o, tile[:], reads=[tile], writes=["dbg_" + name])

    def mark(self):
        return len(self._ctx)

    def release(self, mark):
        self.k.barrier()
        while len(self._ctx) > mark:
            self._ctx.pop().__exit__(None, None, None)


def stage_mod(P, l, cT, w_ada, b_adaT, modT):
    nc, k = P.nc, P.k
    m = P.mark()
    c_sb = P.alloc_sb("mod_c", [128, NCH, 2], F32)
    sc = P.alloc_sb("mod_silu", [128, NCH, 2], F32)
    bsb = P.alloc_sb("mod_b", [128, 96], F32)
    slabs = [P.alloc_sb("mod_w%d" % i, [128, NCH, 512], F32) for i in range(2)]
    pss = [P.alloc_ps("mod_ps%d" % i, [128, 4, 2]) for i in range(2)]
    k.dma('sp', c_sb[:], cT)
    k.dma('sp', bsb[:], b_adaT[l])
    k.op('act', lambda e: e.activation(out=sc[:], in_=c_sb[:], func=AF.Silu), reads=[c_sb], writes=[sc])
    wv = w_ada[l].rearrange("(kc p) c -> p kc c", p=128)
    for cs in range(24):
        slab = slabs[cs % 2]
        pst = pss[cs % 2]
        k.dma('sp' if cs % 2 == 0 else 'act', slab[:], wv[:, :, cs * 512:(cs + 1) * 512])
        for j in range(4):
            for kc in range(NCH):
                k.op('pe', lambda e, j=j, kc=kc: e.matmul(pst[:, j, :], lhsT=slab[:, kc, j * 128:(j + 1) * 128],
                                                       rhs=sc[:, kc, :], start=(kc == 0), stop=(kc == NCH - 1)),
                     reads=[slab, sc], writes=[pst])
        for r in range(2):
            k.op('dve', lambda e, r=r: e.tensor_tensor(out=modT[:, cs * 4:(cs + 1) * 4, r], in0=pst[:, :, r],
                                                      in1=bsb[:, cs * 4:(cs + 1) * 4], op=ALU.add),
                 reads=[pst, bsb], writes=[modT])
    P.release(m)


TOK_BLOCKS = [(0, 512, 0), (512, 512, 0), (1024, 512, 0), (1536, 512, 0), (2048, 256, 1)]


def make_consts(P):
    nc, k = P.nc, P.k
    C = {}
    C['ones_bf'] = sb(nc, "c_ones_bf", [128, 128], BF16)
    C['ones_f'] = sb(nc, "c_ones_f", [128, 128], F32)
    C['id_f'] = sb(nc, "c_id_f", [128, 128], F32)
    C['id_bf'] = sb(nc, "c_id_bf", [128, 128], BF16)
    k.op('pool', lambda e: e.memset(C['ones_f'][:], 1.0), writes=[C['ones_f']])
    k.op('dve', lambda e: e.tensor_copy(out=C['ones_bf'][:], in_=C['ones_f'][:]), reads=[C['ones_f']], writes=[C['ones_bf']])
    k.op('pool', lambda e: e.affine_select(out=C['id_f'][:], in_=C['ones_f'][:], pattern=[[-1, 128]],
                                           compare_op=ALU.is_equal, fill=0.0, base=0, channel_multiplier=1),
         reads=[C['ones_f']], writes=[C['id_f']])
    k.op('dve', lambda e: e.tensor_copy(out=C['id_bf'][:], in_=C['id_f'][:]), reads=[C['id_f']], writes=[C['id_bf']])
    return C


def ln_block(P, C, tmp, xt, nt, dst_fn, scale_fn, bias_fn, src_key=None):
    nc, k = P.nc, P.k
    xb, sq, ps_s, ps_q, mean, rstd, t1 = tmp['xb'], tmp['sq'], tmp['ps_s'], tmp['ps_q'], tmp['mean'], tmp['rstd'], tmp['t1']
    k.op('act', lambda e: e.activation(out=xb[:, :, :nt], in_=xt[:, :, :nt], func=AF.Copy), reads=[xt], writes=[xb])
    k.op('pool', lambda e: e.tensor_tensor(out=sq[:, :, :nt], in0=xt[:, :, :nt], in1=xt[:, :, :nt], op=ALU.mult),
         reads=[xt], writes=[sq])
    for c in range(NCH):
        k.op('pe', lambda e, c=c: e.matmul(ps_s[:, :nt], lhsT=C['ones_bf'][:], rhs=xb[:, c, :nt], start=(c == 0), stop=(c == NCH - 1)),
             reads=[xb, C['ones_bf']], writes=[ps_s])
    for c in range(NCH):
        k.op('pe', lambda e, c=c: e.matmul(ps_q[:, :nt], lhsT=C['ones_bf'][:], rhs=sq[:, c, :nt], start=(c == 0), stop=(c == NCH - 1)),
             reads=[sq, C['ones_bf']], writes=[ps_q])
    k.op('act', lambda e: e.mul(out=mean[:, :nt], in_=ps_s[:, :nt], mul=1.0 / D), reads=[ps_s], writes=[mean])
    k.op('dve', lambda e: e.tensor_tensor(out=rstd[:, :nt], in0=mean[:, :nt], in1=mean[:, :nt], op=ALU.mult),
         reads=[mean], writes=[rstd])
    k.op('dve', lambda e: e.scalar_tensor_tensor(out=rstd[:, :nt], in0=ps_q[:, :nt], scalar=1.0 / D, in1=rstd[:, :nt],
                                                 op0=ALU.mult, op1=ALU.subtract), reads=[ps_q, rstd], writes=[rstd])
    k.op('dve', lambda e: e.tensor_scalar_add(out=rstd[:, :nt], in0=rstd[:, :nt], scalar1=EPS), reads=[rstd], writes=[rstd])
    k.op('act', lambda e: e.activation(out=rstd[:, :nt], in_=rstd[:, :nt], func=AF.Ln), reads=[rstd], writes=[rstd])
    k.op('act', lambda e: e.activation(out=rstd[:, :nt], in_=rstd[:, :nt], func=AF.Exp, scale=-0.5), reads=[rstd], writes=[rstd])
    for c in range(NCH):
        tt = t1[c % 2]
        k.op('dve', lambda e, c=c, tt=tt: e.tensor_tensor(out=tt[:, :nt], in0=xt[:, c, :nt], in1=mean[:, :nt], op=ALU.subtract),
             reads=[xt, mean], writes=[tt])
        k.op('pool', lambda e, tt=tt: e.tensor_tensor(out=tt[:, :nt], in0=tt[:, :nt], in1=rstd[:, :nt], op=ALU.mult),
             reads=[tt, rstd], writes=[tt])
        dst, dkeys = dst_fn(c)
        s_ap, skeys = scale_fn(c)
        b_ap, bkeys = bias_fn(c)
        k.op('act', lambda e, tt=tt, dst=dst, s_ap=s_ap, b_ap=b_ap: e.activation(out=dst, in_=tt[:, :nt], func=AF.Identity,
                                                                               scale=s_ap, bias=b_ap),
             reads=[tt] + skeys + bkeys, writes=dkeys)


def ln_tmp(P, pfx, nmax=512):
    return {
        'xb': P.alloc_sb(pfx + "_xb", [128, NCH, nmax], BF16),
        'sq': P.alloc_sb(pfx + "_sq", [128, NCH, nmax], BF16),
        'ps_s': P.alloc_ps(pfx + "_pss", [128, nmax]),
        'ps_q': P.alloc_ps(pfx + "_psq", [128, nmax]),
        'mean': P.alloc_sb(pfx + "_mean", [128, nmax], F32),
        'rstd': P.alloc_sb(pfx + "_rstd", [128, nmax], F32),
        't1': [P.alloc_sb(pfx + "_t1%d" % i, [128, nmax], F32) for i in range(2)],
    }


def stage_inproj(P, C, l, xT, w_in, modp, pfm, ptm):
    nc, k = P.nc, P.k
    m = P.mark()
    hT = P.alloc_sb("ip_hT", [128, NCH, T], BF16)
    m2 = P.mark()
    tmp = ln_tmp(P, "ip")
    xts = [P.alloc_sb("ip_xt%d" % i, [128, NCH, 512], F32) for i in range(2)]
    xv = xT.rearrange("(c p) t -> p c t", p=128)
    for bi, (t0, nt, r) in enumerate(TOK_BLOCKS):
        xt = xts[bi % 2]
        k.dma('sp', xt[:, :, :nt], xv[:, :, t0:t0 + nt], writes=[xt])
        ln_block(P, C, tmp, xt, nt,
                 lambda c: (hT[:, c, t0:t0 + nt], [hT]),
                 lambda c: (modp['sc1p'][:, c, r:r + 1], [modp['sc1p']]),
                 lambda c: (modp['sh1'][:, c, r:r + 1], [modp['sh1']]))
    P.release(m2)
    slabs = [P.alloc_sb("ip_w%d" % i, [128, NCH, 512], BF16) for i in range(2)]
    pss = [P.alloc_ps("ip_ps%d" % i, [128, 512]) for i in range(4)]
    stg = [P.alloc_sb("ip_stg%d" % i, [128, 512], F32) for i in range(4)]
    wv = w_in[l].rearrange("(kc p) c -> p kc c", p=128)
    ev = 0
    for s in range(13):
        slab = slabs[s % 2]
        ncol = 512 if s < 12 else 32
        k.dma('pool', slab[:, :, :ncol], wv[:, :, s * 512:s * 512 + ncol], writes=[slab])
        if s < 6 or s == 12:
            row0 = s * 512 if s < 6 else 3072
            for j in range((ncol + 127) // 128):
                cw = min(128, ncol - j * 128)
                for (t0, nt, r) in TOK_BLOCKS:
                    pst = pss[ev % 4]; st = stg[ev % 4]
                    for kc in range(NCH):
                        k.op('pe', lambda e, kc=kc, pst=pst: e.matmul(pst[:cw, :nt], lhsT=slab[:, kc, j * 128:j * 128 + cw],
                                                                     rhs=hT[:, kc, t0:t0 + nt], start=(kc == 0), stop=(kc == NCH - 1)),
                             reads=[slab, hT], writes=[pst])
                    if ev % 2 == 0:
                        k.op('act', lambda e, pst=pst, st=st: e.copy(out=st[:cw, :nt], in_=pst[:cw, :nt]), reads=[pst], writes=[st])
                    else:
                        k.op('dve', lambda e, pst=pst, st=st: e.tensor_copy(out=st[:cw, :nt], in_=pst[:cw, :nt]), reads=[pst], writes=[st])
                    k.dma('sp', pfm[row0 + j * 128:row0 + j * 128 + cw, t0:t0 + nt], st[:cw, :nt], reads=[st], writes=[pfm])
                    ev += 1
        else:
            col0 = (s - 6) * 512
            for tt in range(T // 128):
                pst = pss[ev % 4]; st = stg[ev % 4]
                for kc in range(NCH):
                    k.op('pe', lambda e, kc=kc, pst=pst: e.matmul(pst[:, :], lhsT=hT[:, kc, tt * 128:(tt + 1) * 128],
                                                                 rhs=slab[:, kc, :], start=(kc == 0), stop=(kc == NCH - 1)),
                         reads=[slab, hT], writes=[pst])
                if ev % 2 == 0:
                    k.op('act', lambda e, pst=pst, st=st: e.copy(out=st[:, :], in_=pst[:, :]), reads=[pst], writes=[st])
                else:
                    k.op('dve', lambda e, pst=pst, st=st: e.tensor_copy(out=st[:, :], in_=pst[:, :]), reads=[pst], writes=[st])
                k.dma('sp', ptm[tt * 128:(tt + 1) * 128, col0:col0 + 512], st[:, :], reads=[st], writes=[ptm])
                ev += 1
    P.release(m)


def load_mod(P, modT):
    nc, k = P.nc, P.k
    mp = {}
    names = ['sh1', 'sc1p', 'g1', 'sh2', 'sc2p', 'g2']
    for j, n in enumerate(names):
        t = P.alloc_sb("modp_" + n, [128, NCH, 2], F32)
        if n.startswith('sc'):
            k.op('dve', lambda e, t=t, j=j: e.tensor_scalar_add(out=t[:], in0=modT[:, j * 16:(j + 1) * 16, :], scalar1=1.0),
                 reads=[modT], writes=[t])
        else:
            k.op('dve', lambda e, t=t, j=j: e.tensor_copy(out=t[:], in_=modT[:, j * 16:(j + 1) * 16, :]), reads=[modT], writes=[t])
        mp[n] = t
    return mp


HY_MIN = math.log(1e-2) / 1.5
HY_MAX = math.log(1e-2) / 0.3


def stage_hyena(P, C, l, W, pfm, mixT, tok0, Ls):
    nc, k = P.nc, P.k
    NT = Ls // 128
    M = 2 * Ls
    pf = "hy%d_" % Ls
    m0 = P.mark()
    hfb = P.alloc_sb(pf + "hfb", [128, NT, 1024], BF16)
    uT = P.alloc_sb(pf + "uT", [128, NT, 512], BF16)
    x0T = P.alloc_sb(pf + "x0T", [128, NT, 512], F32)
    frow = P.alloc_sb(pf + "frow", [128, Ls], F32)
    ncol = P.alloc_sb(pf + "ncol", [128, NT], F32)
    pa = [P.alloc_ps(pf + "pa%d" % i, [128, 512]) for i in range(7)]
    ptb = P.alloc_ps(pf + "ptb", [128, 512], BF16)
    ti = P.alloc_sb(pf + "ti", [128, Ls], I32)
    k.op('pool', lambda e: e.iota(ti[:], pattern=[[1, Ls]], base=0, channel_multiplier=0), writes=[ti])
    k.op('dve', lambda e: e.tensor_copy(out=frow[:], in_=ti[:]), reads=[ti], writes=[frow])
    k.op('pool', lambda e: e.iota(ti[:, :NT], pattern=[[128, NT]], base=0, channel_multiplier=1), reads=[frow], writes=[ti])
    k.op('dve', lambda e: e.tensor_copy(out=ncol[:], in_=ti[:, :NT]), reads=[ti], writes=[ncol])

    m1 = P.mark()
    w1 = P.alloc_sb(pf + "w1", [33, 64], F32)
    w2 = P.alloc_sb(pf + "w2", [64, 64], F32)
    w3 = P.alloc_sb(pf + "w3", [64, 1024], F32)
    fv = P.alloc_sb(pf + "fv", [64, 3], F32)
    fc = P.alloc_sb(pf + "fc", [64, 4], F32)
    hb = P.alloc_sb(pf + "hb", [1, 512], F32)
    k.dma('sp', w1[:], W['hy_f_w1'][l])
    k.dma('sp', w2[:], W['hy_f_w2'][l])
    k.dma('sp', w3[:], W['hy_f_w3'][l])
    k.dma('sp', fv[:], W['hy_fv'][l])
    k.dma('sp', hb[:], W['hy_bias'][l:l + 1, :])
    k.op('dve', lambda e: e.tensor_scalar_mul(out=fc[:, 0:1], in0=fv[:, 2:3], scalar1=1.0 / (2 * math.pi)), reads=[fv], writes=[fc])
    for i in range(2):
        k.op('dve', lambda e, i=i: e.tensor_scalar(out=fc[:, 1 + i:2 + i], in0=fv[:, i:i + 1], scalar1=fc[:, 0:1], scalar2=0.0,
                                                  op0=ALU.mult, op1=ALU.add), reads=[fv, fc], writes=[fc])
    pc = P.alloc_sb(pf + "pc", [33, 4], F32)
    pi_ = P.alloc_sb(pf + "pi", [33, 1], I32)
    k.op('pool', lambda e: e.iota(pi_[:], pattern=[[0, 1]], base=0, channel_multiplier=1), writes=[pi_])
    k.op('dve', lambda e: e.tensor_copy(out=pc[:, 0:1], in_=pi_[:]), reads=[pi_], writes=[pc])
    step = (15.0 - 1e-4) / 15.0
    k.op('dve', lambda e: e.tensor_scalar(out=pc[:, 3:4], in0=pc[:, 0:1], scalar1=16.5, scalar2=-16.0, op0=ALU.is_gt, op1=ALU.mult),
         reads=[pc], writes=[pc])
    k.op('dve', lambda e: e.tensor_tensor(out=pc[:, 1:2], in0=pc[:, 0:1], in1=pc[:, 3:4], op=ALU.add), reads=[pc], writes=[pc])
    k.op('dve', lambda e: e.tensor_scalar(out=pc[:, 1:2], in0=pc[:, 1:2], scalar1=step / Ls, scalar2=(1e-4 - step) / Ls, op0=ALU.mult, op1=ALU.add),
         reads=[pc], writes=[pc])
    k.op('dve', lambda e: e.tensor_scalar(out=pc[:, 2:3], in0=pc[:, 0:1], scalar1=16.5, scalar2=-0.25, op0=ALU.is_lt, op1=ALU.mult),
         reads=[pc], writes=[pc])
    k.op('dve', lambda e: e.tensor_scalar_add(out=pc[:, 2:3], in0=pc[:, 2:3], scalar1=0.5), reads=[pc], writes=[pc])
    wi_ = P.alloc_sb(pf + "wi", [64, Ls], I32)
    wf_ = P.alloc_sb(pf + "wff", [64, Ls], F32)

    def wrap(t, np_):
        k.op('dve', lambda e: e.tensor_copy(out=wi_[:np_, :], in_=t[:np_, :]), reads=[t], writes=[wi_])
        k.op('pool', lambda e: e.tensor_copy(out=wf_[:np_, :], in_=wi_[:np_, :]), reads=[wi_], writes=[wf_])
        k.op('dve', lambda e: e.tensor_tensor(out=t[:np_, :], in0=t[:np_, :], in1=wf_[:np_, :], op=ALU.subtract), reads=[t, wf_], writes=[t])
    zT = P.alloc_sb(pf + "zT", [33, Ls], F32)
    h1 = P.alloc_sb(pf + "h1", [64, Ls], F32)
    h2 = P.alloc_sb(pf + "h2", [64, Ls], F32)
    k.op('dve', lambda e: e.tensor_scalar(out=zT[:], in0=frow[:33, :], scalar1=pc[:, 1:2], scalar2=pc[:, 2:3], op0=ALU.mult, op1=ALU.add),
         reads=[frow, pc], writes=[zT])
    wrap(zT, 33)
    k.op('act', lambda e: e.activation(out=zT[:], in_=zT[:], func=AF.Sin, scale=2 * math.pi), reads=[zT], writes=[zT])
    k.op('act', lambda e: e.mul(out=zT[0:1, :], in_=frow[0:1, :], mul=1.0 / (Ls - 1)), reads=[frow, zT], writes=[zT])
    BL = min(512, Ls)
    for (src, wt, dst, ci) in ((zT, w1, h1, 1), (h1, w2, h2, 2)):
        for b0 in range(0, Ls, BL):
            pst = pa[(b0 // BL) % 2]
            k.op('pe', lambda e, pst=pst, src=src, wt=wt, b0=b0: e.matmul(pst[:64, :BL], lhsT=wt[:], rhs=src[:, b0:b0 + BL], start=True, stop=True),
                 reads=[wt, src], writes=[pst])
            k.op('dve', lambda e, pst=pst, dst=dst, b0=b0, ci=ci: e.tensor_scalar(out=dst[:, b0:b0 + BL], in0=pst[:64, :BL], scalar1=fc[:, 0:1],
                                                                                 scalar2=fc[:, ci:ci + 1], op0=ALU.mult, op1=ALU.add),
                 reads=[pst, fc], writes=[dst])
        wrap(dst, 64)
        k.op('act', lambda e, dst=dst: e.activation(out=dst[:], in_=dst[:], func=AF.Sin, scale=2 * math.pi), reads=[dst], writes=[dst])
    drow = P.alloc_sb(pf + "drow", [128, 512], F32)
    negt = P.alloc_sb(pf + "negt", [128, NT], F32)
    k.op('dve', lambda e: e.tensor_scalar(out=drow[:], in0=frow[:, :512] if Ls >= 512 else frow[:, :], scalar1=-(HY_MAX - HY_MIN) / 511.0, scalar2=-HY_MIN,
                                          op0=ALU.mult, op1=ALU.add), reads=[frow], writes=[drow]) if Ls >= 512 else None
    if Ls < 512:
        ti2 = P.alloc_sb(pf + "ti2", [128, 512], I32)
        k.op('pool', lambda e: e.iota(ti2[:], pattern=[[1, 512]], base=0, channel_multiplier=0), writes=[ti2])
        k.op('dve', lambda e: e.tensor_copy(out=drow[:], in_=ti2[:]), reads=[ti2], writes=[drow])
        k.op('dve', lambda e: e.tensor_scalar(out=drow[:], in0=drow[:], scalar1=-(HY_MAX - HY_MIN) / 511.0, scalar2=-HY_MIN,
                                              op0=ALU.mult, op1=ALU.add), reads=[drow], writes=[drow])
    k.op('dve', lambda e: e.tensor_scalar_mul(out=negt[:], in0=ncol[:], scalar1=-1.0 / (Ls - 1)), reads=[ncol], writes=[negt])
    dec = P.alloc_sb(pf + "dec", [128, 512], F32)
    h0 = P.alloc_sb(pf + "h0", [128, 1024], F32)
    for tc in range(NT):
        k.op('act', lambda e, tc=tc: e.activation(out=dec[:], in_=drow[:], func=AF.Exp, scale=negt[:, tc:tc + 1]), reads=[drow, negt], writes=[dec])
        for hf in range(2):
            pst = pa[2 + hf]
            k.op('pe', lambda e, pst=pst, tc=tc, hf=hf: e.matmul(pst[:, :], lhsT=h2[:, tc * 128:(tc + 1) * 128], rhs=w3[:, hf * 512:(hf + 1) * 512],
                                                               start=True, stop=True), reads=[h2, w3], writes=[pst])
            if tc == 0:
                k.op('dve', lambda e, pst=pst, hf=hf: e.tensor_tensor(out=h0[:, hf * 512:(hf + 1) * 512], in0=pst[:, :], in1=dec[:], op=ALU.mult),
                     reads=[pst, dec], writes=[h0])
            else:
                k.op('dve', lambda e, pst=pst, tc=tc, hf=hf: e.tensor_tensor(out=hfb[:, tc, hf * 512:(hf + 1) * 512], in0=pst[:, :], in1=dec[:], op=ALU.mult),
                     reads=[pst, dec], writes=[hfb])
        if tc == 0:
            k.op('dve', lambda e: e.tensor_tensor(out=h0[0:1, 0:512], in0=h0[0:1, 0:512], in1=hb[:], op=ALU.add), reads=[h0, hb], writes=[h0])
            k.op('pool', lambda e: e.memset(h0[0:1, 512:1024], 0.0), reads=[h0], writes=[h0])
            k.op('act', lambda e: e.copy(out=hfb[:, 0, :], in_=h0[:]), reads=[h0], writes=[hfb])
    P.release(m1)

    m2 = P.mark()
    sw = P.alloc_sb(pf + "sw", [128, 12, 4], F32)
    k.dma('sp', sw[:], W['hy_sw'][l])
    raws = [P.alloc_sb(pf + "raw%d" % i, [128, Ls], F32) for i in range(2)]
    cvs = [P.alloc_sb(pf + "cv%d" % i, [128, Ls], F32) for i in range(3)]
    ub = P.alloc_sb(pf + "ub", [128, Ls], BF16)

    def conv(j, dst, ri):
        raw = raws[ri]
        k.dma('sp', raw[:], pfm[j * 128:(j + 1) * 128, tok0:tok0 + Ls], writes=[raw])
        k.op('act', lambda e: e.activation(out=dst[:], in_=raw[:], func=AF.Identity, scale=sw[:, j, 1:2], bias=sw[:, j, 3:4]),
             reads=[raw, sw], writes=[dst])
        k.op('dve', lambda e: e.scalar_tensor_tensor(out=dst[:, 1:], in0=raw[:, :Ls - 1], scalar=sw[:, j, 0:1], in1=dst[:, 1:],
                                                     op0=ALU.mult, op1=ALU.add), reads=[raw, sw, dst], writes=[dst])
        k.op('dve', lambda e: e.scalar_tensor_tensor(out=dst[:, :Ls - 1], in0=raw[:, 1:], scalar=sw[:, j, 2:3], in1=dst[:, :Ls - 1],
                                                     op0=ALU.mult, op1=ALU.add), reads=[raw, sw, dst], writes=[dst])

    for j in range(4):
        conv(j, cvs[0], 0)
        for tc in range(NT):
            pst = pa[tc % 2]
            k.op('pe', lambda e, pst=pst, tc=tc: e.transpose(out=pst[:, :128], in_=cvs[0][:, tc * 128:(tc + 1) * 128], identity=C['id_f'][:]),
                 reads=[cvs[0], C['id_f']], writes=[pst])
            k.op('act', lambda e, pst=pst, tc=tc, j=j: e.copy(out=x0T[:, tc, j * 128:(j + 1) * 128], in_=pst[:, :128]), reads=[pst], writes=[x0T])
        conv(4 + j, cvs[1], 1)
        conv(8 + j, cvs[2], 0)
        k.op('pool', lambda e: e.tensor_tensor(out=ub[:], in0=cvs[1][:], in1=cvs[2][:], op=ALU.mult), reads=[cvs[1], cvs[2]], writes=[ub])
        for tc in range(NT):
            k.op('pe', lambda e, tc=tc: e.transpose(out=ptb[:, :128], in_=ub[:, tc * 128:(tc + 1) * 128], identity=C['id_bf'][:]),
                 reads=[ub, C['id_bf']], writes=[ptb])
            k.op('dve', lambda e, tc=tc, j=j: e.tensor_copy(out=uT[:, tc, j * 128:(j + 1) * 128], in_=ptb[:, :128]), reads=[ptb], writes=[uT])
    P.release(m2)

    Asb = P.alloc_sb(pf + "A", [128, NT, 512], BF16)
    Bsb = P.alloc_sb(pf + "B", [128, NT, 512], BF16)
    csb = [P.alloc_sb(pf + "cs%d" % i, [128, 2, 128], BF16) for i in range(3)]
    pm = [P.alloc_sb(pf + "pm%d" % i, [128, 2, 128], F32) for i in range(3)]
    pmi = [P.alloc_sb(pf + "pmi%d" % i, [128, 2, 128], I32) for i in range(3)]
    pmf = [P.alloc_sb(pf + "pmf%d" % i, [128, 2, 128], F32) for i in range(3)]
    wf = P.alloc_sb(pf + "wf", [128, NT], F32)
    nwf = P.alloc_sb(pf + "nwf", [128, NT], F32)
    k.op('pool', lambda e: e.memset(wf[:], 2.0 / M), writes=[wf])
    k.op('pool', lambda e: e.memset(wf[0:1, 0:1], 1.0 / M), reads=[wf], writes=[wf])
    k.op('dve', lambda e: e.tensor_scalar_mul(out=nwf[:], in0=wf[:], scalar1=-1.0), reads=[wf], writes=[nwf])
    gi = [0]

    def gen(a, b):
        i = gi[0] % 3
        gi[0] += 1
        k.op('dve', lambda e: e.tensor_scalar(out=pm[i][:, 0, :], in0=frow[:, b * 128:(b + 1) * 128], scalar1=ncol[:, a:a + 1], scalar2=1.0 / M,
                                              op0=ALU.mult, op1=ALU.mult), reads=[frow, ncol], writes=[pm[i]])
        k.op('pool', lambda e: e.tensor_scalar_add(out=pm[i][:, 1, :], in0=pm[i][:, 0, :], scalar1=0.25), reads=[pm[i]], writes=[pm[i]])
        k.op('dve', lambda e: e.tensor_copy(out=pmi[i][:], in_=pm[i][:]), reads=[pm[i]], writes=[pmi[i]])
        k.op('pool', lambda e: e.tensor_copy(out=pmf[i][:], in_=pmi[i][:]), reads=[pmi[i]], writes=[pmf[i]])
        k.op('dve', lambda e: e.tensor_tensor(out=pm[i][:], in0=pm[i][:], in1=pmf[i][:], op=ALU.subtract), reads=[pm[i], pmf[i]], writes=[pm[i]])
        k.op('act', lambda e: e.activation(out=csb[i][:], in_=pm[i][:], func=AF.Sin, scale=2 * math.pi), reads=[pm[i]], writes=[csb[i]])
        return csb[i][:, 1, :], csb[i][:, 0, :], csb[i]

    def cols(N, j):
        return uT[:, N, :] if j == 0 else hfb[:, N, (j - 1) * 512:j * 512]

    alt = P.alloc_sb(pf + "alt", [128, 128], F32)
    altb = P.alloc_sb(pf + "altb", [128, 128], BF16)
    alti = P.alloc_sb(pf + "alti", [128, 128], I32)
    altf = P.alloc_sb(pf + "altf", [128, 128], F32)
    k.op('dve', lambda e: e.tensor_scalar(out=alt[:], in0=frow[:, :128], scalar1=ncol[:, 0:1], scalar2=0.5, op0=ALU.add, op1=ALU.mult),
         reads=[frow, ncol], writes=[alt])
    k.op('dve', lambda e: e.tensor_scalar_add(out=alt[:], in0=alt[:], scalar1=0.25), reads=[alt], writes=[alt])
    k.op('dve', lambda e: e.tensor_copy(out=alti[:], in_=alt[:]), reads=[alt], writes=[alti])
    k.op('dve', lambda e: e.tensor_copy(out=altf[:], in_=alti[:]), reads=[alti], writes=[altf])
    k.op('dve', lambda e: e.tensor_tensor(out=alt[:], in0=alt[:], in1=altf[:], op=ALU.subtract), reads=[alt, altf], writes=[alt])
    k.op('act', lambda e: e.activation(out=altb[:], in_=alt[:], func=AF.Sin, scale=2 * math.pi), reads=[alt], writes=[altb])
    for j in range(3):
        for N in range(NT):
            k.op('pe', lambda e, j=j, N=N: e.matmul(pa[j][0:1, :], lhsT=altb[:, 0:1], rhs=cols(N, j), start=(N == 0), stop=(N == NT - 1)),
                 reads=[altb, uT, hfb], writes=[pa[j]])
    nyq = P.alloc_sb(pf + "nyq", [1, 2, 512], F32)
    nyqb = P.alloc_sb(pf + "nyqb", [1, 512], BF16)
    k.op('act', lambda e: e.copy(out=nyq[:, 0, :], in_=pa[1][0:1, :]), reads=[pa[1]], writes=[nyq])
    k.op('dve', lambda e: e.tensor_tensor(out=nyq[:, 0, :], in0=pa[2][0:1, :], in1=nyq[:, 0, :], op=ALU.add), reads=[pa[2], nyq], writes=[nyq])
    k.op('dve', lambda e: e.tensor_tensor(out=nyq[:, 1, :], in0=pa[0][0:1, :], in1=nyq[:, 0, :], op=ALU.mult), reads=[pa[0], nyq], writes=[nyq])
    k.op('act', lambda e: e.mul(out=nyqb[:], in_=nyq[:, 1, :], mul=1.0 / M), reads=[nyq], writes=[nyqb])

    ev = [P.alloc_sb(pf + "ev%d" % i, [128, 512], F32) for i in range(4)]
    tt = [P.alloc_sb(pf + "tt%d" % i, [128, 512], F32) for i in range(4)]
    for F in range(NT):
        for N in range(NT):
            cbk, sbl, ck = gen(N, F)
            for j in range(3):
                k.op('pe', lambda e, j=j, N=N, cbk=cbk: e.matmul(pa[j][:, :], lhsT=cbk, rhs=cols(N, j), start=(N == 0), stop=(N == NT - 1)),
                     reads=[ck, uT, hfb], writes=[pa[j]])
            for j in range(3):
                k.op('pe', lambda e, j=j, N=N, sbl=sbl: e.matmul(pa[3 + j][:, :], lhsT=sbl, rhs=cols(N, j), start=(N == 0), stop=(N == NT - 1)),
                     reads=[ck, uT, hfb], writes=[pa[3 + j]])
        k.op('act', lambda e: e.copy(out=ev[0][:], in_=pa[1][:, :]), reads=[pa[1]], writes=[ev[0]])
        k.op('act', lambda e: e.copy(out=ev[1][:], in_=pa[4][:, :]), reads=[pa[4]], writes=[ev[1]])
        k.op('dve', lambda e: e.tensor_tensor(out=ev[0][:], in0=pa[2][:, :], in1=ev[0][:], op=ALU.add), reads=[pa[2], ev[0]], writes=[ev[0]])
        k.op('dve', lambda e: e.tensor_tensor(out=ev[1][:], in0=pa[5][:, :], in1=ev[1][:], op=ALU.subtract), reads=[pa[5], ev[1]], writes=[ev[1]])
        k.op('dve', lambda e: e.tensor_tensor(out=tt[0][:], in0=pa[0][:, :], in1=ev[0][:], op=ALU.mult), reads=[pa[0], ev[0]], writes=[tt[0]])
        k.op('dve', lambda e: e.tensor_tensor(out=tt[1][:], in0=pa[3][:, :], in1=ev[1][:], op=ALU.mult), reads=[pa[3], ev[1]], writes=[tt[1]])
        k.op('pool', lambda e: e.tensor_tensor(out=tt[0][:], in0=tt[0][:], in1=tt[1][:], op=ALU.add), reads=[tt[0], tt[1]], writes=[tt[0]])
        k.op('act', lambda e, F=F: e.activation(out=Asb[:, F, :], in_=tt[0][:], func=AF.Copy, scale=wf[:, F:F + 1]), reads=[tt[0], wf], writes=[Asb])
        k.op('dve', lambda e: e.tensor_tensor(out=tt[2][:], in0=pa[0][:, :], in1=ev[1][:], op=ALU.mult), reads=[pa[0], ev[1]], writes=[tt[2]])
        k.op('dve', lambda e: e.tensor_tensor(out=tt[3][:], in0=pa[3][:, :], in1=ev[0][:], op=ALU.mult), reads=[pa[3], ev[0]], writes=[tt[3]])
        k.op('pool', lambda e: e.tensor_tensor(out=tt[2][:], in0=tt[2][:], in1=tt[3][:], op=ALU.subtract), reads=[tt[2], tt[3]], writes=[tt[2]])
        k.op('act', lambda e, F=F: e.activation(out=Bsb[:, F, :], in_=tt[2][:], func=AF.Copy, scale=nwf[:, F:F + 1]), reads=[tt[2], nwf], writes=[Bsb])
    nw = P.alloc_sb(pf + "nw", [128, 4], F32)
    k.dma('sp', nw[:], W['hy_norm_wT'][l])
    ys = [P.alloc_sb(pf + "y%d" % i, [128, 512], F32) for i in range(2)]
    yb = [P.alloc_sb(pf + "yb%d" % i, [128, 512], BF16) for i in range(2)]
    junk = P.alloc_sb(pf + "junk", [128, 512], F32)
    ss = P.alloc_sb(pf + "ss", [128, 2], F32)
    mst = [P.alloc_sb(pf + "mst%d" % i, [128, 4, 128], BF16) for i in range(2)]
    for N in range(NT):
        py = pa[N % 2]
        for F in range(NT):
            cbk, sbl, ck = gen(F, N)
            k.op('pe', lambda e, F=F, cbk=cbk: e.matmul(py[:, :], lhsT=cbk, rhs=Asb[:, F, :], start=(F == 0), stop=False),
                 reads=[ck, Asb], writes=[py])
            k.op('pe', lambda e, F=F, sbl=sbl: e.matmul(py[:, :], lhsT=sbl, rhs=Bsb[:, F, :], start=False, stop=False),
                 reads=[ck, Bsb], writes=[py])
        k.op('pe', lambda e: e.matmul(py[:, :], lhsT=altb[0:1, :], rhs=nyqb[:], start=False, stop=True), reads=[altb, nyqb], writes=[py])
        y = ys[N % 2]; ybf = yb[N % 2]; ms = mst[N % 2]
        k.op('dve', lambda e, N=N: e.tensor_tensor(out=y[:], in0=py[:, :], in1=x0T[:, N, :], op=ALU.mult), reads=[py, x0T], writes=[y])
        k.op('act', lambda e: e.activation(out=junk[:], in_=y[:], func=AF.Square, accum_out=ss[:, 0:1]), reads=[y], writes=[junk, ss])
        k.op('dve', lambda e: e.tensor_scalar(out=ss[:, 1:2], in0=ss[:, 0:1], scalar1=1.0 / HY, scalar2=EPS, op0=ALU.mult, op1=ALU.add), reads=[ss], writes=[ss])
        k.op('act', lambda e: e.activation(out=ss[:, 1:2], in_=ss[:, 1:2], func=AF.Ln), reads=[ss], writes=[ss])
        k.op('act', lambda e: e.activation(out=ss[:, 1:2], in_=ss[:, 1:2], func=AF.Exp, scale=-0.5), reads=[ss], writes=[ss])
        k.op('dve', lambda e: e.tensor_scalar_mul(out=ybf[:], in0=y[:], scalar1=ss[:, 1:2]), reads=[y, ss], writes=[ybf])
        for j in range(4):
            k.op('pe', lambda e, j=j: e.transpose(out=ptb[:, j * 128:(j + 1) * 128], in_=ybf[:, j * 128:(j + 1) * 128], identity=C['id_bf'][:]),
                 reads=[ybf, C['id_bf']], writes=[ptb])
        for j in range(4):
            k.op('act', lambda e, j=j: e.activation(out=ms[:, j, :], in_=ptb[:, j * 128:(j + 1) * 128], func=AF.Copy, scale=nw[:, j:j + 1]),
                 reads=[ptb, nw], writes=[ms])
        k.dma('sp', mixT[0:512, tok0 + N * 128:tok0 + (N + 1) * 128].rearrange("(j p) t -> p j t", p=128), ms[:], reads=[ms], writes=[mixT])
    P.release(m0)


NEGM = -1.0e4


def stage_na(P, C, l, W, pfm, ptm, mixT, rpbp, with_ctx):
    nc, k = P.nc, P.k
    pf = "na_"
    m0 = P.mark()
    scale = 128 ** -0.5
    qT = P.alloc_sb(pf + "qT", [128, NAH, T], BF16)
    kT = P.alloc_sb(pf + "kT", [128, NAH, T], BF16)
    v1 = P.alloc_sb(pf + "v1", [128, T // 128, NAH, 129], BF16)
    R = P.alloc_sb(pf + "R", [128, NAH, 9, 128], F32)
    k.op('pool', lambda e: e.memset(v1[:], 1.0), writes=[v1])
    for h in range(NAH):
        k.dma('pool', qT[:, h, :], pfm[O_NQ + h * 128:O_NQ + (h + 1) * 128, :], writes=[qT])
        k.dma('pool', kT[:, h, :], pfm[O_NK + h * 128:O_NK + (h + 1) * 128, :], writes=[kT])
    for tt in range(T // 128):
        k.dma('pool', v1[:, tt, :, 0:128], ptm[tt * 128:(tt + 1) * 128, 0:768].rearrange("p (h d) -> p h d", h=NAH), writes=[v1])
    m1 = P.mark()
    zr = P.alloc_sb(pf + "zr", [90, 160], F32)
    k.op('pool', lambda e: e.memset(zr[:], 0.0), writes=[zr])
    k.dma('sp', zr[:, 48:79], W['na_rpb'][l].rearrange("h r m -> (h r) m"), writes=[zr])
    k.dma('sp', rpbp.rearrange("h r m -> (h r) m"), zr[:], reads=[zr], writes=[rpbp])
    Hk = P.alloc_sb(pf + "Hk", [64, NAH, 15, 64], F32)
    for h in range(NAH):
        src = bass.AP(rpbp.tensor, rpbp[h, 0, 0:1].offset, [[1, 64], [160, 15], [1, 64]])
        k.dma('sp', Hk[:, h, :, :], src, reads=[rpbp], writes=[Hk])
    J = P.alloc_sb(pf + "J", [64, 64], F32)
    k.op('pool', lambda e: e.affine_select(out=J[:], in_=C['ones_f'][:64, :64], pattern=[[1, 64]], compare_op=ALU.is_equal, fill=0.0,
                                           base=-63, channel_multiplier=1), reads=[C['ones_f']], writes=[J])
    ms = [P.alloc_sb(pf + "ms%d" % i, [128, 2, 64], F32) for i in range(4)]
    CM = P.alloc_sb(pf + "CM", [128, 2, 64], F32)
    ones3 = C['ones_f'][:, :].rearrange("p (c q) -> p c q", c=2)
    for a in range(2):
        sl = slice(a * 64, (a + 1) * 64)
        k.op('pool', lambda e, sl=sl, a=a: e.affine_select(out=ms[0][sl], in_=ones3[sl], pattern=[[0, 2], [-1, 64]], compare_op=ALU.is_ge, fill=0.0,
                                                          base=8, channel_multiplier=1), reads=[C['ones_f']], writes=[ms[0]])
        k.op('pool', lambda e, sl=sl, a=a: e.affine_select(out=ms[1][sl], in_=ones3[sl], pattern=[[0, 2], [0, 64]], compare_op=ALU.is_ge, fill=0.0,
                                                          base=-48, channel_multiplier=1), reads=[C['ones_f']], writes=[ms[1]])
        k.op('pool', lambda e, sl=sl, a=a: e.affine_select(out=ms[2][sl], in_=ones3[sl], pattern=[[0, 2], [1, 64]], compare_op=ALU.is_ge, fill=0.0,
                                                          base=7, channel_multiplier=-1), reads=[C['ones_f']], writes=[ms[2]])
        k.op('pool', lambda e, sl=sl, a=a: e.affine_select(out=ms[3][sl], in_=ones3[sl], pattern=[[0, 2], [0, 64]], compare_op=ALU.is_ge, fill=0.0,
                                                          base=15, channel_multiplier=-1), reads=[C['ones_f']], writes=[ms[3]])
    k.op('dve', lambda e: e.tensor_tensor(out=ms[0][:], in0=ms[0][:], in1=ms[1][:], op=ALU.max), reads=[ms[0], ms[1]], writes=[ms[0]])
    k.op('dve', lambda e: e.tensor_tensor(out=ms[2][:], in0=ms[2][:], in1=ms[3][:], op=ALU.max), reads=[ms[2], ms[3]], writes=[ms[2]])
    k.op('dve', lambda e: e.tensor_tensor(out=CM[:], in0=ms[0][:], in1=ms[2][:], op=ALU.mult), reads=[ms[0], ms[2]], writes=[CM])
    k.op('dve', lambda e: e.tensor_copy(out=ms[0][:], in_=CM[:]), reads=[CM], writes=[ms[0]])
    k.op('dve', lambda e: e.tensor_scalar(out=CM[:], in0=CM[:], scalar1=-1.0, scalar2=-NEGM, op0=ALU.add, op1=ALU.mult), reads=[CM], writes=[CM])
    pr = [P.alloc_ps(pf + "pr%d" % i, [128, 2, 64]) for i in range(2)]
    cnt = 0
    for h in range(NAH):
        for vi in range(9):
            d = vi - 3 if vi < 7 else (-2 if vi == 7 else 2)
            pst = pr[cnt % 2]
            cnt += 1
            for c in range(2):
                di = 2 * d - c + 7
                k.op('pe', lambda e, pst=pst, h=h, di=di, c=c: e.matmul(pst[:, c, :], lhsT=Hk[:, h, di:di + 2, :], rhs=J[:], start=True, stop=True),
                     reads=[Hk, J], writes=[pst])
            k.op('dve', lambda e, pst=pst, h=h, vi=vi: e.tensor_tensor(out=R[:, h, vi, :].rearrange("p (c q) -> p c q", c=2), in0=pst[:], in1=ms[0][:], op=ALU.mult),
                 reads=[pst, ms[0]], writes=[R])
            k.op('pool', lambda e, h=h, vi=vi: e.tensor_tensor(out=R[:, h, vi, :].rearrange("p (c q) -> p c q", c=2), in0=R[:, h, vi, :].rearrange("p (c q) -> p c q", c=2),
                                                              in1=CM[:], op=ALU.add), reads=[R, CM], writes=[R])
            if vi == 7:
                k.op('pool', lambda e, h=h, vi=vi: e.memset(R[0:64, h, vi, 64:128], NEGM), reads=[R], writes=[R])
            if vi == 8:
                k.op('pool', lambda e, h=h, vi=vi: e.memset(R[:, h, vi, 0:64], NEGM), reads=[R], writes=[R])
                k.op('pool', lambda e, h=h, vi=vi: e.memset(R[64:128, h, vi, 64:128], NEGM), reads=[R], writes=[R])
    P.dump('na_R', R)
    P.dump('na_Hk', Hk)
    P.release(m1)
    nw = P.alloc_sb(pf + "nw", [128, NAH], F32)
    k.dma('sp', nw[:], W['na_norm_wT'][l])
    pS = [P.alloc_ps(pf + "pS%d" % i, [128, 7, 128]) for i in range(2)]
    pO = [P.alloc_ps(pf + "pO%d" % i, [128, 129]) for i in range(2)]
    ptb = P.alloc_ps(pf + "ptb", [128, NAH, 128], BF16)
    ein = [P.alloc_sb(pf + "ein%d" % i, [128, 5, 128], F32) for i in range(2)]
    PT = [P.alloc_sb(pf + "PT%d" % i, [128, 7, 128], BF16) for i in range(2)]
    ob = [P.alloc_sb(pf + "ob%d" % i, [128, NAH * 128], F32) for i in range(2)]
    obb = [P.alloc_sb(pf + "obb%d" % i, [128, NAH * 128], BF16) for i in range(2)]
    rc = P.alloc_sb(pf + "rc", [128, 2], F32)
    ss = P.alloc_sb(pf + "ss", [128, 2], F32)
    junk = P.alloc_sb(pf + "junk", [128, NAH * 128], F32)
    mst = [P.alloc_sb(pf + "mst%d" % i, [128, NAH, 128], BF16) for i in range(2)]
    CT = [L // 128, L // 128 + 1]
    units = []
    for i in range(16):
        if 2 <= i <= 13:
            loc = [(i - 2, 7), (i - 1, 2), (i, 3), (i + 1, 4), (i + 2, 8)]
        elif i < 2:
            loc = [(j, j - i + 3) for j in range(4)]
        else:
            loc = [(j, j - i + 3) for j in range(12, 16)]
        units.append((i, loc, True))
    if with_ctx:
        units += [(16, [], True), (17, [], True)]
    u = 0
    for (qi, loc, _) in units:
        o_t = ob[u % 2]; o_b = obb[u % 2]; ms_ = mst[u % 2]
        for h in range(NAH):
            g = (u * NAH + h) % 2
            ps_, po, ei, pt = pS[g], pO[g], ein[g], PT[g]
            tiles = [j for (j, _) in loc] + CT
            nl = len(loc)
            for jj, j in enumerate(tiles):
                k.op('pe', lambda e, jj=jj, j=j, h=h, ps_=ps_: e.matmul(ps_[:, jj, :], lhsT=kT[:, h, j * 128:(j + 1) * 128], rhs=qT[:, h, qi * 128:(qi + 1) * 128],
                                                                       start=True, stop=True), reads=[kT, qT], writes=[ps_])
            for jj, (j, vi) in enumerate(loc):
                k.op('dve', lambda e, jj=jj, vi=vi, h=h, ps_=ps_, ei=ei: e.scalar_tensor_tensor(out=ei[:, jj, :], in0=ps_[:, jj, :], scalar=scale, in1=R[:, h, vi, :],
                                                                                               op0=ALU.mult, op1=ALU.add), reads=[ps_, R], writes=[ei])
            if nl:
                k.op('act', lambda e, nl=nl, ei=ei, pt=pt: e.activation(out=pt[:, 0:nl, :], in_=ei[:, 0:nl, :], func=AF.Exp), reads=[ei], writes=[pt])
            k.op('act', lambda e, nl=nl, ps_=ps_, pt=pt: e.activation(out=pt[:, nl:nl + 2, :], in_=ps_[:, nl:nl + 2, :], func=AF.Exp, scale=scale),
                 reads=[ps_], writes=[pt])
            for jj, j in enumerate(tiles):
                k.op('pe', lambda e, jj=jj, j=j, h=h, pt=pt, po=po: e.matmul(po[:, :], lhsT=pt[:, jj, :], rhs=v1[:, j, h, :], start=(jj == 0), stop=(jj == len(tiles) - 1)),
                     reads=[pt, v1], writes=[po])
            k.op('dve', lambda e, po=po: e.reciprocal(out=rc[:, 0:1], in_=po[:, 128:129]), reads=[po], writes=[rc])
            k.op('dve', lambda e, po=po, h=h, o_t=o_t: e.tensor_scalar_mul(out=o_t[:, h * 128:(h + 1) * 128], in0=po[:, 0:128], scalar1=rc[:, 0:1]),
                 reads=[po, rc], writes=[o_t])
        k.op('act', lambda e, o_t=o_t: e.activation(out=junk[:], in_=o_t[:], func=AF.Square, accum_out=ss[:, 0:1]), reads=[o_t], writes=[junk, ss])
        k.op('dve', lambda e: e.tensor_scalar(out=ss[:, 1:2], in0=ss[:, 0:1], scalar1=1.0 / 768, scalar2=EPS, op0=ALU.mult, op1=ALU.add), reads=[ss], writes=[ss])
        k.op('act', lambda e: e.activation(out=ss[:, 1:2], in_=ss[:, 1:2], func=AF.Ln), reads=[ss], writes=[ss])
        k.op('act', lambda e: e.activation(out=ss[:, 1:2], in_=ss[:, 1:2], func=AF.Exp, scale=-0.5), reads=[ss], writes=[ss])
        k.op('dve', lambda e, o_t=o_t, o_b=o_b: e.tensor_scalar_mul(out=o_b[:], in0=o_t[:], scalar1=ss[:, 1:2]), reads=[o_t, ss], writes=[o_b])
        for h in range(NAH):
            k.op('pe', lambda e, h=h, o_b=o_b: e.transpose(out=ptb[:, h, :], in_=o_b[:, h * 128:(h + 1) * 128], identity=C['id_bf'][:]),
                 reads=[o_b, C['id_bf']], writes=[ptb])
        for h in range(NAH):
            k.op('act', lambda e, h=h, ms_=ms_: e.activation(out=ms_[:, h, :], in_=ptb[:, h, :], func=AF.Copy, scale=nw[:, h:h + 1]), reads=[ptb, nw], writes=[ms_])
        k.dma('sp', mixT[512:1280, qi * 128:(qi + 1) * 128].rearrange("(j p) t -> p j t", p=128), ms_[:], reads=[ms_], writes=[mixT])
        u += 1
    P.release(m0)


def stage_gla(P, C, l, W, pfm, ptm, mixT, ofs, with_ctx):
    nc, k = P.nc, P.k
    pf = "gl_"
    m0 = P.mark()
    NTL = L // 128
    NTT = T // 128
    qs = GDK ** -0.5
    Mf = P.alloc_sb(pf + "Mf", [128, 128], F32)
    Mb = P.alloc_sb(pf + "Mb", [128, 128], F32)
    k.op('pool', lambda e: e.affine_select(out=Mf[:], in_=C['ones_f'][:], pattern=[[1, 128]], compare_op=ALU.is_ge, fill=0.0, base=0, channel_multiplier=-1),
         reads=[C['ones_f']], writes=[Mf])
    k.op('pool', lambda e: e.memset(Mf[0:64, 64:128], 0.0), reads=[Mf], writes=[Mf])
    k.op('pool', lambda e: e.affine_select(out=Mb[:], in_=C['ones_f'][:], pattern=[[-1, 128]], compare_op=ALU.is_ge, fill=0.0, base=0, channel_multiplier=1),
         reads=[C['ones_f']], writes=[Mb])
    k.op('pool', lambda e: e.memset(Mb[64:128, 0:64], 0.0), reads=[Mb], writes=[Mb])
    Lf = P.alloc_sb(pf + "Lf", [128, 128], F32)
    Lb = P.alloc_sb(pf + "Lb", [128, 128], F32)
    k.op('dve', lambda e: e.tensor_scalar_mul(out=Lf[:], in0=Mf[:], scalar1=-1.0 / 16), reads=[Mf], writes=[Lf])
    k.op('dve', lambda e: e.tensor_scalar_mul(out=Lb[:], in0=Mb[:], scalar1=-1.0 / 16), reads=[Mb], writes=[Lb])
    ind = P.alloc_sb(pf + "ind", [128, 2], F32)
    k.op('pool', lambda e: e.memset(ind[:], 0.0), writes=[ind])
    k.op('pool', lambda e: e.memset(ind[0:64, 0:1], -1.0 / 16), reads=[ind], writes=[ind])
    k.op('pool', lambda e: e.memset(ind[64:128, 1:2], -1.0 / 16), reads=[ind], writes=[ind])
    one1 = P.alloc_sb(pf + "one1", [128, 1], F32)
    k.op('pool', lambda e: e.memset(one1[:], 1.0), writes=[one1])
    ga1T = P.alloc_sb(pf + "ga1T", [33, T], F32)
    k.op('pool', lambda e: e.memset(ga1T[:], 1.0), writes=[ga1T])
    k.dma('sp', ga1T[0:32, :], pfm[3072:3104, :], writes=[ga1T])
    w2b = P.alloc_sb(pf + "w2b", [33, 2, 384], F32)
    k.op('pool', lambda e: e.memset(w2b[:], 0.0), writes=[w2b])
    for d in range(2):
        k.dma('sp', w2b[16 * d:16 * d + 16, d, :], W['gla_a_w2'][l, d], writes=[w2b])
        k.dma('sp', w2b[32:33, d, :], W['gla_a_b'][l, d:d + 1, :], writes=[w2b])
    gnw = P.alloc_sb(pf + "gnw", [128, 768], F32)
    for h in range(GH):
        k.dma('sp', gnw[:, h * 128:(h + 1) * 128], W['gla_norm_w'][l:l + 1, :].partition_broadcast(128), writes=[gnw])
    m1 = P.mark()
    ii = P.alloc_sb(pf + "ii", [128, 16], I32)
    inv = P.alloc_sb(pf + "inv", [128, 16], F32)
    k.op('pool', lambda e: e.iota(ii[:], pattern=[[1, 16]], base=0, channel_multiplier=0), writes=[ii])
    k.op('dve', lambda e: e.tensor_copy(out=inv[:], in_=ii[:]), reads=[ii], writes=[inv])
    k.op('act', lambda e: e.activation(out=inv[:], in_=inv[:], func=AF.Exp, scale=-math.log(10000.0) / 16), reads=[inv], writes=[inv])
    k.op('dve', lambda e: e.tensor_scalar_mul(out=inv[:], in0=inv[:], scalar1=1.0 / (2 * math.pi)), reads=[inv], writes=[inv])
    pp = P.alloc_sb(pf + "pp", [128, 4], F32)
    pi2 = P.alloc_sb(pf + "pi2", [128, 1], I32)
    k.op('pool', lambda e: e.iota(pi2[:], pattern=[[0, 1]], base=0, channel_multiplier=1), writes=[pi2])
    k.op('dve', lambda e: e.tensor_copy(out=pp[:, 0:1], in_=pi2[:]), reads=[pi2], writes=[pp])
    k.op('dve', lambda e: e.tensor_single_scalar(out=pp[:, 1:2], in_=pp[:, 0:1], scalar=63.5, op=ALU.is_gt), reads=[pp], writes=[pp])
    k.op('dve', lambda e: e.scalar_tensor_tensor(out=pp[:, 2:3], in0=pp[:, 1:2], scalar=-64.0, in1=pp[:, 0:1], op0=ALU.mult, op1=ALU.add),
         reads=[pp], writes=[pp])
    tab = P.alloc_sb(pf + "tab", [128, NTL, 2, 16], F32)
    rp = P.alloc_sb(pf + "rp", [128, 1], F32)
    for tt in range(NTL):
        k.op('dve', lambda e, tt=tt: e.tensor_scalar_add(out=rp[:], in0=pp[:, 1:2], scalar1=float(2 * tt)), reads=[pp], writes=[rp])
        k.op('dve', lambda e, tt=tt: e.tensor_scalar_mul(out=tab[:, tt, 0, :], in0=inv[:], scalar1=rp[:, 0:1]), reads=[inv, rp], writes=[tab])
        k.op('dve', lambda e, tt=tt: e.tensor_scalar_mul(out=tab[:, tt, 1, :], in0=inv[:], scalar1=pp[:, 2:3]), reads=[inv, pp], writes=[tab])
    sinT = P.alloc_sb(pf + "sinT", [128, NTL, 2, 16], F32)
    cosT = P.alloc_sb(pf + "cosT", [128, NTL, 2, 16], F32)
    ti = P.alloc_sb(pf + "ti", [128, NTL, 2, 16], I32)
    tf = P.alloc_sb(pf + "tf", [128, NTL, 2, 16], F32)
    for (dst, sh) in ((sinT, 0.0), (cosT, 0.25)):
        if sh:
            k.op('dve', lambda e: e.tensor_scalar_add(out=tab[:], in0=tab[:], scalar1=sh), reads=[tab], writes=[tab])
        k.op('dve', lambda e: e.tensor_copy(out=ti[:], in_=tab[:]), reads=[tab], writes=[ti])
        k.op('dve', lambda e: e.tensor_copy(out=tf[:], in_=ti[:]), reads=[ti], writes=[tf])
        k.op('dve', lambda e: e.tensor_tensor(out=tf[:], in0=tab[:], in1=tf[:], op=ALU.subtract), reads=[tab, tf], writes=[tf])
        k.op('act', lambda e, dst=dst: e.activation(out=dst[:], in_=tf[:], func=AF.Sin, scale=2 * math.pi), reads=[tf], writes=[dst])
    ld = [P.alloc_sb(pf + "ld%d" % i, [128, 2304], F32) for i in range(2)]
    vb = [P.alloc_sb(pf + "vb%d" % i, [128, GH, 128], BF16) for i in range(2)]
    ls_ = P.alloc_sb(pf + "ls", [128, 384], F32)
    ecp = P.alloc_sb(pf + "ecp", [128, 384], F32)
    ecn = P.alloc_sb(pf + "ecn", [128, 384], F32)
    qr = P.alloc_sb(pf + "qr", [128, 768], F32)
    rt = [P.alloc_sb(pf + "rt%d" % i, [128, 2, 16], F32) for i in range(4)]
    qkd = P.alloc_sb(pf + "qkd", [128, 2, 384], BF16)
    qdz = P.alloc_sb(pf + "qdz", [64, GH, 2, 128], BF16)
    k.op('pool', lambda e: e.memset(qdz[:], 0.0), writes=[qdz])
    kdT = P.alloc_sb(pf + "kdT", [64, GH, 128], BF16)
    qdT = P.alloc_sb(pf + "qdT", [64, GH, 128], BF16)
    ecl = P.alloc_sb(pf + "ecl", [64, GH, 2], F32)
    Am = [P.alloc_sb(pf + "Am%d" % i, [128, 128], BF16) for i in range(2)]
    S = {d: P.alloc_sb(pf + "S%d" % d, [64, GH, 128], F32) for d in range(2)}
    Sb = [P.alloc_sb(pf + "Sb%d" % i, [64, GH, 128], BF16) for i in range(3)]
    Stmp = P.alloc_sb(pf + "Stmp", [64, 128], F32)
    osb = P.alloc_sb(pf + "osb", [128, 768], F32)
    ofl = P.alloc_sb(pf + "ofl", [128, 768], F32)
    sq = P.alloc_sb(pf + "sq", [128, GH, 128], F32)
    ss = P.alloc_sb(pf + "ss", [128, 2, GH], F32)
    sg = P.alloc_sb(pf + "sg", [128, 768], F32)
    yb = P.alloc_sb(pf + "yb", [128, 768], BF16)
    mst = [P.alloc_sb(pf + "mst%d" % i, [128, GH, 128], BF16) for i in range(2)]
    bA = P.alloc_ps(pf + "bA", [128, 512])
    pz, pzk = bA[:, 0:384], bA
    pe_, pek = bA[0:64, 384:396].rearrange("p (h c) -> p h c", h=GH), bA
    bB = P.alloc_ps(pf + "bB", [128, 512])
    pc_, pck = bB[:, 0:384], bB
    pTq = P.alloc_ps(pf + "pTq", [64, GH, 128], BF16)
    pTk = P.alloc_ps(pf + "pTk", [64, GH, 128], BF16)
    bE = [P.alloc_ps(pf + "bE%d" % i, [128, 2, 128]) for i in range(2)]
    pA = [(bE[i][:, 0, :], bE[i]) for i in range(2)]
    pO = [(bE[i][:, 1, :], bE[i]) for i in range(2)]
    bF = P.alloc_ps(pf + "bF", [64, 2, 128])
    pK = [(bF[:, i, :], bF) for i in range(2)]
    ptb = P.alloc_ps(pf + "ptb", [128, GH, 128], BF16)
    cnt = [0]

    def tile_pass(tt, d, with_out, final):
        i = cnt[0] % 2
        cnt[0] += 1
        t0 = tt * 128
        buf = ld[i]; v_b = vb[i]
        rope = tt < NTL
        Mm = Mf if d == 0 else Mb
        Lm = Lf if d == 0 else Lb
        k.dma('sp', buf[:], ptm[t0:t0 + 128, 768:3072], writes=[buf])
        k.op('pool', lambda e: e.tensor_copy(out=v_b[:], in_=buf[:, 768:1536].rearrange("p (h d) -> p h d", h=GH)), reads=[buf], writes=[v_b])
        k.op('pe', lambda e: e.matmul(pz, lhsT=ga1T[:, t0:t0 + 128], rhs=w2b[:, d, :], start=True, stop=True), reads=[ga1T, w2b], writes=[pzk])
        k.op('act', lambda e: e.activation(out=ls_[:], in_=pz, func=AF.Exp, scale=-1.0), reads=[pzk], writes=[ls_])
        k.op('act', lambda e: e.activation(out=ls_[:], in_=ls_[:], func=AF.Ln, bias=one1[:]), reads=[ls_, one1], writes=[ls_])
        k.op('pe', lambda e: e.matmul(pc_, lhsT=Lm[:], rhs=ls_[:], start=True, stop=True), reads=[Lm, ls_], writes=[pck])
        for h in range(GH):
            k.op('pe', lambda e, h=h: e.matmul(pe_[:, h, :], lhsT=ls_[:, h * 64:(h + 1) * 64], rhs=ind[:], start=True, stop=True), reads=[ls_, ind], writes=[pek])
        k.op('act', lambda e: e.activation(out=ecl[:], in_=pe_, func=AF.Exp), reads=[pek], writes=[ecl])
        k.op('act', lambda e: e.activation(out=ecp[:], in_=pc_, func=AF.Exp), reads=[pck], writes=[ecp])
        k.op('act', lambda e: e.activation(out=ecn[:], in_=pc_, func=AF.Exp, scale=-1.0), reads=[pck], writes=[ecn])
        if rope:
            cs = cosT[:, tt, :, :]; sn = sinT[:, tt, :, :]
            for which in range(2):
                for h in range(GH):
                    o0 = which * 384 + h * 64
                    xv = buf[:, o0:o0 + 64].rearrange("p (hf two i) -> p hf two i", hf=2, two=2)
                    ov = qr[:, o0:o0 + 64].rearrange("p (hf two i) -> p hf two i", hf=2, two=2)
                    ea, eb = ('dve', 'pool') if (h % 2 == 0) else ('pool', 'dve')
                    k.op(ea, lambda e, xv=xv: e.tensor_tensor(out=rt[0][:], in0=xv[:, :, 0, :], in1=cs, op=ALU.mult), reads=[buf, cosT], writes=[rt[0]])
                    k.op(eb, lambda e, xv=xv: e.tensor_tensor(out=rt[1][:], in0=xv[:, :, 1, :], in1=sn, op=ALU.mult), reads=[buf, sinT], writes=[rt[1]])
                    k.op(ea, lambda e, ov=ov: e.tensor_tensor(out=ov[:, :, 0, :], in0=rt[0][:], in1=rt[1][:], op=ALU.subtract), reads=[rt[0], rt[1]], writes=[qr])
                    k.op(eb, lambda e, xv=xv: e.tensor_tensor(out=rt[2][:], in0=xv[:, :, 0, :], in1=sn, op=ALU.mult), reads=[buf, sinT], writes=[rt[2]])
                    k.op(ea, lambda e, xv=xv: e.tensor_tensor(out=rt[3][:], in0=xv[:, :, 1, :], in1=cs, op=ALU.mult), reads=[buf, cosT], writes=[rt[3]])
                    k.op(eb, lambda e, ov=ov: e.tensor_tensor(out=ov[:, :, 1, :], in0=rt[2][:], in1=rt[3][:], op=ALU.add), reads=[rt[2], rt[3]], writes=[qr])
            src = qr
        else:
            src = buf
        k.op('dve', lambda e: e.scalar_tensor_tensor(out=qkd[:, 0, :], in0=src[:, 0:384], scalar=qs, in1=ecp[:], op0=ALU.mult, op1=ALU.mult),
             reads=[src, ecp], writes=[qkd])
        k.op('pool', lambda e: e.tensor_tensor(out=qkd[:, 1, :], in0=src[:, 384:768], in1=ecn[:], op=ALU.mult), reads=[src, ecn], writes=[qkd])
        for w in range(2):
            for h in range(GH):
                k.op('pe', lambda e, w=w, h=h: e.transpose(out=(pTq if w == 0 else pTk)[:, h, :], in_=qkd[:, w, h * 64:(h + 1) * 64], identity=C['id_bf'][:]),
                     reads=[qkd, C['id_bf']], writes=[pTq if w == 0 else pTk])
        k.op('act', lambda e: e.copy(out=qdT[:], in_=pTq[:]), reads=[pTq], writes=[qdT])
        k.op('dve', lambda e: e.tensor_copy(out=kdT[:], in_=pTk[:]), reads=[pTk], writes=[kdT])
        for c in range(2):
            k.op('pool', lambda e, c=c: e.tensor_copy(out=qdz[:, :, c, 64 * c:64 * c + 64], in_=qdT[:, :, 64 * c:64 * c + 64]), reads=[qdT], writes=[qdz])
        Sd = S[d]
        order = (0, 1) if d == 0 else (1, 0)
        k.op('act', lambda e: e.copy(out=Sb[0][:], in_=Sd[:]), reads=[Sd], writes=[Sb[0]])
        for ci, c in enumerate(order):
            for h in range(GH):
                pk, pkk = pK[h % 2]
                k.op('pe', lambda e, c=c, h=h, pk=pk: e.matmul(pk, lhsT=qkd[64 * c:64 * c + 64, 1, h * 64:(h + 1) * 64], rhs=v_b[64 * c:64 * c + 64, h, :],
                                                              start=True, stop=True), reads=[qkd, v_b], writes=[pkk])
                k.op('dve', lambda e, c=c, h=h: e.tensor_scalar_mul(out=Stmp[:], in0=Sd[:, h, :], scalar1=ecl[:, h, c:c + 1]), reads=[Sd, ecl], writes=[Stmp])
                k.op('dve', lambda e, c=c, h=h, pk=pk: e.scalar_tensor_tensor(out=Sd[:, h, :], in0=pk, scalar=ecl[:, h, c:c + 1], in1=Stmp[:],
                                                                             op0=ALU.mult, op1=ALU.add), reads=[pkk, ecl, Stmp], writes=[Sd])
            if ci == 0 and with_out:
                k.op('act', lambda e: e.copy(out=Sb[1][:], in_=Sd[:]), reads=[Sd], writes=[Sb[1]])
        if not with_out:
            return
        for h in range(GH):
            (pa_, pak), (po, pok) = pA[h % 2], pO[h % 2]
            am = Am[h % 2]
            k.op('pe', lambda e, h=h, pa_=pa_: e.matmul(pa_, lhsT=kdT[:, h, :], rhs=qdT[:, h, :], start=True, stop=True), reads=[kdT, qdT], writes=[pak])
            k.op('dve', lambda e, pa_=pa_, am=am: e.tensor_tensor(out=am[:], in0=pa_, in1=Mm[:], op=ALU.mult), reads=[pak, Mm], writes=[am])
            k.op('pe', lambda e, h=h, po=po, am=am: e.matmul(po, lhsT=am[:], rhs=v_b[:, h, :], start=True, stop=False), reads=[am, v_b], writes=[pok])
            for ci, c in enumerate(order):
                k.op('pe', lambda e, h=h, po=po, c=c, ci=ci: e.matmul(po, lhsT=qdz[:, h, c, :], rhs=Sb[ci][:, h, :], start=False, stop=(ci == 1)),
                     reads=[qdz, Sb[ci]], writes=[pok])
            if not final:
                k.op('act', lambda e, h=h, po=po: e.copy(out=osb[:, h * 128:(h + 1) * 128], in_=po), reads=[pok], writes=[osb])
            else:
                k.op('dve', lambda e, h=h, po=po: e.tensor_tensor(out=osb[:, h * 128:(h + 1) * 128], in0=po, in1=ofl[:, h * 128:(h + 1) * 128], op=ALU.add),
                     reads=[pok, ofl], writes=[osb])
        if not final:
            k.dma('sp', ofs[t0:t0 + 128, :], osb[:], reads=[osb], writes=[ofs])
            return
        o3 = osb[:, :].rearrange("p (h d) -> p h d", h=GH)
        k.op('pool', lambda e: e.tensor_tensor(out=sq[:], in0=o3, in1=o3, op=ALU.mult), reads=[osb], writes=[sq])
        k.op('dve', lambda e: e.tensor_reduce(out=ss[:, 0, :], in_=sq[:], axis=AX.X, op=ALU.add), reads=[sq], writes=[ss])
        k.op('dve', lambda e: e.tensor_scalar(out=ss[:, 1, :], in0=ss[:, 0, :], scalar1=1.0 / 128, scalar2=EPS, op0=ALU.mult, op1=ALU.add), reads=[ss], writes=[ss])
        k.op('act', lambda e: e.activation(out=ss[:, 1, :], in_=ss[:, 1, :], func=AF.Ln), reads=[ss], writes=[ss])
        k.op('act', lambda e: e.activation(out=ss[:, 1, :], in_=ss[:, 1, :], func=AF.Exp, scale=-0.5), reads=[ss], writes=[ss])
        k.op('act', lambda e: e.activation(out=sg[:], in_=buf[:, 1536:2304], func=AF.Silu), reads=[buf], writes=[sg])
        for h in range(GH):
            k.op('dve', lambda e, h=h: e.tensor_scalar_mul(out=osb[:, h * 128:(h + 1) * 128], in0=osb[:, h * 128:(h + 1) * 128], scalar1=ss[:, 1, h:h + 1]),
                 reads=[osb, ss], writes=[osb])
        k.op('pool', lambda e: e.tensor_tensor(out=sg[:], in0=sg[:], in1=gnw[:], op=ALU.mult), reads=[sg, gnw], writes=[sg])
        k.op('dve', lambda e: e.tensor_tensor(out=yb[:], in0=osb[:], in1=sg[:], op=ALU.mult), reads=[osb, sg], writes=[yb])
        ms_ = mst[tt % 2]
        for h in range(GH):
            k.op('pe', lambda e, h=h: e.transpose(out=ptb[:, h, :], in_=yb[:, h * 128:(h + 1) * 128], identity=C['id_bf'][:]), reads=[yb, C['id_bf']], writes=[ptb])
        k.op('act', lambda e, ms_=ms_: e.copy(out=ms_[:], in_=ptb[:]), reads=[ptb], writes=[ms_])
        k.dma('sp', mixT[1280:2048, t0:t0 + 128].rearrange("(j p) t -> p j t", p=128), ms_[:], reads=[ms_], writes=[mixT])

    k.op('pool', lambda e: e.memset(S[0][:], 0.0), writes=[S[0]])
    k.op('pool', lambda e: e.memset(S[1][:], 0.0), writes=[S[1]])
    for tt in (NTL, NTL + 1):
        tile_pass(tt, 0, with_ctx, False)
    for tt in range(NTL):
        tile_pass(tt, 0, True, False)
    for tt in (NTL + 1, NTL):
        if with_ctx:
            k.dma('sp', ofl[:], ofs[tt * 128:(tt + 1) * 128, :], reads=[ofs], writes=[ofl])
        tile_pass(tt, 1, with_ctx, True)
    for tt in range(NTL - 1, -1, -1):
        k.dma('sp', ofl[:], ofs[tt * 128:(tt + 1) * 128, :], reads=[ofs], writes=[ofl])
        tile_pass(tt, 1, True, True)
    P.release(m0)


def stage_post(P, C, l, W, mp, xT, mixT, x1T, h2f_d, h2b_d, ntok):
    nc, k = P.nc, P.k
    pf = "po_"
    m0 = P.mark()
    NB = 256
    wo = P.alloc_sb(pf + "wo", [128, NCH, D], BF16)
    wv = W['w_out'][l].rearrange("(kc p) c -> p kc c", p=128)
    for q in range(4):
        k.dma('pool', wo[:, :, q * 512:(q + 1) * 512], wv[:, :, q * 512:(q + 1) * 512], writes=[wo])
    lnp = P.alloc_sb(pf + "lnp", [128, 4, NCH], F32)
    k.dma('sp', lnp[:], W['lnT'][l])
    tmp = ln_tmp(P, pf + "ln", NB)
    mxb = [P.alloc_sb(pf + "mxb%d" % i, [128, NCH, NB], BF16) for i in range(2)]
    xt = [P.alloc_sb(pf + "xt%d" % i, [128, NCH, NB], F32) for i in range(2)]
    x1t = P.alloc_sb(pf + "x1t", [128, NCH, NB], F32)
    pss = [P.alloc_ps(pf + "ps%d" % i, [128, NB]) for i in range(2)]
    tq = [P.alloc_sb(pf + "tq%d" % i, [128, NB], F32) for i in range(2)]
    mxv = mixT.rearrange("(c p) t -> p c t", p=128)
    xv = xT.rearrange("(c p) t -> p c t", p=128)
    x1v = x1T.rearrange("(c p) t -> p c t", p=128)
    hfv = h2f_d.rearrange("(c p) t -> p c t", p=128)
    hbv = h2b_d.rearrange("(c p) t -> p c t", p=128)
    for bi, t0 in enumerate(range(0, ntok, NB)):
        r = 0 if t0 < L else 1
        mb = mxb[bi % 2]; x_ = xt[bi % 2]
        k.dma('sp', mb[:], mxv[:, :, t0:t0 + NB], writes=[mb])
        k.dma('act', x_[:], xv[:, :, t0:t0 + NB], writes=[x_])
        for dc in range(NCH):
            pst = pss[dc % 2]; tt = tq[dc % 2]
            for kc in range(NCH):
                k.op('pe', lambda e, kc=kc, dc=dc, pst=pst: e.matmul(pst[:, :], lhsT=wo[:, kc, dc * 128:(dc + 1) * 128], rhs=mb[:, kc, :],
                                                                    start=(kc == 0), stop=(kc == NCH - 1)), reads=[wo, mb], writes=[pst])
            k.op('act', lambda e, dc=dc, pst=pst, tt=tt: e.activation(out=tt[:], in_=pst[:, :], func=AF.Copy, scale=mp['g1'][:, dc, r:r + 1]),
                 reads=[pst, mp['g1']], writes=[tt])
            k.op('dve', lambda e, dc=dc, tt=tt: e.scalar_tensor_tensor(out=x_[:, dc, :], in0=x_[:, dc, :], scalar=ALPHA, in1=tt[:], op0=ALU.mult, op1=ALU.add),
                 reads=[x_, tt], writes=[x_])
        ln_block(P, C, tmp, x_, NB,
                 lambda c: (x1t[:, c, :], [x1t]),
                 lambda c: (lnp[:, 0, c:c + 1], [lnp]),
                 lambda c: (lnp[:, 1, c:c + 1], [lnp]))
        k.dma('sp', x1v[:, :, t0:t0 + NB], x1t[:], reads=[x1t], writes=[x1T])
        ln_block(P, C, tmp, x1t, NB,
                 lambda c: (x_[:, c, :], [x_]),
                 lambda c: (mp['sc2p'][:, c, r:r + 1], [mp['sc2p']]),
                 lambda c: (mp['sh2'][:, c, r:r + 1], [mp['sh2']]))
        k.dma('sp', hfv[:, :, t0:t0 + NB], x_[:], reads=[x_], writes=[h2f_d])
        k.dma('pool', hbv[:, :, t0:t0 + NB], x_[:], reads=[x_], writes=[h2b_d])
    P.release(m0)


def stage_moe(P, C, l, W, mp, x1T, h2f_d, h2b_d, outT, ntok):
    nc, k = P.nc, P.k
    pf = "mo_"
    m0 = P.mark()
    NT_ = ntok // 128
    gate = P.alloc_sb(pf + "gate", [128, NT_, NE], F32)
    m1 = P.mark()
    wr = P.alloc_sb(pf + "wr", [128, NCH, 36], F32)
    k.dma('sp', wr[:, :, 0:4], W['w_rg'][l].rearrange("(kc p) c -> p kc c", p=128), writes=[wr])
    k.dma('sp', wr[:, :, 4:36], W['w_re'][l].rearrange("(kc p) c -> p kc c", p=128), writes=[wr])
    br = P.alloc_sb(pf + "br", [128, 36], F32)
    k.dma('sp', br[:, 0:4], W['b_rg'][l:l + 1, :].partition_broadcast(128), writes=[br])
    k.dma('sp', br[:, 4:36], W['b_re'][l:l + 1, :].partition_broadcast(128), writes=[br])
    hf = [P.alloc_sb(pf + "hf%d" % i, [128, NCH, 128], F32) for i in range(2)]
    pl = [P.alloc_ps(pf + "pl%d" % i, [128, 36]) for i in range(2)]
    lg = P.alloc_sb(pf + "lg", [128, 36], F32)
    sm = P.alloc_sb(pf + "sm", [128, 16], F32)
    gs = P.alloc_sb(pf + "gs", [128, 4], F32)
    eg = P.alloc_sb(pf + "eg", [128, 4], F32)
    les = P.alloc_sb(pf + "les", [128, 8], F32)
    le2 = P.alloc_sb(pf + "le2", [128, 8], F32)
    mk1 = P.alloc_sb(pf + "mk1", [128, 8], F32)
    mk2 = P.alloc_sb(pf + "mk2", [128, 8], F32)
    egt = P.alloc_sb(pf + "egt", [128, 8], F32)
    hfv = h2f_d.rearrange("(c p) t -> p c t", p=128)
    for tt in range(NT_):
        h_ = hf[tt % 2]; pst = pl[tt % 2]
        k.dma('sp', h_[:], hfv[:, :, tt * 128:(tt + 1) * 128], writes=[h_])
        for kc in range(NCH):
            k.op('pe', lambda e, kc=kc, h_=h_, pst=pst: e.matmul(pst[:, :], lhsT=h_[:, kc, :], rhs=wr[:, kc, :], start=(kc == 0), stop=(kc == NCH - 1)),
                 reads=[h_, wr], writes=[pst])
        k.op('dve', lambda e, pst=pst: e.tensor_tensor(out=lg[:], in0=pst[:, :], in1=br[:], op=ALU.add), reads=[pst, br], writes=[lg])
        k.op('dve', lambda e: e.tensor_reduce(out=sm[:, 0:1], in_=lg[:, 0:4], axis=AX.X, op=ALU.max), reads=[lg], writes=[sm])
        k.op('dve', lambda e: e.tensor_scalar_mul(out=sm[:, 1:2], in0=sm[:, 0:1], scalar1=-1.0), reads=[sm], writes=[sm])
        k.op('act', lambda e: e.activation(out=eg[:], in_=lg[:, 0:4], func=AF.Exp, bias=sm[:, 1:2], accum_out=sm[:, 2:3]), reads=[lg, sm], writes=[eg, sm])
        k.op('dve', lambda e: e.reciprocal(out=sm[:, 3:4], in_=sm[:, 2:3]), reads=[sm], writes=[sm])
        k.op('dve', lambda e: e.tensor_scalar(out=gs[:], in0=lg[:, 0:4], scalar1=sm[:, 0:1], scalar2=None, op0=ALU.is_ge), reads=[lg, sm], writes=[gs])
        k.op('dve', lambda e: e.tensor_scalar_mul(out=les[:], in0=lg[:, 4:12], scalar1=gs[:, 0:1]), reads=[lg, gs], writes=[les])
        for g in range(1, 4):
            k.op('dve', lambda e, g=g: e.scalar_tensor_tensor(out=les[:], in0=lg[:, 4 + 8 * g:12 + 8 * g], scalar=gs[:, g:g + 1], in1=les[:],
                                                             op0=ALU.mult, op1=ALU.add), reads=[lg, gs, les], writes=[les])
        k.op('dve', lambda e: e.tensor_reduce(out=sm[:, 4:5], in_=les[:], axis=AX.X, op=ALU.max), reads=[les], writes=[sm])
        k.op('dve', lambda e: e.tensor_scalar(out=mk1[:], in0=les[:], scalar1=sm[:, 4:5], scalar2=None, op0=ALU.is_ge), reads=[les, sm], writes=[mk1])
        k.op('dve', lambda e: e.scalar_tensor_tensor(out=le2[:], in0=mk1[:], scalar=-1.0e9, in1=les[:], op0=ALU.mult, op1=ALU.add),
             reads=[mk1, les], writes=[le2])
        k.op('dve', lambda e: e.tensor_reduce(out=sm[:, 5:6], in_=le2[:], axis=AX.X, op=ALU.max), reads=[le2], writes=[sm])
        k.op('dve', lambda e: e.tensor_scalar(out=mk2[:], in0=le2[:], scalar1=sm[:, 5:6], scalar2=None, op0=ALU.is_ge), reads=[le2, sm], writes=[mk2])
        k.op('dve', lambda e: e.tensor_tensor(out=sm[:, 6:7], in0=sm[:, 5:6], in1=sm[:, 4:5], op=ALU.subtract), reads=[sm], writes=[sm])
        k.op('act', lambda e: e.activation(out=sm[:, 7:8], in_=sm[:, 6:7], func=AF.Exp), reads=[sm], writes=[sm])
        k.op('dve', lambda e: e.tensor_scalar_add(out=sm[:, 8:9], in0=sm[:, 7:8], scalar1=1.0), reads=[sm], writes=[sm])
        k.op('dve', lambda e: e.reciprocal(out=sm[:, 9:10], in_=sm[:, 8:9]), reads=[sm], writes=[sm])
        k.op('dve', lambda e: e.tensor_tensor(out=sm[:, 10:11], in0=sm[:, 7:8], in1=sm[:, 9:10], op=ALU.mult), reads=[sm], writes=[sm])
        k.op('dve', lambda e: e.tensor_tensor(out=sm[:, 11:12], in0=sm[:, 9:10], in1=sm[:, 3:4], op=ALU.mult), reads=[sm], writes=[sm])
        k.op('dve', lambda e: e.tensor_tensor(out=sm[:, 12:13], in0=sm[:, 10:11], in1=sm[:, 3:4], op=ALU.mult), reads=[sm], writes=[sm])
        k.op('dve', lambda e: e.tensor_scalar_mul(out=egt[:], in0=mk1[:], scalar1=sm[:, 11:12]), reads=[mk1, sm], writes=[egt])
        k.op('dve', lambda e: e.scalar_tensor_tensor(out=egt[:], in0=mk2[:], scalar=sm[:, 12:13], in1=egt[:], op0=ALU.mult, op1=ALU.add),
             reads=[mk2, sm, egt], writes=[egt])
        for g in range(4):
            k.op('dve', lambda e, g=g, tt=tt: e.tensor_scalar_mul(out=gate[:, tt, 8 * g:8 * g + 8], in0=egt[:], scalar1=gs[:, g:g + 1]),
                 reads=[egt, gs], writes=[gate])
    P.dump('moe_gate', gate)
    P.release(m1)
    PASS = 768
    BLK = 384
    acc = P.alloc_sb(pf + "acc", [128, PASS // 128, D], F32)
    lnp = P.alloc_sb(pf + "lnp", [128, 4, NCH], F32)
    k.dma('sp', lnp[:], W['lnT'][l])
    hbv = h2b_d.rearrange("(c p) t -> p c t", p=128)
    x1v = x1T.rearrange("(c p) t -> p c t", p=128)
    ov = outT.rearrange("(c p) t -> p c t", p=128)
    ei = 0
    oi = 0
    for p0 in range(0, ntok, PASS):
        pn = min(PASS, ntok - p0)
        mE = P.mark()
        hT = P.alloc_sb(pf + "hT", [128, NCH, PASS], BF16)
        wu = [P.alloc_sb(pf + "wu%d" % i, [128, NCH, 1024], BF16) for i in range(2)]
        wd = [P.alloc_sb(pf + "wd%d" % i, [128, 4, D], BF16) for i in range(2)]
        actT = [P.alloc_sb(pf + "actT%d" % i, [128, 4, BLK], BF16) for i in range(2)]
        sgb = [P.alloc_sb(pf + "sg%d" % i, [128, BLK], F32) for i in range(2)]
        pg = [P.alloc_ps(pf + "pg%d" % i, [128, BLK]) for i in range(2)]
        pu = [P.alloc_ps(pf + "pu%d" % i, [128, BLK]) for i in range(2)]
        po = [P.alloc_ps(pf + "po%d" % i, [128, 512]) for i in range(4)]
        k.dma('sp', hT[:, :, :pn], hbv[:, :, p0:p0 + pn], writes=[hT])
        k.op('pool', lambda e: e.memset(acc[:], 0.0), writes=[acc])
        for ex in range(NE):
            g_, e_ = ex // 8, ex % 8
            wu_ = wu[ei % 2]; wd_ = wd[ei % 2]
            ei += 1
            k.dma('pool', wu_[:], W['w_up'][l, g_, e_].rearrange("(kc p) c -> p kc c", p=128), writes=[wu_])
            k.dma('pool', wd_[:], W['w_down'][l, g_, e_].rearrange("(j p) c -> p j c", p=128), writes=[wd_])
            for b0 in range(0, pn, BLK):
                bn = min(BLK, pn - b0)
                at = actT[(b0 // BLK) % 2]
                for j in range(4):
                    pg_ = pg[j % 2]; pu_ = pu[j % 2]; sg_ = sgb[j % 2]
                    for kc in range(NCH):
                        k.op('pe', lambda e, kc=kc, j=j, pg_=pg_: e.matmul(pg_[:, :bn], lhsT=wu_[:, kc, j * 128:(j + 1) * 128], rhs=hT[:, kc, b0:b0 + bn],
                                                                          start=(kc == 0), stop=(kc == NCH - 1)), reads=[wu_, hT], writes=[pg_])
                    for kc in range(NCH):
                        k.op('pe', lambda e, kc=kc, j=j, pu_=pu_: e.matmul(pu_[:, :bn], lhsT=wu_[:, kc, 512 + j * 128:512 + (j + 1) * 128], rhs=hT[:, kc, b0:b0 + bn],
                                                                          start=(kc == 0), stop=(kc == NCH - 1)), reads=[wu_, hT], writes=[pu_])
                    k.op('act', lambda e, pg_=pg_, sg_=sg_: e.activation(out=sg_[:, :bn], in_=pg_[:, :bn], func=AF.Silu), reads=[pg_], writes=[sg_])
                    k.op('dve', lambda e, j=j, pu_=pu_, sg_=sg_, at=at: e.tensor_tensor(out=at[:, j, :bn], in0=pu_[:, :bn], in1=sg_[:, :bn], op=ALU.mult),
                         reads=[pu_, sg_], writes=[at])
                for t3 in range(bn // 128):
                    tl = (b0 // 128) + t3
                    tg = (p0 // 128) + tl
                    for dh in range(4):
                        po_ = po[oi % 4]
                        oi += 1
                        for j in range(4):
                            k.op('pe', lambda e, j=j, dh=dh, po_=po_, t3=t3: e.matmul(po_[:, :], lhsT=at[:, j, t3 * 128:(t3 + 1) * 128], rhs=wd_[:, j, dh * 512:(dh + 1) * 512],
                                                                                   start=(j == 0), stop=(j == 3)), reads=[at, wd_], writes=[po_])
                        k.op('dve', lambda e, dh=dh, po_=po_, tl=tl, tg=tg, ex=ex: e.scalar_tensor_tensor(out=acc[:, tl, dh * 512:(dh + 1) * 512], in0=po_[:, :],
                                                                                                        scalar=gate[:, tg, ex:ex + 1], in1=acc[:, tl, dh * 512:(dh + 1) * 512],
                                                                                                        op0=ALU.mult, op1=ALU.add), reads=[po_, gate, acc], writes=[acc])
        P.release(mE)
        m2 = P.mark()
        tmp = ln_tmp(P, pf + "ln", BLK)
        vt = P.alloc_sb(pf + "vt", [128, NCH, BLK], F32)
        x1b = P.alloc_sb(pf + "x1b", [128, NCH, BLK], F32)
        ot = P.alloc_sb(pf + "ot", [128, NCH, BLK], F32)
        ptf = [P.alloc_ps(pf + "ptf%d" % i, [128, 512]) for i in range(2)]
        tq = [P.alloc_sb(pf + "tq%d" % i, [128, 128], F32) for i in range(2)]
        for b0 in range(0, pn, BLK):
            bn = min(BLK, pn - b0)
            r = 0 if (p0 + b0) < L else 1
            k.dma('sp', x1b[:, :, :bn], x1v[:, :, p0 + b0:p0 + b0 + bn], writes=[x1b])
            for t3 in range(bn // 128):
                tl = (b0 // 128) + t3
                r = 0 if (p0 + b0 + t3 * 128) < L else 1
                for c in range(NCH):
                    pt_ = ptf[c % 2]; tq_ = tq[c % 2]
                    k.op('pe', lambda e, c=c, tl=tl, pt_=pt_: e.transpose(out=pt_[:, :128], in_=acc[:, tl, c * 128:(c + 1) * 128], identity=C['id_f'][:]),
                         reads=[acc, C['id_f']], writes=[pt_])
                    k.op('act', lambda e, c=c, pt_=pt_, tq_=tq_: e.activation(out=tq_[:], in_=pt_[:, :128], func=AF.Copy, scale=mp['g2'][:, c, r:r + 1]),
                         reads=[pt_, mp['g2']], writes=[tq_])
                    k.op('dve', lambda e, c=c, t3=t3, tq_=tq_: e.scalar_tensor_tensor(out=vt[:, c, t3 * 128:(t3 + 1) * 128], in0=x1b[:, c, t3 * 128:(t3 + 1) * 128],
                                                                                      scalar=ALPHA, in1=tq_[:], op0=ALU.mult, op1=ALU.add), reads=[x1b, tq_], writes=[vt])
            ln_block(P, C, tmp, vt, bn,
                     lambda c: (ot[:, c, :bn], [ot]),
                     lambda c: (lnp[:, 2, c:c + 1], [lnp]),
                     lambda c: (lnp[:, 3, c:c + 1], [lnp]))
            k.dma('sp', ov[:, :, p0 + b0:p0 + b0 + bn], ot[:, :, :bn], reads=[ot], writes=[outT])
        P.release(m2)
    P.release(m0)


W_SHAPES = {
    'hy_f_w1': [DEPTH, 33, 64], 'hy_f_w2': [DEPTH, 64, 64], 'hy_f_w3': [DEPTH, 64, 1024], 'hy_fv': [DEPTH, 64, 3],
    'hy_bias': [DEPTH, 512], 'hy_sw': [DEPTH, 128, 12, 4], 'hy_norm_wT': [DEPTH, 128, 4],
    'na_rpb': [DEPTH, 6, 15, 31], 'na_norm_wT': [DEPTH, 128, 6],
    'gla_a_w2': [DEPTH, 2, 16, 384], 'gla_a_b': [DEPTH, 2, 384], 'gla_norm_w': [DEPTH, 128],
    'w_out': [DEPTH, D, D], 'lnT': [DEPTH, 128, 4, NCH],
    'w_rg': [DEPTH, D, 4], 'b_rg': [DEPTH, 4], 'w_re': [DEPTH, D, 32], 'b_re': [DEPTH, 32],
    'w_up': [DEPTH, 4, 8, D, 1024], 'w_down': [DEPTH, 4, 8, DE, D],
    'w_ada': [DEPTH, D, 6 * D], 'b_adaT': [DEPTH, 128, 96], 'w_in': [DEPTH, D, INC],
}


def host_layout(inp, layers=(0, 1)):
    ls = list(layers)
    n = len(ls)
    W = {}
    for nm in ('hy_f_w1', 'hy_f_w2', 'hy_f_w3', 'hy_bias', 'na_rpb', 'gla_a_w2', 'gla_a_b', 'gla_norm_w', 'w_out', 'w_rg', 'b_rg',
               'w_re', 'b_re', 'w_up', 'w_down', 'w_ada', 'w_in'):
        W[nm] = np.ascontiguousarray(inp[nm][ls])
    W['hy_fv'] = np.ascontiguousarray(np.stack([inp['hy_f_b1'][ls], inp['hy_f_b2'][ls], inp['hy_sin_freq'][ls]], -1))
    sw = np.concatenate([inp['hy_short_w'][ls], inp['hy_short_b'][ls][:, None, :]], 1)
    W['hy_sw'] = np.ascontiguousarray(sw.reshape(n, 4, 12, 128).transpose(0, 3, 2, 1))
    W['hy_norm_wT'] = np.ascontiguousarray(inp['hy_norm_w'][ls].reshape(n, 4, 128).transpose(0, 2, 1))
    W['na_norm_wT'] = np.ascontiguousarray(inp['na_norm_w'][ls].reshape(n, 6, 128).transpose(0, 2, 1))
    lnT = np.stack([inp['ln1_g'][ls], inp['ln1_b'][ls], inp['ln2_g'][ls], inp['ln2_b'][ls]], 1)
    W['lnT'] = np.ascontiguousarray(lnT.reshape(n, 4, NCH, 128).transpose(0, 3, 1, 2))
    W['b_adaT'] = np.ascontiguousarray(inp['b_ada'][ls].reshape(n, 96, 128).transpose(0, 2, 1))
    return W


def build(layers=(0, 1), dbg=(), upto=None):
    P = Prog(dbg=dbg)
    nl = len(layers)
    W = {}
    for nm, shp in W_SHAPES.items():
        W[nm] = P.inp(nm, [nl] + shp[1:])
    xT_in = P.inp("xT", [D, T])
    cT = P.inp("cT", [128, NCH, 2])
    outT = P.outp("outT", [D, L])
    pfm = P.scratch("pfm", [3104, T])
    ptm = P.scratch("ptm", [T, 3072])
    mixT = P.scratch("mixT", [D, T], BF16)
    rpbp = P.scratch("rpbp", [6, 15, 160])
    ofs = P.scratch("ofs", [T, 768])
    x1T = P.scratch("x1T", [D, T])
    h2f = P.scratch("h2f", [D, T])
    h2b = P.scratch("h2b", [D, T], BF16)
    xmid = P.scratch("xmid", [D, T])
    C = make_consts(P)
    modT = sb(P.nc, "modT", [128, 96, 2], F32)
    for li in range(nl):
        last = (li == nl - 1)
        xin = xT_in if li == 0 else xmid
        xout = outT if last else xmid
        ntok = L if last else T
        mk = P.mark()
        stage_mod(P, li, cT, W['w_ada'], W['b_adaT'], modT)
        mp = load_mod(P, modT)
        stage_inproj(P, C, li, xin, W['w_in'], mp, pfm, ptm)
        if upto == 'inproj':
            break
        stage_hyena(P, C, li, W, pfm, mixT, 0, L)
        if not last:
            stage_hyena(P, C, li, W, pfm, mixT, L, LC)
        stage_na(P, C, li, W, pfm, ptm, mixT, rpbp, not last)
        stage_gla(P, C, li, W, pfm, ptm, mixT, ofs, not last)
        if upto == 'mix':
            break
        stage_post(P, C, li, W, mp, xin, mixT, x1T, h2f, h2b, ntok)
        if upto == 'post':
            break
        stage_moe(P, C, li, W, mp, x1T, h2f, h2b, xout, ntok)
        P.release(mk)
    P.k.finish()
    return P


def kernel(**inputs):
    inp = {k_: np.asarray(v) for k_, v in inputs.items()}
    Wn = host_layout(inp)
    P = build()
    in_maps = []
    for core in range(8):
        b = core % 4
        m = dict(Wn)
        m['xT'] = np.ascontiguousarray(np.concatenate([inp['x'][b], inp['ctx'][b]], 0).T)
        m['cT'] = np.ascontiguousarray(np.stack([inp['c'][b], inp['c_ctx']], -1).reshape(NCH, 128, 2).transpose(1, 0, 2))
        in_maps.append(m)
    res = run_bass_kernel_spmd(P.nc, in_maps, core_ids=list(range(8)))
    out = np.stack([np.ascontiguousarray(res.results[b]["outT"].T) for b in range(4)], 0)
    return out.astype(np.float32)
```

```python
import math
import numpy as np
import concourse.bass as bass
import concourse.mybir as mybir
from concourse.bass_utils import run_bass_kernel_spmd

F32 = mybir.dt.float32
BF16 = mybir.dt.bfloat16
I32 = mybir.dt.int32
AF = mybir.ActivationFunctionType
ALU = mybir.AluOpType
AX = mybir.AxisListType

D = 2048
L = 2048
LC = 256
T = L + LC
NCH = D // 128
DEPTH = 2
INC = 6176
HY = 512
NAH = 6
GH = 6
GDK = 64
EPS = 1e-6
ALPHA = (2 * DEPTH) ** 0.25
NE = 32
DE = 512

O_HY, O_NQ, O_NK, O_NV, O_GQ, O_GK, O_GV, O_GG, O_GA = 0, 1536, 2304, 3072, 3840, 4224, 4608, 5376, 6144


class K:
    NDMA = 24

    def __init__(self, nc):
        self.nc = nc
        self.eng = {'pe': nc.tensor, 'act': nc.scalar, 'dve': nc.vector, 'pool': nc.gpsimd, 'sp': nc.sync}
        self.sem = {e: nc.semaphore("s_" + e).__enter__() for e in ('pe', 'act', 'dve', 'pool')}
        self.cnt = {e: 0 for e in self.sem}
        self.dsem = [nc.semaphore("d%d" % i).__enter__() for i in range(self.NDMA)]
        self.dcnt = [0] * self.NDMA
        self.dnext = 0
        self.seen = {e: {} for e in self.eng}
        self.lastw = {}
        self.readers = {}
        self.nins = 0

    @staticmethod
    def key(x):
        if isinstance(x, (str, tuple)):
            return x
        return x.tensor.name if hasattr(x, 'tensor') else x.name

    def _wait(self, e, tok):
        if tok is None:
            return
        sem, val, src = tok
        if src == e and e == 'pe':
            return
        if self.seen[e].get(id(sem), 0) >= val:
            return
        self.eng[e].wait_ge(sem, val)
        self.seen[e][id(sem)] = val

    def _deps(self, e, reads, writes):
        for r in reads:
            self._wait(e, self.lastw.get(r))
        for w in writes:
            self._wait(e, self.lastw.get(w))
            for tok in self.readers.get(w, {}).values():
                self._wait(e, tok)

    def _record(self, tok, reads, writes):
        for w in writes:
            self.lastw[w] = tok
            self.readers[w] = {}
        for r in reads:
            if r in writes:
                continue
            self.readers.setdefault(r, {})[id(tok[0])] = tok

    def op(self, e, fn, reads=(), writes=()):
        reads = [self.key(r) for r in reads]
        writes = [self.key(w) for w in writes]
        self._deps(e, reads, writes)
        ins = fn(self.eng[e])
        self.cnt[e] += 1
        ins.then_inc(self.sem[e], 1)
        self._record((self.sem[e], self.cnt[e], e), reads, writes)
        self.nins += 1
        return ins

    def dma(self, e, out, in_, reads=None, writes=None, **kw):
        reads = [self.key(r) for r in (reads if reads is not None else [in_])]
        writes = [self.key(w) for w in (writes if writes is not None else [out])]
        self._deps(e, reads, writes)
        i = self.dnext
        self.dnext = (self.dnext + 1) % self.NDMA
        sem = self.dsem[i]
        self._wait(e, (sem, self.dcnt[i], 'dma'))
        self.eng[e].dma_start(out=out, in_=in_, **kw).then_inc(sem, 16)
        self.dcnt[i] += 16
        self._record((sem, self.dcnt[i], 'dma'), reads, writes)
        self.nins += 1

    def barrier(self):
        toks = [(self.sem[e], self.cnt[e], e) for e in self.sem if self.cnt[e] > 0]
        toks += [(self.dsem[i], self.dcnt[i], 'dma') for i in range(self.NDMA) if self.dcnt[i] > 0]
        for e in self.eng:
            for t in toks:
                if t[2] == e:
                    continue
                self._wait(e, t)
        self.lastw.clear()
        self.readers.clear()

    def finish(self):
        self.barrier()


def sb(nc, name, shape, dt):
    return nc.sbuf_tensor(name, list(shape), dt).__enter__()


def ps(nc, name, shape, dt=F32):
    return nc.psum_tensor(name, list(shape), dt).__enter__()


class Prog:
    def __init__(self, dbg=()):
        self.nc = nc = bass.Bass("TRN2", target_bir_lowering=False)
        self.k = K(nc)
        self.dbg = set(dbg)
        self.dram = {}
        self._ctx = []

    def inp(self, name, shape, dt=F32):
        t = self.nc.dram_tensor(name, list(shape), dt, kind="ExternalInput")
        self.dram[name] = t
        return t.ap()

    def outp(self, name, shape, dt=F32):
        t = self.nc.dram_tensor(name, list(shape), dt, kind="ExternalOutput")
        self.dram[name] = t
        return t.ap()

    def scratch(self, name, shape, dt=F32):
        kind = "ExternalOutput" if name in self.dbg else "Internal"
        t = self.nc.dram_tensor(name, list(shape), dt, kind=kind)
        self.dram[name] = t
        return t.ap()

    def alloc_sb(self, name, shape, dt):
        self._uid = getattr(self, '_uid', 0) + 1
        name = "%s_u%d" % (name, self._uid)
        g = self.nc.sbuf_tensor(name, list(shape), dt)
        t = g.__enter__()
        self._ctx.append(g)
        return t

    def alloc_ps(self, name, shape, dt=F32):
        self._uid = getattr(self, '_uid', 0) + 1
        name = "%s_u%d" % (name, self._uid)
        g = self.nc.psum_tensor(name, list(shape), dt)
        t = g.__enter__()
        self._ctx.append(g)
        return t

    def dump(self, name, tile, dt=F32):
        if name in self.dbg:
            o = self.outp("dbg_" + name, list(tile.shape), dt)
            self.k.dma('sp', o, tile[:], reads=[tile], writes=["dbg_" + name])

    def mark(self):
        return len(self._ctx)

    def release(self, mark):
        self.k.barrier()
        while len(self._ctx) > mark:
            self._ctx.pop().__exit__(None, None, None)


def stage_mod(P, l, cT, w_ada, b_adaT, modT):
    nc, k = P.nc, P.k
    m = P.mark()
    c_sb = P.alloc_sb("mod_c", [128, NCH, 2], F32)
    sc = P.alloc_sb("mod_silu", [128, NCH, 2], F32)
    bsb = P.alloc_sb("mod_b", [128, 96], F32)
    slabs = [P.alloc_sb("mod_w%d" % i, [128, NCH, 512], F32) for i in range(2)]
    pss = [P.alloc_ps("mod_ps%d" % i, [128, 4, 2]) for i in range(2)]
    k.dma('sp', c_sb[:], cT)
    k.dma('sp', bsb[:], b_adaT[l])
    k.op('act', lambda e: e.activation(out=sc[:], in_=c_sb[:], func=AF.Silu), reads=[c_sb], writes=[sc])
    wv = w_ada[l].rearrange("(kc p) c -> p kc c", p=128)
    for cs in range(24):
        slab = slabs[cs % 2]
        pst = pss[cs % 2]
        k.dma('sp' if cs % 2 == 0 else 'act', slab[:], wv[:, :, cs * 512:(cs + 1) * 512])
        for j in range(4):
            for kc in range(NCH):
                k.op('pe', lambda e, j=j, kc=kc: e.matmul(pst[:, j, :], lhsT=slab[:, kc, j * 128:(j + 1) * 128],
                                                       rhs=sc[:, kc, :], start=(kc == 0), stop=(kc == NCH - 1)),
                     reads=[slab, sc], writes=[pst])
        for r in range(2):
            k.op('dve', lambda e, r=r: e.tensor_tensor(out=modT[:, cs * 4:(cs + 1) * 4, r], in0=pst[:, :, r],
                                                      in1=bsb[:, cs * 4:(cs + 1) * 4], op=ALU.add),
                 reads=[pst, bsb], writes=[modT])
    P.release(m)


TOK_BLOCKS = [(0, 512, 0), (512, 512, 0), (1024, 512, 0), (1536, 512, 0), (2048, 256, 1)]


def make_consts(P):
    nc, k = P.nc, P.k
    C = {}
    C['ones_bf'] = sb(nc, "c_ones_bf", [128, 128], BF16)
    C['ones_f'] = sb(nc, "c_ones_f", [128, 128], F32)
    C['id_f'] = sb(nc, "c_id_f", [128, 128], F32)
    C['id_bf'] = sb(nc, "c_id_bf", [128, 128], BF16)
    k.op('pool', lambda e: e.memset(C['ones_f'][:], 1.0), writes=[C['ones_f']])
    k.op('dve', lambda e: e.tensor_copy(out=C['ones_bf'][:], in_=C['ones_f'][:]), reads=[C['ones_f']], writes=[C['ones_bf']])
    k.op('pool', lambda e: e.affine_select(out=C['id_f'][:], in_=C['ones_f'][:], pattern=[[-1, 128]],
                                           compare_op=ALU.is_equal, fill=0.0, base=0, channel_multiplier=1),
         reads=[C['ones_f']], writes=[C['id_f']])
    k.op('dve', lambda e: e.tensor_copy(out=C['id_bf'][:], in_=C['id_f'][:]), reads=[C['id_f']], writes=[C['id_bf']])
    return C


def ln_block(P, C, tmp, xt, nt, dst_fn, scale_fn, bias_fn, src_key=None):
    nc, k = P.nc, P.k
    xb, sq, ps_s, ps_q, mean, rstd, t1 = tmp['xb'], tmp['sq'], tmp['ps_s'], tmp['ps_q'], tmp['mean'], tmp['rstd'], tmp['t1']
    k.op('act', lambda e: e.activation(out=xb[:, :, :nt], in_=xt[:, :, :nt], func=AF.Copy), reads=[xt], writes=[xb])
    k.op('pool', lambda e: e.tensor_tensor(out=sq[:, :, :nt], in0=xt[:, :, :nt], in1=xt[:, :, :nt], op=ALU.mult),
         reads=[xt], writes=[sq])
    for c in range(NCH):
        k.op('pe', lambda e, c=c: e.matmul(ps_s[:, :nt], lhsT=C['ones_bf'][:], rhs=xb[:, c, :nt], start=(c == 0), stop=(c == NCH - 1)),
             reads=[xb, C['ones_bf']], writes=[ps_s])
    for c in range(NCH):
        k.op('pe', lambda e, c=c: e.matmul(ps_q[:, :nt], lhsT=C['ones_bf'][:], rhs=sq[:, c, :nt], start=(c == 0), stop=(c == NCH - 1)),
             reads=[sq, C['ones_bf']], writes=[ps_q])
    k.op('act', lambda e: e.mul(out=mean[:, :nt], in_=ps_s[:, :nt], mul=1.0 / D), reads=[ps_s], writes=[mean])
    k.op('dve', lambda e: e.tensor_tensor(out=rstd[:, :nt], in0=mean[:, :nt], in1=mean[:, :nt], op=ALU.mult),
         reads=[mean], writes=[rstd])
    k.op('dve', lambda e: e.scalar_tensor_tensor(out=rstd[:, :nt], in0=ps_q[:, :nt], scalar=1.0 / D, in1=rstd[:, :nt],
                                                 op0=ALU.mult, op1=ALU.subtract), reads=[ps_q, rstd], writes=[rstd])
    k.op('dve', lambda e: e.tensor_scalar_add(out=rstd[:, :nt], in0=rstd[:, :nt], scalar1=EPS), reads=[rstd], writes=[rstd])
    k.op('act', lambda e: e.activation(out=rstd[:, :nt], in_=rstd[:, :nt], func=AF.Ln), reads=[rstd], writes=[rstd])
    k.op('act', lambda e: e.activation(out=rstd[:, :nt], in_=rstd[:, :nt], func=AF.Exp, scale=-0.5), reads=[rstd], writes=[rstd])
    for c in range(NCH):
        tt = t1[c % 2]
        k.op('dve', lambda e, c=c, tt=tt: e.tensor_tensor(out=tt[:, :nt], in0=xt[:, c, :nt], in1=mean[:, :nt], op=ALU.subtract),
             reads=[xt, mean], writes=[tt])
        k.op('pool', lambda e, tt=tt: e.tensor_tensor(out=tt[:, :nt], in0=tt[:, :nt], in1=rstd[:, :nt], op=ALU.mult),
             reads=[tt, rstd], writes=[tt])
        dst, dkeys = dst_fn(c)
        s_ap, skeys = scale_fn(c)
        b_ap, bkeys = bias_fn(c)
        k.op('act', lambda e, tt=tt, dst=dst, s_ap=s_ap, b_ap=b_ap: e.activation(out=dst, in_=tt[:, :nt], func=AF.Identity,
                                                                               scale=s_ap, bias=b_ap),
             reads=[tt] + skeys + bkeys, writes=dkeys)


def ln_tmp(P, pfx, nmax=512):
    return {
        'xb': P.alloc_sb(pfx + "_xb", [128, NCH, nmax], BF16),
        'sq': P.alloc_sb(pfx + "_sq", [128, NCH, nmax], BF16),
        'ps_s': P.alloc_ps(pfx + "_pss", [128, nmax]),
        'ps_q': P.alloc_ps(pfx + "_psq", [128, nmax]),
        'mean': P.alloc_sb(pfx + "_mean", [128, nmax], F32),
        'rstd': P.alloc_sb(pfx + "_rstd", [128, nmax], F32),
        't1': [P.alloc_sb(pfx + "_t1%d" % i, [128, nmax], F32) for i in range(2)],
    }


def stage_inproj(P, C, l, xT, w_in, modp, pfm, ptm):
    nc, k = P.nc, P.k
    m = P.mark()
    hT = P.alloc_sb("ip_hT", [128, NCH, T], BF16)
    m2 = P.mark()
    tmp = ln_tmp(P, "ip")
    xts = [P.alloc_sb("ip_xt%d" % i, [128, NCH, 512], F32) for i in range(2)]
    xv = xT.rearrange("(c p) t -> p c t", p=128)
    for bi, (t0, nt, r) in enumerate(TOK_BLOCKS):
        xt = xts[bi % 2]
        k.dma('sp', xt[:, :, :nt], xv[:, :, t0:t0 + nt], writes=[xt])
        ln_block(P, C, tmp, xt, nt,
                 lambda c: (hT[:, c, t0:t0 + nt], [hT]),
                 lambda c: (modp['sc1p'][:, c, r:r + 1], [modp['sc1p']]),
                 lambda c: (modp['sh1'][:, c, r:r + 1], [modp['sh1']]))
    P.release(m2)
    slabs = [P.alloc_sb("ip_w%d" % i, [128, NCH, 512], BF16) for i in range(2)]
    pss = [P.alloc_ps("ip_ps%d" % i, [128, 512]) for i in range(4)]
    stg = [P.alloc_sb("ip_stg%d" % i, [128, 512], F32) for i in range(4)]
    wv = w_in[l].rearrange("(kc p) c -> p kc c", p=128)
    ev = 0
    for s in range(13):
        slab = slabs[s % 2]
        ncol = 512 if s < 12 else 32
        k.dma('pool', slab[:, :, :ncol], wv[:, :, s * 512:s * 512 + ncol], writes=[slab])
        if s < 6 or s == 12:
            row0 = s * 512 if s < 6 else 3072
            for j in range((ncol + 127) // 128):
                cw = min(128, ncol - j * 128)
                for (t0, nt, r) in TOK_BLOCKS:
                    pst = pss[ev % 4]; st = stg[ev % 4]
                    for kc in range(NCH):
                        k.op('pe', lambda e, kc=kc, pst=pst: e.matmul(pst[:cw, :nt], lhsT=slab[:, kc, j * 128:j * 128 + cw],
                                                                     rhs=hT[:, kc, t0:t0 + nt], start=(kc == 0), stop=(kc == NCH - 1)),
                             reads=[slab, hT], writes=[pst])
                    if ev % 2 == 0:
                        k.op('act', lambda e, pst=pst, st=st: e.copy(out=st[:cw, :nt], in_=pst[:cw, :nt]), reads=[pst], writes=[st])
                    else:
                        k.op('dve', lambda e, pst=pst, st=st: e.tensor_copy(out=st[:cw, :nt], in_=pst[:cw, :nt]), reads=[pst], writes=[st])
                    k.dma('sp', pfm[row0 + j * 128:row0 + j * 128 + cw, t0:t0 + nt], st[:cw, :nt], reads=[st], writes=[pfm])
                    ev += 1
        else:
            col0 = (s - 6) * 512
            for tt in range(T // 128):
                pst = pss[ev % 4]; st = stg[ev % 4]
                for kc in range(NCH):
                    k.op('pe', lambda e, kc=kc, pst=pst: e.matmul(pst[:, :], lhsT=hT[:, kc, tt * 128:(tt + 1) * 128],
                                                                 rhs=slab[:, kc, :], start=(kc == 0), stop=(kc == NCH - 1)),
                         reads=[slab, hT], writes=[pst])
                if ev % 2 == 0:
                    k.op('act', lambda e, pst=pst, st=st: e.copy(out=st[:, :], in_=pst[:, :]), reads=[pst], writes=[st])
                else:
                    k.op('dve', lambda e, pst=pst, st=st: e.tensor_copy(out=st[:, :], in_=pst[:, :]), reads=[pst], writes=[st])
                k.dma('sp', ptm[tt * 128:(tt + 1) * 128, col0:col0 + 512], st[:, :], reads=[st], writes=[ptm])
                ev += 1
    P.release(m)


def load_mod(P, modT):
    nc, k = P.nc, P.k
    mp = {}
    names = ['sh1', 'sc1p', 'g1', 'sh2', 'sc2p', 'g2']
    for j, n in enumerate(names):
        t = P.alloc_sb("modp_" + n, [128, NCH, 2], F32)
        if n.startswith('sc'):
            k.op('dve', lambda e, t=t, j=j: e.tensor_scalar_add(out=t[:], in0=modT[:, j * 16:(j + 1) * 16, :], scalar1=1.0),
                 reads=[modT], writes=[t])
        else:
            k.op('dve', lambda e, t=t, j=j: e.tensor_copy(out=t[:], in_=modT[:, j * 16:(j + 1) * 16, :]), reads=[modT], writes=[t])
        mp[n] = t
    return mp


HY_MIN = math.log(1e-2) / 1.5
HY_MAX = math.log(1e-2) / 0.3


def stage_hyena(P, C, l, W, pfm, mixT, tok0, Ls):
    nc, k = P.nc, P.k
    NT = Ls // 128
    M = 2 * Ls
    pf = "hy%d_" % Ls
    m0 = P.mark()
    hfb = P.alloc_sb(pf + "hfb", [128, NT, 1024], BF16)
    uT = P.alloc_sb(pf + "uT", [128, NT, 512], BF16)
    x0T = P.alloc_sb(pf + "x0T", [128, NT, 512], F32)
    frow = P.alloc_sb(pf + "frow", [128, Ls], F32)
    ncol = P.alloc_sb(pf + "ncol", [128, NT], F32)
    pa = [P.alloc_ps(pf + "pa%d" % i, [128, 512]) for i in range(7)]
    ptb = P.alloc_ps(pf + "ptb", [128, 512], BF16)
    ti = P.alloc_sb(pf + "ti", [128, Ls], I32)
    k.op('pool', lambda e: e.iota(ti[:], pattern=[[1, Ls]], base=0, channel_multiplier=0), writes=[ti])
    k.op('dve', lambda e: e.tensor_copy(out=frow[:], in_=ti[:]), reads=[ti], writes=[frow])
    k.op('pool', lambda e: e.iota(ti[:, :NT], pattern=[[128, NT]], base=0, channel_multiplier=1), reads=[frow], writes=[ti])
    k.op('dve', lambda e: e.tensor_copy(out=ncol[:], in_=ti[:, :NT]), reads=[ti], writes=[ncol])

    m1 = P.mark()
    w1 = P.alloc_sb(pf + "w1", [33, 64], F32)
    w2 = P.alloc_sb(pf + "w2", [64, 64], F32)
    w3 = P.alloc_sb(pf + "w3", [64, 1024], F32)
    fv = P.alloc_sb(pf + "fv", [64, 3], F32)
    fc = P.alloc_sb(pf + "fc", [64, 4], F32)
    hb = P.alloc_sb(pf + "hb", [1, 512], F32)
    k.dma('sp', w1[:], W['hy_f_w1'][l])
    k.dma('sp', w2[:], W['hy_f_w2'][l])
    k.dma('sp', w3[:], W['hy_f_w3'][l])
    k.dma('sp', fv[:], W['hy_fv'][l])
    k.dma('sp', hb[:], W['hy_bias'][l:l + 1, :])
    k.op('dve', lambda e: e.tensor_scalar_mul(out=fc[:, 0:1], in0=fv[:, 2:3], scalar1=1.0 / (2 * math.pi)), reads=[fv], writes=[fc])
    for i in range(2):
        k.op('dve', lambda e, i=i: e.tensor_scalar(out=fc[:, 1 + i:2 + i], in0=fv[:, i:i + 1], scalar1=fc[:, 0:1], scalar2=0.0,
                                                  op0=ALU.mult, op1=ALU.add), reads=[fv, fc], writes=[fc])
    pc = P.alloc_sb(pf + "pc", [33, 4], F32)
    pi_ = P.alloc_sb(pf + "pi", [33, 1], I32)
    k.op('pool', lambda e: e.iota(pi_[:], pattern=[[0, 1]], base=0, channel_multiplier=1), writes=[pi_])
    k.op('dve', lambda e: e.tensor_copy(out=pc[:, 0:1], in_=pi_[:]), reads=[pi_], writes=[pc])
    step = (15.0 - 1e-4) / 15.0
    k.op('dve', lambda e: e.tensor_scalar(out=pc[:, 3:4], in0=pc[:, 0:1], scalar1=16.5, scalar2=-16.0, op0=ALU.is_gt, op1=ALU.mult),
         reads=[pc], writes=[pc])
    k.op('dve', lambda e: e.tensor_tensor(out=pc[:, 1:2], in0=pc[:, 0:1], in1=pc[:, 3:4], op=ALU.add), reads=[pc], writes=[pc])
    k.op('dve', lambda e: e.tensor_scalar(out=pc[:, 1:2], in0=pc[:, 1:2], scalar1=step / Ls, scalar2=(1e-4 - step) / Ls, op0=ALU.mult, op1=ALU.add),
         reads=[pc], writes=[pc])
    k.op('dve', lambda e: e.tensor_scalar(out=pc[:, 2:3], in0=pc[:, 0:1], scalar1=16.5, scalar2=-0.25, op0=ALU.is_lt, op1=ALU.mult),
         reads=[pc], writes=[pc])
    k.op('dve', lambda e: e.tensor_scalar_add(out=pc[:, 2:3], in0=pc[:, 2:3], scalar1=0.5), reads=[pc], writes=[pc])
    wi_ = P.alloc_sb(pf + "wi", [64, Ls], I32)
    wf_ = P.alloc_sb(pf + "wff", [64, Ls], F32)

    def wrap(t, np_):
        k.op('dve', lambda e: e.tensor_copy(out=wi_[:np_, :], in_=t[:np_, :]), reads=[t], writes=[wi_])
        k.op('pool', lambda e: e.tensor_copy(out=wf_[:np_, :], in_=wi_[:np_, :]), reads=[wi_], writes=[wf_])
        k.op('dve', lambda e: e.tensor_tensor(out=t[:np_, :], in0=t[:np_, :], in1=wf_[:np_, :], op=ALU.subtract), reads=[t, wf_], writes=[t])
    zT = P.alloc_sb(pf + "zT", [33, Ls], F32)
    h1 = P.alloc_sb(pf + "h1", [64, Ls], F32)
    h2 = P.alloc_sb(pf + "h2", [64, Ls], F32)
    k.op('dve', lambda e: e.tensor_scalar(out=zT[:], in0=frow[:33, :], scalar1=pc[:, 1:2], scalar2=pc[:, 2:3], op0=ALU.mult, op1=ALU.add),
         reads=[frow, pc], writes=[zT])
    wrap(zT, 33)
    k.op('act', lambda e: e.activation(out=zT[:], in_=zT[:], func=AF.Sin, scale=2 * math.pi), reads=[zT], writes=[zT])
    k.op('act', lambda e: e.mul(out=zT[0:1, :], in_=frow[0:1, :], mul=1.0 / (Ls - 1)), reads=[frow, zT], writes=[zT])
    BL = min(512, Ls)
    for (src, wt, dst, ci) in ((zT, w1, h1, 1), (h1, w2, h2, 2)):
        for b0 in range(0, Ls, BL):
            pst = pa[(b0 // BL) % 2]
            k.op('pe', lambda e, pst=pst, src=src, wt=wt, b0=b0: e.matmul(pst[:64, :BL], lhsT=wt[:], rhs=src[:, b0:b0 + BL], start=True, stop=True),
                 reads=[wt, src], writes=[pst])
            k.op('dve', lambda e, pst=pst, dst=dst, b0=b0, ci=ci: e.tensor_scalar(out=dst[:, b0:b0 + BL], in0=pst[:64, :BL], scalar1=fc[:, 0:1],
                                                                                 scalar2=fc[:, ci:ci + 1], op0=ALU.mult, op1=ALU.add),
                 reads=[pst, fc], writes=[dst])
        wrap(dst, 64)
        k.op('act', lambda e, dst=dst: e.activation(out=dst[:], in_=dst[:], func=AF.Sin, scale=2 * math.pi), reads=[dst], writes=[dst])
    drow = P.alloc_sb(pf + "drow", [128, 512], F32)
    negt = P.alloc_sb(pf + "negt", [128, NT], F32)
    k.op('dve', lambda e: e.tensor_scalar(out=drow[:], in0=frow[:, :512] if Ls >= 512 else frow[:, :], scalar1=-(HY_MAX - HY_MIN) / 511.0, scalar2=-HY_MIN,
                                          op0=ALU.mult, op1=ALU.add), reads=[frow], writes=[drow]) if Ls >= 512 else None
    if Ls < 512:
        ti2 = P.alloc_sb(pf + "ti2", [128, 512], I32)
        k.op('pool', lambda e: e.iota(ti2[:], pattern=[[1, 512]], base=0, channel_multiplier=0), writes=[ti2])
        k.op('dve', lambda e: e.tensor_copy(out=drow[:], in_=ti2[:]), reads=[ti2], writes=[drow])
        k.op('dve', lambda e: e.tensor_scalar(out=drow[:], in0=drow[:], scalar1=-(HY_MAX - HY_MIN) / 511.0, scalar2=-HY_MIN,
                                              op0=ALU.mult, op1=ALU.add), reads=[drow], writes=[drow])
    k.op('dve', lambda e: e.tensor_scalar_mul(out=negt[:], in0=ncol[:], scalar1=-1.0 / (Ls - 1)), reads=[ncol], writes=[negt])
    dec = P.alloc_sb(pf + "dec", [128, 512], F32)
    h0 = P.alloc_sb(pf + "h0", [128, 1024], F32)
    for tc in range(NT):
        k.op('act', lambda e, tc=tc: e.activation(out=dec[:], in_=drow[:], func=AF.Exp, scale=negt[:, tc:tc + 1]), reads=[drow, negt], writes=[dec])
        for hf in range(2):
            pst = pa[2 + hf]
            k.op('pe', lambda e, pst=pst, tc=tc, hf=hf: e.matmul(pst[:, :], lhsT=h2[:, tc * 128:(tc + 1) * 128], rhs=w3[:, hf * 512:(hf + 1) * 512],
                                                               start=True, stop=True), reads=[h2, w3], writes=[pst])
            if tc == 0:
                k.op('dve', lambda e, pst=pst, hf=hf: e.tensor_tensor(out=h0[:, hf * 512:(hf + 1) * 512], in0=pst[:, :], in1=dec[:], op=ALU.mult),
                     reads=[pst, dec], writes=[h0])
            else:
                k.op('dve', lambda e, pst=pst, tc=tc, hf=hf: e.tensor_tensor(out=hfb[:, tc, hf * 512:(hf + 1) * 512], in0=pst[:, :], in1=dec[:], op=ALU.mult),
                     reads=[pst, dec], writes=[hfb])
        if tc == 0:
            k.op('dve', lambda e: e.tensor_tensor(out=h0[0:1, 0:512], in0=h0[0:1, 0:512], in1=hb[:], op=ALU.add), reads=[h0, hb], writes=[h0])
            k.op('pool', lambda e: e.memset(h0[0:1, 512:1024], 0.0), reads=[h0], writes=[h0])
            k.op('act', lambda e: e.copy(out=hfb[:, 0, :], in_=h0[:]), reads=[h0], writes=[hfb])
    P.release(m1)

    m2 = P.mark()
    sw = P.alloc_sb(pf + "sw", [128, 12, 4], F32)
    k.dma('sp', sw[:], W['hy_sw'][l])
    raws = [P.alloc_sb(pf + "raw%d" % i, [128, Ls], F32) for i in range(2)]
    cvs = [P.alloc_sb(pf + "cv%d" % i, [128, Ls], F32) for i in range(3)]
    ub = P.alloc_sb(pf + "ub", [128, Ls], BF16)

    def conv(j, dst, ri):
        raw = raws[ri]
        k.dma('sp', raw[:], pfm[j * 128:(j + 1) * 128, tok0:tok0 + Ls], writes=[raw])
        k.op('act', lambda e: e.activation(out=dst[:], in_=raw[:], func=AF.Identity, scale=sw[:, j, 1:2], bias=sw[:, j, 3:4]),
             reads=[raw, sw], writes=[dst])
        k.op('dve', lambda e: e.scalar_tensor_tensor(out=dst[:, 1:], in0=raw[:, :Ls - 1], scalar=sw[:, j, 0:1], in1=dst[:, 1:],
                                                     op0=ALU.mult, op1=ALU.add), reads=[raw, sw, dst], writes=[dst])
        k.op('dve', lambda e: e.scalar_tensor_tensor(out=dst[:, :Ls - 1], in0=raw[:, 1:], scalar=sw[:, j, 2:3], in1=dst[:, :Ls - 1],
                                                     op0=ALU.mult, op1=ALU.add), reads=[raw, sw, dst], writes=[dst])

    for j in range(4):
        conv(j, cvs[0], 0)
        for tc in range(NT):
            pst = pa[tc % 2]
            k.op('pe', lambda e, pst=pst, tc=tc: e.transpose(out=pst[:, :128], in_=cvs[0][:, tc * 128:(tc + 1) * 128], identity=C['id_f'][:]),
                 reads=[cvs[0], C['id_f']], writes=[pst])
            k.op('act', lambda e, pst=pst, tc=tc, j=j: e.copy(out=x0T[:, tc, j * 128:(j + 1) * 128], in_=pst[:, :128]), reads=[pst], writes=[x0T])
        conv(4 + j, cvs[1], 1)
        conv(8 + j, cvs[2], 0)
        k.op('pool', lambda e: e.tensor_tensor(out=ub[:], in0=cvs[1][:], in1=cvs[2][:], op=ALU.mult), reads=[cvs[1], cvs[2]], writes=[ub])
        for tc in range(NT):
            k.op('pe', lambda e, tc=tc: e.transpose(out=ptb[:, :128], in_=ub[:, tc * 128:(tc + 1) * 128], identity=C['id_bf'][:]),
                 reads=[ub, C['id_bf']], writes=[ptb])
            k.op('dve', lambda e, tc=tc, j=j: e.tensor_copy(out=uT[:, tc, j * 128:(j + 1) * 128], in_=ptb[:, :128]), reads=[ptb], writes=[uT])
    P.release(m2)

    Asb = P.alloc_sb(pf + "A", [128, NT, 512], BF16)
    Bsb = P.alloc_sb(pf + "B", [128, NT, 512], BF16)
    csb = [P.alloc_sb(pf + "cs%d" % i, [128, 2, 128], BF16) for i in range(3)]
    pm = [P.alloc_sb(pf + "pm%d" % i, [128, 2, 128], F32) for i in range(3)]
    pmi = [P.alloc_sb(pf + "pmi%d" % i, [128, 2, 128], I32) for i in range(3)]
    pmf = [P.alloc_sb(pf + "pmf%d" % i, [128, 2, 128], F32) for i in range(3)]
    wf = P.alloc_sb(pf + "wf", [128, NT], F32)
    nwf = P.alloc_sb(pf + "nwf", [128, NT], F32)
    k.op('pool', lambda e: e.memset(wf[:], 2.0 / M), writes=[wf])
    k.op('pool', lambda e: e.memset(wf[0:1, 0:1], 1.0 / M), reads=[wf], writes=[wf])
    k.op('dve', lambda e: e.tensor_scalar_mul(out=nwf[:], in0=wf[:], scalar1=-1.0), reads=[wf], writes=[nwf])
    gi = [0]

    def gen(a, b):
        i = gi[0] % 3
        gi[0] += 1
        k.op('dve', lambda e: e.tensor_scalar(out=pm[i][:, 0, :], in0=frow[:, b * 128:(b + 1) * 128], scalar1=ncol[:, a:a + 1], scalar2=1.0 / M,
                                              op0=ALU.mult, op1=ALU.mult), reads=[frow, ncol], writes=[pm[i]])
        k.op('pool', lambda e: e.tensor_scalar_add(out=pm[i][:, 1, :], in0=pm[i][:, 0, :], scalar1=0.25), reads=[pm[i]], writes=[pm[i]])
        k.op('dve', lambda e: e.tensor_copy(out=pmi[i][:], in_=pm[i][:]), reads=[pm[i]], writes=[pmi[i]])
        k.op('pool', lambda e: e.tensor_copy(out=pmf[i][:], in_=pmi[i][:]), reads=[pmi[i]], writes=[pmf[i]])
        k.op('dve', lambda e: e.tensor_tensor(out=pm[i][:], in0=pm[i][:], in1=pmf[i][:], op=ALU.subtract), reads=[pm[i], pmf[i]], writes=[pm[i]])
        k.op('act', lambda e: e.activation(out=csb[i][:], in_=pm[i][:], func=AF.Sin, scale=2 * math.pi), reads=[pm[i]], writes=[csb[i]])
        return csb[i][:, 1, :], csb[i][:, 0, :], csb[i]

    def cols(N, j):
        return uT[:, N, :] if j == 0 else hfb[:, N, (j - 1) * 512:j * 512]

    alt = P.alloc_sb(pf + "alt", [128, 128], F32)
    altb = P.alloc_sb(pf + "altb", [128, 128], BF16)
    alti = P.alloc_sb(pf + "alti", [128, 128], I32)
    altf = P.alloc_sb(pf + "altf", [128, 128], F32)
    k.op('dve', lambda e: e.tensor_scalar(out=alt[:], in0=frow[:, :128], scalar1=ncol[:, 0:1], scalar2=0.5, op0=ALU.add, op1=ALU.mult),
         reads=[frow, ncol], writes=[alt])
    k.op('dve', lambda e: e.tensor_scalar_add(out=alt[:], in0=alt[:], scalar1=0.25), reads=[alt], writes=[alt])
    k.op('dve', lambda e: e.tensor_copy(out=alti[:], in_=alt[:]), reads=[alt], writes=[alti])
    k.op('dve', lambda e: e.tensor_copy(out=altf[:], in_=alti[:]), reads=[alti], writes=[altf])
    k.op('dve', lambda e: e.tensor_tensor(out=alt[:], in0=alt[:], in1=altf[:], op=ALU.subtract), reads=[alt, altf], writes=[alt])
    k.op('act', lambda e: e.activation(out=altb[:], in_=alt[:], func=AF.Sin, scale=2 * math.pi), reads=[alt], writes=[altb])
    for j in range(3):
        for N in range(NT):
            k.op('pe', lambda e, j=j, N=N: e.matmul(pa[j][0:1, :], lhsT=altb[:, 0:1], rhs=cols(N, j), start=(N == 0), stop=(N == NT - 1)),
                 reads=[altb, uT, hfb], writes=[pa[j]])
    nyq = P.alloc_sb(pf + "nyq", [1, 2, 512], F32)
    nyqb = P.alloc_sb(pf + "nyqb", [1, 512], BF16)
    k.op('act', lambda e: e.copy(out=nyq[:, 0, :], in_=pa[1][0:1, :]), reads=[pa[1]], writes=[nyq])
    k.op('dve', lambda e: e.tensor_tensor(out=nyq[:, 0, :], in0=pa[2][0:1, :], in1=nyq[:, 0, :], op=ALU.add), reads=[pa[2], nyq], writes=[nyq])
    k.op('dve', lambda e: e.tensor_tensor(out=nyq[:, 1, :], in0=pa[0][0:1, :], in1=nyq[:, 0, :], op=ALU.mult), reads=[pa[0], nyq], writes=[nyq])
    k.op('act', lambda e: e.mul(out=nyqb[:], in_=nyq[:, 1, :], mul=1.0 / M), reads=[nyq], writes=[nyqb])

    ev = [P.alloc_sb(pf + "ev%d" % i, [128, 512], F32) for i in range(4)]
    tt = [P.alloc_sb(pf + "tt%d" % i, [128, 512], F32) for i in range(4)]
    for F in range(NT):
        for N in range(NT):
            cbk, sbl, ck = gen(N, F)
            for j in range(3):
                k.op('pe', lambda e, j=j, N=N, cbk=cbk: e.matmul(pa[j][:, :], lhsT=cbk, rhs=cols(N, j), start=(N == 0), stop=(N == NT - 1)),
                     reads=[ck, uT, hfb], writes=[pa[j]])
            for j in range(3):
                k.op('pe', lambda e, j=j, N=N, sbl=sbl: e.matmul(pa[3 + j][:, :], lhsT=sbl, rhs=cols(N, j), start=(N == 0), stop=(N == NT - 1)),
                     reads=[ck, uT, hfb], writes=[pa[3 + j]])
        k.op('act', lambda e: e.copy(out=ev[0][:], in_=pa[1][:, :]), reads=[pa[1]], writes=[ev[0]])
        k.op('act', lambda e: e.copy(out=ev[1][:], in_=pa[4][:, :]), reads=[pa[4]], writes=[ev[1]])
        k.op('dve', lambda e: e.tensor_tensor(out=ev[0][:], in0=pa[2][:, :], in1=ev[0][:], op=ALU.add), reads=[pa[2], ev[0]], writes=[ev[0]])
        k.op('dve', lambda e: e.tensor_tensor(out=ev[1][:], in0=pa[5][:, :], in1=ev[1][:], op=ALU.subtract), reads=[pa[5], ev[1]], writes=[ev[1]])
        k.op('dve', lambda e: e.tensor_tensor(out=tt[0][:], in0=pa[0][:, :], in1=ev[0][:], op=ALU.mult), reads=[pa[0], ev[0]], writes=[tt[0]])
        k.op('dve', lambda e: e.tensor_tensor(out=tt[1][:], in0=pa[3][:, :], in1=ev[1][:], op=ALU.mult), reads=[pa[3], ev[1]], writes=[tt[1]])
        k.op('pool', lambda e: e.tensor_tensor(out=tt[0][:], in0=tt[0][:], in1=tt[1][:], op=ALU.add), reads=[tt[0], tt[1]], writes=[tt[0]])
        k.op('act', lambda e, F=F: e.activation(out=Asb[:, F, :], in_=tt[0][:], func=AF.Copy, scale=wf[:, F:F + 1]), reads=[tt[0], wf], writes=[Asb])
        k.op('dve', lambda e: e.tensor_tensor(out=tt[2][:], in0=pa[0][:, :], in1=ev[1][:], op=ALU.mult), reads=[pa[0], ev[1]], writes=[tt[2]])
        k.op('dve', lambda e: e.tensor_tensor(out=tt[3][:], in0=pa[3][:, :], in1=ev[0][:], op=ALU.mult), reads=[pa[3], ev[0]], writes=[tt[3]])
        k.op('pool', lambda e: e.tensor_tensor(out=tt[2][:], in0=tt[2][:], in1=tt[3][:], op=ALU.subtract), reads=[tt[2], tt[3]], writes=[tt[2]])
        k.op('act', lambda e, F=F: e.activation(out=Bsb[:, F, :], in_=tt[2][:], func=AF.Copy, scale=nwf[:, F:F + 1]), reads=[tt[2], nwf], writes=[Bsb])
    nw = P.alloc_sb(pf + "nw", [128, 4], F32)
    k.dma('sp', nw[:], W['hy_norm_wT'][l])
    ys = [P.alloc_sb(pf + "y%d" % i, [128, 512], F32) for i in range(2)]
    yb = [P.alloc_sb(pf + "yb%d" % i, [128, 512], BF16) for i in range(2)]
    junk = P.alloc_sb(pf + "junk", [128, 512], F32)
    ss = P.alloc_sb(pf + "ss", [128, 2], F32)
    mst = [P.alloc_sb(pf + "mst%d" % i, [128, 4, 128], BF16) for i in range(2)]
    for N in range(NT):
        py = pa[N % 2]
        for F in range(NT):
            cbk, sbl, ck = gen(F, N)
            k.op('pe', lambda e, F=F, cbk=cbk: e.matmul(py[:, :], lhsT=cbk, rhs=Asb[:, F, :], start=(F == 0), stop=False),
                 reads=[ck, Asb], writes=[py])
            k.op('pe', lambda e, F=F, sbl=sbl: e.matmul(py[:, :], lhsT=sbl, rhs=Bsb[:, F, :], start=False, stop=False),
                 reads=[ck, Bsb], writes=[py])
        k.op('pe', lambda e: e.matmul(py[:, :], lhsT=altb[0:1, :], rhs=nyqb[:], start=False, stop=True), reads=[altb, nyqb], writes=[py])
        y = ys[N % 2]; ybf = yb[N % 2]; ms = mst[N % 2]
        k.op('dve', lambda e, N=N: e.tensor_tensor(out=y[:], in0=py[:, :], in1=x0T[:, N, :], op=ALU.mult), reads=[py, x0T], writes=[y])
        k.op('act', lambda e: e.activation(out=junk[:], in_=y[:], func=AF.Square, accum_out=ss[:, 0:1]), reads=[y], writes=[junk, ss])
        k.op('dve', lambda e: e.tensor_scalar(out=ss[:, 1:2], in0=ss[:, 0:1], scalar1=1.0 / HY, scalar2=EPS, op0=ALU.mult, op1=ALU.add), reads=[ss], writes=[ss])
        k.op('act', lambda e: e.activation(out=ss[:, 1:2], in_=ss[:, 1:2], func=AF.Ln), reads=[ss], writes=[ss])
        k.op('act', lambda e: e.activation(out=ss[:, 1:2], in_=ss[:, 1:2], func=AF.Exp, scale=-0.5), reads=[ss], writes=[ss])
        k.op('dve', lambda e: e.tensor_scalar_mul(out=ybf[:], in0=y[:], scalar1=ss[:, 1:2]), reads=[y, ss], writes=[ybf])
        for j in range(4):
            k.op('pe', lambda e, j=j: e.transpose(out=ptb[:, j * 128:(j + 1) * 128], in_=ybf[:, j * 128:(j + 1) * 128], identity=C['id_bf'][:]),
                 reads=[ybf, C['id_bf']], writes=[ptb])
        for j in range(4):
            k.op('act', lambda e, j=j: e.activation(out=ms[:, j, :], in_=ptb[:, j * 128:(j + 1) * 128], func=AF.Copy, scale=nw[:, j:j + 1]),
                 reads=[ptb, nw], writes=[ms])
        k.dma('sp', mixT[0:512, tok0 + N * 128:tok0 + (N + 1) * 128].rearrange("(j p) t -> p j t", p=128), ms[:], reads=[ms], writes=[mixT])
    P.release(m0)


NEGM = -1.0e4


def stage_na(P, C, l, W, pfm, ptm, mixT, rpbp, with_ctx):
    nc, k = P.nc, P.k
    pf = "na_"
    m0 = P.mark()
    scale = 128 ** -0.5
    qT = P.alloc_sb(pf + "qT", [128, NAH, T], BF16)
    kT = P.alloc_sb(pf + "kT", [128, NAH, T], BF16)
    v1 = P.alloc_sb(pf + "v1", [128, T // 128, NAH, 129], BF16)
    R = P.alloc_sb(pf + "R", [128, NAH, 9, 128], F32)
    k.op('pool', lambda e: e.memset(v1[:], 1.0), writes=[v1])
    for h in range(NAH):
        k.dma('pool', qT[:, h, :], pfm[O_NQ + h * 128:O_NQ + (h + 1) * 128, :], writes=[qT])
        k.dma('pool', kT[:, h, :], pfm[O_NK + h * 128:O_NK + (h + 1) * 128, :], writes=[kT])
    for tt in range(T // 128):
        k.dma('pool', v1[:, tt, :, 0:128], ptm[tt * 128:(tt + 1) * 128, 0:768].rearrange("p (h d) -> p h d", h=NAH), writes=[v1])
    m1 = P.mark()
    zr = P.alloc_sb(pf + "zr", [90, 160], F32)
    k.op('pool', lambda e: e.memset(zr[:], 0.0), writes=[zr])
    k.dma('sp', zr[:, 48:79], W['na_rpb'][l].rearrange("h r m -> (h r) m"), writes=[zr])
    k.dma('sp', rpbp.rearrange("h r m -> (h r) m"), zr[:], reads=[zr], writes=[rpbp])
    Hk = P.alloc_sb(pf + "Hk", [64, NAH, 15, 64], F32)
    for h in range(NAH):
        src = bass.AP(rpbp.tensor, rpbp[h, 0, 0:1].offset, [[1, 64], [160, 15], [1, 64]])
        k.dma('sp', Hk[:, h, :, :], src, reads=[rpbp], writes=[Hk])
    J = P.alloc_sb(pf + "J", [64, 64], F32)
    k.op('pool', lambda e: e.affine_select(out=J[:], in_=C['ones_f'][:64, :64], pattern=[[1, 64]], compare_op=ALU.is_equal, fill=0.0,
                                           base=-63, channel_multiplier=1), reads=[C['ones_f']], writes=[J])
    ms = [P.alloc_sb(pf + "ms%d" % i, [128, 2, 64], F32) for i in range(4)]
    CM = P.alloc_sb(pf + "CM", [128, 2, 64], F32)
    ones3 = C['ones_f'][:, :].rearrange("p (c q) -> p c q", c=2)
    for a in range(2):
        sl = slice(a * 64, (a + 1) * 64)
        k.op('pool', lambda e, sl=sl, a=a: e.affine_select(out=ms[0][sl], in_=ones3[sl], pattern=[[0, 2], [-1, 64]], compare_op=ALU.is_ge, fill=0.0,
                                                          base=8, channel_multiplier=1), reads=[C['ones_f']], writes=[ms[0]])
        k.op('pool', lambda e, sl=sl, a=a: e.affine_select(out=ms[1][sl], in_=ones3[sl], pattern=[[0, 2], [0, 64]], compare_op=ALU.is_ge, fill=0.0,
                                                          base=-48, channel_multiplier=1), reads=[C['ones_f']], writes=[ms[1]])
        k.op('pool', lambda e, sl=sl, a=a: e.affine_select(out=ms[2][sl], in_=ones3[sl], pattern=[[0, 2], [1, 64]], compare_op=ALU.is_ge, fill=0.0,
                                                          base=7, channel_multiplier=-1), reads=[C['ones_f']], writes=[ms[2]])
        k.op('pool', lambda e, sl=sl, a=a: e.affine_select(out=ms[3][sl], in_=ones3[sl], pattern=[[0, 2], [0, 64]], compare_op=ALU.is_ge, fill=0.0,
                                                          base=15, channel_multiplier=-1), reads=[C['ones_f']], writes=[ms[3]])
    k.op('dve', lambda e: e.tensor_tensor(out=ms[0][:], in0=ms[0][:], in1=ms[1][:], op=ALU.max), reads=[ms[0], ms[1]], writes=[ms[0]])
    k.op('dve', lambda e: e.tensor_tensor(out=ms[2][:], in0=ms[2][:], in1=ms[3][:], op=ALU.max), reads=[ms[2], ms[3]], writes=[ms[2]])
    k.op('dve', lambda e: e.tensor_tensor(out=CM[:], in0=ms[0][:], in1=ms[2][:], op=ALU.mult), reads=[ms[0], ms[2]], writes=[CM])
    k.op('dve', lambda e: e.tensor_copy(out=ms[0][:], in_=CM[:]), reads=[CM], writes=[ms[0]])
    k.op('dve', lambda e: e.tensor_scalar(out=CM[:], in0=CM[:], scalar1=-1.0, scalar2=-NEGM, op0=ALU.add, op1=ALU.mult), reads=[CM], writes=[CM])
    pr = [P.alloc_ps(pf + "pr%d" % i, [128, 2, 64]) for i in range(2)]
    cnt = 0
    for h in range(NAH):
        for vi in range(9):
            d = vi - 3 if vi < 7 else (-2 if vi == 7 else 2)
            pst = pr[cnt % 2]
            cnt += 1
            for c in range(2):
                di = 2 * d - c + 7
                k.op('pe', lambda e, pst=pst, h=h, di=di, c=c: e.matmul(pst[:, c, :], lhsT=Hk[:, h, di:di + 2, :], rhs=J[:], start=True, stop=True),
                     reads=[Hk, J], writes=[pst])
            k.op('dve', lambda e, pst=pst, h=h, vi=vi: e.tensor_tensor(out=R[:, h, vi, :].rearrange("p (c q) -> p c q", c=2), in0=pst[:], in1=ms[0][:], op=ALU.mult),
                 reads=[pst, ms[0]], writes=[R])
            k.op('pool', lambda e, h=h, vi=vi: e.tensor_tensor(out=R[:, h, vi, :].rearrange("p (c q) -> p c q", c=2), in0=R[:, h, vi, :].rearrange("p (c q) -> p c q", c=2),
                                                              in1=CM[:], op=ALU.add), reads=[R, CM], writes=[R])
            if vi == 7:
                k.op('pool', lambda e, h=h, vi=vi: e.memset(R[0:64, h, vi, 64:128], NEGM), reads=[R], writes=[R])
            if vi == 8:
                k.op('pool', lambda e, h=h, vi=vi: e.memset(R[:, h, vi, 0:64], NEGM), reads=[R], writes=[R])
                k.op('pool', lambda e, h=h, vi=vi: e.memset(R[64:128, h, vi, 64:128], NEGM), reads=[R], writes=[R])
    P.dump('na_R', R)
    P.dump('na_Hk', Hk)
    P.release(m1)
    nw = P.alloc_sb(pf + "nw", [128, NAH], F32)
    k.dma('sp', nw[:], W['na_norm_wT'][l])
    pS = [P.alloc_ps(pf + "pS%d" % i, [128, 7, 128]) for i in range(2)]
    pO = [P.alloc_ps(pf + "pO%d" % i, [128, 129]) for i in range(2)]
    ptb = P.alloc_ps(pf + "ptb", [128, NAH, 128], BF16)
    ein = [P.alloc_sb(pf + "ein%d" % i, [128, 5, 128], F32) for i in range(2)]
    PT = [P.alloc_sb(pf + "PT%d" % i, [128, 7, 128], BF16) for i in range(2)]
    ob = [P.alloc_sb(pf + "ob%d" % i, [128, NAH * 128], F32) for i in range(2)]
    obb = [P.alloc_sb(pf + "obb%d" % i, [128, NAH * 128], BF16) for i in range(2)]
    rc = P.alloc_sb(pf + "rc", [128, 2], F32)
    ss = P.alloc_sb(pf + "ss", [128, 2], F32)
    junk = P.alloc_sb(pf + "junk", [128, NAH * 128], F32)
    mst = [P.alloc_sb(pf + "mst%d" % i, [128, NAH, 128], BF16) for i in range(2)]
    CT = [L // 128, L // 128 + 1]
    units = []
    for i in range(16):
        if 2 <= i <= 13:
            loc = [(i - 2, 7), (i - 1, 2), (i, 3), (i + 1, 4), (i + 2, 8)]
        elif i < 2:
            loc = [(j, j - i + 3) for j in range(4)]
        else:
            loc = [(j, j - i + 3) for j in range(12, 16)]
        units.append((i, loc, True))
    if with_ctx:
        units += [(16, [], True), (17, [], True)]
    u = 0
    for (qi, loc, _) in units:
        o_t = ob[u % 2]; o_b = obb[u % 2]; ms_ = mst[u % 2]
        for h in range(NAH):
            g = (u * NAH + h) % 2
            ps_, po, ei, pt = pS[g], pO[g], ein[g], PT[g]
            tiles = [j for (j, _) in loc] + CT
            nl = len(loc)
            for jj, j in enumerate(tiles):
                k.op('pe', lambda e, jj=jj, j=j, h=h, ps_=ps_: e.matmul(ps_[:, jj, :], lhsT=kT[:, h, j * 128:(j + 1) * 128], rhs=qT[:, h, qi * 128:(qi + 1) * 128],
                                                                       start=True, stop=True), reads=[kT, qT], writes=[ps_])
            for jj, (j, vi) in enumerate(loc):
                k.op('dve', lambda e, jj=jj, vi=vi, h=h, ps_=ps_, ei=ei: e.scalar_tensor_tensor(out=ei[:, jj, :], in0=ps_[:, jj, :], scalar=scale, in1=R[:, h, vi, :],
                                                                                               op0=ALU.mult, op1=ALU.add), reads=[ps_, R], writes=[ei])
            if nl:
                k.op('act', lambda e, nl=nl, ei=ei, pt=pt: e.activation(out=pt[:, 0:nl, :], in_=ei[:, 0:nl, :], func=AF.Exp), reads=[ei], writes=[pt])
            k.op('act', lambda e, nl=nl, ps_=ps_, pt=pt: e.activation(out=pt[:, nl:nl + 2, :], in_=ps_[:, nl:nl + 2, :], func=AF.Exp, scale=scale),
                 reads=[ps_], writes=[pt])
            for jj, j in enumerate(tiles):
                k.op('pe', lambda e, jj=jj, j=j, h=h, pt=pt, po=po: e.matmul(po[:, :], lhsT=pt[:, jj, :], rhs=v1[:, j, h, :], start=(jj == 0), stop=(jj == len(tiles) - 1)),
                     reads=[pt, v1], writes=[po])
            k.op('dve', lambda e, po=po: e.reciprocal(out=rc[:, 0:1], in_=po[:, 128:129]), reads=[po], writes=[rc])
            k.op('dve', lambda e, po=po, h=h, o_t=o_t: e.tensor_scalar_mul(out=o_t[:, h * 128:(h + 1) * 128], in0=po[:, 0:128], scalar1=rc[:, 0:1]),
                 reads=[po, rc], writes=[o_t])
        k.op('act', lambda e, o_t=o_t: e.activation(out=junk[:], in_=o_t[:], func=AF.Square, accum_out=ss[:, 0:1]), reads=[o_t], writes=[junk, ss])
        k.op('dve', lambda e: e.tensor_scalar(out=ss[:, 1:2], in0=ss[:, 0:1], scalar1=1.0 / 768, scalar2=EPS, op0=ALU.mult, op1=ALU.add), reads=[ss], writes=[ss])
        k.op('act', lambda e: e.activation(out=ss[:, 1:2], in_=ss[:, 1:2], func=AF.Ln), reads=[ss], writes=[ss])
        k.op('act', lambda e: e.activation(out=ss[:, 1:2], in_=ss[:, 1:2], func=AF.Exp, scale=-0.5), reads=[ss], writes=[ss])
        k.op('dve', lambda e, o_t=o_t, o_b=o_b: e.tensor_scalar_mul(out=o_b[:], in0=o_t[:], scalar1=ss[:, 1:2]), reads=[o_t, ss], writes=[o_b])
        for h in range(NAH):
            k.op('pe', lambda e, h=h, o_b=o_b: e.transpose(out=ptb[:, h, :], in_=o_b[:, h * 128:(h + 1) * 128], identity=C['id_bf'][:]),
                 reads=[o_b, C['id_bf']], writes=[ptb])
        for h in range(NAH):
            k.op('act', lambda e, h=h, ms_=ms_: e.activation(out=ms_[:, h, :], in_=ptb[:, h, :], func=AF.Copy, scale=nw[:, h:h + 1]), reads=[ptb, nw], writes=[ms_])
        k.dma('sp', mixT[512:1280, qi * 128:(qi + 1) * 128].rearrange("(j p) t -> p j t", p=128), ms_[:], reads=[ms_], writes=[mixT])
        u += 1
    P.release(m0)


def stage_gla(P, C, l, W, pfm, ptm, mixT, ofs, with_ctx):
    nc, k = P.nc, P.k
    pf = "gl_"
    m0 = P.mark()
    NTL = L // 128
    NTT = T // 128
    qs = GDK ** -0.5
    Mf = P.alloc_sb(pf + "Mf", [128, 128], F32)
    Mb = P.alloc_sb(pf + "Mb", [128, 128], F32)
    k.op('pool', lambda e: e.affine_select(out=Mf[:], in_=C['ones_f'][:], pattern=[[1, 128]], compare_op=ALU.is_ge, fill=0.0, base=0, channel_multiplier=-1),
         reads=[C['ones_f']], writes=[Mf])
    k.op('pool', lambda e: e.memset(Mf[0:64, 64:128], 0.0), reads=[Mf], writes=[Mf])
    k.op('pool', lambda e: e.affine_select(out=Mb[:], in_=C['ones_f'][:], pattern=[[-1, 128]], compare_op=ALU.is_ge, fill=0.0, base=0, channel_multiplier=1),
         reads=[C['ones_f']], writes=[Mb])
    k.op('pool', lambda e: e.memset(Mb[64:128, 0:64], 0.0), reads=[Mb], writes=[Mb])
    Lf = P.alloc_sb(pf + "Lf", [128, 128], F32)
    Lb = P.alloc_sb(pf + "Lb", [128, 128], F32)
    k.op('dve', lambda e: e.tensor_scalar_mul(out=Lf[:], in0=Mf[:], scalar1=-1.0 / 16), reads=[Mf], writes=[Lf])
    k.op('dve', lambda e: e.tensor_scalar_mul(out=Lb[:], in0=Mb[:], scalar1=-1.0 / 16), reads=[Mb], writes=[Lb])
    ind = P.alloc_sb(pf + "ind", [128, 2], F32)
    k.op('pool', lambda e: e.memset(ind[:], 0.0), writes=[ind])
    k.op('pool', lambda e: e.memset(ind[0:64, 0:1], -1.0 / 16), reads=[ind], writes=[ind])
    k.op('pool', lambda e: e.memset(ind[64:128, 1:2], -1.0 / 16), reads=[ind], writes=[ind])
    one1 = P.alloc_sb(pf + "one1", [128, 1], F32)
    k.op('pool', lambda e: e.memset(one1[:], 1.0), writes=[one1])
    ga1T = P.alloc_sb(pf + "ga1T", [33, T], F32)
    k.op('pool', lambda e: e.memset(ga1T[:], 1.0), writes=[ga1T])
    k.dma('sp', ga1T[0:32, :], pfm[3072:3104, :], writes=[ga1T])
    w2b = P.alloc_sb(pf + "w2b", [33, 2, 384], F32)
    k.op('pool', lambda e: e.memset(w2b[:], 0.0), writes=[w2b])
    for d in range(2):
        k.dma('sp', w2b[16 * d:16 * d + 16, d, :], W['gla_a_w2'][l, d], writes=[w2b])
        k.dma('sp', w2b[32:33, d, :], W['gla_a_b'][l, d:d + 1, :], writes=[w2b])
    gnw = P.alloc_sb(pf + "gnw", [128, 768], F32)
    for h in range(GH):
        k.dma('sp', gnw[:, h * 128:(h + 1) * 128], W['gla_norm_w'][l:l + 1, :].partition_broadcast(128), writes=[gnw])
    m1 = P.mark()
    ii = P.alloc_sb(pf + "ii", [128, 16], I32)
    inv = P.alloc_sb(pf + "inv", [128, 16], F32)
    k.op('pool', lambda e: e.iota(ii[:], pattern=[[1, 16]], base=0, channel_multiplier=0), writes=[ii])
    k.op('dve', lambda e: e.tensor_copy(out=inv[:], in_=ii[:]), reads=[ii], writes=[inv])
    k.op('act', lambda e: e.activation(out=inv[:], in_=inv[:], func=AF.Exp, scale=-math.log(10000.0) / 16), reads=[inv], writes=[inv])
    k.op('dve', lambda e: e.tensor_scalar_mul(out=inv[:], in0=inv[:], scalar1=1.0 / (2 * math.pi)), reads=[inv], writes=[inv])
    pp = P.alloc_sb(pf + "pp", [128, 4], F32)
    pi2 = P.alloc_sb(pf + "pi2", [128, 1], I32)
    k.op('pool', lambda e: e.iota(pi2[:], pattern=[[0, 1]], base=0, channel_multiplier=1), writes=[pi2])
    k.op('dve', lambda e: e.tensor_copy(out=pp[:, 0:1], in_=pi2[:]), reads=[pi2], writes=[pp])
    k.op('dve', lambda e: e.tensor_single_scalar(out=pp[:, 1:2], in_=pp[:, 0:1], scalar=63.5, op=ALU.is_gt), reads=[pp], writes=[pp])
    k.op('dve', lambda e: e.scalar_tensor_tensor(out=pp[:, 2:3], in0=pp[:, 1:2], scalar=-64.0, in1=pp[:, 0:1], op0=ALU.mult, op1=ALU.add),
         reads=[pp], writes=[pp])
    tab = P.alloc_sb(pf + "tab", [128, NTL, 2, 16], F32)
    rp = P.alloc_sb(pf + "rp", [128, 1], F32)
    for tt in range(NTL):
        k.op('dve', lambda e, tt=tt: e.tensor_scalar_add(out=rp[:], in0=pp[:, 1:2], scalar1=float(2 * tt)), reads=[pp], writes=[rp])
        k.op('dve', lambda e, tt=tt: e.tensor_scalar_mul(out=tab[:, tt, 0, :], in0=inv[:], scalar1=rp[:, 0:1]), reads=[inv, rp], writes=[tab])
        k.op('dve', lambda e, tt=tt: e.tensor_scalar_mul(out=tab[:, tt, 1, :], in0=inv[:], scalar1=pp[:, 2:3]), reads=[inv, pp], writes=[tab])
    sinT = P.alloc_sb(pf + "sinT", [128, NTL, 2, 16], F32)
    cosT = P.alloc_sb(pf + "cosT", [128, NTL, 2, 16], F32)
    ti = P.alloc_sb(pf + "ti", [128, NTL, 2, 16], I32)
    tf = P.alloc_sb(pf + "tf", [128, NTL, 2, 16], F32)
    for (dst, sh) in ((sinT, 0.0), (cosT, 0.25)):
        if sh:
            k.op('dve', lambda e: e.tensor_scalar_add(out=tab[:], in0=tab[:], scalar1=sh), reads=[tab], writes=[tab])
        k.op('dve', lambda e: e.tensor_copy(out=ti[:], in_=tab[:]), reads=[tab], writes=[ti])
        k.op('dve', lambda e: e.tensor_copy(out=tf[:], in_=ti[:]), reads=[ti], writes=[tf])
        k.op('dve', lambda e: e.tensor_tensor(out=tf[:], in0=tab[:], in1=tf[:], op=ALU.subtract), reads=[tab, tf], writes=[tf])
        k.op('act', lambda e, dst=dst: e.activation(out=dst[:], in_=tf[:], func=AF.Sin, scale=2 * math.pi), reads=[tf], writes=[dst])
    ld = [P.alloc_sb(pf + "ld%d" % i, [128, 2304], F32) for i in range(2)]
    vb = [P.alloc_sb(pf + "vb%d" % i, [128, GH, 128], BF16) for i in range(2)]
    ls_ = P.alloc_sb(pf + "ls", [128, 384], F32)
    ecp = P.alloc_sb(pf + "ecp", [128, 384], F32)
    ecn = P.alloc_sb(pf + "ecn", [128, 384], F32)
    qr = P.alloc_sb(pf + "qr", [128, 768], F32)
    rt = [P.alloc_sb(pf + "rt%d" % i, [128, 2, 16], F32) for i in range(4)]
    qkd = P.alloc_sb(pf + "qkd", [128, 2, 384], BF16)
    qdz = P.alloc_sb(pf + "qdz", [64, GH, 2, 128], BF16)
    k.op('pool', lambda e: e.memset(qdz[:], 0.0), writes=[qdz])
    kdT = P.alloc_sb(pf + "kdT", [64, GH, 128], BF16)
    qdT = P.alloc_sb(pf + "qdT", [64, GH, 128], BF16)
    ecl = P.alloc_sb(pf + "ecl", [64, GH, 2], F32)
    Am = [P.alloc_sb(pf + "Am%d" % i, [128, 128], BF16) for i in range(2)]
    S = {d: P.alloc_sb(pf + "S%d" % d, [64, GH, 128], F32) for d in range(2)}
    Sb = [P.alloc_sb(pf + "Sb%d" % i, [64, GH, 128], BF16) for i in range(3)]
    Stmp = P.alloc_sb(pf + "Stmp", [64, 128], F32)
    osb = P.alloc_sb(pf + "osb", [128, 768], F32)
    ofl = P.alloc_sb(pf + "ofl", [128, 768], F32)
    sq = P.alloc_sb(pf + "sq", [128, GH, 128], F32)
    ss = P.alloc_sb(pf + "ss", [128, 2, GH], F32)
    sg = P.alloc_sb(pf + "sg", [128, 768], F32)
    yb = P.alloc_sb(pf + "yb", [128, 768], BF16)
    mst = [P.alloc_sb(pf + "mst%d" % i, [128, GH, 128], BF16) for i in range(2)]
    bA = P.alloc_ps(pf + "bA", [128, 512])
    pz, pzk = bA[:, 0:384], bA
    pe_, pek = bA[0:64, 384:396].rearrange("p (h c) -> p h c", h=GH), bA
    bB = P.alloc_ps(pf + "bB", [128, 512])
    pc_, pck = bB[:, 0:384], bB
    pTq = P.alloc_ps(pf + "pTq", [64, GH, 128], BF16)
    pTk = P.alloc_ps(pf + "pTk", [64, GH, 128], BF16)
    bE = [P.alloc_ps(pf + "bE%d" % i, [128, 2, 128]) for i in range(2)]
    pA = [(bE[i][:, 0, :], bE[i]) for i in range(2)]
    pO = [(bE[i][:, 1, :], bE[i]) for i in range(2)]
    bF = P.alloc_ps(pf + "bF", [64, 2, 128])
    pK = [(bF[:, i, :], bF) for i in range(2)]
    ptb = P.alloc_ps(pf + "ptb", [128, GH, 128], BF16)
    cnt = [0]

    def tile_pass(tt, d, with_out, final):
        i = cnt[0] % 2
        cnt[0] += 1
        t0 = tt * 128
        buf = ld[i]; v_b = vb[i]
        rope = tt < NTL
        Mm = Mf if d == 0 else Mb
        Lm = Lf if d == 0 else Lb
        k.dma('sp', buf[:], ptm[t0:t0 + 128, 768:3072], writes=[buf])
        k.op('pool', lambda e: e.tensor_copy(out=v_b[:], in_=buf[:, 768:1536].rearrange("p (h d) -> p h d", h=GH)), reads=[buf], writes=[v_b])
        k.op('pe', lambda e: e.matmul(pz, lhsT=ga1T[:, t0:t0 + 128], rhs=w2b[:, d, :], start=True, stop=True), reads=[ga1T, w2b], writes=[pzk])
        k.op('act', lambda e: e.activation(out=ls_[:], in_=pz, func=AF.Exp, scale=-1.0), reads=[pzk], writes=[ls_])
        k.op('act', lambda e: e.activation(out=ls_[:], in_=ls_[:], func=AF.Ln, bias=one1[:]), reads=[ls_, one1], writes=[ls_])
        k.op('pe', lambda e: e.matmul(pc_, lhsT=Lm[:], rhs=ls_[:], start=True, stop=True), reads=[Lm, ls_], writes=[pck])
        for h in range(GH):
            k.op('pe', lambda e, h=h: e.matmul(pe_[:, h, :], lhsT=ls_[:, h * 64:(h + 1) * 64], rhs=ind[:], start=True, stop=True), reads=[ls_, ind], writes=[pek])
        k.op('act', lambda e: e.activation(out=ecl[:], in_=pe_, func=AF.Exp), reads=[pek], writes=[ecl])
        k.op('act', lambda e: e.activation(out=ecp[:], in_=pc_, func=AF.Exp), reads=[pck], writes=[ecp])
        k.op('act', lambda e: e.activation(out=ecn[:], in_=pc_, func=AF.Exp, scale=-1.0), reads=[pck], writes=[ecn])
        if rope:
            cs = cosT[:, tt, :, :]; sn = sinT[:, tt, :, :]
            for which in range(2):
                for h in range(GH):
                    o0 = which * 384 + h * 64
                    xv = buf[:, o0:o0 + 64].rearrange("p (hf two i) -> p hf two i", hf=2, two=2)
                    ov = qr[:, o0:o0 + 64].rearrange("p (hf two i) -> p hf two i", hf=2, two=2)
                    ea, eb = ('dve', 'pool') if (h % 2 == 0) else ('pool', 'dve')
                    k.op(ea, lambda e, xv=xv: e.tensor_tensor(out=rt[0][:], in0=xv[:, :, 0, :], in1=cs, op=ALU.mult), reads=[buf, cosT], writes=[rt[0]])
                    k.op(eb, lambda e, xv=xv: e.tensor_tensor(out=rt[1][:], in0=xv[:, :, 1, :], in1=sn, op=ALU.mult), reads=[buf, sinT], writes=[rt[1]])
                    k.op(ea, lambda e, ov=ov: e.tensor_tensor(out=ov[:, :, 0, :], in0=rt[0][:], in1=rt[1][:], op=ALU.subtract), reads=[rt[0], rt[1]], writes=[qr])
                    k.op(eb, lambda e, xv=xv: e.tensor_tensor(out=rt[2][:], in0=xv[:, :, 0, :], in1=sn, op=ALU.mult), reads=[buf, sinT], writes=[rt[2]])
                    k.op(ea, lambda e, xv=xv: e.tensor_tensor(out=rt[3][:], in0=xv[:, :, 1, :], in1=cs, op=ALU.mult), reads=[buf, cosT], writes=[rt[3]])
                    k.op(eb, lambda e, ov=ov: e.tensor_tensor(out=ov[:, :, 1, :], in0=rt[2][:], in1=rt[3][:], op=ALU.add), reads=[rt[2], rt[3]], writes=[qr])
            src = qr
        else:
            src = buf
        k.op('dve', lambda e: e.scalar_tensor_tensor(out=qkd[:, 0, :], in0=src[:, 0:384], scalar=qs, in1=ecp[:], op0=ALU.mult, op1=ALU.mult),
             reads=[src, ecp], writes=[qkd])
        k.op('pool', lambda e: e.tensor_tensor(out=qkd[:, 1, :], in0=src[:, 384:768], in1=ecn[:], op=ALU.mult), reads=[src, ecn], writes=[qkd])
        for w in range(2):
            for h in range(GH):
                k.op('pe', lambda e, w=w, h=h: e.transpose(out=(pTq if w == 0 else pTk)[:, h, :], in_=qkd[:, w, h * 64:(h + 1) * 64], identity=C['id_bf'][:]),
                     reads=[qkd, C['id_bf']], writes=[pTq if w == 0 else pTk])
        k.op('act', lambda e: e.copy(out=qdT[:], in_=pTq[:]), reads=[pTq], writes=[qdT])
        k.op('dve', lambda e: e.tensor_copy(out=kdT[:], in_=pTk[:]), reads=[pTk], writes=[kdT])
        for c in range(2):
            k.op('pool', lambda e, c=c: e.tensor_copy(out=qdz[:, :, c, 64 * c:64 * c + 64], in_=qdT[:, :, 64 * c:64 * c + 64]), reads=[qdT], writes=[qdz])
        Sd = S[d]
        order = (0, 1) if d == 0 else (1, 0)
        k.op('act', lambda e: e.copy(out=Sb[0][:], in_=Sd[:]), reads=[Sd], writes=[Sb[0]])
        for ci, c in enumerate(order):
            for h in range(GH):
                pk, pkk = pK[h % 2]
                k.op('pe', lambda e, c=c, h=h, pk=pk: e.matmul(pk, lhsT=qkd[64 * c:64 * c + 64, 1, h * 64:(h + 1) * 64], rhs=v_b[64 * c:64 * c + 64, h, :],
                                                              start=True, stop=True), reads=[qkd, v_b], writes=[pkk])
                k.op('dve', lambda e, c=c, h=h: e.tensor_scalar_mul(out=Stmp[:], in0=Sd[:, h, :], scalar1=ecl[:, h, c:c + 1]), reads=[Sd, ecl], writes=[Stmp])
                k.op('dve', lambda e, c=c, h=h, pk=pk: e.scalar_tensor_tensor(out=Sd[:, h, :], in0=pk, scalar=ecl[:, h, c:c + 1], in1=Stmp[:],
                                                                             op0=ALU.mult, op1=ALU.add), reads=[pkk, ecl, Stmp], writes=[Sd])
            if ci == 0 and with_out:
                k.op('act', lambda e: e.copy(out=Sb[1][:], in_=Sd[:]), reads=[Sd], writes=[Sb[1]])
        if not with_out:
            return
        for h in range(GH):
            (pa_, pak), (po, pok) = pA[h % 2], pO[h % 2]
            am = Am[h % 2]
            k.op('pe', lambda e, h=h, pa_=pa_: e.matmul(pa_, lhsT=kdT[:, h, :], rhs=qdT[:, h, :], start=True, stop=True), reads=[kdT, qdT], writes=[pak])
            k.op('dve', lambda e, pa_=pa_, am=am: e.tensor_tensor(out=am[:], in0=pa_, in1=Mm[:], op=ALU.mult), reads=[pak, Mm], writes=[am])
            k.op('pe', lambda e, h=h, po=po, am=am: e.matmul(po, lhsT=am[:], rhs=v_b[:, h, :], start=True, stop=False), reads=[am, v_b], writes=[pok])
            for ci, c in enumerate(order):
                k.op('pe', lambda e, h=h, po=po, c=c, ci=ci: e.matmul(po, lhsT=qdz[:, h, c, :], rhs=Sb[ci][:, h, :], start=False, stop=(ci == 1)),
                     reads=[qdz, Sb[ci]], writes=[pok])
            if not final:
                k.op('act', lambda e, h=h, po=po: e.copy(out=osb[:, h * 128:(h + 1) * 128], in_=po), reads=[pok], writes=[osb])
            else:
                k.op('dve', lambda e, h=h, po=po: e.tensor_tensor(out=osb[:, h * 128:(h + 1) * 128], in0=po, in1=ofl[:, h * 128:(h + 1) * 128], op=ALU.add),
                     reads=[pok, ofl], writes=[osb])
        if not final:
            k.dma('sp', ofs[t0:t0 + 128, :], osb[:], reads=[osb], writes=[ofs])
            return
        o3 = osb[:, :].rearrange("p (h d) -> p h d", h=GH)
        k.op('pool', lambda e: e.tensor_tensor(out=sq[:], in0=o3, in1=o3, op=ALU.mult), reads=[osb], writes=[sq])
        k.op('dve', lambda e: e.tensor_reduce(out=ss[:, 0, :], in_=sq[:], axis=AX.X, op=ALU.add), reads=[sq], writes=[ss])
        k.op('dve', lambda e: e.tensor_scalar(out=ss[:, 1, :], in0=ss[:, 0, :], scalar1=1.0 / 128, scalar2=EPS, op0=ALU.mult, op1=ALU.add), reads=[ss], writes=[ss])
        k.op('act', lambda e: e.activation(out=ss[:, 1, :], in_=ss[:, 1, :], func=AF.Ln), reads=[ss], writes=[ss])
        k.op('act', lambda e: e.activation(out=ss[:, 1, :], in_=ss[:, 1, :], func=AF.Exp, scale=-0.5), reads=[ss], writes=[ss])
        k.op('act', lambda e: e.activation(out=sg[:], in_=buf[:, 1536:2304], func=AF.Silu), reads=[buf], writes=[sg])
        for h in range(GH):
            k.op('dve', lambda e, h=h: e.tensor_scalar_mul(out=osb[:, h * 128:(h + 1) * 128], in0=osb[:, h * 128:(h + 1) * 128], scalar1=ss[:, 1, h:h + 1]),
                 reads=[osb, ss], writes=[osb])
        k.op('pool', lambda e: e.tensor_tensor(out=sg[:], in0=sg[:], in1=gnw[:], op=ALU.mult), reads=[sg, gnw], writes=[sg])
        k.op('dve', lambda e: e.tensor_tensor(out=yb[:], in0=osb[:], in1=sg[:], op=ALU.mult), reads=[osb, sg], writes=[yb])
        ms_ = mst[tt % 2]
        for h in range(GH):
            k.op('pe', lambda e, h=h: e.transpose(out=ptb[:, h, :], in_=yb[:, h * 128:(h + 1) * 128], identity=C['id_bf'][:]), reads=[yb, C['id_bf']], writes=[ptb])
        k.op('act', lambda e, ms_=ms_: e.copy(out=ms_[:], in_=ptb[:]), reads=[ptb], writes=[ms_])
        k.dma('sp', mixT[1280:2048, t0:t0 + 128].rearrange("(j p) t -> p j t", p=128), ms_[:], reads=[ms_], writes=[mixT])

    k.op('pool', lambda e: e.memset(S[0][:], 0.0), writes=[S[0]])
    k.op('pool', lambda e: e.memset(S[1][:], 0.0), writes=[S[1]])
    for tt in (NTL, NTL + 1):
        tile_pass(tt, 0, with_ctx, False)
    for tt in range(NTL):
        tile_pass(tt, 0, True, False)
    for tt in (NTL + 1, NTL):
        if with_ctx:
            k.dma('sp', ofl[:], ofs[tt * 128:(tt + 1) * 128, :], reads=[ofs], writes=[ofl])
        tile_pass(tt, 1, with_ctx, True)
    for tt in range(NTL - 1, -1, -1):
        k.dma('sp', ofl[:], ofs[tt * 128:(tt + 1) * 128, :], reads=[ofs], writes=[ofl])
        tile_pass(tt, 1, True, True)
    P.release(m0)


def stage_post(P, C, l, W, mp, xT, mixT, x1T, h2f_d, h2b_d, ntok):
    nc, k = P.nc, P.k
    pf = "po_"
    m0 = P.mark()
    NB = 256
    wo = P.alloc_sb(pf + "wo", [128, NCH, D], BF16)
    wv = W['w_out'][l].rearrange("(kc p) c -> p kc c", p=128)
    for q in range(4):
        k.dma('pool', wo[:, :, q * 512:(q + 1) * 512], wv[:, :, q * 512:(q + 1) * 512], writes=[wo])
    lnp = P.alloc_sb(pf + "lnp", [128, 4, NCH], F32)
    k.dma('sp', lnp[:], W['lnT'][l])
    tmp = ln_tmp(P, pf + "ln", NB)
    mxb = [P.alloc_sb(pf + "mxb%d" % i, [128, NCH, NB], BF16) for i in range(2)]
    xt = [P.alloc_sb(pf + "xt%d" % i, [128, NCH, NB], F32) for i in range(2)]
    x1t = P.alloc_sb(pf + "x1t", [128, NCH, NB], F32)
    pss = [P.alloc_ps(pf + "ps%d" % i, [128, NB]) for i in range(2)]
    tq = [P.alloc_sb(pf + "tq%d" % i, [128, NB], F32) for i in range(2)]
    mxv = mixT.rearrange("(c p) t -> p c t", p=128)
    xv = xT.rearrange("(c p) t -> p c t", p=128)
    x1v = x1T.rearrange("(c p) t -> p c t", p=128)
    hfv = h2f_d.rearrange("(c p) t -> p c t", p=128)
    hbv = h2b_d.rearrange("(c p) t -> p c t", p=128)
    for bi, t0 in enumerate(range(0, ntok, NB)):
        r = 0 if t0 < L else 1
        mb = mxb[bi % 2]; x_ = xt[bi % 2]
        k.dma('sp', mb[:], mxv[:, :, t0:t0 + NB], writes=[mb])
        k.dma('act', x_[:], xv[:, :, t0:t0 + NB], writes=[x_])
        for dc in range(NCH):
            pst = pss[dc % 2]; tt = tq[dc % 2]
            for kc in range(NCH):
                k.op('pe', lambda e, kc=kc, dc=dc, pst=pst: e.matmul(pst[:, :], lhsT=wo[:, kc, dc * 128:(dc + 1) * 128], rhs=mb[:, kc, :],
                                                                    start=(kc == 0), stop=(kc == NCH - 1)), reads=[wo, mb], writes=[pst])
            k.op('act', lambda e, dc=dc, pst=pst, tt=tt: e.activation(out=tt[:], in_=pst[:, :], func=AF.Copy, scale=mp['g1'][:, dc, r:r + 1]),
                 reads=[pst, mp['g1']], writes=[tt])
            k.op('dve', lambda e, dc=dc, tt=tt: e.scalar_tensor_tensor(out=x_[:, dc, :], in0=x_[:, dc, :], scalar=ALPHA, in1=tt[:], op0=ALU.mult, op1=ALU.add),
                 reads=[x_, tt], writes=[x_])
        ln_block(P, C, tmp, x_, NB,
                 lambda c: (x1t[:, c, :], [x1t]),
                 lambda c: (lnp[:, 0, c:c + 1], [lnp]),
                 lambda c: (lnp[:, 1, c:c + 1], [lnp]))
        k.dma('sp', x1v[:, :, t0:t0 + NB], x1t[:], reads=[x1t], writes=[x1T])
        ln_block(P, C, tmp, x1t, NB,
                 lambda c: (x_[:, c, :], [x_]),
                 lambda c: (mp['sc2p'][:, c, r:r + 1], [mp['sc2p']]),
                 lambda c: (mp['sh2'][:, c, r:r + 1], [mp['sh2']]))
        k.dma('sp', hfv[:, :, t0:t0 + NB], x_[:], reads=[x_], writes=[h2f_d])
        k.dma('pool', hbv[:, :, t0:t0 + NB], x_[:], reads=[x_], writes=[h2b_d])
    P.release(m0)


def stage_moe(P, C, l, W, mp, x1T, h2f_d, h2b_d, outT, ntok):
    nc, k = P.nc, P.k
    pf = "mo_"
    m0 = P.mark()
    NT_ = ntok // 128
    gate = P.alloc_sb(pf + "gate", [128, NT_, NE], F32)
    m1 = P.mark()
    wr = P.alloc_sb(pf + "wr", [128, NCH, 36], F32)
    k.dma('sp', wr[:, :, 0:4], W['w_rg'][l].rearrange("(kc p) c -> p kc c", p=128), writes=[wr])
    k.dma('sp', wr[:, :, 4:36], W['w_re'][l].rearrange("(kc p) c -> p kc c", p=128), writes=[wr])
    br = P.alloc_sb(pf + "br", [128, 36], F32)
    k.dma('sp', br[:, 0:4], W['b_rg'][l:l + 1, :].partition_broadcast(128), writes=[br])
    k.dma('sp', br[:, 4:36], W['b_re'][l:l + 1, :].partition_broadcast(128), writes=[br])
    hf = [P.alloc_sb(pf + "hf%d" % i, [128, NCH, 128], F32) for i in range(2)]
    pl = [P.alloc_ps(pf + "pl%d" % i, [128, 36]) for i in range(2)]
    lg = P.alloc_sb(pf + "lg", [128, 36], F32)
    sm = P.alloc_sb(pf + "sm", [128, 16], F32)
    gs = P.alloc_sb(pf + "gs", [128, 4], F32)
    eg = P.alloc_sb(pf + "eg", [128, 4], F32)
    les = P.alloc_sb(pf + "les", [128, 8], F32)
    le2 = P.alloc_sb(pf + "le2", [128, 8], F32)
    mk1 = P.alloc_sb(pf + "mk1", [128, 8], F32)
    mk2 = P.alloc_sb(pf + "mk2", [128, 8], F32)
    egt = P.alloc_sb(pf + "egt", [128, 8], F32)
    hfv = h2f_d.rearrange("(c p) t -> p c t", p=128)
    for tt in range(NT_):
        h_ = hf[tt % 2]; pst = pl[tt % 2]
        k.dma('sp', h_[:], hfv[:, :, tt * 128:(tt + 1) * 128], writes=[h_])
        for kc in range(NCH):
            k.op('pe', lambda e, kc=kc, h_=h_, pst=pst: e.matmul(pst[:, :], lhsT=h_[:, kc, :], rhs=wr[:, kc, :], start=(kc == 0), stop=(kc == NCH - 1)),
                 reads=[h_, wr], writes=[pst])
        k.op('dve', lambda e, pst=pst: e.tensor_tensor(out=lg[:], in0=pst[:, :], in1=br[:], op=ALU.add), reads=[pst, br], writes=[lg])
        k.op('dve', lambda e: e.tensor_reduce(out=sm[:, 0:1], in_=lg[:, 0:4], axis=AX.X, op=ALU.max), reads=[lg], writes=[sm])
        k.op('dve', lambda e: e.tensor_scalar_mul(out=sm[:, 1:2], in0=sm[:, 0:1], scalar1=-1.0), reads=[sm], writes=[sm])
        k.op('act', lambda e: e.activation(out=eg[:], in_=lg[:, 0:4], func=AF.Exp, bias=sm[:, 1:2], accum_out=sm[:, 2:3]), reads=[lg, sm], writes=[eg, sm])
        k.op('dve', lambda e: e.reciprocal(out=sm[:, 3:4], in_=sm[:, 2:3]), reads=[sm], writes=[sm])
        k.op('dve', lambda e: e.tensor_scalar(out=gs[:], in0=lg[:, 0:4], scalar1=sm[:, 0:1], scalar2=None, op0=ALU.is_ge), reads=[lg, sm], writes=[gs])
        k.op('dve', lambda e: e.tensor_scalar_mul(out=les[:], in0=lg[:, 4:12], scalar1=gs[:, 0:1]), reads=[lg, gs], writes=[les])
        for g in range(1, 4):
            k.op('dve', lambda e, g=g: e.scalar_tensor_tensor(out=les[:], in0=lg[:, 4 + 8 * g:12 + 8 * g], scalar=gs[:, g:g + 1], in1=les[:],
                                                             op0=ALU.mult, op1=ALU.add), reads=[lg, gs, les], writes=[les])
        k.op('dve', lambda e: e.tensor_reduce(out=sm[:, 4:5], in_=les[:], axis=AX.X, op=ALU.max), reads=[les], writes=[sm])
        k.op('dve', lambda e: e.tensor_scalar(out=mk1[:], in0=les[:], scalar1=sm[:, 4:5], scalar2=None, op0=ALU.is_ge), reads=[les, sm], writes=[mk1])
        k.op('dve', lambda e: e.scalar_tensor_tensor(out=le2[:], in0=mk1[:], scalar=-1.0e9, in1=les[:], op0=ALU.mult, op1=ALU.add),
             reads=[mk1, les], writes=[le2])
        k.op('dve', lambda e: e.tensor_reduce(out=sm[:, 5:6], in_=le2[:], axis=AX.X, op=ALU.max), reads=[le2], writes=[sm])
        k.op('dve', lambda e: e.tensor_scalar(out=mk2[:], in0=le2[:], scalar1=sm[:, 5:6], scalar2=None, op0=ALU.is_ge), reads=[le2, sm], writes=[mk2])
        k.op('dve', lambda e: e.tensor_tensor(out=sm[:, 6:7], in0=sm[:, 5:6], in1=sm[:, 4:5], op=ALU.subtract), reads=[sm], writes=[sm])
        k.op('act', lambda e: e.activation(out=sm[:, 7:8], in_=sm[:, 6:7], func=AF.Exp), reads=[sm], writes=[sm])
        k.op('dve', lambda e: e.tensor_scalar_add(out=sm[:, 8:9], in0=sm[:, 7:8], scalar1=1.0), reads=[sm], writes=[sm])
        k.op('dve', lambda e: e.reciprocal(out=sm[:, 9:10], in_=sm[:, 8:9]), reads=[sm], writes=[sm])
        k.op('dve', lambda e: e.tensor_tensor(out=sm[:, 10:11], in0=sm[:, 7:8], in1=sm[:, 9:10], op=ALU.mult), reads=[sm], writes=[sm])
        k.op('dve', lambda e: e.tensor_tensor(out=sm[:, 11:12], in0=sm[:, 9:10], in1=sm[:, 3:4], op=ALU.mult), reads=[sm], writes=[sm])
        k.op('dve', lambda e: e.tensor_tensor(out=sm[:, 12:13], in0=sm[:, 10:11], in1=sm[:, 3:4], op=ALU.mult), reads=[sm], writes=[sm])
        k.op('dve', lambda e: e.tensor_scalar_mul(out=egt[:], in0=mk1[:], scalar1=sm[:, 11:12]), reads=[mk1, sm], writes=[egt])
        k.op('dve', lambda e: e.scalar_tensor_tensor(out=egt[:], in0=mk2[:], scalar=sm[:, 12:13], in1=egt[:], op0=ALU.mult, op1=ALU.add),
             reads=[mk2, sm, egt], writes=[egt])
        for g in range(4):
            k.op('dve', lambda e, g=g, tt=tt: e.tensor_scalar_mul(out=gate[:, tt, 8 * g:8 * g + 8], in0=egt[:], scalar1=gs[:, g:g + 1]),
                 reads=[egt, gs], writes=[gate])
    P.dump('moe_gate', gate)
    P.release(m1)
    PASS = 1024 if ntok == L else 768
    BLK = 384
    acc = P.alloc_sb(pf + "acc", [128, PASS // 128, D], F32)
    lnp = P.alloc_sb(pf + "lnp", [128, 4, NCH], F32)
    k.dma('sp', lnp[:], W['lnT'][l])
    hbv = h2b_d.rearrange("(c p) t -> p c t", p=128)
    x1v = x1T.rearrange("(c p) t -> p c t", p=128)
    ov = outT.rearrange("(c p) t -> p c t", p=128)
    ei = 0
    oi = 0
    for p0 in range(0, ntok, PASS):
        pn = min(PASS, ntok - p0)
        mE = P.mark()
        hT = P.alloc_sb(pf + "hT", [128, NCH, PASS], BF16)
        wu = [P.alloc_sb(pf + "wu%d" % i, [128, NCH, 1024], BF16) for i in range(2)]
        wd = [P.alloc_sb(pf + "wd%d" % i, [128, 4, D], BF16) for i in range(2)]
        actT = [P.alloc_sb(pf + "actT%d" % i, [128, 4, BLK], BF16) for i in range(2)]
        sgb = [P.alloc_sb(pf + "sg%d" % i, [128, BLK], F32) for i in range(2)]
        pg = [P.alloc_ps(pf + "pg%d" % i, [128, BLK]) for i in range(2)]
        pu = [P.alloc_ps(pf + "pu%d" % i, [128, BLK]) for i in range(2)]
        po = [P.alloc_ps(pf + "po%d" % i, [128, 512]) for i in range(4)]
        k.dma('sp', hT[:, :, :pn], hbv[:, :, p0:p0 + pn], writes=[hT])
        k.op('pool', lambda e: e.memset(acc[:], 0.0), writes=[acc])
        for ex in range(NE):
            g_, e_ = ex // 8, ex % 8
            wu_ = wu[ei % 2]; wd_ = wd[ei % 2]
            ei += 1
            k.dma('pool', wu_[:], W['w_up'][l, g_, e_].rearrange("(kc p) c -> p kc c", p=128), writes=[wu_])
            k.dma('pool', wd_[:], W['w_down'][l, g_, e_].rearrange("(j p) c -> p j c", p=128), writes=[wd_])
            for b0 in range(0, pn, BLK):
                bn = min(BLK, pn - b0)
                at = actT[(b0 // BLK) % 2]
                for j in range(4):
                    pg_ = pg[j % 2]; pu_ = pu[j % 2]; sg_ = sgb[j % 2]
                    for kc in range(NCH):
                        k.op('pe', lambda e, kc=kc, j=j, pg_=pg_: e.matmul(pg_[:, :bn], lhsT=wu_[:, kc, j * 128:(j + 1) * 128], rhs=hT[:, kc, b0:b0 + bn],
                                                                          start=(kc == 0), stop=(kc == NCH - 1)), reads=[wu_, hT], writes=[pg_])
                    for kc in range(NCH):
                        k.op('pe', lambda e, kc=kc, j=j, pu_=pu_: e.matmul(pu_[:, :bn], lhsT=wu_[:, kc, 512 + j * 128:512 + (j + 1) * 128], rhs=hT[:, kc, b0:b0 + bn],
                                                                          start=(kc == 0), stop=(kc == NCH - 1)), reads=[wu_, hT], writes=[pu_])
                    k.op('act', lambda e, pg_=pg_, sg_=sg_: e.activation(out=sg_[:, :bn], in_=pg_[:, :bn], func=AF.Silu), reads=[pg_], writes=[sg_])
                    k.op('dve', lambda e, j=j, pu_=pu_, sg_=sg_, at=at: e.tensor_tensor(out=at[:, j, :bn], in0=pu_[:, :bn], in1=sg_[:, :bn], op=ALU.mult),
                         reads=[pu_, sg_], writes=[at])
                for t3 in range(bn // 128):
                    tl = (b0 // 128) + t3
                    tg = (p0 // 128) + tl
                    for dh in range(4):
                        po_ = po[oi % 4]
                        oi += 1
                        for j in range(4):
                            k.op('pe', lambda e, j=j, dh=dh, po_=po_, t3=t3: e.matmul(po_[:, :], lhsT=at[:, j, t3 * 128:(t3 + 1) * 128], rhs=wd_[:, j, dh * 512:(dh + 1) * 512],
                                                                                   start=(j == 0), stop=(j == 3)), reads=[at, wd_], writes=[po_])
                        k.op('dve', lambda e, dh=dh, po_=po_, tl=tl, tg=tg, ex=ex: e.scalar_tensor_tensor(out=acc[:, tl, dh * 512:(dh + 1) * 512], in0=po_[:, :],
                                                                                                        scalar=gate[:, tg, ex:ex + 1], in1=acc[:, tl, dh * 512:(dh + 1) * 512],
                                                                                                        op0=ALU.mult, op1=ALU.add), reads=[po_, gate, acc], writes=[acc])
        P.release(mE)
        m2 = P.mark()
        tmp = ln_tmp(P, pf + "ln", BLK)
        vt = P.alloc_sb(pf + "vt", [128, NCH, BLK], F32)
        x1b = P.alloc_sb(pf + "x1b", [128, NCH, BLK], F32)
        ot = P.alloc_sb(pf + "ot", [128, NCH, BLK], F32)
        ptf = [P.alloc_ps(pf + "ptf%d" % i, [128, 512]) for i in range(2)]
        tq = [P.alloc_sb(pf + "tq%d" % i, [128, 128], F32) for i in range(2)]
        for b0 in range(0, pn, BLK):
            bn = min(BLK, pn - b0)
            r = 0 if (p0 + b0) < L else 1
            k.dma('sp', x1b[:, :, :bn], x1v[:, :, p0 + b0:p0 + b0 + bn], writes=[x1b])
            for t3 in range(bn // 128):
                tl = (b0 // 128) + t3
                r = 0 if (p0 + b0 + t3 * 128) < L else 1
                for c in range(NCH):
                    pt_ = ptf[c % 2]; tq_ = tq[c % 2]
                    k.op('pe', lambda e, c=c, tl=tl, pt_=pt_: e.transpose(out=pt_[:, :128], in_=acc[:, tl, c * 128:(c + 1) * 128], identity=C['id_f'][:]),
                         reads=[acc, C['id_f']], writes=[pt_])
                    k.op('act', lambda e, c=c, pt_=pt_, tq_=tq_: e.activation(out=tq_[:], in_=pt_[:, :128], func=AF.Copy, scale=mp['g2'][:, c, r:r + 1]),
                         reads=[pt_, mp['g2']], writes=[tq_])
                    k.op('dve', lambda e, c=c, t3=t3, tq_=tq_: e.scalar_tensor_tensor(out=vt[:, c, t3 * 128:(t3 + 1) * 128], in0=x1b[:, c, t3 * 128:(t3 + 1) * 128],
                                                                                      scalar=ALPHA, in1=tq_[:], op0=ALU.mult, op1=ALU.add), reads=[x1b, tq_], writes=[vt])
            ln_block(P, C, tmp, vt, bn,
                     lambda c: (ot[:, c, :bn], [ot]),
                     lambda c: (lnp[:, 2, c:c + 1], [lnp]),
                     lambda c: (lnp[:, 3, c:c + 1], [lnp]))
            k.dma('sp', ov[:, :, p0 + b0:p0 + b0 + bn], ot[:, :, :bn], reads=[ot], writes=[outT])
        P.release(m2)
    P.release(m0)


W_SHAPES = {
    'hy_f_w1': [DEPTH, 33, 64], 'hy_f_w2': [DEPTH, 64, 64], 'hy_f_w3': [DEPTH, 64, 1024], 'hy_fv': [DEPTH, 64, 3],
    'hy_bias': [DEPTH, 512], 'hy_sw': [DEPTH, 128, 12, 4], 'hy_norm_wT': [DEPTH, 128, 4],
    'na_rpb': [DEPTH, 6, 15, 31], 'na_norm_wT': [DEPTH, 128, 6],
    'gla_a_w2': [DEPTH, 2, 16, 384], 'gla_a_b': [DEPTH, 2, 384], 'gla_norm_w': [DEPTH, 128],
    'w_out': [DEPTH, D, D], 'lnT': [DEPTH, 128, 4, NCH],
    'w_rg': [DEPTH, D, 4], 'b_rg': [DEPTH, 4], 'w_re': [DEPTH, D, 32], 'b_re': [DEPTH, 32],
    'w_up': [DEPTH, 4, 8, D, 1024], 'w_down': [DEPTH, 4, 8, DE, D],
    'w_ada': [DEPTH, D, 6 * D], 'b_adaT': [DEPTH, 128, 96], 'w_in': [DEPTH, D, INC],
}


def host_layout(inp, layers=(0, 1)):
    ls = list(layers)
    n = len(ls)
    W = {}
    for nm in ('hy_f_w1', 'hy_f_w2', 'hy_f_w3', 'hy_bias', 'na_rpb', 'gla_a_w2', 'gla_a_b', 'gla_norm_w', 'w_out', 'w_rg', 'b_rg',
               'w_re', 'b_re', 'w_up', 'w_down', 'w_ada', 'w_in'):
        W[nm] = np.ascontiguousarray(inp[nm][ls])
    W['hy_fv'] = np.ascontiguousarray(np.stack([inp['hy_f_b1'][ls], inp['hy_f_b2'][ls], inp['hy_sin_freq'][ls]], -1))
    sw = np.concatenate([inp['hy_short_w'][ls], inp['hy_short_b'][ls][:, None, :]], 1)
    W['hy_sw'] = np.ascontiguousarray(sw.reshape(n, 4, 12, 128).transpose(0, 3, 2, 1))
    W['hy_norm_wT'] = np.ascontiguousarray(inp['hy_norm_w'][ls].reshape(n, 4, 128).transpose(0, 2, 1))
    W['na_norm_wT'] = np.ascontiguousarray(inp['na_norm_w'][ls].reshape(n, 6, 128).transpose(0, 2, 1))
    lnT = np.stack([inp['ln1_g'][ls], inp['ln1_b'][ls], inp['ln2_g'][ls], inp['ln2_b'][ls]], 1)
    W['lnT'] = np.ascontiguousarray(lnT.reshape(n, 4, NCH, 128).transpose(0, 3, 1, 2))
    W['b_adaT'] = np.ascontiguousarray(inp['b_ada'][ls].reshape(n, 96, 128).transpose(0, 2, 1))
    return W


def build(layers=(0, 1), dbg=(), upto=None):
    P = Prog(dbg=dbg)
    nl = len(layers)
    W = {}
    for nm, shp in W_SHAPES.items():
        W[nm] = P.inp(nm, [nl] + shp[1:])
    xT_in = P.inp("xT", [D, T])
    cT = P.inp("cT", [128, NCH, 2])
    outT = P.outp("outT", [D, L])
    pfm = P.scratch("pfm", [3104, T])
    ptm = P.scratch("ptm", [T, 3072])
    mixT = P.scratch("mixT", [D, T], BF16)
    rpbp = P.scratch("rpbp", [6, 15, 160])
    ofs = P.scratch("ofs", [T, 768])
    x1T = P.scratch("x1T", [D, T])
    h2f = P.scratch("h2f", [D, T])
    h2b = P.scratch("h2b", [D, T], BF16)
    xmid = P.scratch("xmid", [D, T])
    C = make_consts(P)
    modT = sb(P.nc, "modT", [128, 96, 2], F32)
    for li in range(nl):
        last = (li == nl - 1)
        xin = xT_in if li == 0 else xmid
        xout = outT if last else xmid
        ntok = L if last else T
        mk = P.mark()
        stage_mod(P, li, cT, W['w_ada'], W['b_adaT'], modT)
        mp = load_mod(P, modT)
        stage_inproj(P, C, li, xin, W['w_in'], mp, pfm, ptm)
        if upto == 'inproj':
            break
        stage_hyena(P, C, li, W, pfm, mixT, 0, L)
        if not last:
            stage_hyena(P, C, li, W, pfm, mixT, L, LC)
        stage_na(P, C, li, W, pfm, ptm, mixT, rpbp, not last)
        stage_gla(P, C, li, W, pfm, ptm, mixT, ofs, not last)
        if upto == 'mix':
            break
        stage_post(P, C, li, W, mp, xin, mixT, x1T, h2f, h2b, ntok)
        if upto == 'post':
            break
        stage_moe(P, C, li, W, mp, x1T, h2f, h2b, xout, ntok)
        P.release(mk)
    P.k.finish()
    return P


def kernel(**inputs):
    inp = {k_: np.asarray(v) for k_, v in inputs.items()}
    Wn = host_layout(inp)
    P = build()
    in_maps = []
    for core in range(8):
        b = core % 4
        m = dict(Wn)
        m['xT'] = np.ascontiguousarray(np.concatenate([inp['x'][b], inp['ctx'][b]], 0).T)
        m['cT'] = np.ascontiguousarray(np.stack([inp['c'][b], inp['c_ctx']], -1).reshape(NCH, 128, 2).transpose(1, 0, 2))
        in_maps.append(m)
    res = run_bass_kernel_spmd(P.nc, in_maps, core_ids=list(range(8)))
    out = np.stack([np.ascontiguousarray(res.results[b]["outT"].T) for b in range(4)], 0)
    return out.astype(np.float32)
```

```python
import math
import numpy as np
import concourse.bass as bass
import concourse.mybir as mybir
from concourse.bass_utils import run_bass_kernel_spmd

F32 = mybir.dt.float32
BF16 = mybir.dt.bfloat16
I32 = mybir.dt.int32
AF = mybir.ActivationFunctionType
ALU = mybir.AluOpType
AX = mybir.AxisListType

D = 2048
L = 2048
LC = 256
T = L + LC
NCH = D // 128
DEPTH = 2
INC = 6176
HY = 512
NAH = 6
GH = 6
GDK = 64
EPS = 1e-6
ALPHA = (2 * DEPTH) ** 0.25
NE = 32
DE = 512

O_HY, O_NQ, O_NK, O_NV, O_GQ, O_GK, O_GV, O_GG, O_GA = 0, 1536, 2304, 3072, 3840, 4224, 4608, 5376, 6144


class K:
    NDMA = 24

    def __init__(self, nc):
        self.nc = nc
        self.eng = {'pe': nc.tensor, 'act': nc.scalar, 'dve': nc.vector, 'pool': nc.gpsimd, 'sp': nc.sync}
        self.sem = {e: nc.semaphore("s_" + e).__enter__() for e in ('pe', 'act', 'dve', 'pool')}
        self.cnt = {e: 0 for e in self.sem}
        self.dsem = [nc.semaphore("d%d" % i).__enter__() for i in range(self.NDMA)]
        self.dcnt = [0] * self.NDMA
        self.dnext = 0
        self.seen = {e: {} for e in self.eng}
        self.lastw = {}
        self.readers = {}
        self.nins = 0

    @staticmethod
    def key(x):
        if isinstance(x, (str, tuple)):
            return x
        return x.tensor.name if hasattr(x, 'tensor') else x.name

    def _wait(self, e, tok):
        if tok is None:
            return
        sem, val, src = tok
        if src == e and e == 'pe':
            return
        if self.seen[e].get(id(sem), 0) >= val:
            return
        self.eng[e].wait_ge(sem, val)
        self.seen[e][id(sem)] = val

    def _deps(self, e, reads, writes):
        for r in reads:
            self._wait(e, self.lastw.get(r))
        for w in writes:
            self._wait(e, self.lastw.get(w))
            for tok in self.readers.get(w, {}).values():
                self._wait(e, tok)

    def _record(self, tok, reads, writes):
        for w in writes:
            self.lastw[w] = tok
            self.readers[w] = {}
        for r in reads:
            if r in writes:
                continue
            self.readers.setdefault(r, {})[id(tok[0])] = tok

    def op(self, e, fn, reads=(), writes=()):
        reads = [self.key(r) for r in reads]
        writes = [self.key(w) for w in writes]
        self._deps(e, reads, writes)
        ins = fn(self.eng[e])
        self.cnt[e] += 1
        ins.then_inc(self.sem[e], 1)
        self._record((self.sem[e], self.cnt[e], e), reads, writes)
        self.nins += 1
        return ins

    def dma(self, e, out, in_, reads=None, writes=None, **kw):
        reads = [self.key(r) for r in (reads if reads is not None else [in_])]
        writes = [self.key(w) for w in (writes if writes is not None else [out])]
        self._deps(e, reads, writes)
        i = self.dnext
        self.dnext = (self.dnext + 1) % self.NDMA
        sem = self.dsem[i]
        self._wait(e, (sem, self.dcnt[i], 'dma'))
        self.eng[e].dma_start(out=out, in_=in_, **kw).then_inc(sem, 16)
        self.dcnt[i] += 16
        self._record((sem, self.dcnt[i], 'dma'), reads, writes)
        self.nins += 1

    def barrier(self):
        toks = [(self.sem[e], self.cnt[e], e) for e in self.sem if self.cnt[e] > 0]
        toks += [(self.dsem[i], self.dcnt[i], 'dma') for i in range(self.NDMA) if self.dcnt[i] > 0]
        for e in self.eng:
            for t in toks:
                if t[2] == e:
                    continue
                self._wait(e, t)
        self.lastw.clear()
        self.readers.clear()

    def finish(self):
        self.barrier()


def sb(nc, name, shape, dt):
    return nc.sbuf_tensor(name, list(shape), dt).__enter__()


def ps(nc, name, shape, dt=F32):
    return nc.psum_tensor(name, list(shape), dt).__enter__()


class Prog:
    def __init__(self, dbg=()):
        self.nc = nc = bass.Bass("TRN2", target_bir_lowering=False)
        self.k = K(nc)
        self.dbg = set(dbg)
        self.dram = {}
        self._ctx = []

    def inp(self, name, shape, dt=F32):
        t = self.nc.dram_tensor(name, list(shape), dt, kind="ExternalInput")
        self.dram[name] = t
        return t.ap()

    def outp(self, name, shape, dt=F32):
        t = self.nc.dram_tensor(name, list(shape), dt, kind="ExternalOutput")
        self.dram[name] = t
        return t.ap()

    def scratch(self, name, shape, dt=F32):
        kind = "ExternalOutput" if name in self.dbg else "Internal"
        t = self.nc.dram_tensor(name, list(shape), dt, kind=kind)
        self.dram[name] = t
        return t.ap()

    def alloc_sb(self, name, shape, dt):
        self._uid = getattr(self, '_uid', 0) + 1
        name = "%s_u%d" % (name, self._uid)
        g = self.nc.sbuf_tensor(name, list(shape), dt)
        t = g.__enter__()
        self._ctx.append(g)
        return t

    def alloc_ps(self, name, shape, dt=F32):
        self._uid = getattr(self, '_uid', 0) + 1
        name = "%s_u%d" % (name, self._uid)
        g = self.nc.psum_tensor(name, list(shape), dt)
        t = g.__enter__()
        self._ctx.append(g)
        return t

    def dump(self, name, tile, dt=F32):
        if name in self.dbg:
            o = self.outp("dbg_" + name, list(tile.shape), dt)
            self.k.dma('sp', o, tile[:], reads=[tile], writes=["dbg_" + name])

    def mark(self):
        return len(self._ctx)

    def release(self, mark):
        self.k.barrier()
        while len(self._ctx) > mark:
            self._ctx.pop().__exit__(None, None, None)


def stage_mod(P, l, cT, w_ada, b_adaT, modT):
    nc, k = P.nc, P.k
    m = P.mark()
    c_sb = P.alloc_sb("mod_c", [128, NCH, 2], F32)
    sc = P.alloc_sb("mod_silu", [128, NCH, 2], F32)
    bsb = P.alloc_sb("mod_b", [128, 96], F32)
    slabs = [P.alloc_sb("mod_w%d" % i, [128, NCH, 512], F32) for i in range(2)]
    pss = [P.alloc_ps("mod_ps%d" % i, [128, 4, 2]) for i in range(2)]
    k.dma('sp', c_sb[:], cT)
    k.dma('sp', bsb[:], b_adaT[l])
    k.op('act', lambda e: e.activation(out=sc[:], in_=c_sb[:], func=AF.Silu), reads=[c_sb], writes=[sc])
    wv = w_ada[l].rearrange("(kc p) c -> p kc c", p=128)
    for cs in range(24):
        slab = slabs[cs % 2]
        pst = pss[cs % 2]
        k.dma('sp' if cs % 2 == 0 else 'act', slab[:], wv[:, :, cs * 512:(cs + 1) * 512])
        for j in range(4):
            for kc in range(NCH):
                k.op('pe', lambda e, j=j, kc=kc: e.matmul(pst[:, j, :], lhsT=slab[:, kc, j * 128:(j + 1) * 128],
                                                       rhs=sc[:, kc, :], start=(kc == 0), stop=(kc == NCH - 1)),
                     reads=[slab, sc], writes=[pst])
        for r in range(2):
            k.op('dve', lambda e, r=r: e.tensor_tensor(out=modT[:, cs * 4:(cs + 1) * 4, r], in0=pst[:, :, r],
                                                      in1=bsb[:, cs * 4:(cs + 1) * 4], op=ALU.add),
                 reads=[pst, bsb], writes=[modT])
    P.release(m)


TOK_BLOCKS = [(0, 512, 0), (512, 512, 0), (1024, 512, 0), (1536, 512, 0), (2048, 256, 1)]


def make_consts(P):
    nc, k = P.nc, P.k
    C = {}
    C['ones_bf'] = sb(nc, "c_ones_bf", [128, 128], BF16)
    C['ones_f'] = sb(nc, "c_ones_f", [128, 128], F32)
    C['id_f'] = sb(nc, "c_id_f", [128, 128], F32)
    C['id_bf'] = sb(nc, "c_id_bf", [128, 128], BF16)
    k.op('pool', lambda e: e.memset(C['ones_f'][:], 1.0), writes=[C['ones_f']])
    k.op('dve', lambda e: e.tensor_copy(out=C['ones_bf'][:], in_=C['ones_f'][:]), reads=[C['ones_f']], writes=[C['ones_bf']])
    k.op('pool', lambda e: e.affine_select(out=C['id_f'][:], in_=C['ones_f'][:], pattern=[[-1, 128]],
                                           compare_op=ALU.is_equal, fill=0.0, base=0, channel_multiplier=1),
         reads=[C['ones_f']], writes=[C['id_f']])
    k.op('dve', lambda e: e.tensor_copy(out=C['id_bf'][:], in_=C['id_f'][:]), reads=[C['id_f']], writes=[C['id_bf']])
    return C


def ln_block(P, C, tmp, xt, nt, dst_fn, scale_fn, bias_fn, src_key=None):
    nc, k = P.nc, P.k
    xb, sq, ps_s, ps_q, mean, rstd, t1 = tmp['xb'], tmp['sq'], tmp['ps_s'], tmp['ps_q'], tmp['mean'], tmp['rstd'], tmp['t1']
    k.op('act', lambda e: e.activation(out=xb[:, :, :nt], in_=xt[:, :, :nt], func=AF.Copy), reads=[xt], writes=[xb])
    k.op('pool', lambda e: e.tensor_tensor(out=sq[:, :, :nt], in0=xt[:, :, :nt], in1=xt[:, :, :nt], op=ALU.mult),
         reads=[xt], writes=[sq])
    for c in range(NCH):
        k.op('pe', lambda e, c=c: e.matmul(ps_s[:, :nt], lhsT=C['ones_bf'][:], rhs=xb[:, c, :nt], start=(c == 0), stop=(c == NCH - 1)),
             reads=[xb, C['ones_bf']], writes=[ps_s])
    for c in range(NCH):
        k.op('pe', lambda e, c=c: e.matmul(ps_q[:, :nt], lhsT=C['ones_bf'][:], rhs=sq[:, c, :nt], start=(c == 0), stop=(c == NCH - 1)),
             reads=[sq, C['ones_bf']], writes=[ps_q])
    k.op('act', lambda e: e.mul(out=mean[:, :nt], in_=ps_s[:, :nt], mul=1.0 / D), reads=[ps_s], writes=[mean])
    k.op('dve', lambda e: e.tensor_tensor(out=rstd[:, :nt], in0=mean[:, :nt], in1=mean[:, :nt], op=ALU.mult),
         reads=[mean], writes=[rstd])
    k.op('dve', lambda e: e.scalar_tensor_tensor(out=rstd[:, :nt], in0=ps_q[:, :nt], scalar=1.0 / D, in1=rstd[:, :nt],
                                                 op0=ALU.mult, op1=ALU.subtract), reads=[ps_q, rstd], writes=[rstd])
    k.op('dve', lambda e: e.tensor_scalar_add(out=rstd[:, :nt], in0=rstd[:, :nt], scalar1=EPS), reads=[rstd], writes=[rstd])
    k.op('act', lambda e: e.activation(out=rstd[:, :nt], in_=rstd[:, :nt], func=AF.Ln), reads=[rstd], writes=[rstd])
    k.op('act', lambda e: e.activation(out=rstd[:, :nt], in_=rstd[:, :nt], func=AF.Exp, scale=-0.5), reads=[rstd], writes=[rstd])
    for c in range(NCH):
        tt = t1[c % 2]
        k.op('dve', lambda e, c=c, tt=tt: e.tensor_tensor(out=tt[:, :nt], in0=xt[:, c, :nt], in1=mean[:, :nt], op=ALU.subtract),
             reads=[xt, mean], writes=[tt])
        k.op('pool', lambda e, tt=tt: e.tensor_tensor(out=tt[:, :nt], in0=tt[:, :nt], in1=rstd[:, :nt], op=ALU.mult),
             reads=[tt, rstd], writes=[tt])
        dst, dkeys = dst_fn(c)
        s_ap, skeys = scale_fn(c)
        b_ap, bkeys = bias_fn(c)
        k.op('act', lambda e, tt=tt, dst=dst, s_ap=s_ap, b_ap=b_ap: e.activation(out=dst, in_=tt[:, :nt], func=AF.Identity,
                                                                               scale=s_ap, bias=b_ap),
             reads=[tt] + skeys + bkeys, writes=dkeys)


def ln_tmp(P, pfx, nmax=512):
    return {
        'xb': P.alloc_sb(pfx + "_xb", [128, NCH, nmax], BF16),
        'sq': P.alloc_sb(pfx + "_sq", [128, NCH, nmax], BF16),
        'ps_s': P.alloc_ps(pfx + "_pss", [128, nmax]),
        'ps_q': P.alloc_ps(pfx + "_psq", [128, nmax]),
        'mean': P.alloc_sb(pfx + "_mean", [128, nmax], F32),
        'rstd': P.alloc_sb(pfx + "_rstd", [128, nmax], F32),
        't1': [P.alloc_sb(pfx + "_t1%d" % i, [128, nmax], F32) for i in range(2)],
    }


def stage_inproj(P, C, l, xT, w_in, modp, pfm, ptm):
    nc, k = P.nc, P.k
    m = P.mark()
    hT = P.alloc_sb("ip_hT", [128, NCH, T], BF16)
    m2 = P.mark()
    tmp = ln_tmp(P, "ip")
    xts = [P.alloc_sb("ip_xt%d" % i, [128, NCH, 512], F32) for i in range(2)]
    xv = xT.rearrange("(c p) t -> p c t", p=128)
    for bi, (t0, nt, r) in enumerate(TOK_BLOCKS):
        xt = xts[bi % 2]
        k.dma('sp', xt[:, :, :nt], xv[:, :, t0:t0 + nt], writes=[xt])
        ln_block(P, C, tmp, xt, nt,
                 lambda c: (hT[:, c, t0:t0 + nt], [hT]),
                 lambda c: (modp['sc1p'][:, c, r:r + 1], [modp['sc1p']]),
                 lambda c: (modp['sh1'][:, c, r:r + 1], [modp['sh1']]))
    P.release(m2)
    slabs = [P.alloc_sb("ip_w%d" % i, [128, NCH, 512], BF16) for i in range(2)]
    pss = [P.alloc_ps("ip_ps%d" % i, [128, 512]) for i in range(4)]
    stg = [P.alloc_sb("ip_stg%d" % i, [128, 512], F32) for i in range(4)]
    wv = w_in[l].rearrange("(kc p) c -> p kc c", p=128)
    ev = 0
    for s in range(13):
        slab = slabs[s % 2]
        ncol = 512 if s < 12 else 32
        k.dma('pool', slab[:, :, :ncol], wv[:, :, s * 512:s * 512 + ncol], writes=[slab])
        if s < 6 or s == 12:
            row0 = s * 512 if s < 6 else 3072
            for j in range((ncol + 127) // 128):
                cw = min(128, ncol - j * 128)
                for (t0, nt, r) in TOK_BLOCKS:
                    pst = pss[ev % 4]; st = stg[ev % 4]
                    for kc in range(NCH):
                        k.op('pe', lambda e, kc=kc, pst=pst: e.matmul(pst[:cw, :nt], lhsT=slab[:, kc, j * 128:j * 128 + cw],
                                                                     rhs=hT[:, kc, t0:t0 + nt], start=(kc == 0), stop=(kc == NCH - 1)),
                             reads=[slab, hT], writes=[pst])
                    if ev % 2 == 0:
                        k.op('act', lambda e, pst=pst, st=st: e.copy(out=st[:cw, :nt], in_=pst[:cw, :nt]), reads=[pst], writes=[st])
                    else:
                        k.op('dve', lambda e, pst=pst, st=st: e.tensor_copy(out=st[:cw, :nt], in_=pst[:cw, :nt]), reads=[pst], writes=[st])
                    k.dma('sp', pfm[row0 + j * 128:row0 + j * 128 + cw, t0:t0 + nt], st[:cw, :nt], reads=[st], writes=[pfm])
                    ev += 1
        else:
            col0 = (s - 6) * 512
            for tt in range(T // 128):
                pst = pss[ev % 4]; st = stg[ev % 4]
                for kc in range(NCH):
                    k.op('pe', lambda e, kc=kc, pst=pst: e.matmul(pst[:, :], lhsT=hT[:, kc, tt * 128:(tt + 1) * 128],
                                                                 rhs=slab[:, kc, :], start=(kc == 0), stop=(kc == NCH - 1)),
                         reads=[slab, hT], writes=[pst])
                if ev % 2 == 0:
                    k.op('act', lambda e, pst=pst, st=st: e.copy(out=st[:, :], in_=pst[:, :]), reads=[pst], writes=[st])
                else:
                    k.op('dve', lambda e, pst=pst, st=st: e.tensor_copy(out=st[:, :], in_=pst[:, :]), reads=[pst], writes=[st])
                k.dma('sp', ptm[tt * 128:(tt + 1) * 128, col0:col0 + 512], st[:, :], reads=[st], writes=[ptm])
                ev += 1
    P.release(m)


def load_mod(P, modT):
    nc, k = P.nc, P.k
    mp = {}
    names = ['sh1', 'sc1p', 'g1', 'sh2', 'sc2p', 'g2']
    for j, n in enumerate(names):
        t = P.alloc_sb("modp_" + n, [128, NCH, 2], F32)
        if n.startswith('sc'):
            k.op('dve', lambda e, t=t, j=j: e.tensor_scalar_add(out=t[:], in0=modT[:, j * 16:(j + 1) * 16, :], scalar1=1.0),
                 reads=[modT], writes=[t])
        else:
            k.op('dve', lambda e, t=t, j=j: e.tensor_copy(out=t[:], in_=modT[:, j * 16:(j + 1) * 16, :]), reads=[modT], writes=[t])
        mp[n] = t
    return mp


HY_MIN = math.log(1e-2) / 1.5
HY_MAX = math.log(1e-2) / 0.3


def stage_hyena(P, C, l, W, pfm, mixT, tok0, Ls):
    nc, k = P.nc, P.k
    NT = Ls // 128
    M = 2 * Ls
    pf = "hy%d_" % Ls
    m0 = P.mark()
    hfb = P.alloc_sb(pf + "hfb", [128, NT, 1024], BF16)
    uT = P.alloc_sb(pf + "uT", [128, NT, 512], BF16)
    x0T = P.alloc_sb(pf + "x0T", [128, NT, 512], F32)
    frow = P.alloc_sb(pf + "frow", [128, Ls], F32)
    ncol = P.alloc_sb(pf + "ncol", [128, NT], F32)
    pa = [P.alloc_ps(pf + "pa%d" % i, [128, 512]) for i in range(7)]
    ptb = P.alloc_ps(pf + "ptb", [128, 512], BF16)
    ti = P.alloc_sb(pf + "ti", [128, Ls], I32)
    k.op('pool', lambda e: e.iota(ti[:], pattern=[[1, Ls]], base=0, channel_multiplier=0), writes=[ti])
    k.op('dve', lambda e: e.tensor_copy(out=frow[:], in_=ti[:]), reads=[ti], writes=[frow])
    k.op('pool', lambda e: e.iota(ti[:, :NT], pattern=[[128, NT]], base=0, channel_multiplier=1), reads=[frow], writes=[ti])
    k.op('dve', lambda e: e.tensor_copy(out=ncol[:], in_=ti[:, :NT]), reads=[ti], writes=[ncol])

    m1 = P.mark()
    w1 = P.alloc_sb(pf + "w1", [33, 64], F32)
    w2 = P.alloc_sb(pf + "w2", [64, 64], F32)
    w3 = P.alloc_sb(pf + "w3", [64, 1024], F32)
    fv = P.alloc_sb(pf + "fv", [64, 3], F32)
    fc = P.alloc_sb(pf + "fc", [64, 4], F32)
    hb = P.alloc_sb(pf + "hb", [1, 512], F32)
    k.dma('sp', w1[:], W['hy_f_w1'][l])
    k.dma('sp', w2[:], W['hy_f_w2'][l])
    k.dma('sp', w3[:], W['hy_f_w3'][l])
    k.dma('sp', fv[:], W['hy_fv'][l])
    k.dma('sp', hb[:], W['hy_bias'][l:l + 1, :])
    k.op('dve', lambda e: e.tensor_scalar_mul(out=fc[:, 0:1], in0=fv[:, 2:3], scalar1=1.0 / (2 * math.pi)), reads=[fv], writes=[fc])
    for i in range(2):
        k.op('dve', lambda e, i=i: e.tensor_scalar(out=fc[:, 1 + i:2 + i], in0=fv[:, i:i + 1], scalar1=fc[:, 0:1], scalar2=0.0,
                                                  op0=ALU.mult, op1=ALU.add), reads=[fv, fc], writes=[fc])
    pc = P.alloc_sb(pf + "pc", [33, 4], F32)
    pi_ = P.alloc_sb(pf + "pi", [33, 1], I32)
    k.op('pool', lambda e: e.iota(pi_[:], pattern=[[0, 1]], base=0, channel_multiplier=1), writes=[pi_])
    k.op('dve', lambda e: e.tensor_copy(out=pc[:, 0:1], in_=pi_[:]), reads=[pi_], writes=[pc])
    step = (15.0 - 1e-4) / 15.0
    k.op('dve', lambda e: e.tensor_scalar(out=pc[:, 3:4], in0=pc[:, 0:1], scalar1=16.5, scalar2=-16.0, op0=ALU.is_gt, op1=ALU.mult),
         reads=[pc], writes=[pc])
    k.op('dve', lambda e: e.tensor_tensor(out=pc[:, 1:2], in0=pc[:, 0:1], in1=pc[:, 3:4], op=ALU.add), reads=[pc], writes=[pc])
    k.op('dve', lambda e: e.tensor_scalar(out=pc[:, 1:2], in0=pc[:, 1:2], scalar1=step / Ls, scalar2=(1e-4 - step) / Ls, op0=ALU.mult, op1=ALU.add),
         reads=[pc], writes=[pc])
    k.op('dve', lambda e: e.tensor_scalar(out=pc[:, 2:3], in0=pc[:, 0:1], scalar1=16.5, scalar2=-0.25, op0=ALU.is_lt, op1=ALU.mult),
         reads=[pc], writes=[pc])
    k.op('dve', lambda e: e.tensor_scalar_add(out=pc[:, 2:3], in0=pc[:, 2:3], scalar1=0.5), reads=[pc], writes=[pc])
    wi_ = P.alloc_sb(pf + "wi", [64, Ls], I32)
    wf_ = P.alloc_sb(pf + "wff", [64, Ls], F32)

    def wrap(t, np_):
        k.op('dve', lambda e: e.tensor_copy(out=wi_[:np_, :], in_=t[:np_, :]), reads=[t], writes=[wi_])
        k.op('pool', lambda e: e.tensor_copy(out=wf_[:np_, :], in_=wi_[:np_, :]), reads=[wi_], writes=[wf_])
        k.op('dve', lambda e: e.tensor_tensor(out=t[:np_, :], in0=t[:np_, :], in1=wf_[:np_, :], op=ALU.subtract), reads=[t, wf_], writes=[t])
    zT = P.alloc_sb(pf + "zT", [33, Ls], F32)
    h1 = P.alloc_sb(pf + "h1", [64, Ls], F32)
    h2 = P.alloc_sb(pf + "h2", [64, Ls], F32)
    k.op('dve', lambda e: e.tensor_scalar(out=zT[:], in0=frow[:33, :], scalar1=pc[:, 1:2], scalar2=pc[:, 2:3], op0=ALU.mult, op1=ALU.add),
         reads=[frow, pc], writes=[zT])
    wrap(zT, 33)
    k.op('act', lambda e: e.activation(out=zT[:], in_=zT[:], func=AF.Sin, scale=2 * math.pi), reads=[zT], writes=[zT])
    k.op('act', lambda e: e.mul(out=zT[0:1, :], in_=frow[0:1, :], mul=1.0 / (Ls - 1)), reads=[frow, zT], writes=[zT])
    BL = min(512, Ls)
    for (src, wt, dst, ci) in ((zT, w1, h1, 1), (h1, w2, h2, 2)):
        for b0 in range(0, Ls, BL):
            pst = pa[(b0 // BL) % 2]
            k.op('pe', lambda e, pst=pst, src=src, wt=wt, b0=b0: e.matmul(pst[:64, :BL], lhsT=wt[:], rhs=src[:, b0:b0 + BL], start=True, stop=True),
                 reads=[wt, src], writes=[pst])
            k.op('dve', lambda e, pst=pst, dst=dst, b0=b0, ci=ci: e.tensor_scalar(out=dst[:, b0:b0 + BL], in0=pst[:64, :BL], scalar1=fc[:, 0:1],
                                                                                 scalar2=fc[:, ci:ci + 1], op0=ALU.mult, op1=ALU.add),
                 reads=[pst, fc], writes=[dst])
        wrap(dst, 64)
        k.op('act', lambda e, dst=dst: e.activation(out=dst[:], in_=dst[:], func=AF.Sin, scale=2 * math.pi), reads=[dst], writes=[dst])
    drow = P.alloc_sb(pf + "drow", [128, 512], F32)
    negt = P.alloc_sb(pf + "negt", [128, NT], F32)
    k.op('dve', lambda e: e.tensor_scalar(out=drow[:], in0=frow[:, :512] if Ls >= 512 else frow[:, :], scalar1=-(HY_MAX - HY_MIN) / 511.0, scalar2=-HY_MIN,
                                          op0=ALU.mult, op1=ALU.add), reads=[frow], writes=[drow]) if Ls >= 512 else None
    if Ls < 512:
        ti2 = P.alloc_sb(pf + "ti2", [128, 512], I32)
        k.op('pool', lambda e: e.iota(ti2[:], pattern=[[1, 512]], base=0, channel_multiplier=0), writes=[ti2])
        k.op('dve', lambda e: e.tensor_copy(out=drow[:], in_=ti2[:]), reads=[ti2], writes=[drow])
        k.op('dve', lambda e: e.tensor_scalar(out=drow[:], in0=drow[:], scalar1=-(HY_MAX - HY_MIN) / 511.0, scalar2=-HY_MIN,
                                              op0=ALU.mult, op1=ALU.add), reads=[drow], writes=[drow])
    k.op('dve', lambda e: e.tensor_scalar_mul(out=negt[:], in0=ncol[:], scalar1=-1.0 / (Ls - 1)), reads=[ncol], writes=[negt])
    dec = P.alloc_sb(pf + "dec", [128, 512], F32)
    h0 = P.alloc_sb(pf + "h0", [128, 1024], F32)
    for tc in range(NT):
        k.op('act', lambda e, tc=tc: e.activation(out=dec[:], in_=drow[:], func=AF.Exp, scale=negt[:, tc:tc + 1]), reads=[drow, negt], writes=[dec])
        for hf in range(2):
            pst = pa[2 + hf]
            k.op('pe', lambda e, pst=pst, tc=tc, hf=hf: e.matmul(pst[:, :], lhsT=h2[:, tc * 128:(tc + 1) * 128], rhs=w3[:, hf * 512:(hf + 1) * 512],
                                                               start=True, stop=True), reads=[h2, w3], writes=[pst])
            if tc == 0:
                k.op('dve', lambda e, pst=pst, hf=hf: e.tensor_tensor(out=h0[:, hf * 512:(hf + 1) * 512], in0=pst[:, :], in1=dec[:], op=ALU.mult),
                     reads=[pst, dec], writes=[h0])
            else:
                k.op('dve', lambda e, pst=pst, tc=tc, hf=hf: e.tensor_tensor(out=hfb[:, tc, hf * 512:(hf + 1) * 512], in0=pst[:, :], in1=dec[:], op=ALU.mult),
                     reads=[pst, dec], writes=[hfb])
        if tc == 0:
            k.op('dve', lambda e: e.tensor_tensor(out=h0[0:1, 0:512], in0=h0[0:1, 0:512], in1=hb[:], op=ALU.add), reads=[h0, hb], writes=[h0])
            k.op('pool', lambda e: e.memset(h0[0:1, 512:1024], 0.0), reads=[h0], writes=[h0])
            k.op('act', lambda e: e.copy(out=hfb[:, 0, :], in_=h0[:]), reads=[h0], writes=[hfb])
    P.release(m1)

    m2 = P.mark()
    sw = P.alloc_sb(pf + "sw", [128, 12, 4], F32)
    k.dma('sp', sw[:], W['hy_sw'][l])
    raws = [P.alloc_sb(pf + "raw%d" % i, [128, Ls], F32) for i in range(2)]
    cvs = [P.alloc_sb(pf + "cv%d" % i, [128, Ls], F32) for i in range(3)]
    ub = P.alloc_sb(pf + "ub", [128, Ls], BF16)

    def conv(j, dst, ri):
        raw = raws[ri]
        k.dma('sp', raw[:], pfm[j * 128:(j + 1) * 128, tok0:tok0 + Ls], writes=[raw])
        k.op('act', lambda e: e.activation(out=dst[:], in_=raw[:], func=AF.Identity, scale=sw[:, j, 1:2], bias=sw[:, j, 3:4]),
             reads=[raw, sw], writes=[dst])
        k.op('dve', lambda e: e.scalar_tensor_tensor(out=dst[:, 1:], in0=raw[:, :Ls - 1], scalar=sw[:, j, 0:1], in1=dst[:, 1:],
                                                     op0=ALU.mult, op1=ALU.add), reads=[raw, sw, dst], writes=[dst])
        k.op('dve', lambda e: e.scalar_tensor_tensor(out=dst[:, :Ls - 1], in0=raw[:, 1:], scalar=sw[:, j, 2:3], in1=dst[:, :Ls - 1],
                                                     op0=ALU.mult, op1=ALU.add), reads=[raw, sw, dst], writes=[dst])

    for j in range(4):
        conv(j, cvs[0], 0)
        for tc in range(NT):
            pst = pa[tc % 2]
            k.op('pe', lambda e, pst=pst, tc=tc: e.transpose(out=pst[:, :128], in_=cvs[0][:, tc * 128:(tc + 1) * 128], identity=C['id_f'][:]),
                 reads=[cvs[0], C['id_f']], writes=[pst])
            k.op('act', lambda e, pst=pst, tc=tc, j=j: e.copy(out=x0T[:, tc, j * 128:(j + 1) * 128], in_=pst[:, :128]), reads=[pst], writes=[x0T])
        conv(4 + j, cvs[1], 1)
        conv(8 + j, cvs[2], 0)
        k.op('pool', lambda e: e.tensor_tensor(out=ub[:], in0=cvs[1][:], in1=cvs[2][:], op=ALU.mult), reads=[cvs[1], cvs[2]], writes=[ub])
        for tc in range(NT):
            k.op('pe', lambda e, tc=tc: e.transpose(out=ptb[:, :128], in_=ub[:, tc * 128:(tc + 1) * 128], identity=C['id_bf'][:]),
                 reads=[ub, C['id_bf']], writes=[ptb])
            k.op('dve', lambda e, tc=tc, j=j: e.tensor_copy(out=uT[:, tc, j * 128:(j + 1) * 128], in_=ptb[:, :128]), reads=[ptb], writes=[uT])
    P.release(m2)

    Asb = P.alloc_sb(pf + "A", [128, NT, 512], BF16)
    Bsb = P.alloc_sb(pf + "B", [128, NT, 512], BF16)
    csb = [P.alloc_sb(pf + "cs%d" % i, [128, 2, 128], BF16) for i in range(3)]
    pm = [P.alloc_sb(pf + "pm%d" % i, [128, 2, 128], F32) for i in range(3)]
    pmi = [P.alloc_sb(pf + "pmi%d" % i, [128, 2, 128], I32) for i in range(3)]
    pmf = [P.alloc_sb(pf + "pmf%d" % i, [128, 2, 128], F32) for i in range(3)]
    wf = P.alloc_sb(pf + "wf", [128, NT], F32)
    nwf = P.alloc_sb(pf + "nwf", [128, NT], F32)
    k.op('pool', lambda e: e.memset(wf[:], 2.0 / M), writes=[wf])
    k.op('pool', lambda e: e.memset(wf[0:1, 0:1], 1.0 / M), reads=[wf], writes=[wf])
    k.op('dve', lambda e: e.tensor_scalar_mul(out=nwf[:], in0=wf[:], scalar1=-1.0), reads=[wf], writes=[nwf])
    gi = [0]

    def gen(a, b):
        i = gi[0] % 3
        gi[0] += 1
        k.op('dve', lambda e: e.tensor_scalar(out=pm[i][:, 0, :], in0=frow[:, b * 128:(b + 1) * 128], scalar1=ncol[:, a:a + 1], scalar2=1.0 / M,
                                              op0=ALU.mult, op1=ALU.mult), reads=[frow, ncol], writes=[pm[i]])
        k.op('pool', lambda e: e.tensor_scalar_add(out=pm[i][:, 1, :], in0=pm[i][:, 0, :], scalar1=0.25), reads=[pm[i]], writes=[pm[i]])
        k.op('dve', lambda e: e.tensor_copy(out=pmi[i][:], in_=pm[i][:]), reads=[pm[i]], writes=[pmi[i]])
        k.op('pool', lambda e: e.tensor_copy(out=pmf[i][:], in_=pmi[i][:]), reads=[pmi[i]], writes=[pmf[i]])
        k.op('dve', lambda e: e.tensor_tensor(out=pm[i][:], in0=pm[i][:], in1=pmf[i][:], op=ALU.subtract), reads=[pm[i], pmf[i]], writes=[pm[i]])
        k.op('act', lambda e: e.activation(out=csb[i][:], in_=pm[i][:], func=AF.Sin, scale=2 * math.pi), reads=[pm[i]], writes=[csb[i]])
        return csb[i][:, 1, :], csb[i][:, 0, :], csb[i]

    def cols(N, j):
        return uT[:, N, :] if j == 0 else hfb[:, N, (j - 1) * 512:j * 512]

    alt = P.alloc_sb(pf + "alt", [128, 128], F32)
    altb = P.alloc_sb(pf + "altb", [128, 128], BF16)
    alti = P.alloc_sb(pf + "alti", [128, 128], I32)
    altf = P.alloc_sb(pf + "altf", [128, 128], F32)
    k.op('dve', lambda e: e.tensor_scalar(out=alt[:], in0=frow[:, :128], scalar1=ncol[:, 0:1], scalar2=0.5, op0=ALU.add, op1=ALU.mult),
         reads=[frow, ncol], writes=[alt])
    k.op('dve', lambda e: e.tensor_scalar_add(out=alt[:], in0=alt[:], scalar1=0.25), reads=[alt], writes=[alt])
    k.op('dve', lambda e: e.tensor_copy(out=alti[:], in_=alt[:]), reads=[alt], writes=[alti])
    k.op('dve', lambda e: e.tensor_copy(out=altf[:], in_=alti[:]), reads=[alti], writes=[altf])
    k.op('dve', lambda e: e.tensor_tensor(out=alt[:], in0=alt[:], in1=altf[:], op=ALU.subtract), reads=[alt, altf], writes=[alt])
    k.op('act', lambda e: e.activation(out=altb[:], in_=alt[:], func=AF.Sin, scale=2 * math.pi), reads=[alt], writes=[altb])
    for j in range(3):
        for N in range(NT):
            k.op('pe', lambda e, j=j, N=N: e.matmul(pa[j][0:1, :], lhsT=altb[:, 0:1], rhs=cols(N, j), start=(N == 0), stop=(N == NT - 1)),
                 reads=[altb, uT, hfb], writes=[pa[j]])
    nyq = P.alloc_sb(pf + "nyq", [1, 2, 512], F32)
    nyqb = P.alloc_sb(pf + "nyqb", [1, 512], BF16)
    k.op('act', lambda e: e.copy(out=nyq[:, 0, :], in_=pa[1][0:1, :]), reads=[pa[1]], writes=[nyq])
    k.op('dve', lambda e: e.tensor_tensor(out=nyq[:, 0, :], in0=pa[2][0:1, :], in1=nyq[:, 0, :], op=ALU.add), reads=[pa[2], nyq], writes=[nyq])
    k.op('dve', lambda e: e.tensor_tensor(out=nyq[:, 1, :], in0=pa[0][0:1, :], in1=nyq[:, 0, :], op=ALU.mult), reads=[pa[0], nyq], writes=[nyq])
    k.op('act', lambda e: e.mul(out=nyqb[:], in_=nyq[:, 1, :], mul=1.0 / M), reads=[nyq], writes=[nyqb])

    ev = [P.alloc_sb(pf + "ev%d" % i, [128, 512], F32) for i in range(4)]
    tt = [P.alloc_sb(pf + "tt%d" % i, [128, 512], F32) for i in range(4)]
    for F in range(NT):
        for N in range(NT):
            cbk, sbl, ck = gen(N, F)
            for j in range(3):
                k.op('pe', lambda e, j=j, N=N, cbk=cbk: e.matmul(pa[j][:, :], lhsT=cbk, rhs=cols(N, j), start=(N == 0), stop=(N == NT - 1)),
                     reads=[ck, uT, hfb], writes=[pa[j]])
            for j in range(3):
                k.op('pe', lambda e, j=j, N=N, sbl=sbl: e.matmul(pa[3 + j][:, :], lhsT=sbl, rhs=cols(N, j), start=(N == 0), stop=(N == NT - 1)),
                     reads=[ck, uT, hfb], writes=[pa[3 + j]])
        k.op('act', lambda e: e.copy(out=ev[0][:], in_=pa[1][:, :]), reads=[pa[1]], writes=[ev[0]])
        k.op('act', lambda e: e.copy(out=ev[1][:], in_=pa[4][:, :]), reads=[pa[4]], writes=[ev[1]])
        k.op('dve', lambda e: e.tensor_tensor(out=ev[0][:], in0=pa[2][:, :], in1=ev[0][:], op=ALU.add), reads=[pa[2], ev[0]], writes=[ev[0]])
        k.op('dve', lambda e: e.tensor_tensor(out=ev[1][:], in0=pa[5][:, :], in1=ev[1][:], op=ALU.subtract), reads=[pa[5], ev[1]], writes=[ev[1]])
        k.op('dve', lambda e: e.tensor_tensor(out=tt[0][:], in0=pa[0][:, :], in1=ev[0][:], op=ALU.mult), reads=[pa[0], ev[0]], writes=[tt[0]])
        k.op('dve', lambda e: e.tensor_tensor(out=tt[1][:], in0=pa[3][:, :], in1=ev[1][:], op=ALU.mult), reads=[pa[3], ev[1]], writes=[tt[1]])
        k.op('pool', lambda e: e.tensor_tensor(out=tt[0][:], in0=tt[0][:], in1=tt[1][:], op=ALU.add), reads=[tt[0], tt[1]], writes=[tt[0]])
        k.op('act', lambda e, F=F: e.activation(out=Asb[:, F, :], in_=tt[0][:], func=AF.Copy, scale=wf[:, F:F + 1]), reads=[tt[0], wf], writes=[Asb])
        k.op('dve', lambda e: e.tensor_tensor(out=tt[2][:], in0=pa[0][:, :], in1=ev[1][:], op=ALU.mult), reads=[pa[0], ev[1]], writes=[tt[2]])
        k.op('dve', lambda e: e.tensor_tensor(out=tt[3][:], in0=pa[3][:, :], in1=ev[0][:], op=ALU.mult), reads=[pa[3], ev[0]], writes=[tt[3]])
        k.op('pool', lambda e: e.tensor_tensor(out=tt[2][:], in0=tt[2][:], in1=tt[3][:], op=ALU.subtract), reads=[tt[2], tt[3]], writes=[tt[2]])
        k.op('act', lambda e, F=F: e.activation(out=Bsb[:, F, :], in_=tt[2][:], func=AF.Copy, scale=nwf[:, F:F + 1]), reads=[tt[2], nwf], writes=[Bsb])
    nw = P.alloc_sb(pf + "nw", [128, 4], F32)
    k.dma('sp', nw[:], W['hy_norm_wT'][l])
    ys = [P.alloc_sb(pf + "y%d" % i, [128, 512], F32) for i in range(2)]
    yb = [P.alloc_sb(pf + "yb%d" % i, [128, 512], BF16) for i in range(2)]
    junk = P.alloc_sb(pf + "junk", [128, 512], F32)
    ss = P.alloc_sb(pf + "ss", [128, 2], F32)
    mst = [P.alloc_sb(pf + "mst%d" % i, [128, 4, 128], BF16) for i in range(2)]
    for N in range(NT):
        py = pa[N % 2]
        for F in range(NT):
            cbk, sbl, ck = gen(F, N)
            k.op('pe', lambda e, F=F, cbk=cbk: e.matmul(py[:, :], lhsT=cbk, rhs=Asb[:, F, :], start=(F == 0), stop=False),
                 reads=[ck, Asb], writes=[py])
            k.op('pe', lambda e, F=F, sbl=sbl: e.matmul(py[:, :], lhsT=sbl, rhs=Bsb[:, F, :], start=False, stop=False),
                 reads=[ck, Bsb], writes=[py])
        k.op('pe', lambda e: e.matmul(py[:, :], lhsT=altb[0:1, :], rhs=nyqb[:], start=False, stop=True), reads=[altb, nyqb], writes=[py])
        y = ys[N % 2]; ybf = yb[N % 2]; ms = mst[N % 2]
        k.op('dve', lambda e, N=N: e.tensor_tensor(out=y[:], in0=py[:, :], in1=x0T[:, N, :], op=ALU.mult), reads=[py, x0T], writes=[y])
        k.op('act', lambda e: e.activation(out=junk[:], in_=y[:], func=AF.Square, accum_out=ss[:, 0:1]), reads=[y], writes=[junk, ss])
        k.op('dve', lambda e: e.tensor_scalar(out=ss[:, 1:2], in0=ss[:, 0:1], scalar1=1.0 / HY, scalar2=EPS, op0=ALU.mult, op1=ALU.add), reads=[ss], writes=[ss])
        k.op('act', lambda e: e.activation(out=ss[:, 1:2], in_=ss[:, 1:2], func=AF.Ln), reads=[ss], writes=[ss])
        k.op('act', lambda e: e.activation(out=ss[:, 1:2], in_=ss[:, 1:2], func=AF.Exp, scale=-0.5), reads=[ss], writes=[ss])
        k.op('dve', lambda e: e.tensor_scalar_mul(out=ybf[:], in0=y[:], scalar1=ss[:, 1:2]), reads=[y, ss], writes=[ybf])
        for j in range(4):
            k.op('pe', lambda e, j=j: e.transpose(out=ptb[:, j * 128:(j + 1) * 128], in_=ybf[:, j * 128:(j + 1) * 128], identity=C['id_bf'][:]),
                 reads=[ybf, C['id_bf']], writes=[ptb])
        for j in range(4):
            k.op('act', lambda e, j=j: e.activation(out=ms[:, j, :], in_=ptb[:, j * 128:(j + 1) * 128], func=AF.Copy, scale=nw[:, j:j + 1]),
                 reads=[ptb, nw], writes=[ms])
        k.dma('sp', mixT[0:512, tok0 + N * 128:tok0 + (N + 1) * 128].rearrange("(j p) t -> p j t", p=128), ms[:], reads=[ms], writes=[mixT])
    P.release(m0)


NEGM = -1.0e4


def stage_na(P, C, l, W, pfm, ptm, mixT, rpbp, with_ctx):
    nc, k = P.nc, P.k
    pf = "na_"
    m0 = P.mark()
    scale = 128 ** -0.5
    qT = P.alloc_sb(pf + "qT", [128, NAH, T], BF16)
    kT = P.alloc_sb(pf + "kT", [128, NAH, T], BF16)
    v1 = P.alloc_sb(pf + "v1", [128, T // 128, NAH, 129], BF16)
    R = P.alloc_sb(pf + "R", [128, NAH, 9, 128], F32)
    k.op('pool', lambda e: e.memset(v1[:], 1.0), writes=[v1])
    for h in range(NAH):
        k.dma('pool', qT[:, h, :], pfm[O_NQ + h * 128:O_NQ + (h + 1) * 128, :], writes=[qT])
        k.dma('pool', kT[:, h, :], pfm[O_NK + h * 128:O_NK + (h + 1) * 128, :], writes=[kT])
    for tt in range(T // 128):
        k.dma('pool', v1[:, tt, :, 0:128], ptm[tt * 128:(tt + 1) * 128, 0:768].rearrange("p (h d) -> p h d", h=NAH), writes=[v1])
    m1 = P.mark()
    zr = P.alloc_sb(pf + "zr", [90, 160], F32)
    k.op('pool', lambda e: e.memset(zr[:], 0.0), writes=[zr])
    k.dma('sp', zr[:, 48:79], W['na_rpb'][l].rearrange("h r m -> (h r) m"), writes=[zr])
    k.dma('sp', rpbp.rearrange("h r m -> (h r) m"), zr[:], reads=[zr], writes=[rpbp])
    Hk = P.alloc_sb(pf + "Hk", [64, NAH, 15, 64], F32)
    for h in range(NAH):
        src = bass.AP(rpbp.tensor, rpbp[h, 0, 0:1].offset, [[1, 64], [160, 15], [1, 64]])
        k.dma('sp', Hk[:, h, :, :], src, reads=[rpbp], writes=[Hk])
    J = P.alloc_sb(pf + "J", [64, 64], F32)
    k.op('pool', lambda e: e.affine_select(out=J[:], in_=C['ones_f'][:64, :64], pattern=[[1, 64]], compare_op=ALU.is_equal, fill=0.0,
                                           base=-63, channel_multiplier=1), reads=[C['ones_f']], writes=[J])
    ms = [P.alloc_sb(pf + "ms%d" % i, [128, 2, 64], F32) for i in range(4)]
    CM = P.alloc_sb(pf + "CM", [128, 2, 64], F32)
    ones3 = C['ones_f'][:, :].rearrange("p (c q) -> p c q", c=2)
    for a in range(2):
        sl = slice(a * 64, (a + 1) * 64)
        k.op('pool', lambda e, sl=sl, a=a: e.affine_select(out=ms[0][sl], in_=ones3[sl], pattern=[[0, 2], [-1, 64]], compare_op=ALU.is_ge, fill=0.0,
                                                          base=8, channel_multiplier=1), reads=[C['ones_f']], writes=[ms[0]])
        k.op('pool', lambda e, sl=sl, a=a: e.affine_select(out=ms[1][sl], in_=ones3[sl], pattern=[[0, 2], [0, 64]], compare_op=ALU.is_ge, fill=0.0,
                                                          base=-48, channel_multiplier=1), reads=[C['ones_f']], writes=[ms[1]])
        k.op('pool', lambda e, sl=sl, a=a: e.affine_select(out=ms[2][sl], in_=ones3[sl], pattern=[[0, 2], [1, 64]], compare_op=ALU.is_ge, fill=0.0,
                                                          base=7, channel_multiplier=-1), reads=[C['ones_f']], writes=[ms[2]])
        k.op('pool', lambda e, sl=sl, a=a: e.affine_select(out=ms[3][sl], in_=ones3[sl], pattern=[[0, 2], [0, 64]], compare_op=ALU.is_ge, fill=0.0,
                                                          base=15, channel_multiplier=-1), reads=[C['ones_f']], writes=[ms[3]])
    k.op('dve', lambda e: e.tensor_tensor(out=ms[0][:], in0=ms[0][:], in1=ms[1][:], op=ALU.max), reads=[ms[0], ms[1]], writes=[ms[0]])
    k.op('dve', lambda e: e.tensor_tensor(out=ms[2][:], in0=ms[2][:], in1=ms[3][:], op=ALU.max), reads=[ms[2], ms[3]], writes=[ms[2]])
    k.op('dve', lambda e: e.tensor_tensor(out=CM[:], in0=ms[0][:], in1=ms[2][:], op=ALU.mult), reads=[ms[0], ms[2]], writes=[CM])
    k.op('dve', lambda e: e.tensor_copy(out=ms[0][:], in_=CM[:]), reads=[CM], writes=[ms[0]])
    k.op('dve', lambda e: e.tensor_scalar(out=CM[:], in0=CM[:], scalar1=-1.0, scalar2=-NEGM, op0=ALU.add, op1=ALU.mult), reads=[CM], writes=[CM])
    pr = [P.alloc_ps(pf + "pr%d" % i, [128, 2, 64]) for i in range(2)]
    cnt = 0
    for h in range(NAH):
        for vi in range(9):
            d = vi - 3 if vi < 7 else (-2 if vi == 7 else 2)
            pst = pr[cnt % 2]
            cnt += 1
            for c in range(2):
                di = 2 * d - c + 7
                k.op('pe', lambda e, pst=pst, h=h, di=di, c=c: e.matmul(pst[:, c, :], lhsT=Hk[:, h, di:di + 2, :], rhs=J[:], start=True, stop=True),
                     reads=[Hk, J], writes=[pst])
            k.op('dve', lambda e, pst=pst, h=h, vi=vi: e.tensor_tensor(out=R[:, h, vi, :].rearrange("p (c q) -> p c q", c=2), in0=pst[:], in1=ms[0][:], op=ALU.mult),
                 reads=[pst, ms[0]], writes=[R])
            k.op('pool', lambda e, h=h, vi=vi: e.tensor_tensor(out=R[:, h, vi, :].rearrange("p (c q) -> p c q", c=2), in0=R[:, h, vi, :].rearrange("p (c q) -> p c q", c=2),
                                                              in1=CM[:], op=ALU.add), reads=[R, CM], writes=[R])
            if vi == 7:
                k.op('pool', lambda e, h=h, vi=vi: e.memset(R[0:64, h, vi, 64:128], NEGM), reads=[R], writes=[R])
            if vi == 8:
                k.op('pool', lambda e, h=h, vi=vi: e.memset(R[:, h, vi, 0:64], NEGM), reads=[R], writes=[R])
                k.op('pool', lambda e, h=h, vi=vi: e.memset(R[64:128, h, vi, 64:128], NEGM), reads=[R], writes=[R])
    P.dump('na_R', R)
    P.dump('na_Hk', Hk)
    P.release(m1)
    nw = P.alloc_sb(pf + "nw", [128, NAH], F32)
    k.dma('sp', nw[:], W['na_norm_wT'][l])
    pS = [P.alloc_ps(pf + "pS%d" % i, [128, 7, 128]) for i in range(2)]
    pO = [P.alloc_ps(pf + "pO%d" % i, [128, 129]) for i in range(2)]
    ptb = P.alloc_ps(pf + "ptb", [128, NAH, 128], BF16)
    ein = [P.alloc_sb(pf + "ein%d" % i, [128, 5, 128], F32) for i in range(2)]
    PT = [P.alloc_sb(pf + "PT%d" % i, [128, 7, 128], BF16) for i in range(2)]
    ob = [P.alloc_sb(pf + "ob%d" % i, [128, NAH * 128], F32) for i in range(2)]
    obb = [P.alloc_sb(pf + "obb%d" % i, [128, NAH * 128], BF16) for i in range(2)]
    rc = P.alloc_sb(pf + "rc", [128, 2], F32)
    ss = P.alloc_sb(pf + "ss", [128, 2], F32)
    junk = P.alloc_sb(pf + "junk", [128, NAH * 128], F32)
    mst = [P.alloc_sb(pf + "mst%d" % i, [128, NAH, 128], BF16) for i in range(2)]
    CT = [L // 128, L // 128 + 1]
    units = []
    for i in range(16):
        if 2 <= i <= 13:
            loc = [(i - 2, 7), (i - 1, 2), (i, 3), (i + 1, 4), (i + 2, 8)]
        elif i < 2:
            loc = [(j, j - i + 3) for j in range(4)]
        else:
            loc = [(j, j - i + 3) for j in range(12, 16)]
        units.append((i, loc, True))
    if with_ctx:
        units += [(16, [], True), (17, [], True)]
    u = 0
    for (qi, loc, _) in units:
        o_t = ob[u % 2]; o_b = obb[u % 2]; ms_ = mst[u % 2]
        for h in range(NAH):
            g = (u * NAH + h) % 2
            ps_, po, ei, pt = pS[g], pO[g], ein[g], PT[g]
            tiles = [j for (j, _) in loc] + CT
            nl = len(loc)
            for jj, j in enumerate(tiles):
                k.op('pe', lambda e, jj=jj, j=j, h=h, ps_=ps_: e.matmul(ps_[:, jj, :], lhsT=kT[:, h, j * 128:(j + 1) * 128], rhs=qT[:, h, qi * 128:(qi + 1) * 128],
                                                                       start=True, stop=True), reads=[kT, qT], writes=[ps_])
            for jj, (j, vi) in enumerate(loc):
                k.op('dve', lambda e, jj=jj, vi=vi, h=h, ps_=ps_, ei=ei: e.scalar_tensor_tensor(out=ei[:, jj, :], in0=ps_[:, jj, :], scalar=scale, in1=R[:, h, vi, :],
                                                                                               op0=ALU.mult, op1=ALU.add), reads=[ps_, R], writes=[ei])
            if nl:
                k.op('act', lambda e, nl=nl, ei=ei, pt=pt: e.activation(out=pt[:, 0:nl, :], in_=ei[:, 0:nl, :], func=AF.Exp), reads=[ei], writes=[pt])
            k.op('act', lambda e, nl=nl, ps_=ps_, pt=pt: e.activation(out=pt[:, nl:nl + 2, :], in_=ps_[:, nl:nl + 2, :], func=AF.Exp, scale=scale),
                 reads=[ps_], writes=[pt])
            for jj, j in enumerate(tiles):
                k.op('pe', lambda e, jj=jj, j=j, h=h, pt=pt, po=po: e.matmul(po[:, :], lhsT=pt[:, jj, :], rhs=v1[:, j, h, :], start=(jj == 0), stop=(jj == len(tiles) - 1)),
                     reads=[pt, v1], writes=[po])
            k.op('dve', lambda e, po=po: e.reciprocal(out=rc[:, 0:1], in_=po[:, 128:129]), reads=[po], writes=[rc])
            k.op('dve', lambda e, po=po, h=h, o_t=o_t: e.tensor_scalar_mul(out=o_t[:, h * 128:(h + 1) * 128], in0=po[:, 0:128], scalar1=rc[:, 0:1]),
                 reads=[po, rc], writes=[o_t])
        k.op('act', lambda e, o_t=o_t: e.activation(out=junk[:], in_=o_t[:], func=AF.Square, accum_out=ss[:, 0:1]), reads=[o_t], writes=[junk, ss])
        k.op('dve', lambda e: e.tensor_scalar(out=ss[:, 1:2], in0=ss[:, 0:1], scalar1=1.0 / 768, scalar2=EPS, op0=ALU.mult, op1=ALU.add), reads=[ss], writes=[ss])
        k.op('act', lambda e: e.activation(out=ss[:, 1:2], in_=ss[:, 1:2], func=AF.Ln), reads=[ss], writes=[ss])
        k.op('act', lambda e: e.activation(out=ss[:, 1:2], in_=ss[:, 1:2], func=AF.Exp, scale=-0.5), reads=[ss], writes=[ss])
        k.op('dve', lambda e, o_t=o_t, o_b=o_b: e.tensor_scalar_mul(out=o_b[:], in0=o_t[:], scalar1=ss[:, 1:2]), reads=[o_t, ss], writes=[o_b])
        for h in range(NAH):
            k.op('pe', lambda e, h=h, o_b=o_b: e.transpose(out=ptb[:, h, :], in_=o_b[:, h * 128:(h + 1) * 128], identity=C['id_bf'][:]),
                 reads=[o_b, C['id_bf']], writes=[ptb])
        for h in range(NAH):
            k.op('act', lambda e, h=h, ms_=ms_: e.activation(out=ms_[:, h, :], in_=ptb[:, h, :], func=AF.Copy, scale=nw[:, h:h + 1]), reads=[ptb, nw], writes=[ms_])
        k.dma('sp', mixT[512:1280, qi * 128:(qi + 1) * 128].rearrange("(j p) t -> p j t", p=128), ms_[:], reads=[ms_], writes=[mixT])
        u += 1
    P.release(m0)


def stage_gla(P, C, l, W, pfm, ptm, mixT, ofs, with_ctx):
    nc, k = P.nc, P.k
    pf = "gl_"
    m0 = P.mark()
    NTL = L // 128
    NTT = T // 128
    qs = GDK ** -0.5
    Mf = P.alloc_sb(pf + "Mf", [128, 128], F32)
    Mb = P.alloc_sb(pf + "Mb", [128, 128], F32)
    k.op('pool', lambda e: e.affine_select(out=Mf[:], in_=C['ones_f'][:], pattern=[[1, 128]], compare_op=ALU.is_ge, fill=0.0, base=0, channel_multiplier=-1),
         reads=[C['ones_f']], writes=[Mf])
    k.op('pool', lambda e: e.memset(Mf[0:64, 64:128], 0.0), reads=[Mf], writes=[Mf])
    k.op('pool', lambda e: e.affine_select(out=Mb[:], in_=C['ones_f'][:], pattern=[[-1, 128]], compare_op=ALU.is_ge, fill=0.0, base=0, channel_multiplier=1),
         reads=[C['ones_f']], writes=[Mb])
    k.op('pool', lambda e: e.memset(Mb[64:128, 0:64], 0.0), reads=[Mb], writes=[Mb])
    Lf = P.alloc_sb(pf + "Lf", [128, 128], F32)
    Lb = P.alloc_sb(pf + "Lb", [128, 128], F32)
    k.op('dve', lambda e: e.tensor_scalar_mul(out=Lf[:], in0=Mf[:], scalar1=-1.0 / 16), reads=[Mf], writes=[Lf])
    k.op('dve', lambda e: e.tensor_scalar_mul(out=Lb[:], in0=Mb[:], scalar1=-1.0 / 16), reads=[Mb], writes=[Lb])
    ind = P.alloc_sb(pf + "ind", [128, 2], F32)
    k.op('pool', lambda e: e.memset(ind[:], 0.0), writes=[ind])
    k.op('pool', lambda e: e.memset(ind[0:64, 0:1], -1.0 / 16), reads=[ind], writes=[ind])
    k.op('pool', lambda e: e.memset(ind[64:128, 1:2], -1.0 / 16), reads=[ind], writes=[ind])
    one1 = P.alloc_sb(pf + "one1", [128, 1], F32)
    k.op('pool', lambda e: e.memset(one1[:], 1.0), writes=[one1])
    ga1T = P.alloc_sb(pf + "ga1T", [33, T], F32)
    k.op('pool', lambda e: e.memset(ga1T[:], 1.0), writes=[ga1T])
    k.dma('sp', ga1T[0:32, :], pfm[3072:3104, :], writes=[ga1T])
    w2b = P.alloc_sb(pf + "w2b", [33, 2, 384], F32)
    k.op('pool', lambda e: e.memset(w2b[:], 0.0), writes=[w2b])
    for d in range(2):
        k.dma('sp', w2b[16 * d:16 * d + 16, d, :], W['gla_a_w2'][l, d], writes=[w2b])
        k.dma('sp', w2b[32:33, d, :], W['gla_a_b'][l, d:d + 1, :], writes=[w2b])
    gnw = P.alloc_sb(pf + "gnw", [128, 768], F32)
    for h in range(GH):
        k.dma('sp', gnw[:, h * 128:(h + 1) * 128], W['gla_norm_w'][l:l + 1, :].partition_broadcast(128), writes=[gnw])
    m1 = P.mark()
    ii = P.alloc_sb(pf + "ii", [128, 16], I32)
    inv = P.alloc_sb(pf + "inv", [128, 16], F32)
    k.op('pool', lambda e: e.iota(ii[:], pattern=[[1, 16]], base=0, channel_multiplier=0), writes=[ii])
    k.op('dve', lambda e: e.tensor_copy(out=inv[:], in_=ii[:]), reads=[ii], writes=[inv])
    k.op('act', lambda e: e.activation(out=inv[:], in_=inv[:], func=AF.Exp, scale=-math.log(10000.0) / 16), reads=[inv], writes=[inv])
    k.op('dve', lambda e: e.tensor_scalar_mul(out=inv[:], in0=inv[:], scalar1=1.0 / (2 * math.pi)), reads=[inv], writes=[inv])
    pp = P.alloc_sb(pf + "pp", [128, 4], F32)
    pi2 = P.alloc_sb(pf + "pi2", [128, 1], I32)
    k.op('pool', lambda e: e.iota(pi2[:], pattern=[[0, 1]], base=0, channel_multiplier=1), writes=[pi2])
    k.op('dve', lambda e: e.tensor_copy(out=pp[:, 0:1], in_=pi2[:]), reads=[pi2], writes=[pp])
    k.op('dve', lambda e: e.tensor_single_scalar(out=pp[:, 1:2], in_=pp[:, 0:1], scalar=63.5, op=ALU.is_gt), reads=[pp], writes=[pp])
    k.op('dve', lambda e: e.scalar_tensor_tensor(out=pp[:, 2:3], in0=pp[:, 1:2], scalar=-64.0, in1=pp[:, 0:1], op0=ALU.mult, op1=ALU.add),
         reads=[pp], writes=[pp])
    tab = P.alloc_sb(pf + "tab", [128, NTL, 2, 16], F32)
    rp = P.alloc_sb(pf + "rp", [128, 1], F32)
    for tt in range(NTL):
        k.op('dve', lambda e, tt=tt: e.tensor_scalar_add(out=rp[:], in0=pp[:, 1:2], scalar1=float(2 * tt)), reads=[pp], writes=[rp])
        k.op('dve', lambda e, tt=tt: e.tensor_scalar_mul(out=tab[:, tt, 0, :], in0=inv[:], scalar1=rp[:, 0:1]), reads=[inv, rp], writes=[tab])
        k.op('dve', lambda e, tt=tt: e.tensor_scalar_mul(out=tab[:, tt, 1, :], in0=inv[:], scalar1=pp[:, 2:3]), reads=[inv, pp], writes=[tab])
    sinT = P.alloc_sb(pf + "sinT", [128, NTL, 2, 16], F32)
    cosT = P.alloc_sb(pf + "cosT", [128, NTL, 2, 16], F32)
    ti = P.alloc_sb(pf + "ti", [128, NTL, 2, 16], I32)
    tf = P.alloc_sb(pf + "tf", [128, NTL, 2, 16], F32)
    for (dst, sh) in ((sinT, 0.0), (cosT, 0.25)):
        if sh:
            k.op('dve', lambda e: e.tensor_scalar_add(out=tab[:], in0=tab[:], scalar1=sh), reads=[tab], writes=[tab])
        k.op('dve', lambda e: e.tensor_copy(out=ti[:], in_=tab[:]), reads=[tab], writes=[ti])
        k.op('dve', lambda e: e.tensor_copy(out=tf[:], in_=ti[:]), reads=[ti], writes=[tf])
        k.op('dve', lambda e: e.tensor_tensor(out=tf[:], in0=tab[:], in1=tf[:], op=ALU.subtract), reads=[tab, tf], writes=[tf])
        k.op('act', lambda e, dst=dst: e.activation(out=dst[:], in_=tf[:], func=AF.Sin, scale=2 * math.pi), reads=[tf], writes=[dst])
    ld = [P.alloc_sb(pf + "ld%d" % i, [128, 2304], F32) for i in range(2)]
    vb = [P.alloc_sb(pf + "vb%d" % i, [128, GH, 128], BF16) for i in range(2)]
    ls_ = P.alloc_sb(pf + "ls", [128, 384], F32)
    ecp = P.alloc_sb(pf + "ecp", [128, 384], F32)
    ecn = P.alloc_sb(pf + "ecn", [128, 384], F32)
    qr = P.alloc_sb(pf + "qr", [128, 768], F32)
    rt = [P.alloc_sb(pf + "rt%d" % i, [128, 2, 16], F32) for i in range(4)]
    qkd = P.alloc_sb(pf + "qkd", [128, 2, 384], BF16)
    qdz = P.alloc_sb(pf + "qdz", [64, GH, 2, 128], BF16)
    k.op('pool', lambda e: e.memset(qdz[:], 0.0), writes=[qdz])
    kdT = P.alloc_sb(pf + "kdT", [64, GH, 128], BF16)
    qdT = P.alloc_sb(pf + "qdT", [64, GH, 128], BF16)
    ecl = P.alloc_sb(pf + "ecl", [64, GH, 2], F32)
    Am = [P.alloc_sb(pf + "Am%d" % i, [128, 128], BF16) for i in range(2)]
    S = {d: P.alloc_sb(pf + "S%d" % d, [64, GH, 128], F32) for d in range(2)}
    Sb = [P.alloc_sb(pf + "Sb%d" % i, [64, GH, 128], BF16) for i in range(3)]
    Stmp = P.alloc_sb(pf + "Stmp", [64, 128], F32)
    osb = P.alloc_sb(pf + "osb", [128, 768], F32)
    ofl = P.alloc_sb(pf + "ofl", [128, 768], F32)
    sq = P.alloc_sb(pf + "sq", [128, GH, 128], F32)
    ss = P.alloc_sb(pf + "ss", [128, 2, GH], F32)
    sg = P.alloc_sb(pf + "sg", [128, 768], F32)
    yb = P.alloc_sb(pf + "yb", [128, 768], BF16)
    mst = [P.alloc_sb(pf + "mst%d" % i, [128, GH, 128], BF16) for i in range(2)]
    bA = P.alloc_ps(pf + "bA", [128, 512])
    pz, pzk = bA[:, 0:384], bA
    pe_, pek = bA[0:64, 384:396].rearrange("p (h c) -> p h c", h=GH), bA
    bB = P.alloc_ps(pf + "bB", [128, 512])
    pc_, pck = bB[:, 0:384], bB
    pTq = P.alloc_ps(pf + "pTq", [64, GH, 128], BF16)
    pTk = P.alloc_ps(pf + "pTk", [64, GH, 128], BF16)
    bE = [P.alloc_ps(pf + "bE%d" % i, [128, 2, 128]) for i in range(2)]
    pA = [(bE[i][:, 0, :], bE[i]) for i in range(2)]
    pO = [(bE[i][:, 1, :], bE[i]) for i in range(2)]
    bF = P.alloc_ps(pf + "bF", [64, 2, 128])
    pK = [(bF[:, i, :], bF) for i in range(2)]
    ptb = P.alloc_ps(pf + "ptb", [128, GH, 128], BF16)
    cnt = [0]

    def tile_pass(tt, d, with_out, final):
        i = cnt[0] % 2
        cnt[0] += 1
        t0 = tt * 128
        buf = ld[i]; v_b = vb[i]
        rope = tt < NTL
        Mm = Mf if d == 0 else Mb
        Lm = Lf if d == 0 else Lb
        k.dma('sp', buf[:], ptm[t0:t0 + 128, 768:3072], writes=[buf])
        k.op('pool', lambda e: e.tensor_copy(out=v_b[:], in_=buf[:, 768:1536].rearrange("p (h d) -> p h d", h=GH)), reads=[buf], writes=[v_b])
        k.op('pe', lambda e: e.matmul(pz, lhsT=ga1T[:, t0:t0 + 128], rhs=w2b[:, d, :], start=True, stop=True), reads=[ga1T, w2b], writes=[pzk])
        k.op('act', lambda e: e.activation(out=ls_[:], in_=pz, func=AF.Exp, scale=-1.0), reads=[pzk], writes=[ls_])
        k.op('act', lambda e: e.activation(out=ls_[:], in_=ls_[:], func=AF.Ln, bias=one1[:]), reads=[ls_, one1], writes=[ls_])
        k.op('pe', lambda e: e.matmul(pc_, lhsT=Lm[:], rhs=ls_[:], start=True, stop=True), reads=[Lm, ls_], writes=[pck])
        for h in range(GH):
            k.op('pe', lambda e, h=h: e.matmul(pe_[:, h, :], lhsT=ls_[:, h * 64:(h + 1) * 64], rhs=ind[:], start=True, stop=True), reads=[ls_, ind], writes=[pek])
        k.op('act', lambda e: e.activation(out=ecl[:], in_=pe_, func=AF.Exp), reads=[pek], writes=[ecl])
        k.op('act', lambda e: e.activation(out=ecp[:], in_=pc_, func=AF.Exp), reads=[pck], writes=[ecp])
        k.op('act', lambda e: e.activation(out=ecn[:], in_=pc_, func=AF.Exp, scale=-1.0), reads=[pck], writes=[ecn])
        if rope:
            cs = cosT[:, tt, :, :]; sn = sinT[:, tt, :, :]
            for which in range(2):
                for h in range(GH):
                    o0 = which * 384 + h * 64
                    xv = buf[:, o0:o0 + 64].rearrange("p (hf two i) -> p hf two i", hf=2, two=2)
                    ov = qr[:, o0:o0 + 64].rearrange("p (hf two i) -> p hf two i", hf=2, two=2)
                    ea, eb = ('dve', 'pool') if (h % 2 == 0) else ('pool', 'dve')
                    k.op(ea, lambda e, xv=xv: e.tensor_tensor(out=rt[0][:], in0=xv[:, :, 0, :], in1=cs, op=ALU.mult), reads=[buf, cosT], writes=[rt[0]])
                    k.op(eb, lambda e, xv=xv: e.tensor_tensor(out=rt[1][:], in0=xv[:, :, 1, :], in1=sn, op=ALU.mult), reads=[buf, sinT], writes=[rt[1]])
                    k.op(ea, lambda e, ov=ov: e.tensor_tensor(out=ov[:, :, 0, :], in0=rt[0][:], in1=rt[1][:], op=ALU.subtract), reads=[rt[0], rt[1]], writes=[qr])
                    k.op(eb, lambda e, xv=xv: e.tensor_tensor(out=rt[2][:], in0=xv[:, :, 0, :], in1=sn, op=ALU.mult), reads=[buf, sinT], writes=[rt[2]])
                    k.op(ea, lambda e, xv=xv: e.tensor_tensor(out=rt[3][:], in0=xv[:, :, 1, :], in1=cs, op=ALU.mult), reads=[buf, cosT], writes=[rt[3]])
                    k.op(eb, lambda e, ov=ov: e.tensor_tensor(out=ov[:, :, 1, :], in0=rt[2][:], in1=rt[3][:], op=ALU.add), reads=[rt[2], rt[3]], writes=[qr])
            src = qr
        else:
            src = buf
        k.op('dve', lambda e: e.scalar_tensor_tensor(out=qkd[:, 0, :], in0=src[:, 0:384], scalar=qs, in1=ecp[:], op0=ALU.mult, op1=ALU.mult),
             reads=[src, ecp], writes=[qkd])
        k.op('pool', lambda e: e.tensor_tensor(out=qkd[:, 1, :], in0=src[:, 384:768], in1=ecn[:], op=ALU.mult), reads=[src, ecn], writes=[qkd])
        for w in range(2):
            for h in range(GH):
                k.op('pe', lambda e, w=w, h=h: e.transpose(out=(pTq if w == 0 else pTk)[:, h, :], in_=qkd[:, w, h * 64:(h + 1) * 64], identity=C['id_bf'][:]),
                     reads=[qkd, C['id_bf']], writes=[pTq if w == 0 else pTk])
        k.op('act', lambda e: e.copy(out=qdT[:], in_=pTq[:]), reads=[pTq], writes=[qdT])
        k.op('dve', lambda e: e.tensor_copy(out=kdT[:], in_=pTk[:]), reads=[pTk], writes=[kdT])
        for c in range(2):
            k.op('pool', lambda e, c=c: e.tensor_copy(out=qdz[:, :, c, 64 * c:64 * c + 64], in_=qdT[:, :, 64 * c:64 * c + 64]), reads=[qdT], writes=[qdz])
        Sd = S[d]
        order = (0, 1) if d == 0 else (1, 0)
        k.op('act', lambda e: e.copy(out=Sb[0][:], in_=Sd[:]), reads=[Sd], writes=[Sb[0]])
        for ci, c in enumerate(order):
            for h in range(GH):
                pk, pkk = pK[h % 2]
                k.op('pe', lambda e, c=c, h=h, pk=pk: e.matmul(pk, lhsT=qkd[64 * c:64 * c + 64, 1, h * 64:(h + 1) * 64], rhs=v_b[64 * c:64 * c + 64, h, :],
                                                              start=True, stop=True), reads=[qkd, v_b], writes=[pkk])
                k.op('dve', lambda e, c=c, h=h: e.tensor_scalar_mul(out=Stmp[:], in0=Sd[:, h, :], scalar1=ecl[:, h, c:c + 1]), reads=[Sd, ecl], writes=[Stmp])
                k.op('dve', lambda e, c=c, h=h, pk=pk: e.scalar_tensor_tensor(out=Sd[:, h, :], in0=pk, scalar=ecl[:, h, c:c + 1], in1=Stmp[:],
                                                                             op0=ALU.mult, op1=ALU.add), reads=[pkk, ecl, Stmp], writes=[Sd])
            if ci == 0 and with_out:
                k.op('act', lambda e: e.copy(out=Sb[1][:], in_=Sd[:]), reads=[Sd], writes=[Sb[1]])
        if not with_out:
            return
        for h in range(GH):
            (pa_, pak), (po, pok) = pA[h % 2], pO[h % 2]
            am = Am[h % 2]
            k.op('pe', lambda e, h=h, pa_=pa_: e.matmul(pa_, lhsT=kdT[:, h, :], rhs=qdT[:, h, :], start=True, stop=True), reads=[kdT, qdT], writes=[pak])
            k.op('dve', lambda e, pa_=pa_, am=am: e.tensor_tensor(out=am[:], in0=pa_, in1=Mm[:], op=ALU.mult), reads=[pak, Mm], writes=[am])
            k.op('pe', lambda e, h=h, po=po, am=am: e.matmul(po, lhsT=am[:], rhs=v_b[:, h, :], start=True, stop=False), reads=[am, v_b], writes=[pok])
            for ci, c in enumerate(order):
                k.op('pe', lambda e, h=h, po=po, c=c, ci=ci: e.matmul(po, lhsT=qdz[:, h, c, :], rhs=Sb[ci][:, h, :], start=False, stop=(ci == 1)),
                     reads=[qdz, Sb[ci]], writes=[pok])
            if not final:
                k.op('act', lambda e, h=h, po=po: e.copy(out=osb[:, h * 128:(h + 1) * 128], in_=po), reads=[pok], writes=[osb])
            else:
                k.op('dve', lambda e, h=h, po=po: e.tensor_tensor(out=osb[:, h * 128:(h + 1) * 128], in0=po, in1=ofl[:, h * 128:(h + 1) * 128], op=ALU.add),
                     reads=[pok, ofl], writes=[osb])
        if not final:
            k.dma('sp', ofs[t0:t0 + 128, :], osb[:], reads=[osb], writes=[ofs])
            return
        o3 = osb[:, :].rearrange("p (h d) -> p h d", h=GH)
        k.op('pool', lambda e: e.tensor_tensor(out=sq[:], in0=o3, in1=o3, op=ALU.mult), reads=[osb], writes=[sq])
        k.op('dve', lambda e: e.tensor_reduce(out=ss[:, 0, :], in_=sq[:], axis=AX.X, op=ALU.add), reads=[sq], writes=[ss])
        k.op('dve', lambda e: e.tensor_scalar(out=ss[:, 1, :], in0=ss[:, 0, :], scalar1=1.0 / 128, scalar2=EPS, op0=ALU.mult, op1=ALU.add), reads=[ss], writes=[ss])
        k.op('act', lambda e: e.activation(out=ss[:, 1, :], in_=ss[:, 1, :], func=AF.Ln), reads=[ss], writes=[ss])
        k.op('act', lambda e: e.activation(out=ss[:, 1, :], in_=ss[:, 1, :], func=AF.Exp, scale=-0.5), reads=[ss], writes=[ss])
        k.op('act', lambda e: e.activation(out=sg[:], in_=buf[:, 1536:2304], func=AF.Silu), reads=[buf], writes=[sg])
        for h in range(GH):
            k.op('dve', lambda e, h=h: e.tensor_scalar_mul(out=osb[:, h * 128:(h + 1) * 128], in0=osb[:, h * 128:(h + 1) * 128], scalar1=ss[:, 1, h:h + 1]),
                 reads=[osb, ss], writes=[osb])
        k.op('pool', lambda e: e.tensor_tensor(out=sg[:], in0=sg[:], in1=gnw[:], op=ALU.mult), reads=[sg, gnw], writes=[sg])
        k.op('dve', lambda e: e.tensor_tensor(out=yb[:], in0=osb[:], in1=sg[:], op=ALU.mult), reads=[osb, sg], writes=[yb])
        ms_ = mst[tt % 2]
        for h in range(GH):
            k.op('pe', lambda e, h=h: e.transpose(out=ptb[:, h, :], in_=yb[:, h * 128:(h + 1) * 128], identity=C['id_bf'][:]), reads=[yb, C['id_bf']], writes=[ptb])
        k.op('act', lambda e, ms_=ms_: e.copy(out=ms_[:], in_=ptb[:]), reads=[ptb], writes=[ms_])
        k.dma('sp', mixT[1280:2048, t0:t0 + 128].rearrange("(j p) t -> p j t", p=128), ms_[:], reads=[ms_], writes=[mixT])

    k.op('pool', lambda e: e.memset(S[0][:], 0.0), writes=[S[0]])
    k.op('pool', lambda e: e.memset(S[1][:], 0.0), writes=[S[1]])
    for tt in (NTL, NTL + 1):
        tile_pass(tt, 0, with_ctx, False)
    for tt in range(NTL):
        tile_pass(tt, 0, True, False)
    for tt in (NTL + 1, NTL):
        if with_ctx:
            k.dma('sp', ofl[:], ofs[tt * 128:(tt + 1) * 128, :], reads=[ofs], writes=[ofl])
        tile_pass(tt, 1, with_ctx, True)
    for tt in range(NTL - 1, -1, -1):
        k.dma('sp', ofl[:], ofs[tt * 128:(tt + 1) * 128, :], reads=[ofs], writes=[ofl])
        tile_pass(tt, 1, True, True)
    P.release(m0)


def stage_post(P, C, l, W, mp, xT, mixT, x1T, h2f_d, h2b_d, ntok):
    nc, k = P.nc, P.k
    pf = "po_"
    m0 = P.mark()
    NB = 256
    wo = P.alloc_sb(pf + "wo", [128, NCH, D], BF16)
    wv = W['w_out'][l].rearrange("(kc p) c -> p kc c", p=128)
    for q in range(4):
        k.dma('pool', wo[:, :, q * 512:(q + 1) * 512], wv[:, :, q * 512:(q + 1) * 512], writes=[wo])
    lnp = P.alloc_sb(pf + "lnp", [128, 4, NCH], F32)
    k.dma('sp', lnp[:], W['lnT'][l])
    tmp = ln_tmp(P, pf + "ln", NB)
    mxb = [P.alloc_sb(pf + "mxb%d" % i, [128, NCH, NB], BF16) for i in range(2)]
    xt = [P.alloc_sb(pf + "xt%d" % i, [128, NCH, NB], F32) for i in range(2)]
    x1t = P.alloc_sb(pf + "x1t", [128, NCH, NB], F32)
    pss = [P.alloc_ps(pf + "ps%d" % i, [128, NB]) for i in range(2)]
    tq = [P.alloc_sb(pf + "tq%d" % i, [128, NB], F32) for i in range(2)]
    mxv = mixT.rearrange("(c p) t -> p c t", p=128)
    xv = xT.rearrange("(c p) t -> p c t", p=128)
    x1v = x1T.rearrange("(c p) t -> p c t", p=128)
    hfv = h2f_d.rearrange("(c p) t -> p c t", p=128)
    hbv = h2b_d.rearrange("(c p) t -> p c t", p=128)
    for bi, t0 in enumerate(range(0, ntok, NB)):
        r = 0 if t0 < L else 1
        mb = mxb[bi % 2]; x_ = xt[bi % 2]
        k.dma('sp', mb[:], mxv[:, :, t0:t0 + NB], writes=[mb])
        k.dma('act', x_[:], xv[:, :, t0:t0 + NB], writes=[x_])
        for dc in range(NCH):
            pst = pss[dc % 2]; tt = tq[dc % 2]
            for kc in range(NCH):
                k.op('pe', lambda e, kc=kc, dc=dc, pst=pst: e.matmul(pst[:, :], lhsT=wo[:, kc, dc * 128:(dc + 1) * 128], rhs=mb[:, kc, :],
                                                                    start=(kc == 0), stop=(kc == NCH - 1)), reads=[wo, mb], writes=[pst])
            k.op('act', lambda e, dc=dc, pst=pst, tt=tt: e.activation(out=tt[:], in_=pst[:, :], func=AF.Copy, scale=mp['g1'][:, dc, r:r + 1]),
                 reads=[pst, mp['g1']], writes=[tt])
            k.op('dve', lambda e, dc=dc, tt=tt: e.scalar_tensor_tensor(out=x_[:, dc, :], in0=x_[:, dc, :], scalar=ALPHA, in1=tt[:], op0=ALU.mult, op1=ALU.add),
                 reads=[x_, tt], writes=[x_])
        ln_block(P, C, tmp, x_, NB,
                 lambda c: (x1t[:, c, :], [x1t]),
                 lambda c: (lnp[:, 0, c:c + 1], [lnp]),
                 lambda c: (lnp[:, 1, c:c + 1], [lnp]))
        k.dma('sp', x1v[:, :, t0:t0 + NB], x1t[:], reads=[x1t], writes=[x1T])
        ln_block(P, C, tmp, x1t, NB,
                 lambda c: (x_[:, c, :], [x_]),
                 lambda c: (mp['sc2p'][:, c, r:r + 1], [mp['sc2p']]),
                 lambda c: (mp['sh2'][:, c, r:r + 1], [mp['sh2']]))
        k.dma('sp', hfv[:, :, t0:t0 + NB], x_[:], reads=[x_], writes=[h2f_d])
        k.dma('pool', hbv[:, :, t0:t0 + NB], x_[:], reads=[x_], writes=[h2b_d])
    P.release(m0)


def stage_moe(P, C, l, W, mp, x1T, h2f_d, h2b_d, outT, ntok):
    nc, k = P.nc, P.k
    pf = "mo_"
    m0 = P.mark()
    NT_ = ntok // 128
    gate = P.alloc_sb(pf + "gate", [128, NT_, NE], F32)
    m1 = P.mark()
    wr = P.alloc_sb(pf + "wr", [128, NCH, 36], F32)
    k.dma('sp', wr[:, :, 0:4], W['w_rg'][l].rearrange("(kc p) c -> p kc c", p=128), writes=[wr])
    k.dma('sp', wr[:, :, 4:36], W['w_re'][l].rearrange("(kc p) c -> p kc c", p=128), writes=[wr])
    br = P.alloc_sb(pf + "br", [128, 36], F32)
    k.dma('sp', br[:, 0:4], W['b_rg'][l:l + 1, :].partition_broadcast(128), writes=[br])
    k.dma('sp', br[:, 4:36], W['b_re'][l:l + 1, :].partition_broadcast(128), writes=[br])
    hf = [P.alloc_sb(pf + "hf%d" % i, [128, NCH, 128], F32) for i in range(2)]
    pl = [P.alloc_ps(pf + "pl%d" % i, [128, 36]) for i in range(2)]
    lg = P.alloc_sb(pf + "lg", [128, 36], F32)
    sm = P.alloc_sb(pf + "sm", [128, 16], F32)
    gs = P.alloc_sb(pf + "gs", [128, 4], F32)
    eg = P.alloc_sb(pf + "eg", [128, 4], F32)
    les = P.alloc_sb(pf + "les", [128, 8], F32)
    le2 = P.alloc_sb(pf + "le2", [128, 8], F32)
    mk1 = P.alloc_sb(pf + "mk1", [128, 8], F32)
    mk2 = P.alloc_sb(pf + "mk2", [128, 8], F32)
    egt = P.alloc_sb(pf + "egt", [128, 8], F32)
    hfv = h2f_d.rearrange("(c p) t -> p c t", p=128)
    for tt in range(NT_):
        h_ = hf[tt % 2]; pst = pl[tt % 2]
        k.dma('sp', h_[:], hfv[:, :, tt * 128:(tt + 1) * 128], writes=[h_])
        for kc in range(NCH):
            k.op('pe', lambda e, kc=kc, h_=h_, pst=pst: e.matmul(pst[:, :], lhsT=h_[:, kc, :], rhs=wr[:, kc, :], start=(kc == 0), stop=(kc == NCH - 1)),
                 reads=[h_, wr], writes=[pst])
        k.op('dve', lambda e, pst=pst: e.tensor_tensor(out=lg[:], in0=pst[:, :], in1=br[:], op=ALU.add), reads=[pst, br], writes=[lg])
        k.op('dve', lambda e: e.tensor_reduce(out=sm[:, 0:1], in_=lg[:, 0:4], axis=AX.X, op=ALU.max), reads=[lg], writes=[sm])
        k.op('dve', lambda e: e.tensor_scalar_mul(out=sm[:, 1:2], in0=sm[:, 0:1], scalar1=-1.0), reads=[sm], writes=[sm])
        k.op('act', lambda e: e.activation(out=eg[:], in_=lg[:, 0:4], func=AF.Exp, bias=sm[:, 1:2], accum_out=sm[:, 2:3]), reads=[lg, sm], writes=[eg, sm])
        k.op('dve', lambda e: e.reciprocal(out=sm[:, 3:4], in_=sm[:, 2:3]), reads=[sm], writes=[sm])
        k.op('dve', lambda e: e.tensor_scalar(out=gs[:], in0=lg[:, 0:4], scalar1=sm[:, 0:1], scalar2=None, op0=ALU.is_ge), reads=[lg, sm], writes=[gs])
        k.op('dve', lambda e: e.tensor_scalar_mul(out=les[:], in0=lg[:, 4:12], scalar1=gs[:, 0:1]), reads=[lg, gs], writes=[les])
        for g in range(1, 4):
            k.op('dve', lambda e, g=g: e.scalar_tensor_tensor(out=les[:], in0=lg[:, 4 + 8 * g:12 + 8 * g], scalar=gs[:, g:g + 1], in1=les[:],
                                                             op0=ALU.mult, op1=ALU.add), reads=[lg, gs, les], writes=[les])
        k.op('dve', lambda e: e.tensor_reduce(out=sm[:, 4:5], in_=les[:], axis=AX.X, op=ALU.max), reads=[les], writes=[sm])
        k.op('dve', lambda e: e.tensor_scalar(out=mk1[:], in0=les[:], scalar1=sm[:, 4:5], scalar2=None, op0=ALU.is_ge), reads=[les, sm], writes=[mk1])
        k.op('dve', lambda e: e.scalar_tensor_tensor(out=le2[:], in0=mk1[:], scalar=-1.0e9, in1=les[:], op0=ALU.mult, op1=ALU.add),
             reads=[mk1, les], writes=[le2])
        k.op('dve', lambda e: e.tensor_reduce(out=sm[:, 5:6], in_=le2[:], axis=AX.X, op=ALU.max), reads=[le2], writes=[sm])
        k.op('dve', lambda e: e.tensor_scalar(out=mk2[:], in0=le2[:], scalar1=sm[:, 5:6], scalar2=None, op0=ALU.is_ge), reads=[le2, sm], writes=[mk2])
        k.op('dve', lambda e: e.tensor_tensor(out=sm[:, 6:7], in0=sm[:, 5:6], in1=sm[:, 4:5], op=ALU.subtract), reads=[sm], writes=[sm])
        k.op('act', lambda e: e.activation(out=sm[:, 7:8], in_=sm[:, 6:7], func=AF.Exp), reads=[sm], writes=[sm])
        k.op('dve', lambda e: e.tensor_scalar_add(out=sm[:, 8:9], in0=sm[:, 7:8], scalar1=1.0), reads=[sm], writes=[sm])
        k.op('dve', lambda e: e.reciprocal(out=sm[:, 9:10], in_=sm[:, 8:9]), reads=[sm], writes=[sm])
        k.op('dve', lambda e: e.tensor_tensor(out=sm[:, 10:11], in0=sm[:, 7:8], in1=sm[:, 9:10], op=ALU.mult), reads=[sm], writes=[sm])
        k.op('dve', lambda e: e.tensor_tensor(out=sm[:, 11:12], in0=sm[:, 9:10], in1=sm[:, 3:4], op=ALU.mult), reads=[sm], writes=[sm])
        k.op('dve', lambda e: e.tensor_tensor(out=sm[:, 12:13], in0=sm[:, 10:11], in1=sm[:, 3:4], op=ALU.mult), reads=[sm], writes=[sm])
        k.op('dve', lambda e: e.tensor_scalar_mul(out=egt[:], in0=mk1[:], scalar1=sm[:, 11:12]), reads=[mk1, sm], writes=[egt])
        k.op('dve', lambda e: e.scalar_tensor_tensor(out=egt[:], in0=mk2[:], scalar=sm[:, 12:13], in1=egt[:], op0=ALU.mult, op1=ALU.add),
             reads=[mk2, sm, egt], writes=[egt])
        for g in range(4):
            k.op('dve', lambda e, g=g, tt=tt: e.tensor_scalar_mul(out=gate[:, tt, 8 * g:8 * g + 8], in0=egt[:], scalar1=gs[:, g:g + 1]),
                 reads=[egt, gs], writes=[gate])
    P.dump('moe_gate', gate)
    P.release(m1)
    PASS = 1024 if ntok in (L, L // 2) else 768
    BLK = 384
    acc = P.alloc_sb(pf + "acc", [128, PASS // 128, D], F32)
    lnp = P.alloc_sb(pf + "lnp", [128, 4, NCH], F32)
    k.dma('sp', lnp[:], W['lnT'][l])
    hbv = h2b_d.rearrange("(c p) t -> p c t", p=128)
    x1v = x1T.rearrange("(c p) t -> p c t", p=128)
    ov = outT.rearrange("(c p) t -> p c t", p=128)
    ei = 0
    oi = 0
    for p0 in range(0, ntok, PASS):
        pn = min(PASS, ntok - p0)
        mE = P.mark()
        hT = P.alloc_sb(pf + "hT", [128, NCH, PASS], BF16)
        wu = [P.alloc_sb(pf + "wu%d" % i, [128, NCH, 1024], BF16) for i in range(2)]
        wd = [P.alloc_sb(pf + "wd%d" % i, [128, 4, D], BF16) for i in range(2)]
        actT = [P.alloc_sb(pf + "actT%d" % i, [128, 4, BLK], BF16) for i in range(2)]
        sgb = [P.alloc_sb(pf + "sg%d" % i, [128, BLK], F32) for i in range(2)]
        pg = [P.alloc_ps(pf + "pg%d" % i, [128, BLK]) for i in range(2)]
        pu = [P.alloc_ps(pf + "pu%d" % i, [128, BLK]) for i in range(2)]
        po = [P.alloc_ps(pf + "po%d" % i, [128, 512]) for i in range(4)]
        k.dma('sp', hT[:, :, :pn], hbv[:, :, p0:p0 + pn], writes=[hT])
        k.op('pool', lambda e: e.memset(acc[:], 0.0), writes=[acc])
        for ex in range(NE):
            g_, e_ = ex // 8, ex % 8
            wu_ = wu[ei % 2]; wd_ = wd[ei % 2]
            ei += 1
            k.dma('pool', wu_[:], W['w_up'][l, g_, e_].rearrange("(kc p) c -> p kc c", p=128), writes=[wu_])
            k.dma('pool', wd_[:], W['w_down'][l, g_, e_].rearrange("(j p) c -> p j c", p=128), writes=[wd_])
            for b0 in range(0, pn, BLK):
                bn = min(BLK, pn - b0)
                at = actT[(b0 // BLK) % 2]
                for j in range(4):
                    pg_ = pg[j % 2]; pu_ = pu[j % 2]; sg_ = sgb[j % 2]
                    for kc in range(NCH):
                        k.op('pe', lambda e, kc=kc, j=j, pg_=pg_: e.matmul(pg_[:, :bn], lhsT=wu_[:, kc, j * 128:(j + 1) * 128], rhs=hT[:, kc, b0:b0 + bn],
                                                                          start=(kc == 0), stop=(kc == NCH - 1)), reads=[wu_, hT], writes=[pg_])
                    for kc in range(NCH):
                        k.op('pe', lambda e, kc=kc, j=j, pu_=pu_: e.matmul(pu_[:, :bn], lhsT=wu_[:, kc, 512 + j * 128:512 + (j + 1) * 128], rhs=hT[:, kc, b0:b0 + bn],
                                                                          start=(kc == 0), stop=(kc == NCH - 1)), reads=[wu_, hT], writes=[pu_])
                    k.op('act', lambda e, pg_=pg_, sg_=sg_: e.activation(out=sg_[:, :bn], in_=pg_[:, :bn], func=AF.Silu), reads=[pg_], writes=[sg_])
                    k.op('dve', lambda e, j=j, pu_=pu_, sg_=sg_, at=at: e.tensor_tensor(out=at[:, j, :bn], in0=pu_[:, :bn], in1=sg_[:, :bn], op=ALU.mult),
                         reads=[pu_, sg_], writes=[at])
                for t3 in range(bn // 128):
                    tl = (b0 // 128) + t3
                    tg = (p0 // 128) + tl
                    for dh in range(4):
                        po_ = po[oi % 4]
                        oi += 1
                        for j in range(4):
                            k.op('pe', lambda e, j=j, dh=dh, po_=po_, t3=t3: e.matmul(po_[:, :], lhsT=at[:, j, t3 * 128:(t3 + 1) * 128], rhs=wd_[:, j, dh * 512:(dh + 1) * 512],
                                                                                   start=(j == 0), stop=(j == 3)), reads=[at, wd_], writes=[po_])
                        k.op('dve', lambda e, dh=dh, po_=po_, tl=tl, tg=tg, ex=ex: e.scalar_tensor_tensor(out=acc[:, tl, dh * 512:(dh + 1) * 512], in0=po_[:, :],
                                                                                                        scalar=gate[:, tg, ex:ex + 1], in1=acc[:, tl, dh * 512:(dh + 1) * 512],
                                                                                                        op0=ALU.mult, op1=ALU.add), reads=[po_, gate, acc], writes=[acc])
        P.release(mE)
        m2 = P.mark()
        tmp = ln_tmp(P, pf + "ln", BLK)
        vt = P.alloc_sb(pf + "vt", [128, NCH, BLK], F32)
        x1b = P.alloc_sb(pf + "x1b", [128, NCH, BLK], F32)
        ot = P.alloc_sb(pf + "ot", [128, NCH, BLK], F32)
        ptf = [P.alloc_ps(pf + "ptf%d" % i, [128, 512]) for i in range(2)]
        tq = [P.alloc_sb(pf + "tq%d" % i, [128, 128], F32) for i in range(2)]
        for b0 in range(0, pn, BLK):
            bn = min(BLK, pn - b0)
            r = 0 if (p0 + b0) < L else 1
            k.dma('sp', x1b[:, :, :bn], x1v[:, :, p0 + b0:p0 + b0 + bn], writes=[x1b])
            for t3 in range(bn // 128):
                tl = (b0 // 128) + t3
                r = 0 if (p0 + b0 + t3 * 128) < L else 1
                for c in range(NCH):
                    pt_ = ptf[c % 2]; tq_ = tq[c % 2]
                    k.op('pe', lambda e, c=c, tl=tl, pt_=pt_: e.transpose(out=pt_[:, :128], in_=acc[:, tl, c * 128:(c + 1) * 128], identity=C['id_f'][:]),
                         reads=[acc, C['id_f']], writes=[pt_])
                    k.op('act', lambda e, c=c, pt_=pt_, tq_=tq_: e.activation(out=tq_[:], in_=pt_[:, :128], func=AF.Copy, scale=mp['g2'][:, c, r:r + 1]),
                         reads=[pt_, mp['g2']], writes=[tq_])
                    k.op('dve', lambda e, c=c, t3=t3, tq_=tq_: e.scalar_tensor_tensor(out=vt[:, c, t3 * 128:(t3 + 1) * 128], in0=x1b[:, c, t3 * 128:(t3 + 1) * 128],
                                                                                      scalar=ALPHA, in1=tq_[:], op0=ALU.mult, op1=ALU.add), reads=[x1b, tq_], writes=[vt])
            ln_block(P, C, tmp, vt, bn,
                     lambda c: (ot[:, c, :bn], [ot]),
                     lambda c: (lnp[:, 2, c:c + 1], [lnp]),
                     lambda c: (lnp[:, 3, c:c + 1], [lnp]))
            k.dma('sp', ov[:, :, p0 + b0:p0 + b0 + bn], ot[:, :, :bn], reads=[ot], writes=[outT])
        P.release(m2)
    P.release(m0)


def stage_select(P, selT, items):
    nc, k = P.nc, P.k
    m0 = P.mark()
    HALF = L // 2
    sel = P.alloc_sb("sel", [128, 1], F32)
    k.dma('sp', sel[:], selT)
    A = [P.alloc_sb("selA%d" % i, [128, NCH, 256], F32) for i in range(2)]
    B = [P.alloc_sb("selB%d" % i, [128, NCH, 256], F32) for i in range(2)]
    n = 0
    for (src, dst, dstb) in items:
        sv = src.rearrange("(c p) t -> p c t", p=128)
        dv = dst.rearrange("(c p) t -> p c t", p=128)
        for t0 in range(0, HALF, 256):
            a_, b_ = A[n % 2], B[n % 2]
            n += 1
            k.dma('sp', a_[:], sv[:, :, t0:t0 + 256], writes=[a_])
            k.dma('act', b_[:], sv[:, :, HALF + t0:HALF + t0 + 256], writes=[b_])
            k.op('dve', lambda e, a_=a_, b_=b_: e.tensor_tensor(out=b_[:], in0=b_[:], in1=a_[:], op=ALU.subtract), reads=[a_, b_], writes=[b_])
            k.op('dve', lambda e, a_=a_, b_=b_: e.scalar_tensor_tensor(out=a_[:], in0=b_[:], scalar=sel[:, 0:1], in1=a_[:], op0=ALU.mult, op1=ALU.add),
                 reads=[a_, b_, sel], writes=[a_])
            k.dma('sp', dv[:, :, t0:t0 + 256], a_[:], reads=[a_], writes=[dst])
            if dstb is not None:
                k.dma('pool', dstb.rearrange("(c p) t -> p c t", p=128)[:, :, t0:t0 + 256], a_[:], reads=[a_], writes=[dstb])
    P.release(m0)


W_SHAPES = {
    'hy_f_w1': [DEPTH, 33, 64], 'hy_f_w2': [DEPTH, 64, 64], 'hy_f_w3': [DEPTH, 64, 1024], 'hy_fv': [DEPTH, 64, 3],
    'hy_bias': [DEPTH, 512], 'hy_sw': [DEPTH, 128, 12, 4], 'hy_norm_wT': [DEPTH, 128, 4],
    'na_rpb': [DEPTH, 6, 15, 31], 'na_norm_wT': [DEPTH, 128, 6],
    'gla_a_w2': [DEPTH, 2, 16, 384], 'gla_a_b': [DEPTH, 2, 384], 'gla_norm_w': [DEPTH, 128],
    'w_out': [DEPTH, D, D], 'lnT': [DEPTH, 128, 4, NCH],
    'w_rg': [DEPTH, D, 4], 'b_rg': [DEPTH, 4], 'w_re': [DEPTH, D, 32], 'b_re': [DEPTH, 32],
    'w_up': [DEPTH, 4, 8, D, 1024], 'w_down': [DEPTH, 4, 8, DE, D],
    'w_ada': [DEPTH, D, 6 * D], 'b_adaT': [DEPTH, 128, 96], 'w_in': [DEPTH, D, INC],
}


def host_layout(inp, layers=(0, 1)):
    ls = list(layers)
    n = len(ls)
    W = {}
    for nm in ('hy_f_w1', 'hy_f_w2', 'hy_f_w3', 'hy_bias', 'na_rpb', 'gla_a_w2', 'gla_a_b', 'gla_norm_w', 'w_out', 'w_rg', 'b_rg',
               'w_re', 'b_re', 'w_up', 'w_down', 'w_ada', 'w_in'):
        W[nm] = np.ascontiguousarray(inp[nm][ls])
    W['hy_fv'] = np.ascontiguousarray(np.stack([inp['hy_f_b1'][ls], inp['hy_f_b2'][ls], inp['hy_sin_freq'][ls]], -1))
    sw = np.concatenate([inp['hy_short_w'][ls], inp['hy_short_b'][ls][:, None, :]], 1)
    W['hy_sw'] = np.ascontiguousarray(sw.reshape(n, 4, 12, 128).transpose(0, 3, 2, 1))
    W['hy_norm_wT'] = np.ascontiguousarray(inp['hy_norm_w'][ls].reshape(n, 4, 128).transpose(0, 2, 1))
    W['na_norm_wT'] = np.ascontiguousarray(inp['na_norm_w'][ls].reshape(n, 6, 128).transpose(0, 2, 1))
    lnT = np.stack([inp['ln1_g'][ls], inp['ln1_b'][ls], inp['ln2_g'][ls], inp['ln2_b'][ls]], 1)
    W['lnT'] = np.ascontiguousarray(lnT.reshape(n, 4, NCH, 128).transpose(0, 3, 1, 2))
    W['b_adaT'] = np.ascontiguousarray(inp['b_ada'][ls].reshape(n, 96, 128).transpose(0, 2, 1))
    return W


def build(layers=(0, 1), dbg=(), upto=None):
    P = Prog(dbg=dbg)
    nl = len(layers)
    W = {}
    for nm, shp in W_SHAPES.items():
        W[nm] = P.inp(nm, [nl] + shp[1:])
    xT_in = P.inp("xT", [D, T])
    cT = P.inp("cT", [128, NCH, 2])
    outT = P.outp("outT", [D, L // 2])
    selT = P.inp("selT", [128, 1])
    x1S = P.scratch("x1S", [D, L // 2])
    h2fS = P.scratch("h2fS", [D, L // 2])
    h2bS = P.scratch("h2bS", [D, L // 2], BF16)
    pfm = P.scratch("pfm", [3104, T])
    ptm = P.scratch("ptm", [T, 3072])
    mixT = P.scratch("mixT", [D, T], BF16)
    rpbp = P.scratch("rpbp", [6, 15, 160])
    ofs = P.scratch("ofs", [T, 768])
    x1T = P.scratch("x1T", [D, T])
    h2f = P.scratch("h2f", [D, T])
    h2b = P.scratch("h2b", [D, T], BF16)
    xmid = P.scratch("xmid", [D, T])
    C = make_consts(P)
    modT = sb(P.nc, "modT", [128, 96, 2], F32)
    for li in range(nl):
        last = (li == nl - 1)
        xin = xT_in if li == 0 else xmid
        xout = outT if last else xmid
        ntok = L if last else T
        mk = P.mark()
        stage_mod(P, li, cT, W['w_ada'], W['b_adaT'], modT)
        mp = load_mod(P, modT)
        stage_inproj(P, C, li, xin, W['w_in'], mp, pfm, ptm)
        if upto == 'inproj':
            break
        stage_hyena(P, C, li, W, pfm, mixT, 0, L)
        if not last:
            stage_hyena(P, C, li, W, pfm, mixT, L, LC)
        stage_na(P, C, li, W, pfm, ptm, mixT, rpbp, not last)
        stage_gla(P, C, li, W, pfm, ptm, mixT, ofs, not last)
        if upto == 'mix':
            break
        stage_post(P, C, li, W, mp, xin, mixT, x1T, h2f, h2b, ntok)
        if upto == 'post':
            break
        if last:
            stage_select(P, selT, [(x1T, x1S, None), (h2f, h2fS, h2bS)])
            stage_moe(P, C, li, W, mp, x1S, h2fS, h2bS, xout, L // 2)
        else:
            stage_moe(P, C, li, W, mp, x1T, h2f, h2b, xout, ntok)
        P.release(mk)
    P.k.finish()
    return P


def kernel(**inputs):
    inp = {k_: np.asarray(v) for k_, v in inputs.items()}
    Wn = host_layout(inp)
    P = build()
    in_maps = []
    for core in range(8):
        b = core % 4
        m = dict(Wn)
        m['xT'] = np.ascontiguousarray(np.concatenate([inp['x'][b], inp['ctx'][b]], 0).T)
        m['cT'] = np.ascontiguousarray(np.stack([inp['c'][b], inp['c_ctx']], -1).reshape(NCH, 128, 2).transpose(1, 0, 2))
        m['selT'] = np.full((128, 1), float(core // 4), np.float32)
        in_maps.append(m)
    res = run_bass_kernel_spmd(P.nc, in_maps, core_ids=list(range(8)))
    out = np.stack([np.concatenate([res.results[b]["outT"].T, res.results[b + 4]["outT"].T], 0) for b in range(4)], 0)
    return out.astype(np.float32)
```

```python
import math
import numpy as np
import concourse.bass as bass
import concourse.mybir as mybir
from concourse.bass_utils import run_bass_kernel_spmd

F32 = mybir.dt.float32
BF16 = mybir.dt.bfloat16
I32 = mybir.dt.int32
AF = mybir.ActivationFunctionType
ALU = mybir.AluOpType
AX = mybir.AxisListType

D = 2048
L = 2048
LC = 256
T = L + LC
NCH = D // 128
DEPTH = 2
INC = 6176
HY = 512
NAH = 6
GH = 6
GDK = 64
EPS = 1e-6
ALPHA = (2 * DEPTH) ** 0.25
NE = 32
DE = 512

O_HY, O_NQ, O_NK, O_NV, O_GQ, O_GK, O_GV, O_GG, O_GA = 0, 1536, 2304, 3072, 3840, 4224, 4608, 5376, 6144


class K:
    NDMA = 24

    def __init__(self, nc):
        self.nc = nc
        self.eng = {'pe': nc.tensor, 'act': nc.scalar, 'dve': nc.vector, 'pool': nc.gpsimd, 'sp': nc.sync}
        self.sem = {e: nc.semaphore("s_" + e).__enter__() for e in ('pe', 'act', 'dve', 'pool')}
        self.cnt = {e: 0 for e in self.sem}
        self.dsem = [nc.semaphore("d%d" % i).__enter__() for i in range(self.NDMA)]
        self.dcnt = [0] * self.NDMA
        self.dnext = 0
        self.seen = {e: {} for e in self.eng}
        self.lastw = {}
        self.readers = {}
        self.nins = 0

    @staticmethod
    def key(x):
        if isinstance(x, (str, tuple)):
            return x
        return x.tensor.name if hasattr(x, 'tensor') else x.name

    def _wait(self, e, tok):
        if tok is None:
            return
        sem, val, src = tok
        if src == e and e == 'pe':
            return
        if self.seen[e].get(id(sem), 0) >= val:
            return
        self.eng[e].wait_ge(sem, val)
        self.seen[e][id(sem)] = val

    def _deps(self, e, reads, writes):
        for r in reads:
            self._wait(e, self.lastw.get(r))
        for w in writes:
            self._wait(e, self.lastw.get(w))
            for tok in self.readers.get(w, {}).values():
                self._wait(e, tok)

    def _record(self, tok, reads, writes):
        for w in writes:
            self.lastw[w] = tok
            self.readers[w] = {}
        for r in reads:
            if r in writes:
                continue
            self.readers.setdefault(r, {})[id(tok[0])] = tok

    def op(self, e, fn, reads=(), writes=()):
        reads = [self.key(r) for r in reads]
        writes = [self.key(w) for w in writes]
        self._deps(e, reads, writes)
        ins = fn(self.eng[e])
        self.cnt[e] += 1
        ins.then_inc(self.sem[e], 1)
        self._record((self.sem[e], self.cnt[e], e), reads, writes)
        self.nins += 1
        return ins

    def dma(self, e, out, in_, reads=None, writes=None, **kw):
        reads = [self.key(r) for r in (reads if reads is not None else [in_])]
        writes = [self.key(w) for w in (writes if writes is not None else [out])]
        self._deps(e, reads, writes)
        i = self.dnext
        self.dnext = (self.dnext + 1) % self.NDMA
        sem = self.dsem[i]
        self._wait(e, (sem, self.dcnt[i], 'dma'))
        self.eng[e].dma_start(out=out, in_=in_, **kw).then_inc(sem, 16)
        self.dcnt[i] += 16
        self._record((sem, self.dcnt[i], 'dma'), reads, writes)
        self.nins += 1

    def barrier(self):
        toks = [(self.sem[e], self.cnt[e], e) for e in self.sem if self.cnt[e] > 0]
        toks += [(self.dsem[i], self.dcnt[i], 'dma') for i in range(self.NDMA) if self.dcnt[i] > 0]
        for e in self.eng:
            for t in toks:
                if t[2] == e:
                    continue
                self._wait(e, t)
        self.lastw.clear()
        self.readers.clear()

    def finish(self):
        self.barrier()


def sb(nc, name, shape, dt):
    return nc.sbuf_tensor(name, list(shape), dt).__enter__()


def ps(nc, name, shape, dt=F32):
    return nc.psum_tensor(name, list(shape), dt).__enter__()


class Prog:
    def __init__(self, dbg=()):
        self.nc = nc = bass.Bass("TRN2", target_bir_lowering=False)
        self.k = K(nc)
        self.dbg = set(dbg)
        self.dram = {}
        self._ctx = []

    def inp(self, name, shape, dt=F32):
        t = self.nc.dram_tensor(name, list(shape), dt, kind="ExternalInput")
        self.dram[name] = t
        return t.ap()

    def outp(self, name, shape, dt=F32):
        t = self.nc.dram_tensor(name, list(shape), dt, kind="ExternalOutput")
        self.dram[name] = t
        return t.ap()

    def scratch(self, name, shape, dt=F32):
        kind = "ExternalOutput" if name in self.dbg else "Internal"
        t = self.nc.dram_tensor(name, list(shape), dt, kind=kind)
        self.dram[name] = t
        return t.ap()

    def alloc_sb(self, name, shape, dt):
        self._uid = getattr(self, '_uid', 0) + 1
        name = "%s_u%d" % (name, self._uid)
        g = self.nc.sbuf_tensor(name, list(shape), dt)
        t = g.__enter__()
        self._ctx.append(g)
        return t

    def alloc_ps(self, name, shape, dt=F32):
        self._uid = getattr(self, '_uid', 0) + 1
        name = "%s_u%d" % (name, self._uid)
        g = self.nc.psum_tensor(name, list(shape), dt)
        t = g.__enter__()
        self._ctx.append(g)
        return t

    def dump(self, name, tile, dt=F32):
        if name in self.dbg:
            o = self.outp("dbg_" + name, list(tile.shape), dt)
            self.k.dma('sp', o, tile[:], reads=[tile], writes=["dbg_" + name])

    def mark(self):
        return len(self._ctx)

    def release(self, mark):
        self.k.barrier()
        while len(self._ctx) > mark:
            self._ctx.pop().__exit__(None, None, None)


def stage_mod(P, l, cT, w_ada, b_adaT, modT):
    nc, k = P.nc, P.k
    m = P.mark()
    c_sb = P.alloc_sb("mod_c", [128, NCH, 2], F32)
    sc = P.alloc_sb("mod_silu", [128, NCH, 2], F32)
    bsb = P.alloc_sb("mod_b", [128, 96], F32)
    slabs = [P.alloc_sb("mod_w%d" % i, [128, NCH, 512], F32) for i in range(2)]
    pss = [P.alloc_ps("mod_ps%d" % i, [128, 4, 2]) for i in range(2)]
    k.dma('sp', c_sb[:], cT)
    k.dma('sp', bsb[:], b_adaT[l])
    k.op('act', lambda e: e.activation(out=sc[:], in_=c_sb[:], func=AF.Silu), reads=[c_sb], writes=[sc])
    wv = w_ada[l].rearrange("(kc p) c -> p kc c", p=128)
    for cs in range(24):
        slab = slabs[cs % 2]
        pst = pss[cs % 2]
        k.dma('sp' if cs % 2 == 0 else 'act', slab[:], wv[:, :, cs * 512:(cs + 1) * 512])
        for j in range(4):
            for kc in range(NCH):
                k.op('pe', lambda e, j=j, kc=kc: e.matmul(pst[:, j, :], lhsT=slab[:, kc, j * 128:(j + 1) * 128],
                                                       rhs=sc[:, kc, :], start=(kc == 0), stop=(kc == NCH - 1)),
                     reads=[slab, sc], writes=[pst])
        for r in range(2):
            k.op('dve', lambda e, r=r: e.tensor_tensor(out=modT[:, cs * 4:(cs + 1) * 4, r], in0=pst[:, :, r],
                                                      in1=bsb[:, cs * 4:(cs + 1) * 4], op=ALU.add),
                 reads=[pst, bsb], writes=[modT])
    P.release(m)


TOK_BLOCKS = [(0, 512, 0), (512, 512, 0), (1024, 512, 0), (1536, 512, 0), (2048, 256, 1)]


def make_consts(P):
    nc, k = P.nc, P.k
    C = {}
    C['ones_bf'] = sb(nc, "c_ones_bf", [128, 128], BF16)
    C['ones_f'] = sb(nc, "c_ones_f", [128, 128], F32)
    C['id_f'] = sb(nc, "c_id_f", [128, 128], F32)
    C['id_bf'] = sb(nc, "c_id_bf", [128, 128], BF16)
    k.op('pool', lambda e: e.memset(C['ones_f'][:], 1.0), writes=[C['ones_f']])
    k.op('dve', lambda e: e.tensor_copy(out=C['ones_bf'][:], in_=C['ones_f'][:]), reads=[C['ones_f']], writes=[C['ones_bf']])
    k.op('pool', lambda e: e.affine_select(out=C['id_f'][:], in_=C['ones_f'][:], pattern=[[-1, 128]],
                                           compare_op=ALU.is_equal, fill=0.0, base=0, channel_multiplier=1),
         reads=[C['ones_f']], writes=[C['id_f']])
    k.op('dve', lambda e: e.tensor_copy(out=C['id_bf'][:], in_=C['id_f'][:]), reads=[C['id_f']], writes=[C['id_bf']])
    return C


def ln_block(P, C, tmp, xt, nt, dst_fn, scale_fn, bias_fn, src_key=None):
    nc, k = P.nc, P.k
    xb, sq, ps_s, ps_q, mean, rstd, t1 = tmp['xb'], tmp['sq'], tmp['ps_s'], tmp['ps_q'], tmp['mean'], tmp['rstd'], tmp['t1']
    k.op('act', lambda e: e.activation(out=xb[:, :, :nt], in_=xt[:, :, :nt], func=AF.Copy), reads=[xt], writes=[xb])
    k.op('pool', lambda e: e.tensor_tensor(out=sq[:, :, :nt], in0=xt[:, :, :nt], in1=xt[:, :, :nt], op=ALU.mult),
         reads=[xt], writes=[sq])
    for c in range(NCH):
        k.op('pe', lambda e, c=c: e.matmul(ps_s[:, :nt], lhsT=C['ones_bf'][:], rhs=xb[:, c, :nt], start=(c == 0), stop=(c == NCH - 1)),
             reads=[xb, C['ones_bf']], writes=[ps_s])
    for c in range(NCH):
        k.op('pe', lambda e, c=c: e.matmul(ps_q[:, :nt], lhsT=C['ones_bf'][:], rhs=sq[:, c, :nt], start=(c == 0), stop=(c == NCH - 1)),
             reads=[sq, C['ones_bf']], writes=[ps_q])
    k.op('act', lambda e: e.mul(out=mean[:, :nt], in_=ps_s[:, :nt], mul=1.0 / D), reads=[ps_s], writes=[mean])
    k.op('dve', lambda e: e.tensor_tensor(out=rstd[:, :nt], in0=mean[:, :nt], in1=mean[:, :nt], op=ALU.mult),
         reads=[mean], writes=[rstd])
    k.op('dve', lambda e: e.scalar_tensor_tensor(out=rstd[:, :nt], in0=ps_q[:, :nt], scalar=1.0 / D, in1=rstd[:, :nt],
                                                 op0=ALU.mult, op1=ALU.subtract), reads=[ps_q, rstd], writes=[rstd])
    k.op('dve', lambda e: e.tensor_scalar_add(out=rstd[:, :nt], in0=rstd[:, :nt], scalar1=EPS), reads=[rstd], writes=[rstd])
    k.op('act', lambda e: e.activation(out=rstd[:, :nt], in_=rstd[:, :nt], func=AF.Ln), reads=[rstd], writes=[rstd])
    k.op('act', lambda e: e.activation(out=rstd[:, :nt], in_=rstd[:, :nt], func=AF.Exp, scale=-0.5), reads=[rstd], writes=[rstd])
    for c in range(NCH):
        tt = t1[c % 2]
        k.op('dve', lambda e, c=c, tt=tt: e.tensor_tensor(out=tt[:, :nt], in0=xt[:, c, :nt], in1=mean[:, :nt], op=ALU.subtract),
             reads=[xt, mean], writes=[tt])
        k.op('pool', lambda e, tt=tt: e.tensor_tensor(out=tt[:, :nt], in0=tt[:, :nt], in1=rstd[:, :nt], op=ALU.mult),
             reads=[tt, rstd], writes=[tt])
        dst, dkeys = dst_fn(c)
        s_ap, skeys = scale_fn(c)
        b_ap, bkeys = bias_fn(c)
        k.op('act', lambda e, tt=tt, dst=dst, s_ap=s_ap, b_ap=b_ap: e.activation(out=dst, in_=tt[:, :nt], func=AF.Identity,
                                                                               scale=s_ap, bias=b_ap),
             reads=[tt] + skeys + bkeys, writes=dkeys)


def ln_tmp(P, pfx, nmax=512):
    return {
        'xb': P.alloc_sb(pfx + "_xb", [128, NCH, nmax], BF16),
        'sq': P.alloc_sb(pfx + "_sq", [128, NCH, nmax], BF16),
        'ps_s': P.alloc_ps(pfx + "_pss", [128, nmax]),
        'ps_q': P.alloc_ps(pfx + "_psq", [128, nmax]),
        'mean': P.alloc_sb(pfx + "_mean", [128, nmax], F32),
        'rstd': P.alloc_sb(pfx + "_rstd", [128, nmax], F32),
        't1': [P.alloc_sb(pfx + "_t1%d" % i, [128, nmax], F32) for i in range(2)],
    }


def stage_inproj(P, C, l, xT, w_in, modp, pfm, ptm):
    nc, k = P.nc, P.k
    m = P.mark()
    hT = P.alloc_sb("ip_hT", [128, NCH, T], BF16)
    m2 = P.mark()
    tmp = ln_tmp(P, "ip")
    xts = [P.alloc_sb("ip_xt%d" % i, [128, NCH, 512], F32) for i in range(2)]
    xv = xT.rearrange("(c p) t -> p c t", p=128)
    for bi, (t0, nt, r) in enumerate(TOK_BLOCKS):
        xt = xts[bi % 2]
        k.dma('sp', xt[:, :, :nt], xv[:, :, t0:t0 + nt], writes=[xt])
        ln_block(P, C, tmp, xt, nt,
                 lambda c: (hT[:, c, t0:t0 + nt], [hT]),
                 lambda c: (modp['sc1p'][:, c, r:r + 1], [modp['sc1p']]),
                 lambda c: (modp['sh1'][:, c, r:r + 1], [modp['sh1']]))
    P.release(m2)
    slabs = [P.alloc_sb("ip_w%d" % i, [128, NCH, 512], BF16) for i in range(2)]
    pss = [P.alloc_ps("ip_ps%d" % i, [128, 512]) for i in range(4)]
    stg = [P.alloc_sb("ip_stg%d" % i, [128, 512], F32) for i in range(4)]
    wv = w_in[l].rearrange("(kc p) c -> p kc c", p=128)
    ev = 0
    for s in range(13):
        slab = slabs[s % 2]
        ncol = 512 if s < 12 else 32
        k.dma('pool', slab[:, :, :ncol], wv[:, :, s * 512:s * 512 + ncol], writes=[slab])
        if s < 6 or s == 12:
            row0 = s * 512 if s < 6 else 3072
            for j in range((ncol + 127) // 128):
                cw = min(128, ncol - j * 128)
                for (t0, nt, r) in TOK_BLOCKS:
                    pst = pss[ev % 4]; st = stg[ev % 4]
                    for kc in range(NCH):
                        k.op('pe', lambda e, kc=kc, pst=pst: e.matmul(pst[:cw, :nt], lhsT=slab[:, kc, j * 128:j * 128 + cw],
                                                                     rhs=hT[:, kc, t0:t0 + nt], start=(kc == 0), stop=(kc == NCH - 1)),
                             reads=[slab, hT], writes=[pst])
                    if ev % 2 == 0:
                        k.op('act', lambda e, pst=pst, st=st: e.copy(out=st[:cw, :nt], in_=pst[:cw, :nt]), reads=[pst], writes=[st])
                    else:
                        k.op('dve', lambda e, pst=pst, st=st: e.tensor_copy(out=st[:cw, :nt], in_=pst[:cw, :nt]), reads=[pst], writes=[st])
                    k.dma('sp', pfm[row0 + j * 128:row0 + j * 128 + cw, t0:t0 + nt], st[:cw, :nt], reads=[st], writes=[pfm])
                    ev += 1
        else:
            col0 = (s - 6) * 512
            for tt in range(T // 128):
                pst = pss[ev % 4]; st = stg[ev % 4]
                for kc in range(NCH):
                    k.op('pe', lambda e, kc=kc, pst=pst: e.matmul(pst[:, :], lhsT=hT[:, kc, tt * 128:(tt + 1) * 128],
                                                                 rhs=slab[:, kc, :], start=(kc == 0), stop=(kc == NCH - 1)),
                         reads=[slab, hT], writes=[pst])
                if ev % 2 == 0:
                    k.op('act', lambda e, pst=pst, st=st: e.copy(out=st[:, :], in_=pst[:, :]), reads=[pst], writes=[st])
                else:
                    k.op('dve', lambda e, pst=pst, st=st: e.tensor_copy(out=st[:, :], in_=pst[:, :]), reads=[pst], writes=[st])
                k.dma('sp', ptm[tt * 128:(tt + 1) * 128, col0:col0 + 512], st[:, :], reads=[st], writes=[ptm])
                ev += 1
    P.release(m)


def load_mod(P, modT):
    nc, k = P.nc, P.k
    mp = {}
    names = ['sh1', 'sc1p', 'g1', 'sh2', 'sc2p', 'g2']
    for j, n in enumerate(names):
        t = P.alloc_sb("modp_" + n, [128, NCH, 2], F32)
        if n.startswith('sc'):
            k.op('dve', lambda e, t=t, j=j: e.tensor_scalar_add(out=t[:], in0=modT[:, j * 16:(j + 1) * 16, :], scalar1=1.0),
                 reads=[modT], writes=[t])
        else:
            k.op('dve', lambda e, t=t, j=j: e.tensor_copy(out=t[:], in_=modT[:, j * 16:(j + 1) * 16, :]), reads=[modT], writes=[t])
        mp[n] = t
    return mp


HY_MIN = math.log(1e-2) / 1.5
HY_MAX = math.log(1e-2) / 0.3


def stage_hyena(P, C, l, W, pfm, mixT, tok0, Ls):
    nc, k = P.nc, P.k
    NT = Ls // 128
    M = 2 * Ls
    pf = "hy%d_" % Ls
    m0 = P.mark()
    hfb = P.alloc_sb(pf + "hfb", [128, NT, 1024], BF16)
    uT = P.alloc_sb(pf + "uT", [128, NT, 512], BF16)
    x0T = P.alloc_sb(pf + "x0T", [128, NT, 512], F32)
    frow = P.alloc_sb(pf + "frow", [128, Ls], F32)
    ncol = P.alloc_sb(pf + "ncol", [128, NT], F32)
    pa = [P.alloc_ps(pf + "pa%d" % i, [128, 512]) for i in range(7)]
    ptb = P.alloc_ps(pf + "ptb", [128, 512], BF16)
    ti = P.alloc_sb(pf + "ti", [128, Ls], I32)
    k.op('pool', lambda e: e.iota(ti[:], pattern=[[1, Ls]], base=0, channel_multiplier=0), writes=[ti])
    k.op('dve', lambda e: e.tensor_copy(out=frow[:], in_=ti[:]), reads=[ti], writes=[frow])
    k.op('pool', lambda e: e.iota(ti[:, :NT], pattern=[[128, NT]], base=0, channel_multiplier=1), reads=[frow], writes=[ti])
    k.op('dve', lambda e: e.tensor_copy(out=ncol[:], in_=ti[:, :NT]), reads=[ti], writes=[ncol])

    m1 = P.mark()
    w1 = P.alloc_sb(pf + "w1", [33, 64], F32)
    w2 = P.alloc_sb(pf + "w2", [64, 64], F32)
    w3 = P.alloc_sb(pf + "w3", [64, 1024], F32)
    fv = P.alloc_sb(pf + "fv", [64, 3], F32)
    fc = P.alloc_sb(pf + "fc", [64, 4], F32)
    hb = P.alloc_sb(pf + "hb", [1, 512], F32)
    k.dma('sp', w1[:], W['hy_f_w1'][l])
    k.dma('sp', w2[:], W['hy_f_w2'][l])
    k.dma('sp', w3[:], W['hy_f_w3'][l])
    k.dma('sp', fv[:], W['hy_fv'][l])
    k.dma('sp', hb[:], W['hy_bias'][l:l + 1, :])
    k.op('dve', lambda e: e.tensor_scalar_mul(out=fc[:, 0:1], in0=fv[:, 2:3], scalar1=1.0 / (2 * math.pi)), reads=[fv], writes=[fc])
    for i in range(2):
        k.op('dve', lambda e, i=i: e.tensor_scalar(out=fc[:, 1 + i:2 + i], in0=fv[:, i:i + 1], scalar1=fc[:, 0:1], scalar2=0.0,
                                                  op0=ALU.mult, op1=ALU.add), reads=[fv, fc], writes=[fc])
    pc = P.alloc_sb(pf + "pc", [33, 4], F32)
    pi_ = P.alloc_sb(pf + "pi", [33, 1], I32)
    k.op('pool', lambda e: e.iota(pi_[:], pattern=[[0, 1]], base=0, channel_multiplier=1), writes=[pi_])
    k.op('dve', lambda e: e.tensor_copy(out=pc[:, 0:1], in_=pi_[:]), reads=[pi_], writes=[pc])
    step = (15.0 - 1e-4) / 15.0
    k.op('dve', lambda e: e.tensor_scalar(out=pc[:, 3:4], in0=pc[:, 0:1], scalar1=16.5, scalar2=-16.0, op0=ALU.is_gt, op1=ALU.mult),
         reads=[pc], writes=[pc])
    k.op('dve', lambda e: e.tensor_tensor(out=pc[:, 1:2], in0=pc[:, 0:1], in1=pc[:, 3:4], op=ALU.add), reads=[pc], writes=[pc])
    k.op('dve', lambda e: e.tensor_scalar(out=pc[:, 1:2], in0=pc[:, 1:2], scalar1=step / Ls, scalar2=(1e-4 - step) / Ls, op0=ALU.mult, op1=ALU.add),
         reads=[pc], writes=[pc])
    k.op('dve', lambda e: e.tensor_scalar(out=pc[:, 2:3], in0=pc[:, 0:1], scalar1=16.5, scalar2=-0.25, op0=ALU.is_lt, op1=ALU.mult),
         reads=[pc], writes=[pc])
    k.op('dve', lambda e: e.tensor_scalar_add(out=pc[:, 2:3], in0=pc[:, 2:3], scalar1=0.5), reads=[pc], writes=[pc])
    wi_ = P.alloc_sb(pf + "wi", [64, Ls], I32)
    wf_ = P.alloc_sb(pf + "wff", [64, Ls], F32)

    def wrap(t, np_):
        k.op('dve', lambda e: e.tensor_copy(out=wi_[:np_, :], in_=t[:np_, :]), reads=[t], writes=[wi_])
        k.op('pool', lambda e: e.tensor_copy(out=wf_[:np_, :], in_=wi_[:np_, :]), reads=[wi_], writes=[wf_])
        k.op('dve', lambda e: e.tensor_tensor(out=t[:np_, :], in0=t[:np_, :], in1=wf_[:np_, :], op=ALU.subtract), reads=[t, wf_], writes=[t])
    zT = P.alloc_sb(pf + "zT", [33, Ls], F32)
    h1 = P.alloc_sb(pf + "h1", [64, Ls], F32)
    h2 = P.alloc_sb(pf + "h2", [64, Ls], F32)
    k.op('dve', lambda e: e.tensor_scalar(out=zT[:], in0=frow[:33, :], scalar1=pc[:, 1:2], scalar2=pc[:, 2:3], op0=ALU.mult, op1=ALU.add),
         reads=[frow, pc], writes=[zT])
    wrap(zT, 33)
    k.op('act', lambda e: e.activation(out=zT[:], in_=zT[:], func=AF.Sin, scale=2 * math.pi), reads=[zT], writes=[zT])
    k.op('act', lambda e: e.mul(out=zT[0:1, :], in_=frow[0:1, :], mul=1.0 / (Ls - 1)), reads=[frow, zT], writes=[zT])
    BL = min(512, Ls)
    for (src, wt, dst, ci) in ((zT, w1, h1, 1), (h1, w2, h2, 2)):
        for b0 in range(0, Ls, BL):
            pst = pa[(b0 // BL) % 2]
            k.op('pe', lambda e, pst=pst, src=src, wt=wt, b0=b0: e.matmul(pst[:64, :BL], lhsT=wt[:], rhs=src[:, b0:b0 + BL], start=True, stop=True),
                 reads=[wt, src], writes=[pst])
            k.op('dve', lambda e, pst=pst, dst=dst, b0=b0, ci=ci: e.tensor_scalar(out=dst[:, b0:b0 + BL], in0=pst[:64, :BL], scalar1=fc[:, 0:1],
                                                                                 scalar2=fc[:, ci:ci + 1], op0=ALU.mult, op1=ALU.add),
                 reads=[pst, fc], writes=[dst])
        wrap(dst, 64)
        k.op('act', lambda e, dst=dst: e.activation(out=dst[:], in_=dst[:], func=AF.Sin, scale=2 * math.pi), reads=[dst], writes=[dst])
    drow = P.alloc_sb(pf + "drow", [128, 512], F32)
    negt = P.alloc_sb(pf + "negt", [128, NT], F32)
    k.op('dve', lambda e: e.tensor_scalar(out=drow[:], in0=frow[:, :512] if Ls >= 512 else frow[:, :], scalar1=-(HY_MAX - HY_MIN) / 511.0, scalar2=-HY_MIN,
                                          op0=ALU.mult, op1=ALU.add), reads=[frow], writes=[drow]) if Ls >= 512 else None
    if Ls < 512:
        ti2 = P.alloc_sb(pf + "ti2", [128, 512], I32)
        k.op('pool', lambda e: e.iota(ti2[:], pattern=[[1, 512]], base=0, channel_multiplier=0), writes=[ti2])
        k.op('dve', lambda e: e.tensor_copy(out=drow[:], in_=ti2[:]), reads=[ti2], writes=[drow])
        k.op('dve', lambda e: e.tensor_scalar(out=drow[:], in0=drow[:], scalar1=-(HY_MAX - HY_MIN) / 511.0, scalar2=-HY_MIN,
                                              op0=ALU.mult, op1=ALU.add), reads=[drow], writes=[drow])
    k.op('dve', lambda e: e.tensor_scalar_mul(out=negt[:], in0=ncol[:], scalar1=-1.0 / (Ls - 1)), reads=[ncol], writes=[negt])
    dec = P.alloc_sb(pf + "dec", [128, 512], F32)
    h0 = P.alloc_sb(pf + "h0", [128, 1024], F32)
    for tc in range(NT):
        k.op('act', lambda e, tc=tc: e.activation(out=dec[:], in_=drow[:], func=AF.Exp, scale=negt[:, tc:tc + 1]), reads=[drow, negt], writes=[dec])
        for hf in range(2):
            pst = pa[2 + hf]
            k.op('pe', lambda e, pst=pst, tc=tc, hf=hf: e.matmul(pst[:, :], lhsT=h2[:, tc * 128:(tc + 1) * 128], rhs=w3[:, hf * 512:(hf + 1) * 512],
                                                               start=True, stop=True), reads=[h2, w3], writes=[pst])
            if tc == 0:
                k.op('dve', lambda e, pst=pst, hf=hf: e.tensor_tensor(out=h0[:, hf * 512:(hf + 1) * 512], in0=pst[:, :], in1=dec[:], op=ALU.mult),
                     reads=[pst, dec], writes=[h0])
            else:
                k.op('dve', lambda e, pst=pst, tc=tc, hf=hf: e.tensor_tensor(out=hfb[:, tc, hf * 512:(hf + 1) * 512], in0=pst[:, :], in1=dec[:], op=ALU.mult),
                     reads=[pst, dec], writes=[hfb])
        if tc == 0:
            k.op('dve', lambda e: e.tensor_tensor(out=h0[0:1, 0:512], in0=h0[0:1, 0:512], in1=hb[:], op=ALU.add), reads=[h0, hb], writes=[h0])
            k.op('pool', lambda e: e.memset(h0[0:1, 512:1024], 0.0), reads=[h0], writes=[h0])
            k.op('act', lambda e: e.copy(out=hfb[:, 0, :], in_=h0[:]), reads=[h0], writes=[hfb])
    P.release(m1)

    m2 = P.mark()
    sw = P.alloc_sb(pf + "sw", [128, 12, 4], F32)
    k.dma('sp', sw[:], W['hy_sw'][l])
    raws = [P.alloc_sb(pf + "raw%d" % i, [128, Ls], F32) for i in range(2)]
    cvs = [P.alloc_sb(pf + "cv%d" % i, [128, Ls], F32) for i in range(3)]
    ub = P.alloc_sb(pf + "ub", [128, Ls], BF16)

    def conv(j, dst, ri):
        raw = raws[ri]
        k.dma('sp', raw[:], pfm[j * 128:(j + 1) * 128, tok0:tok0 + Ls], writes=[raw])
        k.op('act', lambda e: e.activation(out=dst[:], in_=raw[:], func=AF.Identity, scale=sw[:, j, 1:2], bias=sw[:, j, 3:4]),
             reads=[raw, sw], writes=[dst])
        k.op('dve', lambda e: e.scalar_tensor_tensor(out=dst[:, 1:], in0=raw[:, :Ls - 1], scalar=sw[:, j, 0:1], in1=dst[:, 1:],
                                                     op0=ALU.mult, op1=ALU.add), reads=[raw, sw, dst], writes=[dst])
        k.op('dve', lambda e: e.scalar_tensor_tensor(out=dst[:, :Ls - 1], in0=raw[:, 1:], scalar=sw[:, j, 2:3], in1=dst[:, :Ls - 1],
                                                     op0=ALU.mult, op1=ALU.add), reads=[raw, sw, dst], writes=[dst])

    for j in range(4):
        conv(j, cvs[0], 0)
        for tc in range(NT):
            pst = pa[tc % 2]
            k.op('pe', lambda e, pst=pst, tc=tc: e.transpose(out=pst[:, :128], in_=cvs[0][:, tc * 128:(tc + 1) * 128], identity=C['id_f'][:]),
                 reads=[cvs[0], C['id_f']], writes=[pst])
            k.op('act', lambda e, pst=pst, tc=tc, j=j: e.copy(out=x0T[:, tc, j * 128:(j + 1) * 128], in_=pst[:, :128]), reads=[pst], writes=[x0T])
        conv(4 + j, cvs[1], 1)
        conv(8 + j, cvs[2], 0)
        k.op('pool', lambda e: e.tensor_tensor(out=ub[:], in0=cvs[1][:], in1=cvs[2][:], op=ALU.mult), reads=[cvs[1], cvs[2]], writes=[ub])
        for tc in range(NT):
            k.op('pe', lambda e, tc=tc: e.transpose(out=ptb[:, :128], in_=ub[:, tc * 128:(tc + 1) * 128], identity=C['id_bf'][:]),
                 reads=[ub, C['id_bf']], writes=[ptb])
            k.op('dve', lambda e, tc=tc, j=j: e.tensor_copy(out=uT[:, tc, j * 128:(j + 1) * 128], in_=ptb[:, :128]), reads=[ptb], writes=[uT])
    P.release(m2)

    Asb = P.alloc_sb(pf + "A", [128, NT, 512], BF16)
    Bsb = P.alloc_sb(pf + "B", [128, NT, 512], BF16)
    csb = [P.alloc_sb(pf + "cs%d" % i, [128, 2, 128], BF16) for i in range(3)]
    pm = [P.alloc_sb(pf + "pm%d" % i, [128, 2, 128], F32) for i in range(3)]
    pmi = [P.alloc_sb(pf + "pmi%d" % i, [128, 2, 128], I32) for i in range(3)]
    pmf = [P.alloc_sb(pf + "pmf%d" % i, [128, 2, 128], F32) for i in range(3)]
    wf = P.alloc_sb(pf + "wf", [128, NT], F32)
    nwf = P.alloc_sb(pf + "nwf", [128, NT], F32)
    k.op('pool', lambda e: e.memset(wf[:], 2.0 / M), writes=[wf])
    k.op('pool', lambda e: e.memset(wf[0:1, 0:1], 1.0 / M), reads=[wf], writes=[wf])
    k.op('dve', lambda e: e.tensor_scalar_mul(out=nwf[:], in0=wf[:], scalar1=-1.0), reads=[wf], writes=[nwf])
    gi = [0]

    def gen(a, b):
        i = gi[0] % 3
        gi[0] += 1
        k.op('dve', lambda e: e.tensor_scalar(out=pm[i][:, 0, :], in0=frow[:, b * 128:(b + 1) * 128], scalar1=ncol[:, a:a + 1], scalar2=1.0 / M,
                                              op0=ALU.mult, op1=ALU.mult), reads=[frow, ncol], writes=[pm[i]])
        k.op('pool', lambda e: e.tensor_scalar_add(out=pm[i][:, 1, :], in0=pm[i][:, 0, :], scalar1=0.25), reads=[pm[i]], writes=[pm[i]])
        k.op('dve', lambda e: e.tensor_copy(out=pmi[i][:], in_=pm[i][:]), reads=[pm[i]], writes=[pmi[i]])
        k.op('pool', lambda e: e.tensor_copy(out=pmf[i][:], in_=pmi[i][:]), reads=[pmi[i]], writes=[pmf[i]])
        k.op('dve', lambda e: e.tensor_tensor(out=pm[i][:], in0=pm[i][:], in1=pmf[i][:], op=ALU.subtract), reads=[pm[i], pmf[i]], writes=[pm[i]])
        k.op('act', lambda e: e.activation(out=csb[i][:], in_=pm[i][:], func=AF.Sin, scale=2 * math.pi), reads=[pm[i]], writes=[csb[i]])
        return csb[i][:, 1, :], csb[i][:, 0, :], csb[i]

    def cols(N, j):
        return uT[:, N, :] if j == 0 else hfb[:, N, (j - 1) * 512:j * 512]

    alt = P.alloc_sb(pf + "alt", [128, 128], F32)
    altb = P.alloc_sb(pf + "altb", [128, 128], BF16)
    alti = P.alloc_sb(pf + "alti", [128, 128], I32)
    altf = P.alloc_sb(pf + "altf", [128, 128], F32)
    k.op('dve', lambda e: e.tensor_scalar(out=alt[:], in0=frow[:, :128], scalar1=ncol[:, 0:1], scalar2=0.5, op0=ALU.add, op1=ALU.mult),
         reads=[frow, ncol], writes=[alt])
    k.op('dve', lambda e: e.tensor_scalar_add(out=alt[:], in0=alt[:], scalar1=0.25), reads=[alt], writes=[alt])
    k.op('dve', lambda e: e.tensor_copy(out=alti[:], in_=alt[:]), reads=[alt], writes=[alti])
    k.op('dve', lambda e: e.tensor_copy(out=altf[:], in_=alti[:]), reads=[alti], writes=[altf])
    k.op('dve', lambda e: e.tensor_tensor(out=alt[:], in0=alt[:], in1=altf[:], op=ALU.subtract), reads=[alt, altf], writes=[alt])
    k.op('act', lambda e: e.activation(out=altb[:], in_=alt[:], func=AF.Sin, scale=2 * math.pi), reads=[alt], writes=[altb])
    for j in range(3):
        for N in range(NT):
            k.op('pe', lambda e, j=j, N=N: e.matmul(pa[j][0:1, :], lhsT=altb[:, 0:1], rhs=cols(N, j), start=(N == 0), stop=(N == NT - 1)),
                 reads=[altb, uT, hfb], writes=[pa[j]])
    nyq = P.alloc_sb(pf + "nyq", [1, 2, 512], F32)
    nyqb = P.alloc_sb(pf + "nyqb", [1, 512], BF16)
    k.op('act', lambda e: e.copy(out=nyq[:, 0, :], in_=pa[1][0:1, :]), reads=[pa[1]], writes=[nyq])
    k.op('dve', lambda e: e.tensor_tensor(out=nyq[:, 0, :], in0=pa[2][0:1, :], in1=nyq[:, 0, :], op=ALU.add), reads=[pa[2], nyq], writes=[nyq])
    k.op('dve', lambda e: e.tensor_tensor(out=nyq[:, 1, :], in0=pa[0][0:1, :], in1=nyq[:, 0, :], op=ALU.mult), reads=[pa[0], nyq], writes=[nyq])
    k.op('act', lambda e: e.mul(out=nyqb[:], in_=nyq[:, 1, :], mul=1.0 / M), reads=[nyq], writes=[nyqb])

    ev = [P.alloc_sb(pf + "ev%d" % i, [128, 512], F32) for i in range(4)]
    tt = [P.alloc_sb(pf + "tt%d" % i, [128, 512], F32) for i in range(4)]
    for F in range(NT):
        for N in range(NT):
            cbk, sbl, ck = gen(N, F)
            for j in range(3):
                k.op('pe', lambda e, j=j, N=N, cbk=cbk: e.matmul(pa[j][:, :], lhsT=cbk, rhs=cols(N, j), start=(N == 0), stop=(N == NT - 1)),
                     reads=[ck, uT, hfb], writes=[pa[j]])
            for j in range(3):
                k.op('pe', lambda e, j=j, N=N, sbl=sbl: e.matmul(pa[3 + j][:, :], lhsT=sbl, rhs=cols(N, j), start=(N == 0), stop=(N == NT - 1)),
                     reads=[ck, uT, hfb], writes=[pa[3 + j]])
        k.op('act', lambda e: e.copy(out=ev[0][:], in_=pa[1][:, :]), reads=[pa[1]], writes=[ev[0]])
        k.op('act', lambda e: e.copy(out=ev[1][:], in_=pa[4][:, :]), reads=[pa[4]], writes=[ev[1]])
        k.op('dve', lambda e: e.tensor_tensor(out=ev[0][:], in0=pa[2][:, :], in1=ev[0][:], op=ALU.add), reads=[pa[2], ev[0]], writes=[ev[0]])
        k.op('dve', lambda e: e.tensor_tensor(out=ev[1][:], in0=pa[5][:, :], in1=ev[1][:], op=ALU.subtract), reads=[pa[5], ev[1]], writes=[ev[1]])
        k.op('dve', lambda e: e.tensor_tensor(out=tt[0][:], in0=pa[0][:, :], in1=ev[0][:], op=ALU.mult), reads=[pa[0], ev[0]], writes=[tt[0]])
        k.op('dve', lambda e: e.tensor_tensor(out=tt[1][:], in0=pa[3][:, :], in1=ev[1][:], op=ALU.mult), reads=[pa[3], ev[1]], writes=[tt[1]])
        k.op('pool', lambda e: e.tensor_tensor(out=tt[0][:], in0=tt[0][:], in1=tt[1][:], op=ALU.add), reads=[tt[0], tt[1]], writes=[tt[0]])
        k.op('act', lambda e, F=F: e.activation(out=Asb[:, F, :], in_=tt[0][:], func=AF.Copy, scale=wf[:, F:F + 1]), reads=[tt[0], wf], writes=[Asb])
        k.op('dve', lambda e: e.tensor_tensor(out=tt[2][:], in0=pa[0][:, :], in1=ev[1][:], op=ALU.mult), reads=[pa[0], ev[1]], writes=[tt[2]])
        k.op('dve', lambda e: e.tensor_tensor(out=tt[3][:], in0=pa[3][:, :], in1=ev[0][:], op=ALU.mult), reads=[pa[3], ev[0]], writes=[tt[3]])
        k.op('pool', lambda e: e.tensor_tensor(out=tt[2][:], in0=tt[2][:], in1=tt[3][:], op=ALU.subtract), reads=[tt[2], tt[3]], writes=[tt[2]])
        k.op('act', lambda e, F=F: e.activation(out=Bsb[:, F, :], in_=tt[2][:], func=AF.Copy, scale=nwf[:, F:F + 1]), reads=[tt[2], nwf], writes=[Bsb])
    nw = P.alloc_sb(pf + "nw", [128, 4], F32)
    k.dma('sp', nw[:], W['hy_norm_wT'][l])
    ys = [P.alloc_sb(pf + "y%d" % i, [128, 512], F32) for i in range(2)]
    yb = [P.alloc_sb(pf + "yb%d" % i, [128, 512], BF16) for i in range(2)]
    junk = P.alloc_sb(pf + "junk", [128, 512], F32)
    ss = P.alloc_sb(pf + "ss", [128, 2], F32)
    mst = [P.alloc_sb(pf + "mst%d" % i, [128, 4, 128], BF16) for i in range(2)]
    for N in range(NT):
        py = pa[N % 2]
        for F in range(NT):
            cbk, sbl, ck = gen(F, N)
            k.op('pe', lambda e, F=F, cbk=cbk: e.matmul(py[:, :], lhsT=cbk, rhs=Asb[:, F, :], start=(F == 0), stop=False),
                 reads=[ck, Asb], writes=[py])
            k.op('pe', lambda e, F=F, sbl=sbl: e.matmul(py[:, :], lhsT=sbl, rhs=Bsb[:, F, :], start=False, stop=False),
                 reads=[ck, Bsb], writes=[py])
        k.op('pe', lambda e: e.matmul(py[:, :], lhsT=altb[0:1, :], rhs=nyqb[:], start=False, stop=True), reads=[altb, nyqb], writes=[py])
        y = ys[N % 2]; ybf = yb[N % 2]; ms = mst[N % 2]
        k.op('dve', lambda e, N=N: e.tensor_tensor(out=y[:], in0=py[:, :], in1=x0T[:, N, :], op=ALU.mult), reads=[py, x0T], writes=[y])
        k.op('act', lambda e: e.activation(out=junk[:], in_=y[:], func=AF.Square, accum_out=ss[:, 0:1]), reads=[y], writes=[junk, ss])
        k.op('dve', lambda e: e.tensor_scalar(out=ss[:, 1:2], in0=ss[:, 0:1], scalar1=1.0 / HY, scalar2=EPS, op0=ALU.mult, op1=ALU.add), reads=[ss], writes=[ss])
        k.op('act', lambda e: e.activation(out=ss[:, 1:2], in_=ss[:, 1:2], func=AF.Ln), reads=[ss], writes=[ss])
        k.op('act', lambda e: e.activation(out=ss[:, 1:2], in_=ss[:, 1:2], func=AF.Exp, scale=-0.5), reads=[ss], writes=[ss])
        k.op('dve', lambda e: e.tensor_scalar_mul(out=ybf[:], in0=y[:], scalar1=ss[:, 1:2]), reads=[y, ss], writes=[ybf])
        for j in range(4):
            k.op('pe', lambda e, j=j: e.transpose(out=ptb[:, j * 128:(j + 1) * 128], in_=ybf[:, j * 128:(j + 1) * 128], identity=C['id_bf'][:]),
                 reads=[ybf, C['id_bf']], writes=[ptb])
        for j in range(4):
            k.op('act', lambda e, j=j: e.activation(out=ms[:, j, :], in_=ptb[:, j * 128:(j + 1) * 128], func=AF.Copy, scale=nw[:, j:j + 1]),
                 reads=[ptb, nw], writes=[ms])
        k.dma('sp', mixT[0:512, tok0 + N * 128:tok0 + (N + 1) * 128].rearrange("(j p) t -> p j t", p=128), ms[:], reads=[ms], writes=[mixT])
    P.release(m0)


NEGM = -1.0e4


def stage_na(P, C, l, W, pfm, ptm, mixT, rpbp, with_ctx):
    nc, k = P.nc, P.k
    pf = "na_"
    m0 = P.mark()
    scale = 128 ** -0.5
    qT = P.alloc_sb(pf + "qT", [128, NAH, T], BF16)
    kT = P.alloc_sb(pf + "kT", [128, NAH, T], BF16)
    v1 = P.alloc_sb(pf + "v1", [128, T // 128, NAH, 129], BF16)
    R = P.alloc_sb(pf + "R", [128, NAH, 9, 128], F32)
    k.op('pool', lambda e: e.memset(v1[:], 1.0), writes=[v1])
    for h in range(NAH):
        k.dma('pool', qT[:, h, :], pfm[O_NQ + h * 128:O_NQ + (h + 1) * 128, :], writes=[qT])
        k.dma('pool', kT[:, h, :], pfm[O_NK + h * 128:O_NK + (h + 1) * 128, :], writes=[kT])
    for tt in range(T // 128):
        k.dma('pool', v1[:, tt, :, 0:128], ptm[tt * 128:(tt + 1) * 128, 0:768].rearrange("p (h d) -> p h d", h=NAH), writes=[v1])
    m1 = P.mark()
    zr = P.alloc_sb(pf + "zr", [90, 160], F32)
    k.op('pool', lambda e: e.memset(zr[:], 0.0), writes=[zr])
    k.dma('sp', zr[:, 48:79], W['na_rpb'][l].rearrange("h r m -> (h r) m"), writes=[zr])
    k.dma('sp', rpbp.rearrange("h r m -> (h r) m"), zr[:], reads=[zr], writes=[rpbp])
    Hk = P.alloc_sb(pf + "Hk", [64, NAH, 15, 64], F32)
    for h in range(NAH):
        src = bass.AP(rpbp.tensor, rpbp[h, 0, 0:1].offset, [[1, 64], [160, 15], [1, 64]])
        k.dma('sp', Hk[:, h, :, :], src, reads=[rpbp], writes=[Hk])
    J = P.alloc_sb(pf + "J", [64, 64], F32)
    k.op('pool', lambda e: e.affine_select(out=J[:], in_=C['ones_f'][:64, :64], pattern=[[1, 64]], compare_op=ALU.is_equal, fill=0.0,
                                           base=-63, channel_multiplier=1), reads=[C['ones_f']], writes=[J])
    ms = [P.alloc_sb(pf + "ms%d" % i, [128, 2, 64], F32) for i in range(4)]
    CM = P.alloc_sb(pf + "CM", [128, 2, 64], F32)
    ones3 = C['ones_f'][:, :].rearrange("p (c q) -> p c q", c=2)
    for a in range(2):
        sl = slice(a * 64, (a + 1) * 64)
        k.op('pool', lambda e, sl=sl, a=a: e.affine_select(out=ms[0][sl], in_=ones3[sl], pattern=[[0, 2], [-1, 64]], compare_op=ALU.is_ge, fill=0.0,
                                                          base=8, channel_multiplier=1), reads=[C['ones_f']], writes=[ms[0]])
        k.op('pool', lambda e, sl=sl, a=a: e.affine_select(out=ms[1][sl], in_=ones3[sl], pattern=[[0, 2], [0, 64]], compare_op=ALU.is_ge, fill=0.0,
                                                          base=-48, channel_multiplier=1), reads=[C['ones_f']], writes=[ms[1]])
        k.op('pool', lambda e, sl=sl, a=a: e.affine_select(out=ms[2][sl], in_=ones3[sl], pattern=[[0, 2], [1, 64]], compare_op=ALU.is_ge, fill=0.0,
                                                          base=7, channel_multiplier=-1), reads=[C['ones_f']], writes=[ms[2]])
        k.op('pool', lambda e, sl=sl, a=a: e.affine_select(out=ms[3][sl], in_=ones3[sl], pattern=[[0, 2], [0, 64]], compare_op=ALU.is_ge, fill=0.0,
                                                          base=15, channel_multiplier=-1), reads=[C['ones_f']], writes=[ms[3]])
    k.op('dve', lambda e: e.tensor_tensor(out=ms[0][:], in0=ms[0][:], in1=ms[1][:], op=ALU.max), reads=[ms[0], ms[1]], writes=[ms[0]])
    k.op('dve', lambda e: e.tensor_tensor(out=ms[2][:], in0=ms[2][:], in1=ms[3][:], op=ALU.max), reads=[ms[2], ms[3]], writes=[ms[2]])
    k.op('dve', lambda e: e.tensor_tensor(out=CM[:], in0=ms[0][:], in1=ms[2][:], op=ALU.mult), reads=[ms[0], ms[2]], writes=[CM])
    k.op('dve', lambda e: e.tensor_copy(out=ms[0][:], in_=CM[:]), reads=[CM], writes=[ms[0]])
    k.op('dve', lambda e: e.tensor_scalar(out=CM[:], in0=CM[:], scalar1=-1.0, scalar2=-NEGM, op0=ALU.add, op1=ALU.mult), reads=[CM], writes=[CM])
    pr = [P.alloc_ps(pf + "pr%d" % i, [128, 2, 64]) for i in range(2)]
    cnt = 0
    for h in range(NAH):
        for vi in range(9):
            d = vi - 3 if vi < 7 else (-2 if vi == 7 else 2)
            pst = pr[cnt % 2]
            cnt += 1
            for c in range(2):
                di = 2 * d - c + 7
                k.op('pe', lambda e, pst=pst, h=h, di=di, c=c: e.matmul(pst[:, c, :], lhsT=Hk[:, h, di:di + 2, :], rhs=J[:], start=True, stop=True),
                     reads=[Hk, J], writes=[pst])
            k.op('dve', lambda e, pst=pst, h=h, vi=vi: e.tensor_tensor(out=R[:, h, vi, :].rearrange("p (c q) -> p c q", c=2), in0=pst[:], in1=ms[0][:], op=ALU.mult),
                 reads=[pst, ms[0]], writes=[R])
            k.op('pool', lambda e, h=h, vi=vi: e.tensor_tensor(out=R[:, h, vi, :].rearrange("p (c q) -> p c q", c=2), in0=R[:, h, vi, :].rearrange("p (c q) -> p c q", c=2),
                                                              in1=CM[:], op=ALU.add), reads=[R, CM], writes=[R])
            if vi == 7:
                k.op('pool', lambda e, h=h, vi=vi: e.memset(R[0:64, h, vi, 64:128], NEGM), reads=[R], writes=[R])
            if vi == 8:
                k.op('pool', lambda e, h=h, vi=vi: e.memset(R[:, h, vi, 0:64], NEGM), reads=[R], writes=[R])
                k.op('pool', lambda e, h=h, vi=vi: e.memset(R[64:128, h, vi, 64:128], NEGM), reads=[R], writes=[R])
    P.dump('na_R', R)
    P.dump('na_Hk', Hk)
    P.release(m1)
    nw = P.alloc_sb(pf + "nw", [128, NAH], F32)
    k.dma('sp', nw[:], W['na_norm_wT'][l])
    pS = [P.alloc_ps(pf + "pS%d" % i, [128, 7, 128]) for i in range(2)]
    pO = [P.alloc_ps(pf + "pO%d" % i, [128, 129]) for i in range(2)]
    ptb = P.alloc_ps(pf + "ptb", [128, NAH, 128], BF16)
    ein = [P.alloc_sb(pf + "ein%d" % i, [128, 5, 128], F32) for i in range(2)]
    PT = [P.alloc_sb(pf + "PT%d" % i, [128, 7, 128], BF16) for i in range(2)]
    ob = [P.alloc_sb(pf + "ob%d" % i, [128, NAH * 128], F32) for i in range(2)]
    obb = [P.alloc_sb(pf + "obb%d" % i, [128, NAH * 128], BF16) for i in range(2)]
    rc = P.alloc_sb(pf + "rc", [128, 2], F32)
    ss = P.alloc_sb(pf + "ss", [128, 2], F32)
    junk = P.alloc_sb(pf + "junk", [128, NAH * 128], F32)
    mst = [P.alloc_sb(pf + "mst%d" % i, [128, NAH, 128], BF16) for i in range(2)]
    CT = [L // 128, L // 128 + 1]
    units = []
    for i in range(16):
        if 2 <= i <= 13:
            loc = [(i - 2, 7), (i - 1, 2), (i, 3), (i + 1, 4), (i + 2, 8)]
        elif i < 2:
            loc = [(j, j - i + 3) for j in range(4)]
        else:
            loc = [(j, j - i + 3) for j in range(12, 16)]
        units.append((i, loc, True))
    if with_ctx:
        units += [(16, [], True), (17, [], True)]
    u = 0
    for (qi, loc, _) in units:
        o_t = ob[u % 2]; o_b = obb[u % 2]; ms_ = mst[u % 2]
        for h in range(NAH):
            g = (u * NAH + h) % 2
            ps_, po, ei, pt = pS[g], pO[g], ein[g], PT[g]
            tiles = [j for (j, _) in loc] + CT
            nl = len(loc)
            for jj, j in enumerate(tiles):
                k.op('pe', lambda e, jj=jj, j=j, h=h, ps_=ps_: e.matmul(ps_[:, jj, :], lhsT=kT[:, h, j * 128:(j + 1) * 128], rhs=qT[:, h, qi * 128:(qi + 1) * 128],
                                                                       start=True, stop=True), reads=[kT, qT], writes=[ps_])
            for jj, (j, vi) in enumerate(loc):
                k.op('dve', lambda e, jj=jj, vi=vi, h=h, ps_=ps_, ei=ei: e.scalar_tensor_tensor(out=ei[:, jj, :], in0=ps_[:, jj, :], scalar=scale, in1=R[:, h, vi, :],
                                                                                               op0=ALU.mult, op1=ALU.add), reads=[ps_, R], writes=[ei])
            if nl:
                k.op('act', lambda e, nl=nl, ei=ei, pt=pt: e.activation(out=pt[:, 0:nl, :], in_=ei[:, 0:nl, :], func=AF.Exp), reads=[ei], writes=[pt])
            k.op('act', lambda e, nl=nl, ps_=ps_, pt=pt: e.activation(out=pt[:, nl:nl + 2, :], in_=ps_[:, nl:nl + 2, :], func=AF.Exp, scale=scale),
                 reads=[ps_], writes=[pt])
            for jj, j in enumerate(tiles):
                k.op('pe', lambda e, jj=jj, j=j, h=h, pt=pt, po=po: e.matmul(po[:, :], lhsT=pt[:, jj, :], rhs=v1[:, j, h, :], start=(jj == 0), stop=(jj == len(tiles) - 1)),
                     reads=[pt, v1], writes=[po])
            k.op('dve', lambda e, po=po: e.reciprocal(out=rc[:, 0:1], in_=po[:, 128:129]), reads=[po], writes=[rc])
            k.op('dve', lambda e, po=po, h=h, o_t=o_t: e.tensor_scalar_mul(out=o_t[:, h * 128:(h + 1) * 128], in0=po[:, 0:128], scalar1=rc[:, 0:1]),
                 reads=[po, rc], writes=[o_t])
        k.op('act', lambda e, o_t=o_t: e.activation(out=junk[:], in_=o_t[:], func=AF.Square, accum_out=ss[:, 0:1]), reads=[o_t], writes=[junk, ss])
        k.op('dve', lambda e: e.tensor_scalar(out=ss[:, 1:2], in0=ss[:, 0:1], scalar1=1.0 / 768, scalar2=EPS, op0=ALU.mult, op1=ALU.add), reads=[ss], writes=[ss])
        k.op('act', lambda e: e.activation(out=ss[:, 1:2], in_=ss[:, 1:2], func=AF.Ln), reads=[ss], writes=[ss])
        k.op('act', lambda e: e.activation(out=ss[:, 1:2], in_=ss[:, 1:2], func=AF.Exp, scale=-0.5), reads=[ss], writes=[ss])
        k.op('dve', lambda e, o_t=o_t, o_b=o_b: e.tensor_scalar_mul(out=o_b[:], in0=o_t[:], scalar1=ss[:, 1:2]), reads=[o_t, ss], writes=[o_b])
        for h in range(NAH):
            k.op('pe', lambda e, h=h, o_b=o_b: e.transpose(out=ptb[:, h, :], in_=o_b[:, h * 128:(h + 1) * 128], identity=C['id_bf'][:]),
                 reads=[o_b, C['id_bf']], writes=[ptb])
        for h in range(NAH):
            k.op('act', lambda e, h=h, ms_=ms_: e.activation(out=ms_[:, h, :], in_=ptb[:, h, :], func=AF.Copy, scale=nw[:, h:h + 1]), reads=[ptb, nw], writes=[ms_])
        k.dma('sp', mixT[512:1280, qi * 128:(qi + 1) * 128].rearrange("(j p) t -> p j t", p=128), ms_[:], reads=[ms_], writes=[mixT])
        u += 1
    P.release(m0)


def stage_gla(P, C, l, W, pfm, ptm, mixT, ofs, with_ctx):
    nc, k = P.nc, P.k
    pf = "gl_"
    m0 = P.mark()
    NTL = L // 128
    NTT = T // 128
    qs = GDK ** -0.5
    Mf = P.alloc_sb(pf + "Mf", [128, 128], F32)
    Mb = P.alloc_sb(pf + "Mb", [128, 128], F32)
    k.op('pool', lambda e: e.affine_select(out=Mf[:], in_=C['ones_f'][:], pattern=[[1, 128]], compare_op=ALU.is_ge, fill=0.0, base=0, channel_multiplier=-1),
         reads=[C['ones_f']], writes=[Mf])
    k.op('pool', lambda e: e.memset(Mf[0:64, 64:128], 0.0), reads=[Mf], writes=[Mf])
    k.op('pool', lambda e: e.affine_select(out=Mb[:], in_=C['ones_f'][:], pattern=[[-1, 128]], compare_op=ALU.is_ge, fill=0.0, base=0, channel_multiplier=1),
         reads=[C['ones_f']], writes=[Mb])
    k.op('pool', lambda e: e.memset(Mb[64:128, 0:64], 0.0), reads=[Mb], writes=[Mb])
    Lf = P.alloc_sb(pf + "Lf", [128, 128], F32)
    Lb = P.alloc_sb(pf + "Lb", [128, 128], F32)
    k.op('dve', lambda e: e.tensor_scalar_mul(out=Lf[:], in0=Mf[:], scalar1=-1.0 / 16), reads=[Mf], writes=[Lf])
    k.op('dve', lambda e: e.tensor_scalar_mul(out=Lb[:], in0=Mb[:], scalar1=-1.0 / 16), reads=[Mb], writes=[Lb])
    ind = P.alloc_sb(pf + "ind", [128, 2], F32)
    k.op('pool', lambda e: e.memset(ind[:], 0.0), writes=[ind])
    k.op('pool', lambda e: e.memset(ind[0:64, 0:1], -1.0 / 16), reads=[ind], writes=[ind])
    k.op('pool', lambda e: e.memset(ind[64:128, 1:2], -1.0 / 16), reads=[ind], writes=[ind])
    one1 = P.alloc_sb(pf + "one1", [128, 1], F32)
    k.op('pool', lambda e: e.memset(one1[:], 1.0), writes=[one1])
    ga1T = P.alloc_sb(pf + "ga1T", [33, T], F32)
    k.op('pool', lambda e: e.memset(ga1T[:], 1.0), writes=[ga1T])
    k.dma('sp', ga1T[0:32, :], pfm[3072:3104, :], writes=[ga1T])
    w2b = P.alloc_sb(pf + "w2b", [33, 2, 384], F32)
    k.op('pool', lambda e: e.memset(w2b[:], 0.0), writes=[w2b])
    for d in range(2):
        k.dma('sp', w2b[16 * d:16 * d + 16, d, :], W['gla_a_w2'][l, d], writes=[w2b])
        k.dma('sp', w2b[32:33, d, :], W['gla_a_b'][l, d:d + 1, :], writes=[w2b])
    gnw = P.alloc_sb(pf + "gnw", [128, 768], F32)
    for h in range(GH):
        k.dma('sp', gnw[:, h * 128:(h + 1) * 128], W['gla_norm_w'][l:l + 1, :].partition_broadcast(128), writes=[gnw])
    m1 = P.mark()
    ii = P.alloc_sb(pf + "ii", [128, 16], I32)
    inv = P.alloc_sb(pf + "inv", [128, 16], F32)
    k.op('pool', lambda e: e.iota(ii[:], pattern=[[1, 16]], base=0, channel_multiplier=0), writes=[ii])
    k.op('dve', lambda e: e.tensor_copy(out=inv[:], in_=ii[:]), reads=[ii], writes=[inv])
    k.op('act', lambda e: e.activation(out=inv[:], in_=inv[:], func=AF.Exp, scale=-math.log(10000.0) / 16), reads=[inv], writes=[inv])
    k.op('dve', lambda e: e.tensor_scalar_mul(out=inv[:], in0=inv[:], scalar1=1.0 / (2 * math.pi)), reads=[inv], writes=[inv])
    pp = P.alloc_sb(pf + "pp", [128, 4], F32)
    pi2 = P.alloc_sb(pf + "pi2", [128, 1], I32)
    k.op('pool', lambda e: e.iota(pi2[:], pattern=[[0, 1]], base=0, channel_multiplier=1), writes=[pi2])
    k.op('dve', lambda e: e.tensor_copy(out=pp[:, 0:1], in_=pi2[:]), reads=[pi2], writes=[pp])
    k.op('dve', lambda e: e.tensor_single_scalar(out=pp[:, 1:2], in_=pp[:, 0:1], scalar=63.5, op=ALU.is_gt), reads=[pp], writes=[pp])
    k.op('dve', lambda e: e.scalar_tensor_tensor(out=pp[:, 2:3], in0=pp[:, 1:2], scalar=-64.0, in1=pp[:, 0:1], op0=ALU.mult, op1=ALU.add),
         reads=[pp], writes=[pp])
    tab = P.alloc_sb(pf + "tab", [128, NTL, 2, 16], F32)
    rp = P.alloc_sb(pf + "rp", [128, 1], F32)
    for tt in range(NTL):
        k.op('dve', lambda e, tt=tt: e.tensor_scalar_add(out=rp[:], in0=pp[:, 1:2], scalar1=float(2 * tt)), reads=[pp], writes=[rp])
        k.op('dve', lambda e, tt=tt: e.tensor_scalar_mul(out=tab[:, tt, 0, :], in0=inv[:], scalar1=rp[:, 0:1]), reads=[inv, rp], writes=[tab])
        k.op('dve', lambda e, tt=tt: e.tensor_scalar_mul(out=tab[:, tt, 1, :], in0=inv[:], scalar1=pp[:, 2:3]), reads=[inv, pp], writes=[tab])
    sinT = P.alloc_sb(pf + "sinT", [128, NTL, 2, 16], F32)
    cosT = P.alloc_sb(pf + "cosT", [128, NTL, 2, 16], F32)
    ti = P.alloc_sb(pf + "ti", [128, NTL, 2, 16], I32)
    tf = P.alloc_sb(pf + "tf", [128, NTL, 2, 16], F32)
    for (dst, sh) in ((sinT, 0.0), (cosT, 0.25)):
        if sh:
            k.op('dve', lambda e: e.tensor_scalar_add(out=tab[:], in0=tab[:], scalar1=sh), reads=[tab], writes=[tab])
        k.op('dve', lambda e: e.tensor_copy(out=ti[:], in_=tab[:]), reads=[tab], writes=[ti])
        k.op('dve', lambda e: e.tensor_copy(out=tf[:], in_=ti[:]), reads=[ti], writes=[tf])
        k.op('dve', lambda e: e.tensor_tensor(out=tf[:], in0=tab[:], in1=tf[:], op=ALU.subtract), reads=[tab, tf], writes=[tf])
        k.op('act', lambda e, dst=dst: e.activation(out=dst[:], in_=tf[:], func=AF.Sin, scale=2 * math.pi), reads=[tf], writes=[dst])
    ld = [P.alloc_sb(pf + "ld%d" % i, [128, 2304], F32) for i in range(2)]
    vb = [P.alloc_sb(pf + "vb%d" % i, [128, GH, 128], BF16) for i in range(2)]
    ls_ = P.alloc_sb(pf + "ls", [128, 384], F32)
    ecp = P.alloc_sb(pf + "ecp", [128, 384], F32)
    ecn = P.alloc_sb(pf + "ecn", [128, 384], F32)
    qr = P.alloc_sb(pf + "qr", [128, 768], F32)
    rt = [P.alloc_sb(pf + "rt%d" % i, [128, 2, 16], F32) for i in range(4)]
    qkd = P.alloc_sb(pf + "qkd", [128, 2, 384], BF16)
    qdz = P.alloc_sb(pf + "qdz", [64, GH, 2, 128], BF16)
    k.op('pool', lambda e: e.memset(qdz[:], 0.0), writes=[qdz])
    kdT = P.alloc_sb(pf + "kdT", [64, GH, 128], BF16)
    qdT = P.alloc_sb(pf + "qdT", [64, GH, 128], BF16)
    ecl = P.alloc_sb(pf + "ecl", [64, GH, 2], F32)
    Am = [P.alloc_sb(pf + "Am%d" % i, [128, 128], BF16) for i in range(2)]
    S = {d: P.alloc_sb(pf + "S%d" % d, [64, GH, 128], F32) for d in range(2)}
    Sb = [P.alloc_sb(pf + "Sb%d" % i, [64, GH, 128], BF16) for i in range(3)]
    Stmp = P.alloc_sb(pf + "Stmp", [64, 128], F32)
    osb = P.alloc_sb(pf + "osb", [128, 768], F32)
    ofl = P.alloc_sb(pf + "ofl", [128, 768], F32)
    sq = P.alloc_sb(pf + "sq", [128, GH, 128], F32)
    ss = P.alloc_sb(pf + "ss", [128, 2, GH], F32)
    sg = P.alloc_sb(pf + "sg", [128, 768], F32)
    yb = P.alloc_sb(pf + "yb", [128, 768], BF16)
    mst = [P.alloc_sb(pf + "mst%d" % i, [128, GH, 128], BF16) for i in range(2)]
    bA = P.alloc_ps(pf + "bA", [128, 512])
    pz, pzk = bA[:, 0:384], bA
    pe_, pek = bA[0:64, 384:396].rearrange("p (h c) -> p h c", h=GH), bA
    bB = P.alloc_ps(pf + "bB", [128, 512])
    pc_, pck = bB[:, 0:384], bB
    pTq = P.alloc_ps(pf + "pTq", [64, GH, 128], BF16)
    pTk = P.alloc_ps(pf + "pTk", [64, GH, 128], BF16)
    bE = [P.alloc_ps(pf + "bE%d" % i, [128, 2, 128]) for i in range(2)]
    pA = [(bE[i][:, 0, :], bE[i]) for i in range(2)]
    pO = [(bE[i][:, 1, :], bE[i]) for i in range(2)]
    bF = P.alloc_ps(pf + "bF", [64, 2, 128])
    pK = [(bF[:, i, :], bF) for i in range(2)]
    ptb = P.alloc_ps(pf + "ptb", [128, GH, 128], BF16)
    cnt = [0]

    def tile_pass(tt, d, with_out, final):
        i = cnt[0] % 2
        cnt[0] += 1
        t0 = tt * 128
        buf = ld[i]; v_b = vb[i]
        rope = tt < NTL
        Mm = Mf if d == 0 else Mb
        Lm = Lf if d == 0 else Lb
        k.dma('sp', buf[:], ptm[t0:t0 + 128, 768:3072], writes=[buf])
        k.op('pool', lambda e: e.tensor_copy(out=v_b[:], in_=buf[:, 768:1536].rearrange("p (h d) -> p h d", h=GH)), reads=[buf], writes=[v_b])
        k.op('pe', lambda e: e.matmul(pz, lhsT=ga1T[:, t0:t0 + 128], rhs=w2b[:, d, :], start=True, stop=True), reads=[ga1T, w2b], writes=[pzk])
        k.op('act', lambda e: e.activation(out=ls_[:], in_=pz, func=AF.Exp, scale=-1.0), reads=[pzk], writes=[ls_])
        k.op('act', lambda e: e.activation(out=ls_[:], in_=ls_[:], func=AF.Ln, bias=one1[:]), reads=[ls_, one1], writes=[ls_])
        k.op('pe', lambda e: e.matmul(pc_, lhsT=Lm[:], rhs=ls_[:], start=True, stop=True), reads=[Lm, ls_], writes=[pck])
        for h in range(GH):
            k.op('pe', lambda e, h=h: e.matmul(pe_[:, h, :], lhsT=ls_[:, h * 64:(h + 1) * 64], rhs=ind[:], start=True, stop=True), reads=[ls_, ind], writes=[pek])
        k.op('act', lambda e: e.activation(out=ecl[:], in_=pe_, func=AF.Exp), reads=[pek], writes=[ecl])
        k.op('act', lambda e: e.activation(out=ecp[:], in_=pc_, func=AF.Exp), reads=[pck], writes=[ecp])
        k.op('act', lambda e: e.activation(out=ecn[:], in_=pc_, func=AF.Exp, scale=-1.0), reads=[pck], writes=[ecn])
        if rope:
            cs = cosT[:, tt, :, :]; sn = sinT[:, tt, :, :]
            for which in range(2):
                for h in range(GH):
                    o0 = which * 384 + h * 64
                    xv = buf[:, o0:o0 + 64].rearrange("p (hf two i) -> p hf two i", hf=2, two=2)
                    ov = qr[:, o0:o0 + 64].rearrange("p (hf two i) -> p hf two i", hf=2, two=2)
                    ea, eb = ('dve', 'pool') if (h % 2 == 0) else ('pool', 'dve')
                    k.op(ea, lambda e, xv=xv: e.tensor_tensor(out=rt[0][:], in0=xv[:, :, 0, :], in1=cs, op=ALU.mult), reads=[buf, cosT], writes=[rt[0]])
                    k.op(eb, lambda e, xv=xv: e.tensor_tensor(out=rt[1][:], in0=xv[:, :, 1, :], in1=sn, op=ALU.mult), reads=[buf, sinT], writes=[rt[1]])
                    k.op(ea, lambda e, ov=ov: e.tensor_tensor(out=ov[:, :, 0, :], in0=rt[0][:], in1=rt[1][:], op=ALU.subtract), reads=[rt[0], rt[1]], writes=[qr])
                    k.op(eb, lambda e, xv=xv: e.tensor_tensor(out=rt[2][:], in0=xv[:, :, 0, :], in1=sn, op=ALU.mult), reads=[buf, sinT], writes=[rt[2]])
                    k.op(ea, lambda e, xv=xv: e.tensor_tensor(out=rt[3][:], in0=xv[:, :, 1, :], in1=cs, op=ALU.mult), reads=[buf, cosT], writes=[rt[3]])
                    k.op(eb, lambda e, ov=ov: e.tensor_tensor(out=ov[:, :, 1, :], in0=rt[2][:], in1=rt[3][:], op=ALU.add), reads=[rt[2], rt[3]], writes=[qr])
            src = qr
        else:
            src = buf
        k.op('dve', lambda e: e.scalar_tensor_tensor(out=qkd[:, 0, :], in0=src[:, 0:384], scalar=qs, in1=ecp[:], op0=ALU.mult, op1=ALU.mult),
             reads=[src, ecp], writes=[qkd])
        k.op('pool', lambda e: e.tensor_tensor(out=qkd[:, 1, :], in0=src[:, 384:768], in1=ecn[:], op=ALU.mult), reads=[src, ecn], writes=[qkd])
        for w in range(2):
            for h in range(GH):
                k.op('pe', lambda e, w=w, h=h: e.transpose(out=(pTq if w == 0 else pTk)[:, h, :], in_=qkd[:, w, h * 64:(h + 1) * 64], identity=C['id_bf'][:]),
                     reads=[qkd, C['id_bf']], writes=[pTq if w == 0 else pTk])
        k.op('act', lambda e: e.copy(out=qdT[:], in_=pTq[:]), reads=[pTq], writes=[qdT])
        k.op('dve', lambda e: e.tensor_copy(out=kdT[:], in_=pTk[:]), reads=[pTk], writes=[kdT])
        for c in range(2):
            k.op('pool', lambda e, c=c: e.tensor_copy(out=qdz[:, :, c, 64 * c:64 * c + 64], in_=qdT[:, :, 64 * c:64 * c + 64]), reads=[qdT], writes=[qdz])
        Sd = S[d]
        order = (0, 1) if d == 0 else (1, 0)
        k.op('act', lambda e: e.copy(out=Sb[0][:], in_=Sd[:]), reads=[Sd], writes=[Sb[0]])
        for ci, c in enumerate(order):
            for h in range(GH):
                pk, pkk = pK[h % 2]
                k.op('pe', lambda e, c=c, h=h, pk=pk: e.matmul(pk, lhsT=qkd[64 * c:64 * c + 64, 1, h * 64:(h + 1) * 64], rhs=v_b[64 * c:64 * c + 64, h, :],
                                                              start=True, stop=True), reads=[qkd, v_b], writes=[pkk])
                k.op('dve', lambda e, c=c, h=h: e.tensor_scalar_mul(out=Stmp[:], in0=Sd[:, h, :], scalar1=ecl[:, h, c:c + 1]), reads=[Sd, ecl], writes=[Stmp])
                k.op('dve', lambda e, c=c, h=h, pk=pk: e.scalar_tensor_tensor(out=Sd[:, h, :], in0=pk, scalar=ecl[:, h, c:c + 1], in1=Stmp[:],
                                                                             op0=ALU.mult, op1=ALU.add), reads=[pkk, ecl, Stmp], writes=[Sd])
            if ci == 0 and with_out:
                k.op('act', lambda e: e.copy(out=Sb[1][:], in_=Sd[:]), reads=[Sd], writes=[Sb[1]])
        if not with_out:
            return
        for h in range(GH):
            (pa_, pak), (po, pok) = pA[h % 2], pO[h % 2]
            am = Am[h % 2]
            k.op('pe', lambda e, h=h, pa_=pa_: e.matmul(pa_, lhsT=kdT[:, h, :], rhs=qdT[:, h, :], start=True, stop=True), reads=[kdT, qdT], writes=[pak])
            k.op('dve', lambda e, pa_=pa_, am=am: e.tensor_tensor(out=am[:], in0=pa_, in1=Mm[:], op=ALU.mult), reads=[pak, Mm], writes=[am])
            k.op('pe', lambda e, h=h, po=po, am=am: e.matmul(po, lhsT=am[:], rhs=v_b[:, h, :], start=True, stop=False), reads=[am, v_b], writes=[pok])
            for ci, c in enumerate(order):
                k.op('pe', lambda e, h=h, po=po, c=c, ci=ci: e.matmul(po, lhsT=qdz[:, h, c, :], rhs=Sb[ci][:, h, :], start=False, stop=(ci == 1)),
                     reads=[qdz, Sb[ci]], writes=[pok])
            if not final:
                k.op('act', lambda e, h=h, po=po: e.copy(out=osb[:, h * 128:(h + 1) * 128], in_=po), reads=[pok], writes=[osb])
            else:
                k.op('dve', lambda e, h=h, po=po: e.tensor_tensor(out=osb[:, h * 128:(h + 1) * 128], in0=po, in1=ofl[:, h * 128:(h + 1) * 128], op=ALU.add),
                     reads=[pok, ofl], writes=[osb])
        if not final:
            k.dma('sp', ofs[t0:t0 + 128, :], osb[:], reads=[osb], writes=[ofs])
            return
        o3 = osb[:, :].rearrange("p (h d) -> p h d", h=GH)
        k.op('pool', lambda e: e.tensor_tensor(out=sq[:], in0=o3, in1=o3, op=ALU.mult), reads=[osb], writes=[sq])
        k.op('dve', lambda e: e.tensor_reduce(out=ss[:, 0, :], in_=sq[:], axis=AX.X, op=ALU.add), reads=[sq], writes=[ss])
        k.op('dve', lambda e: e.tensor_scalar(out=ss[:, 1, :], in0=ss[:, 0, :], scalar1=1.0 / 128, scalar2=EPS, op0=ALU.mult, op1=ALU.add), reads=[ss], writes=[ss])
        k.op('act', lambda e: e.activation(out=ss[:, 1, :], in_=ss[:, 1, :], func=AF.Ln), reads=[ss], writes=[ss])
        k.op('act', lambda e: e.activation(out=ss[:, 1, :], in_=ss[:, 1, :], func=AF.Exp, scale=-0.5), reads=[ss], writes=[ss])
        k.op('act', lambda e: e.activation(out=sg[:], in_=buf[:, 1536:2304], func=AF.Silu), reads=[buf], writes=[sg])
        for h in range(GH):
            k.op('dve', lambda e, h=h: e.tensor_scalar_mul(out=osb[:, h * 128:(h + 1) * 128], in0=osb[:, h * 128:(h + 1) * 128], scalar1=ss[:, 1, h:h + 1]),
                 reads=[osb, ss], writes=[osb])
        k.op('pool', lambda e: e.tensor_tensor(out=sg[:], in0=sg[:], in1=gnw[:], op=ALU.mult), reads=[sg, gnw], writes=[sg])
        k.op('dve', lambda e: e.tensor_tensor(out=yb[:], in0=osb[:], in1=sg[:], op=ALU.mult), reads=[osb, sg], writes=[yb])
        ms_ = mst[tt % 2]
        for h in range(GH):
            k.op('pe', lambda e, h=h: e.transpose(out=ptb[:, h, :], in_=yb[:, h * 128:(h + 1) * 128], identity=C['id_bf'][:]), reads=[yb, C['id_bf']], writes=[ptb])
        k.op('act', lambda e, ms_=ms_: e.copy(out=ms_[:], in_=ptb[:]), reads=[ptb], writes=[ms_])
        k.dma('sp', mixT[1280:2048, t0:t0 + 128].rearrange("(j p) t -> p j t", p=128), ms_[:], reads=[ms_], writes=[mixT])

    k.op('pool', lambda e: e.memset(S[0][:], 0.0), writes=[S[0]])
    k.op('pool', lambda e: e.memset(S[1][:], 0.0), writes=[S[1]])
    for tt in (NTL, NTL + 1):
        tile_pass(tt, 0, with_ctx, False)
    for tt in range(NTL):
        tile_pass(tt, 0, True, False)
    for tt in (NTL + 1, NTL):
        if with_ctx:
            k.dma('sp', ofl[:], ofs[tt * 128:(tt + 1) * 128, :], reads=[ofs], writes=[ofl])
        tile_pass(tt, 1, with_ctx, True)
    for tt in range(NTL - 1, -1, -1):
        k.dma('sp', ofl[:], ofs[tt * 128:(tt + 1) * 128, :], reads=[ofs], writes=[ofl])
        tile_pass(tt, 1, True, True)
    P.release(m0)


def stage_post(P, C, l, W, mp, xT, mixT, x1T, h2f_d, h2b_d, ntok):
    nc, k = P.nc, P.k
    pf = "po_"
    m0 = P.mark()
    NB = 256
    wo = P.alloc_sb(pf + "wo", [128, NCH, D], BF16)
    wv = W['w_out'][l].rearrange("(kc p) c -> p kc c", p=128)
    for q in range(4):
        k.dma('pool', wo[:, :, q * 512:(q + 1) * 512], wv[:, :, q * 512:(q + 1) * 512], writes=[wo])
    lnp = P.alloc_sb(pf + "lnp", [128, 4, NCH], F32)
    k.dma('sp', lnp[:], W['lnT'][l])
    tmp = ln_tmp(P, pf + "ln", NB)
    mxb = [P.alloc_sb(pf + "mxb%d" % i, [128, NCH, NB], BF16) for i in range(2)]
    xt = [P.alloc_sb(pf + "xt%d" % i, [128, NCH, NB], F32) for i in range(2)]
    x1t = P.alloc_sb(pf + "x1t", [128, NCH, NB], F32)
    pss = [P.alloc_ps(pf + "ps%d" % i, [128, NB]) for i in range(2)]
    tq = [P.alloc_sb(pf + "tq%d" % i, [128, NB], F32) for i in range(2)]
    mxv = mixT.rearrange("(c p) t -> p c t", p=128)
    xv = xT.rearrange("(c p) t -> p c t", p=128)
    x1v = x1T.rearrange("(c p) t -> p c t", p=128)
    hfv = h2f_d.rearrange("(c p) t -> p c t", p=128)
    hbv = h2b_d.rearrange("(c p) t -> p c t", p=128)
    for bi, t0 in enumerate(range(0, ntok, NB)):
        r = 0 if t0 < L else 1
        mb = mxb[bi % 2]; x_ = xt[bi % 2]
        k.dma('sp', mb[:], mxv[:, :, t0:t0 + NB], writes=[mb])
        k.dma('act', x_[:], xv[:, :, t0:t0 + NB], writes=[x_])
        for dc in range(NCH):
            pst = pss[dc % 2]; tt = tq[dc % 2]
            for kc in range(NCH):
                k.op('pe', lambda e, kc=kc, dc=dc, pst=pst: e.matmul(pst[:, :], lhsT=wo[:, kc, dc * 128:(dc + 1) * 128], rhs=mb[:, kc, :],
                                                                    start=(kc == 0), stop=(kc == NCH - 1)), reads=[wo, mb], writes=[pst])
            k.op('act', lambda e, dc=dc, pst=pst, tt=tt: e.activation(out=tt[:], in_=pst[:, :], func=AF.Copy, scale=mp['g1'][:, dc, r:r + 1]),
                 reads=[pst, mp['g1']], writes=[tt])
            k.op('dve', lambda e, dc=dc, tt=tt: e.scalar_tensor_tensor(out=x_[:, dc, :], in0=x_[:, dc, :], scalar=ALPHA, in1=tt[:], op0=ALU.mult, op1=ALU.add),
                 reads=[x_, tt], writes=[x_])
        ln_block(P, C, tmp, x_, NB,
                 lambda c: (x1t[:, c, :], [x1t]),
                 lambda c: (lnp[:, 0, c:c + 1], [lnp]),
                 lambda c: (lnp[:, 1, c:c + 1], [lnp]))
        k.dma('sp', x1v[:, :, t0:t0 + NB], x1t[:], reads=[x1t], writes=[x1T])
        ln_block(P, C, tmp, x1t, NB,
                 lambda c: (x_[:, c, :], [x_]),
                 lambda c: (mp['sc2p'][:, c, r:r + 1], [mp['sc2p']]),
                 lambda c: (mp['sh2'][:, c, r:r + 1], [mp['sh2']]))
        k.dma('sp', hfv[:, :, t0:t0 + NB], x_[:], reads=[x_], writes=[h2f_d])
        k.dma('pool', hbv[:, :, t0:t0 + NB], x_[:], reads=[x_], writes=[h2b_d])
    P.release(m0)


_OI = [0]


def stage_moe(P, C, l, W, mp, x1T, h2f_d, h2b_d, outT, ntok):
    nc, k = P.nc, P.k
    pf = "mo_"
    m0 = P.mark()
    NT_ = ntok // 128
    gate = P.alloc_sb(pf + "gate", [128, NT_, NE], F32)
    m1 = P.mark()
    wr = P.alloc_sb(pf + "wr", [128, NCH, 36], F32)
    k.dma('sp', wr[:, :, 0:4], W['w_rg'][l].rearrange("(kc p) c -> p kc c", p=128), writes=[wr])
    k.dma('sp', wr[:, :, 4:36], W['w_re'][l].rearrange("(kc p) c -> p kc c", p=128), writes=[wr])
    br = P.alloc_sb(pf + "br", [128, 36], F32)
    k.dma('sp', br[:, 0:4], W['b_rg'][l:l + 1, :].partition_broadcast(128), writes=[br])
    k.dma('sp', br[:, 4:36], W['b_re'][l:l + 1, :].partition_broadcast(128), writes=[br])
    hf = [P.alloc_sb(pf + "hf%d" % i, [128, NCH, 128], F32) for i in range(2)]
    pl = [P.alloc_ps(pf + "pl%d" % i, [128, 36]) for i in range(2)]
    lg = P.alloc_sb(pf + "lg", [128, 36], F32)
    sm = P.alloc_sb(pf + "sm", [128, 16], F32)
    gs = P.alloc_sb(pf + "gs", [128, 4], F32)
    eg = P.alloc_sb(pf + "eg", [128, 4], F32)
    les = P.alloc_sb(pf + "les", [128, 8], F32)
    le2 = P.alloc_sb(pf + "le2", [128, 8], F32)
    mk1 = P.alloc_sb(pf + "mk1", [128, 8], F32)
    mk2 = P.alloc_sb(pf + "mk2", [128, 8], F32)
    egt = P.alloc_sb(pf + "egt", [128, 8], F32)
    hfv = h2f_d.rearrange("(c p) t -> p c t", p=128)
    for tt in range(NT_):
        h_ = hf[tt % 2]; pst = pl[tt % 2]
        k.dma('sp', h_[:], hfv[:, :, tt * 128:(tt + 1) * 128], writes=[h_])
        for kc in range(NCH):
            k.op('pe', lambda e, kc=kc, h_=h_, pst=pst: e.matmul(pst[:, :], lhsT=h_[:, kc, :], rhs=wr[:, kc, :], start=(kc == 0), stop=(kc == NCH - 1)),
                 reads=[h_, wr], writes=[pst])
        k.op('dve', lambda e, pst=pst: e.tensor_tensor(out=lg[:], in0=pst[:, :], in1=br[:], op=ALU.add), reads=[pst, br], writes=[lg])
        k.op('dve', lambda e: e.tensor_reduce(out=sm[:, 0:1], in_=lg[:, 0:4], axis=AX.X, op=ALU.max), reads=[lg], writes=[sm])
        k.op('dve', lambda e: e.tensor_scalar_mul(out=sm[:, 1:2], in0=sm[:, 0:1], scalar1=-1.0), reads=[sm], writes=[sm])
        k.op('act', lambda e: e.activation(out=eg[:], in_=lg[:, 0:4], func=AF.Exp, bias=sm[:, 1:2], accum_out=sm[:, 2:3]), reads=[lg, sm], writes=[eg, sm])
        k.op('dve', lambda e: e.reciprocal(out=sm[:, 3:4], in_=sm[:, 2:3]), reads=[sm], writes=[sm])
        k.op('dve', lambda e: e.tensor_scalar(out=gs[:], in0=lg[:, 0:4], scalar1=sm[:, 0:1], scalar2=None, op0=ALU.is_ge), reads=[lg, sm], writes=[gs])
        k.op('dve', lambda e: e.tensor_scalar_mul(out=les[:], in0=lg[:, 4:12], scalar1=gs[:, 0:1]), reads=[lg, gs], writes=[les])
        for g in range(1, 4):
            k.op('dve', lambda e, g=g: e.scalar_tensor_tensor(out=les[:], in0=lg[:, 4 + 8 * g:12 + 8 * g], scalar=gs[:, g:g + 1], in1=les[:],
                                                             op0=ALU.mult, op1=ALU.add), reads=[lg, gs, les], writes=[les])
        k.op('dve', lambda e: e.tensor_reduce(out=sm[:, 4:5], in_=les[:], axis=AX.X, op=ALU.max), reads=[les], writes=[sm])
        k.op('dve', lambda e: e.tensor_scalar(out=mk1[:], in0=les[:], scalar1=sm[:, 4:5], scalar2=None, op0=ALU.is_ge), reads=[les, sm], writes=[mk1])
        k.op('dve', lambda e: e.scalar_tensor_tensor(out=le2[:], in0=mk1[:], scalar=-1.0e9, in1=les[:], op0=ALU.mult, op1=ALU.add),
             reads=[mk1, les], writes=[le2])
        k.op('dve', lambda e: e.tensor_reduce(out=sm[:, 5:6], in_=le2[:], axis=AX.X, op=ALU.max), reads=[le2], writes=[sm])
        k.op('dve', lambda e: e.tensor_scalar(out=mk2[:], in0=le2[:], scalar1=sm[:, 5:6], scalar2=None, op0=ALU.is_ge), reads=[le2, sm], writes=[mk2])
        k.op('dve', lambda e: e.tensor_tensor(out=sm[:, 6:7], in0=sm[:, 5:6], in1=sm[:, 4:5], op=ALU.subtract), reads=[sm], writes=[sm])
        k.op('act', lambda e: e.activation(out=sm[:, 7:8], in_=sm[:, 6:7], func=AF.Exp), reads=[sm], writes=[sm])
        k.op('dve', lambda e: e.tensor_scalar_add(out=sm[:, 8:9], in0=sm[:, 7:8], scalar1=1.0), reads=[sm], writes=[sm])
        k.op('dve', lambda e: e.reciprocal(out=sm[:, 9:10], in_=sm[:, 8:9]), reads=[sm], writes=[sm])
        k.op('dve', lambda e: e.tensor_tensor(out=sm[:, 10:11], in0=sm[:, 7:8], in1=sm[:, 9:10], op=ALU.mult), reads=[sm], writes=[sm])
        k.op('dve', lambda e: e.tensor_tensor(out=sm[:, 11:12], in0=sm[:, 9:10], in1=sm[:, 3:4], op=ALU.mult), reads=[sm], writes=[sm])
        k.op('dve', lambda e: e.tensor_tensor(out=sm[:, 12:13], in0=sm[:, 10:11], in1=sm[:, 3:4], op=ALU.mult), reads=[sm], writes=[sm])
        k.op('dve', lambda e: e.tensor_scalar_mul(out=egt[:], in0=mk1[:], scalar1=sm[:, 11:12]), reads=[mk1, sm], writes=[egt])
        k.op('dve', lambda e: e.scalar_tensor_tensor(out=egt[:], in0=mk2[:], scalar=sm[:, 12:13], in1=egt[:], op0=ALU.mult, op1=ALU.add),
             reads=[mk2, sm, egt], writes=[egt])
        for g in range(4):
            k.op('dve', lambda e, g=g, tt=tt: e.tensor_scalar_mul(out=gate[:, tt, 8 * g:8 * g + 8], in0=egt[:], scalar1=gs[:, g:g + 1]),
                 reads=[egt, gs], writes=[gate])
    P.dump('moe_gate', gate)
    P.release(m1)
    PASS = 1024 if ntok in (L, L // 2) else 768
    BLK = 384
    acc = P.alloc_sb(pf + "acc", [128, PASS // 128, D], F32)
    lnp = P.alloc_sb(pf + "lnp", [128, 4, NCH], F32)
    k.dma('sp', lnp[:], W['lnT'][l])
    hbv = h2b_d.rearrange("(c p) t -> p c t", p=128)
    x1v = x1T.rearrange("(c p) t -> p c t", p=128)
    ov = outT.rearrange("(c p) t -> p c t", p=128)
    ei = 0
    oi = 0
    for p0 in range(0, ntok, PASS):
        pn = min(PASS, ntok - p0)
        mE = P.mark()
        hT = P.alloc_sb(pf + "hT", [128, NCH, PASS], BF16)
        wu = [P.alloc_sb(pf + "wu%d" % i, [128, NCH, 1024], BF16) for i in range(2)]
        wd = [P.alloc_sb(pf + "wd%d" % i, [128, 4, D], BF16) for i in range(2)]
        actT = [P.alloc_sb(pf + "actT%d" % i, [128, 4, BLK], BF16) for i in range(2)]
        sgb = [P.alloc_sb(pf + "sg%d" % i, [128, BLK], F32) for i in range(2)]
        pg = [P.alloc_ps(pf + "pg%d" % i, [128, BLK]) for i in range(2)]
        pu = [P.alloc_ps(pf + "pu%d" % i, [128, BLK]) for i in range(2)]
        po = [P.alloc_ps(pf + "po%d" % i, [128, 512]) for i in range(4)]
        k.dma('sp', hT[:, :, :pn], hbv[:, :, p0:p0 + pn], writes=[hT])
        k.op('pool', lambda e: e.memset(acc[:], 0.0), writes=[acc])
        for ex in range(NE):
            g_, e_ = ex // 8, ex % 8
            wu_ = wu[ei % 2]; wd_ = wd[ei % 2]
            ei += 1
            k.dma('pool', wu_[:], W['w_up'][l, g_, e_].rearrange("(kc p) c -> p kc c", p=128), writes=[wu_])
            k.dma('pool', wd_[:], W['w_down'][l, g_, e_].rearrange("(j p) c -> p j c", p=128), writes=[wd_])
            def up_block(b0):
                    bn = min(BLK, pn - b0)
                    at = actT[(b0 // BLK) % 2]
                    for j in range(4):
                        pg_ = pg[j % 2]; pu_ = pu[j % 2]; sg_ = sgb[j % 2]
                        for kc in range(NCH):
                            k.op('pe', lambda e, kc=kc, j=j, pg_=pg_: e.matmul(pg_[:, :bn], lhsT=wu_[:, kc, j * 128:(j + 1) * 128], rhs=hT[:, kc, b0:b0 + bn],
                                                                              start=(kc == 0), stop=(kc == NCH - 1)), reads=[wu_, hT], writes=[pg_])
                        for kc in range(NCH):
                            k.op('pe', lambda e, kc=kc, j=j, pu_=pu_: e.matmul(pu_[:, :bn], lhsT=wu_[:, kc, 512 + j * 128:512 + (j + 1) * 128], rhs=hT[:, kc, b0:b0 + bn],
                                                                              start=(kc == 0), stop=(kc == NCH - 1)), reads=[wu_, hT], writes=[pu_])
                        k.op('act', lambda e, pg_=pg_, sg_=sg_: e.activation(out=sg_[:, :bn], in_=pg_[:, :bn], func=AF.Silu), reads=[pg_], writes=[sg_])
                        k.op('dve', lambda e, j=j, pu_=pu_, sg_=sg_, at=at: e.tensor_tensor(out=at[:, j, :bn], in0=pu_[:, :bn], in1=sg_[:, :bn], op=ALU.mult),
                             reads=[pu_, sg_], writes=[at])
            def down_block(b0):
                    bn = min(BLK, pn - b0)
                    at = actT[(b0 // BLK) % 2]
                    for t3 in range(bn // 128):
                        tl = (b0 // 128) + t3
                        tg = (p0 // 128) + tl
                        for dh in range(4):
                            po_ = po[_OI[0] % 4]
                            _OI[0] += 1
                            for j in range(4):
                                k.op('pe', lambda e, j=j, dh=dh, po_=po_, t3=t3: e.matmul(po_[:, :], lhsT=at[:, j, t3 * 128:(t3 + 1) * 128], rhs=wd_[:, j, dh * 512:(dh + 1) * 512],
                                                                                       start=(j == 0), stop=(j == 3)), reads=[at, wd_], writes=[po_])
                            k.op('dve', lambda e, dh=dh, po_=po_, tl=tl, tg=tg, ex=ex: e.scalar_tensor_tensor(out=acc[:, tl, dh * 512:(dh + 1) * 512], in0=po_[:, :],
                                                                                                            scalar=gate[:, tg, ex:ex + 1], in1=acc[:, tl, dh * 512:(dh + 1) * 512],
                                                                                                            op0=ALU.mult, op1=ALU.add), reads=[po_, gate, acc], writes=[acc])
            blks = list(range(0, pn, BLK))
            up_block(blks[0])
            for bi_ in range(1, len(blks)):
                up_block(blks[bi_])
                down_block(blks[bi_ - 1])
            down_block(blks[-1])
        P.release(mE)
        m2 = P.mark()
        tmp = ln_tmp(P, pf + "ln", BLK)
        vt = P.alloc_sb(pf + "vt", [128, NCH, BLK], F32)
        x1b = P.alloc_sb(pf + "x1b", [128, NCH, BLK], F32)
        ot = P.alloc_sb(pf + "ot", [128, NCH, BLK], F32)
        ptf = [P.alloc_ps(pf + "ptf%d" % i, [128, 512]) for i in range(2)]
        tq = [P.alloc_sb(pf + "tq%d" % i, [128, 128], F32) for i in range(2)]
        for b0 in range(0, pn, BLK):
            bn = min(BLK, pn - b0)
            r = 0 if (p0 + b0) < L else 1
            k.dma('sp', x1b[:, :, :bn], x1v[:, :, p0 + b0:p0 + b0 + bn], writes=[x1b])
            for t3 in range(bn // 128):
                tl = (b0 // 128) + t3
                r = 0 if (p0 + b0 + t3 * 128) < L else 1
                for c in range(NCH):
                    pt_ = ptf[c % 2]; tq_ = tq[c % 2]
                    k.op('pe', lambda e, c=c, tl=tl, pt_=pt_: e.transpose(out=pt_[:, :128], in_=acc[:, tl, c * 128:(c + 1) * 128], identity=C['id_f'][:]),
                         reads=[acc, C['id_f']], writes=[pt_])
                    k.op('act', lambda e, c=c, pt_=pt_, tq_=tq_: e.activation(out=tq_[:], in_=pt_[:, :128], func=AF.Copy, scale=mp['g2'][:, c, r:r + 1]),
                         reads=[pt_, mp['g2']], writes=[tq_])
                    k.op('dve', lambda e, c=c, t3=t3, tq_=tq_: e.scalar_tensor_tensor(out=vt[:, c, t3 * 128:(t3 + 1) * 128], in0=x1b[:, c, t3 * 128:(t3 + 1) * 128],
                                                                                      scalar=ALPHA, in1=tq_[:], op0=ALU.mult, op1=ALU.add), reads=[x1b, tq_], writes=[vt])
            ln_block(P, C, tmp, vt, bn,
                     lambda c: (ot[:, c, :bn], [ot]),
                     lambda c: (lnp[:, 2, c:c + 1], [lnp]),
                     lambda c: (lnp[:, 3, c:c + 1], [lnp]))
            k.dma('sp', ov[:, :, p0 + b0:p0 + b0 + bn], ot[:, :, :bn], reads=[ot], writes=[outT])
        P.release(m2)
    P.release(m0)


def stage_select(P, selT, items):
    nc, k = P.nc, P.k
    m0 = P.mark()
    HALF = L // 2
    sel = P.alloc_sb("sel", [128, 1], F32)
    k.dma('sp', sel[:], selT)
    A = [P.alloc_sb("selA%d" % i, [128, NCH, 256], F32) for i in range(2)]
    B = [P.alloc_sb("selB%d" % i, [128, NCH, 256], F32) for i in range(2)]
    n = 0
    for (src, dst, dstb) in items:
        sv = src.rearrange("(c p) t -> p c t", p=128)
        dv = dst.rearrange("(c p) t -> p c t", p=128)
        for t0 in range(0, HALF, 256):
            a_, b_ = A[n % 2], B[n % 2]
            n += 1
            k.dma('sp', a_[:], sv[:, :, t0:t0 + 256], writes=[a_])
            k.dma('act', b_[:], sv[:, :, HALF + t0:HALF + t0 + 256], writes=[b_])
            k.op('dve', lambda e, a_=a_, b_=b_: e.tensor_tensor(out=b_[:], in0=b_[:], in1=a_[:], op=ALU.subtract), reads=[a_, b_], writes=[b_])
            k.op('dve', lambda e, a_=a_, b_=b_: e.scalar_tensor_tensor(out=a_[:], in0=b_[:], scalar=sel[:, 0:1], in1=a_[:], op0=ALU.mult, op1=ALU.add),
                 reads=[a_, b_, sel], writes=[a_])
            k.dma('sp', dv[:, :, t0:t0 + 256], a_[:], reads=[a_], writes=[dst])
            if dstb is not None:
                k.dma('pool', dstb.rearrange("(c p) t -> p c t", p=128)[:, :, t0:t0 + 256], a_[:], reads=[a_], writes=[dstb])
    P.release(m0)


W_SHAPES = {
    'hy_f_w1': [DEPTH, 33, 64], 'hy_f_w2': [DEPTH, 64, 64], 'hy_f_w3': [DEPTH, 64, 1024], 'hy_fv': [DEPTH, 64, 3],
    'hy_bias': [DEPTH, 512], 'hy_sw': [DEPTH, 128, 12, 4], 'hy_norm_wT': [DEPTH, 128, 4],
    'na_rpb': [DEPTH, 6, 15, 31], 'na_norm_wT': [DEPTH, 128, 6],
    'gla_a_w2': [DEPTH, 2, 16, 384], 'gla_a_b': [DEPTH, 2, 384], 'gla_norm_w': [DEPTH, 128],
    'w_out': [DEPTH, D, D], 'lnT': [DEPTH, 128, 4, NCH],
    'w_rg': [DEPTH, D, 4], 'b_rg': [DEPTH, 4], 'w_re': [DEPTH, D, 32], 'b_re': [DEPTH, 32],
    'w_up': [DEPTH, 4, 8, D, 1024], 'w_down': [DEPTH, 4, 8, DE, D],
    'w_ada': [DEPTH, D, 6 * D], 'b_adaT': [DEPTH, 128, 96], 'w_in': [DEPTH, D, INC],
}


def host_layout(inp, layers=(0, 1)):
    ls = list(layers)
    n = len(ls)
    W = {}
    for nm in ('hy_f_w1', 'hy_f_w2', 'hy_f_w3', 'hy_bias', 'na_rpb', 'gla_a_w2', 'gla_a_b', 'gla_norm_w', 'w_out', 'w_rg', 'b_rg',
               'w_re', 'b_re', 'w_up', 'w_down', 'w_ada', 'w_in'):
        W[nm] = np.ascontiguousarray(inp[nm][ls])
    W['hy_fv'] = np.ascontiguousarray(np.stack([inp['hy_f_b1'][ls], inp['hy_f_b2'][ls], inp['hy_sin_freq'][ls]], -1))
    sw = np.concatenate([inp['hy_short_w'][ls], inp['hy_short_b'][ls][:, None, :]], 1)
    W['hy_sw'] = np.ascontiguousarray(sw.reshape(n, 4, 12, 128).transpose(0, 3, 2, 1))
    W['hy_norm_wT'] = np.ascontiguousarray(inp['hy_norm_w'][ls].reshape(n, 4, 128).transpose(0, 2, 1))
    W['na_norm_wT'] = np.ascontiguousarray(inp['na_norm_w'][ls].reshape(n, 6, 128).transpose(0, 2, 1))
    lnT = np.stack([inp['ln1_g'][ls], inp['ln1_b'][ls], inp['ln2_g'][ls], inp['ln2_b'][ls]], 1)
    W['lnT'] = np.ascontiguousarray(lnT.reshape(n, 4, NCH, 128).transpose(0, 3, 1, 2))
    W['b_adaT'] = np.ascontiguousarray(inp['b_ada'][ls].reshape(n, 96, 128).transpose(0, 2, 1))
    return W


def build(layers=(0, 1), dbg=(), upto=None):
    P = Prog(dbg=dbg)
    nl = len(layers)
    W = {}
    for nm, shp in W_SHAPES.items():
        W[nm] = P.inp(nm, [nl] + shp[1:])
    xT_in = P.inp("xT", [D, T])
    cT = P.inp("cT", [128, NCH, 2])
    outT = P.outp("outT", [D, L // 2])
    selT = P.inp("selT", [128, 1])
    x1S = P.scratch("x1S", [D, L // 2])
    h2fS = P.scratch("h2fS", [D, L // 2])
    h2bS = P.scratch("h2bS", [D, L // 2], BF16)
    pfm = P.scratch("pfm", [3104, T])
    ptm = P.scratch("ptm", [T, 3072])
    mixT = P.scratch("mixT", [D, T], BF16)
    rpbp = P.scratch("rpbp", [6, 15, 160])
    ofs = P.scratch("ofs", [T, 768])
    x1T = P.scratch("x1T", [D, T])
    h2f = P.scratch("h2f", [D, T])
    h2b = P.scratch("h2b", [D, T], BF16)
    xmid = P.scratch("xmid", [D, T])
    C = make_consts(P)
    modT = sb(P.nc, "modT", [128, 96, 2], F32)
    for li in range(nl):
        last = (li == nl - 1)
        xin = xT_in if li == 0 else xmid
        xout = outT if last else xmid
        ntok = L if last else T
        mk = P.mark()
        stage_mod(P, li, cT, W['w_ada'], W['b_adaT'], modT)
        mp = load_mod(P, modT)
        stage_inproj(P, C, li, xin, W['w_in'], mp, pfm, ptm)
        if upto == 'inproj':
            break
        stage_hyena(P, C, li, W, pfm, mixT, 0, L)
        if not last:
            stage_hyena(P, C, li, W, pfm, mixT, L, LC)
        stage_na(P, C, li, W, pfm, ptm, mixT, rpbp, not last)
        stage_gla(P, C, li, W, pfm, ptm, mixT, ofs, not last)
        if upto == 'mix':
            break
        stage_post(P, C, li, W, mp, xin, mixT, x1T, h2f, h2b, ntok)
        if upto == 'post':
            break
        if last:
            stage_select(P, selT, [(x1T, x1S, None), (h2f, h2fS, h2bS)])
            stage_moe(P, C, li, W, mp, x1S, h2fS, h2bS, xout, L // 2)
        else:
            stage_moe(P, C, li, W, mp, x1T, h2f, h2b, xout, ntok)
        P.release(mk)
    P.k.finish()
    return P


def kernel(**inputs):
    inp = {k_: np.asarray(v) for k_, v in inputs.items()}
    Wn = host_layout(inp)
    P = build()
    in_maps = []
    for core in range(8):
        b = core % 4
        m = dict(Wn)
        m['xT'] = np.ascontiguousarray(np.concatenate([inp['x'][b], inp['ctx'][b]], 0).T)
        m['cT'] = np.ascontiguousarray(np.stack([inp['c'][b], inp['c_ctx']], -1).reshape(NCH, 128, 2).transpose(1, 0, 2))
        m['selT'] = np.full((128, 1), float(core // 4), np.float32)
        in_maps.append(m)
    res = run_bass_kernel_spmd(P.nc, in_maps, core_ids=list(range(8)))
    out = np.stack([np.concatenate([res.results[b]["outT"].T, res.results[b + 4]["outT"].T], 0) for b in range(4)], 0)
    return out.astype(np.float32)
```
